# Optimizing a Trainium2 kernel written in Bass

```python
import jax, jax.numpy as jnp
from jax import lax
import numpy as np

D_MODEL = 2048
BATCH = 8
SEQ = 2048
DEPTH = 2

ALPHA = (2 * DEPTH) ** 0.25
BETA = (8 * DEPTH) ** -0.25
LN_EPS = 1e-5

FOX_HEADS = 8
FOX_HEAD_DIM = D_MODEL // 16
FOX_WIDTH = FOX_HEADS * FOX_HEAD_DIM
QUERY_BLOCK = 128
CONV_CH = D_MODEL // 2
CONV_GROUPS = 8
CONV_WIDTH = 31
EVEN_IN = 3 * FOX_WIDTH + FOX_HEADS + 2 * CONV_CH
EVEN_MIX = FOX_WIDTH + CONV_CH

GLA_HEADS = 4
GLA_KEY_WIDTH = D_MODEL // 2
GLA_VAL_WIDTH = D_MODEL
GLA_HK = GLA_KEY_WIDTH // GLA_HEADS
GLA_HV = GLA_VAL_WIDTH // GLA_HEADS
GLA_LOW_RANK = 16
GLA_TAU = 16.0
GLA_CHUNK = 64
ODD_IN = 2 * GLA_KEY_WIDTH + 2 * GLA_VAL_WIDTH + GLA_LOW_RANK

N_EXPERTS = 16
N_GROUPS = 4
EXPERTS_PER_GROUP = N_EXPERTS // N_GROUPS
TOP_K = 2
D_EXPERT = D_MODEL * 11 // 16
EXPERT_BLOCK = 256

N_EVEN = (DEPTH + 1) // 2
N_ODD = DEPTH // 2

kernel_name = "fox_conformer_gla_sharedrouter_moe_deepnorm"


def layer_norm(x, g, b):
    xf = x.astype(jnp.float32)
    mu = jnp.mean(xf, axis=-1, keepdims=True)
    var = jnp.mean(jnp.square(xf - mu), axis=-1, keepdims=True)
    return ((xf - mu) * lax.rsqrt(var + LN_EPS) * g + b).astype(x.dtype)


def forgetting_attention(q, k, v, log_f):
    B, S, H, Dh = q.shape
    c = jnp.cumsum(log_f, axis=1).transpose(0, 2, 1)
    scale = Dh ** -0.5
    outs = []
    for i in range(S // QUERY_BLOCK):
        lo, hi = i * QUERY_BLOCK, (i + 1) * QUERY_BLOCK
        s = jnp.einsum('bqhd,bkhd->bhqk', q[:, lo:hi], k[:, :hi]).astype(jnp.float32) * scale
        s = s + c[:, :, lo:hi, None] - c[:, :, None, :hi]
        causal = (lo + jnp.arange(QUERY_BLOCK))[:, None] >= jnp.arange(hi)[None, :]
        p = jax.nn.softmax(jnp.where(causal, s, -jnp.inf), axis=-1)
        outs.append(jnp.einsum('bhqk,bkhd->bqhd', p.astype(v.dtype), v[:, :hi]))
    return jnp.concatenate(outs, axis=1)


def causal_depthwise_conv(u, w, b):
    y = lax.conv_general_dilated(u, w.astype(u.dtype), window_strides=(1,),
                                 padding=[(CONV_WIDTH - 1, 0)],
                                 dimension_numbers=('NWC', 'WIO', 'NWC'),
                                 feature_group_count=u.shape[-1])
    return y + b


def even_mixer(x, w_in, b_f, conv_w, conv_b, cn_g, cn_b, w_out):
    B, S, _ = x.shape
    h = x @ w_in
    q, k, v, f_logit, glu = jnp.split(
        h, [FOX_WIDTH, 2 * FOX_WIDTH, 3 * FOX_WIDTH, 3 * FOX_WIDTH + FOX_HEADS], axis=-1)
    heads = lambda t: t.reshape(B, S, FOX_HEADS, FOX_HEAD_DIM)
    log_f = jax.nn.log_sigmoid((f_logit + b_f).astype(jnp.float32))
    att = forgetting_attention(heads(q), heads(k), heads(v), log_f).reshape(B, S, FOX_WIDTH)
    a, gate = jnp.split(glu, 2, axis=-1)
    u = causal_depthwise_conv(a * jax.nn.sigmoid(gate), conv_w, conv_b)
    u = layer_norm(u.reshape(B, S, CONV_GROUPS, -1),
                   cn_g.reshape(CONV_GROUPS, -1), cn_b.reshape(CONV_GROUPS, -1))
    u = jax.nn.silu(u.reshape(B, S, CONV_CH))
    return jnp.concatenate([att, u], axis=-1) @ w_out


def gla_chunked(q, k, v, log_a):
    B, S, H, HK = q.shape
    HV = v.shape[-1]
    N, C = S // GLA_CHUNK, GLA_CHUNK
    chunks = lambda t: t.reshape(B, N, C, H, t.shape[-1]).transpose(1, 0, 3, 2, 4)
    q, k, v, log_a = chunks(q), chunks(k), chunks(v), chunks(log_a)
    b = jnp.cumsum(log_a, axis=-2)
    b_last = b[..., -1:, :]
    q_t = q * jnp.exp(b)
    k_t = k * jnp.exp(-b)
    k_end = k * jnp.exp(b_last - b)
    causal = jnp.tril(jnp.ones((C, C), dtype=bool))
    attn = jnp.where(causal, jnp.einsum('nbhtd,nbhsd->nbhts', q_t, k_t), 0.0)
    o_intra = jnp.einsum('nbhts,nbhsv->nbhtv', attn, v)

    def step(state, inp):
        q_n, k_n, v_n, dec_n = inp
        o = jnp.einsum('bhtd,bhdv->bhtv', q_n, state)
        state = state * dec_n[..., 0, :, None] + jnp.einsum('bhsd,bhsv->bhdv', k_n, v_n)
        return state, o

    state0 = jnp.zeros((B, H, HK, HV), jnp.float32)
    _, o_inter = lax.scan(step, state0, (q_t, k_end, v, jnp.exp(b_last)))
    o = o_intra + o_inter
    return o.transpose(1, 0, 3, 2, 4).reshape(B, S, H, HV)


def odd_mixer(x, w_in, w_a2, b_a, norm_g, w_out):
    B, S, _ = x.shape
    h = x @ w_in
    q, k, v, g, a_low = jnp.split(
        h, [GLA_KEY_WIDTH, 2 * GLA_KEY_WIDTH, 2 * GLA_KEY_WIDTH + GLA_VAL_WIDTH,
            2 * GLA_KEY_WIDTH + 2 * GLA_VAL_WIDTH], axis=-1)
    log_a = jax.nn.log_sigmoid((a_low @ w_a2 + b_a).astype(jnp.float32)) / GLA_TAU
    kh = lambda t: t.astype(jnp.float32).reshape(B, S, GLA_HEADS, GLA_HK)
    o = gla_chunked(kh(q) * GLA_HK ** -0.5, kh(k),
                    v.astype(jnp.float32).reshape(B, S, GLA_HEADS, GLA_HV), kh(log_a))
    o = o * lax.rsqrt(jnp.mean(jnp.square(o), axis=-1, keepdims=True) + LN_EPS)
    o = o.reshape(B, S, GLA_VAL_WIDTH) * norm_g * jax.nn.silu(g.astype(jnp.float32))
    return o.astype(x.dtype) @ w_out


def route(x2d, router_w, router_bias):
    N = x2d.shape[0]
    scores = jax.nn.sigmoid((x2d @ router_w).astype(jnp.float32))
    sel = (scores + router_bias.astype(jnp.float32)).reshape(N, N_GROUPS, EXPERTS_PER_GROUP)
    group_score = jnp.sum(lax.top_k(sel, 2)[0], axis=-1)
    grp = jnp.argmax(group_score, axis=-1)
    sel_g = jnp.take_along_axis(sel, grp[:, None, None], axis=1)[:, 0]
    _, local = lax.top_k(sel_g, TOP_K)
    idx = grp[:, None] * EXPERTS_PER_GROUP + local
    w = jnp.take_along_axis(scores, idx, axis=1)
    return idx, w / jnp.sum(w, axis=-1, keepdims=True)


def moe_ffn(x2d, idx, gates, w_gate, w_up, w_down):
    N, D = x2d.shape
    A = N * TOP_K
    P = ((A + N_EXPERTS * EXPERT_BLOCK + EXPERT_BLOCK - 1) // EXPERT_BLOCK) * EXPERT_BLOCK
    NB = P // EXPERT_BLOCK
    flat_e = idx.reshape(-1)
    flat_tok = jnp.repeat(jnp.arange(N), TOP_K)
    order = jnp.argsort(flat_e, stable=True)
    e_sorted = flat_e[order]
    tok_sorted = flat_tok[order]
    counts = jnp.bincount(flat_e, length=N_EXPERTS)
    starts = jnp.cumsum(counts) - counts
    padded = ((counts + EXPERT_BLOCK - 1) // EXPERT_BLOCK) * EXPERT_BLOCK
    pends = jnp.cumsum(padded)
    pstarts = pends - padded
    dest = pstarts[e_sorted] + (jnp.arange(A) - starts[e_sorted])
    slot_tok = jnp.full((P,), N, dtype=jnp.int32).at[dest].set(tok_sorted.astype(jnp.int32))
    x_pad = jnp.concatenate([x2d, jnp.zeros((1, D), x2d.dtype)], axis=0)
    xs = x_pad[slot_tok].reshape(NB, EXPERT_BLOCK, D)
    block_e = jnp.minimum(jnp.searchsorted(pends, jnp.arange(NB) * EXPERT_BLOCK, side='right'),
                          N_EXPERTS - 1)

    def expert_block(args):
        xb, e = args
        hdn = jax.nn.silu(xb @ w_gate[e]) * (xb @ w_up[e])
        return hdn @ w_down[e]

    ys = lax.map(expert_block, (xs, block_e)).reshape(P, D)
    y_sorted = ys[dest] * gates.reshape(-1)[order][:, None].astype(ys.dtype)
    return jax.ops.segment_sum(y_sorted, tok_sorted, num_segments=N)


def _normal(key, shape, scale):
    return jax.random.normal(key, shape, jnp.float32) * scale


def setup_inputs(seed: int = 0) -> dict:
    key = jax.random.key(seed)
    ks = jax.random.split(key, 24)
    D = D_MODEL
    x = _normal(ks[0], (BATCH, SEQ, D), 1.0)
    even_w_in = _normal(ks[1], (N_EVEN, D, EVEN_IN), D ** -0.5)
    even_w_in = even_w_in.at[..., 2 * FOX_WIDTH:3 * FOX_WIDTH].multiply(BETA)
    glu_a0 = 3 * FOX_WIDTH + FOX_HEADS
    even_w_in = even_w_in.at[..., glu_a0:glu_a0 + CONV_CH].multiply(BETA)
    even_b_f = jax.random.uniform(ks[2], (N_EVEN, FOX_HEADS), jnp.float32, 1.0, 4.0)
    even_conv_w = _normal(ks[3], (N_EVEN, CONV_WIDTH, 1, CONV_CH), CONV_WIDTH ** -0.5)
    even_conv_b = _normal(ks[4], (N_EVEN, CONV_CH), 0.02)
    even_conv_norm_g = 1.0 + _normal(ks[5], (N_EVEN, CONV_CH), 0.02)
    even_conv_norm_b = _normal(ks[6], (N_EVEN, CONV_CH), 0.02)
    even_w_out = _normal(ks[7], (N_EVEN, EVEN_MIX, D), EVEN_MIX ** -0.5 * BETA)
    odd_w_in = _normal(ks[8], (N_ODD, D, ODD_IN), D ** -0.5)
    odd_w_in = odd_w_in.at[..., 2 * GLA_KEY_WIDTH:2 * GLA_KEY_WIDTH + GLA_VAL_WIDTH].multiply(BETA)
    odd_w_a2 = _normal(ks[9], (N_ODD, GLA_LOW_RANK, GLA_KEY_WIDTH), GLA_LOW_RANK ** -0.5)
    odd_b_a = _normal(ks[10], (N_ODD, GLA_KEY_WIDTH), 0.1)
    odd_norm_g = 1.0 + _normal(ks[11], (N_ODD, GLA_VAL_WIDTH), 0.02)
    odd_w_out = _normal(ks[12], (N_ODD, GLA_VAL_WIDTH, D), GLA_VAL_WIDTH ** -0.5 * BETA)
    ln_mix_g = 1.0 + _normal(ks[13], (DEPTH, D), 0.02)
    ln_mix_b = _normal(ks[14], (DEPTH, D), 0.02)
    ln_ffn_g = 1.0 + _normal(ks[15], (DEPTH, D), 0.02)
    ln_ffn_b = _normal(ks[16], (DEPTH, D), 0.02)
    router_w = _normal(ks[17], (D, N_EXPERTS), D ** -0.5)
    router_bias = _normal(ks[18], (N_EXPERTS,), 0.01)
    expert_w_gate = _normal(ks[19], (DEPTH, N_EXPERTS, D, D_EXPERT), D ** -0.5)
    expert_w_up = _normal(ks[20], (DEPTH, N_EXPERTS, D, D_EXPERT), D ** -0.5 * BETA)
    expert_w_down = _normal(ks[21], (DEPTH, N_EXPERTS, D_EXPERT, D), D_EXPERT ** -0.5 * BETA)
    return {"x": x, "even_w_in": even_w_in, "even_b_f": even_b_f, "even_conv_w": even_conv_w,
            "even_conv_b": even_conv_b, "even_conv_norm_g": even_conv_norm_g,
            "even_conv_norm_b": even_conv_norm_b, "even_w_out": even_w_out,
            "odd_w_in": odd_w_in, "odd_w_a2": odd_w_a2, "odd_b_a": odd_b_a,
            "odd_norm_g": odd_norm_g, "odd_w_out": odd_w_out,
            "ln_mix_g": ln_mix_g, "ln_mix_b": ln_mix_b, "ln_ffn_g": ln_ffn_g, "ln_ffn_b": ln_ffn_b,
            "router_w": router_w, "router_bias": router_bias,
            "expert_w_gate": expert_w_gate, "expert_w_up": expert_w_up,
            "expert_w_down": expert_w_down}


def reference(x, even_w_in, even_b_f, even_conv_w, even_conv_b, even_conv_norm_g,
              even_conv_norm_b, even_w_out, odd_w_in, odd_w_a2, odd_b_a, odd_norm_g,
              odd_w_out, ln_mix_g, ln_mix_b, ln_ffn_g, ln_ffn_b, router_w, router_bias,
              expert_w_gate, expert_w_up, expert_w_down):
    B, S, D = x.shape
    for layer in range(DEPTH):
        i = layer // 2
        if layer % 2 == 0:
            mix = even_mixer(x, even_w_in[i], even_b_f[i], even_conv_w[i], even_conv_b[i],
                             even_conv_norm_g[i], even_conv_norm_b[i], even_w_out[i])
        else:
            mix = odd_mixer(x, odd_w_in[i], odd_w_a2[i], odd_b_a[i], odd_norm_g[i], odd_w_out[i])
        x = layer_norm(ALPHA * x + mix, ln_mix_g[layer], ln_mix_b[layer])
        x2d = x.reshape(B * S, D)
        idx, gates = route(x2d, router_w, router_bias)
        y = moe_ffn(x2d, idx, gates, expert_w_gate[layer], expert_w_up[layer], expert_w_down[layer])
        x = layer_norm(ALPHA * x + y.reshape(B, S, D), ln_ffn_g[layer], ln_ffn_b[layer])
    return x
```

```python
import numpy as np
import ml_dtypes
import concourse.bass as bass
import concourse.mybir as mybir
from concourse.bass_utils import run_bass_kernel_spmd

F32 = mybir.dt.float32
BF16 = mybir.dt.bfloat16
I32 = mybir.dt.int32
AF = mybir.ActivationFunctionType
ALU = mybir.AluOpType
AX = mybir.AxisListType

S = 2048
D = 2048
NT = S // 128
KC = D // 128
DEPTH = 2
ALPHA = (2 * DEPTH) ** 0.25
LN_EPS = 1e-5
NE = 16
FE = 1408
NF = FE // 128
CAP = 512
NJ = CAP // 128
ZROW = S
YZ = NE * CAP


class Res:
    __slots__ = ("name", "w", "rs", "dsem")

    def __init__(self, name):
        self.name = name
        self.w = None
        self.rs = []
        self.dsem = None


class KB:
    ENGS = ("pe", "act", "dve", "pool", "sp")

    def __init__(self, nc, n_dma_sems=48, same_engine_sync=True):
        self.nc = nc
        self.eng = {"pe": nc.tensor, "act": nc.scalar, "dve": nc.vector,
                    "pool": nc.gpsimd, "sp": nc.sync}
        self.sem = {}
        self.cnt = {}
        self.waited = {}
        self.same_engine_sync = same_engine_sync
        for e in self.ENGS:
            self._mksem("p_" + e)
        self._mksem("bar")
        self.dma_pool = []
        for i in range(n_dma_sems):
            self._mksem("d%d" % i)
            self.dma_pool.append("d%d" % i)
        self.dma_next = 0
        self.dma_res = []

    def _mksem(self, name):
        self.sem[name] = self.nc.alloc_semaphore(name)
        self.cnt[name] = 0

    def _wait(self, e, dep):
        if dep is None:
            return
        s, c = dep
        if s == "p_" + e and (e in ("pe", "sp") or not self.same_engine_sync):
            return
        if self.waited.get((e, s), 0) >= c:
            return
        self.eng[e].wait_ge(self.sem[s], c)
        self.waited[(e, s)] = c

    def _pre(self, e, reads, writes):
        for r in reads:
            self._wait(e, r.w)
        for w in writes:
            self._wait(e, w.w)
            for d in w.rs:
                self._wait(e, d)

    def _post(self, dep, reads, writes):
        for r in reads:
            r.rs.append(dep)
            if len(r.rs) > 8:
                m = {}
                for s, c in r.rs:
                    m[s] = max(m.get(s, 0), c)
                r.rs = list(m.items())
        for w in writes:
            w.w = dep
            w.rs = []

    def op(self, e, fn, reads=(), writes=()):
        self._pre(e, reads, writes)
        ins = fn(self.eng[e])
        s = "p_" + e
        self.cnt[s] += 1
        ins.then_inc(self.sem[s], 1)
        self._post((s, self.cnt[s]), reads, writes)
        return ins

    def dma(self, q, fn, reads=(), writes=(), key=None):
        self._pre(q, reads, writes)
        key = key or (writes[0] if writes else reads[0])
        if key.dsem is None:
            assert self.dma_next < len(self.dma_pool), "out of dma sems"
            key.dsem = self.dma_pool[self.dma_next]
            self.dma_next += 1
            self.dma_res.append(key)
        ins = fn(self.eng[q])
        s = key.dsem
        self.cnt[s] += 16
        ins.then_inc(self.sem[s], 16)
        self._post((s, self.cnt[s]), reads, writes)
        return ins

    def barrier(self):
        sp = self.eng["sp"]
        for s, c in self.cnt.items():
            if s in ("bar", "p_sp") or c == 0:
                continue
            if self.waited.get(("sp", s), 0) >= c:
                continue
            sp.wait_ge(self.sem[s], c)
            self.waited[("sp", s)] = c
        self.cnt["bar"] += 1
        sp.nop().then_inc(self.sem["bar"], 1)
        for e in self.ENGS:
            if e != "sp":
                self.eng[e].wait_ge(self.sem["bar"], self.cnt["bar"])
            for s, c in self.cnt.items():
                self.waited[(e, s)] = c
        for r in self.dma_res:
            r.dsem = None
        self.dma_res = []
        self.dma_next = 0


class Ring:
    def __init__(self, views, name):
        self.v = views
        self.r = [Res("%s%d" % (name, i)) for i in range(len(views))]
        self.i = -1

    def next(self):
        self.i = (self.i + 1) % len(self.v)
        return self.v[self.i], self.r[self.i]


_UNIQ = [0]


def uq(name):
    _UNIQ[0] += 1
    return "%s_u%d" % (name, _UNIQ[0])


def ln_tile(kb, z, zr, gam, bet, rg, st, rst, out, rout, eng2="dve"):
    stats, mv, rstd = st
    for c in range(4):
        kb.op("dve", lambda e: e.bn_stats(out=stats[:, c * 6:(c + 1) * 6], in_=z[:, c * 512:(c + 1) * 512]),
              reads=[zr], writes=[rst])
    kb.op("dve", lambda e: e.bn_aggr(out=mv[:, 0:2], in_=stats[:, 0:24]), reads=[rst], writes=[rst])
    kb.op("dve", lambda e: e.tensor_scalar_add(out=rstd[:, 0:1], in0=mv[:, 1:2], scalar1=LN_EPS), reads=[rst], writes=[rst])
    kb.op("act", lambda e: e.sqrt(out=rstd[:, 0:1], in_=rstd[:, 0:1]), reads=[rst], writes=[rst])
    kb.op("dve", lambda e: e.reciprocal(out=rstd[:, 0:1], in_=rstd[:, 0:1]), reads=[rst], writes=[rst])
    kb.op("dve", lambda e: e.tensor_scalar(out=z[:, :], in0=z[:, :], scalar1=mv[:, 0:1], scalar2=rstd[:, 0:1],
                                           op0=ALU.subtract, op1=ALU.mult), reads=[zr, rst], writes=[zr])
    kb.op(eng2, lambda e: e.tensor_tensor(out=z[:, :], in0=z[:, :], in1=gam[:, :], op=ALU.mult), reads=[zr] + rg, writes=[zr])
    kb.op(eng2, lambda e: e.tensor_tensor(out=out[:, :], in0=z[:, :], in1=bet[:, :], op=ALU.add), reads=[zr] + rg, writes=[rout])


class LNPipe:
    def __init__(self, nc, kb, es, gam, bet, rgs, emit, eps=LN_EPS):
        self.kb = kb
        self.gam, self.bet, self.rgs, self.emit, self.eps = gam, bet, rgs, emit, eps
        st = es.enter_context(nc.sbuf_tensor(uq("lnst"), [128, 2, 32], F32))
        self.st_ring = Ring([st[:, i] for i in range(2)], "lnst")
        zo = es.enter_context(nc.sbuf_tensor(uq("lnzo"), [128, 2, D], F32))
        self.zo_ring = Ring([zo[:, i] for i in range(2)], "lnzo")
        self.pending = None

    def _apply(self):
        kb = self.kb
        z, r_z, tag = self.pending
        o, r_o = self.zo_ring.next()
        kb.op("dve", lambda e: e.tensor_tensor(out=z, in0=z, in1=self.gam[:, :], op=ALU.mult), reads=[r_z] + self.rgs, writes=[r_z])
        kb.op("dve", lambda e: e.tensor_tensor(out=o, in0=z, in1=self.bet[:, :], op=ALU.add), reads=[r_z] + self.rgs, writes=[r_o])
        self.pending = None
        self.emit(o, r_o, tag)

    def feed(self, z, r_z, tag):
        kb = self.kb
        st, r_st = self.st_ring.next()
        for c in range(4):
            kb.op("dve", lambda e: e.bn_stats(out=st[:, c * 6:(c + 1) * 6], in_=z[:, c * 512:(c + 1) * 512]), reads=[r_z], writes=[r_st])
        kb.op("dve", lambda e: e.bn_aggr(out=st[:, 24:26], in_=st[:, 0:24]), reads=[r_st], writes=[r_st])
        kb.op("dve", lambda e: e.tensor_scalar_add(out=st[:, 26:27], in0=st[:, 25:26], scalar1=self.eps), reads=[r_st], writes=[r_st])
        kb.op("act", lambda e: e.sqrt(out=st[:, 26:27], in_=st[:, 26:27]), reads=[r_st], writes=[r_st])
        if self.pending is not None:
            self._apply()
        kb.op("dve", lambda e: e.reciprocal(out=st[:, 27:28], in_=st[:, 26:27]), reads=[r_st], writes=[r_st])
        kb.op("dve", lambda e: e.scalar_tensor_tensor(out=st[:, 28:29], in0=st[:, 24:25], scalar=-1.0, in1=st[:, 27:28], op0=ALU.mult, op1=ALU.mult),
              reads=[r_st], writes=[r_st])
        kb.op("act", lambda e: e.activation(out=z, in_=z, func=AF.Identity, bias=st[:, 28:29], scale=st[:, 27:28]), reads=[r_z, r_st], writes=[r_z])
        self.pending = (z, r_z, tag)

    def flush(self):
        if self.pending is not None:
            self._apply()


def bcast_rows(ap1d, n):
    return ap1d.partition_broadcast(128)


def moe_phase(nc, kb, L, C, xa, xab, ys, xo, xob, w):
    from contextlib import ExitStack
    ident, iota, tokinfo = C["ident"], C["iota"], C["tokinfo"]
    wg_d = w["expert_w_gate"]
    wu_d = w["expert_w_up"]
    wd_d = w["expert_w_down"]

    with ExitStack() as es:
        def sb(name, shape, dt):
            return es.enter_context(nc.sbuf_tensor(uq(name), shape, dt))

        def ps(name, shape, dt):
            return es.enter_context(nc.psum_tensor(uq(name), shape, dt))

        gate_all = sb("gate_all", [128, NT, NE], F32)
        posm_all = sb("posm_all", [128, NT, NE], F32)
        ridx = sb("ridx", [128, NT, 2], I32)
        gsel = sb("gsel", [128, NT, 2], F32)
        tokidx = sb("tokidx", [128, NE * NJ], I32)
        rw_bf = sb("rw_bf", [128, KC, NE], BF16)
        rbias = sb("rbias", [128, NE], F32)
        ecap = sb("ecap", [128, NE], F32)
        ident_s = sb("ident_s", [128, 128], BF16)
        iota_s = sb("iota_s", [128, CAP], F32)
        tokinfo_s = sb("tokinfo_s", [128, NT, 4], BF16)
        lstrict = sb("lstrict", [128, 128], BF16)
        ones_bf = sb("ones_bf", [128, 128], BF16)
        r_const = Res("const")
        r_gate = Res("gate_all")
        r_posm = Res("posm_all")
        r_ridx = Res("ridx")
        r_tok = Res("tokidx")

        kb.dma("pool", lambda e: e.dma_start(out=rw_bf[:], in_=w["router_w"].rearrange("(kc p) e -> p kc e", p=128)), writes=[r_const])
        c2 = Res("c2"); c3 = Res("c3"); c4 = Res("c4"); c5 = Res("c5"); c6 = Res("c6"); c7 = Res("c7")
        kb.dma("sp", lambda e: e.dma_start(out=rbias[:], in_=w["router_bias"].partition_broadcast(128)), writes=[c2])
        kb.dma("sp", lambda e: e.dma_start(out=ident_s[:], in_=ident), writes=[c3])
        kb.dma("sp", lambda e: e.dma_start(out=iota_s[:], in_=iota[:, 0:CAP]), writes=[c4])
        kb.dma("sp", lambda e: e.dma_start(out=tokinfo_s[:], in_=tokinfo), writes=[c5])
        kb.dma("sp", lambda e: e.dma_start(out=lstrict[:], in_=C["lstrict"]), writes=[c6])
        kb.dma("sp", lambda e: e.dma_start(out=ecap[:], in_=C["ecap"]), writes=[c7])
        kb.op("dve", lambda e: e.memset(ones_bf[:], 1.0), writes=[c6])
        consts = [r_const, c2, c3, c4, c5, c6, c7]

        with ExitStack() as es2:
            def sb2(name, shape, dt):
                return es2.enter_context(nc.sbuf_tensor(uq(name), shape, dt))

            def ps2(name, shape, dt):
                return es2.enter_context(nc.psum_tensor(uq(name), shape, dt))

            xt_b = sb2("xt_b", [128, 2, D], BF16)
            xt_ring = Ring([xt_b[:, i] for i in range(2)], "xt_b")
            xT = sb2("xT", [128, 2, KC, 128], BF16)
            xT_ring = Ring([xT[:, i] for i in range(2)], "xT")
            pT = ps2("pT", [128, 2, 1024], BF16)
            pT_ring = Ring([pT[:, i] for i in range(2)], "pT")
            psm = ps2("psm", [128, 512], F32)
            r_psm_r = Res("psm_r"); r_psm_p = Res("psm_p")
            rt = sb2("rt", [128, 16, NE], F32)
            r_rt = Res("rt")
            m4 = sb2("m4", [128, 8, 4], F32)
            mcum = sb2("mcum", [128, NE], BF16)
            m_bf = sb2("m_bf", [128, 2, NE], BF16)
            m_ring = Ring([m_bf[:, i] for i in range(2)], "m_bf")
            r_mcum = Res("mcum")
            oh = sb2("oh", [128, 4, CAP], BF16)
            oh_ring = Ring([oh[:, i] for i in range(4)], "oh")
            kb.op("dve", lambda e: e.memset(mcum[:], 0.0), writes=[r_mcum])

            for i in range(NT):
                xt, r_xt = xt_ring.next()
                kb.dma("sp", lambda e: e.dma_start(out=xt, in_=xab[i * 128:(i + 1) * 128, :]), writes=[r_xt])
                xTt, r_xT = xT_ring.next()
                for g4 in range(4):
                    p, r_p = pT_ring.next()
                    for q in range(4):
                        kc = g4 * 4 + q
                        kb.op("pe", lambda e: e.transpose(out=p[:, q * 128:(q + 1) * 128], in_=xt[:, kc * 128:(kc + 1) * 128], identity=ident_s[:]),
                              reads=[r_xt, c3], writes=[r_p])
                    en = "act" if g4 % 2 == 0 else "dve"
                    if en == "act":
                        kb.op("act", lambda e: e.copy(out=xTt[:, g4 * 4:(g4 + 1) * 4, :], in_=p[:, 0:512].rearrange("p (a b) -> p a b", a=4)),
                              reads=[r_p], writes=[r_xT])
                    else:
                        kb.op("dve", lambda e: e.tensor_copy(out=xTt[:, g4 * 4:(g4 + 1) * 4, :], in_=p[:, 0:512].rearrange("p (a b) -> p a b", a=4)),
                              reads=[r_p], writes=[r_xT])
                for kc in range(KC):
                    kb.op("pe", lambda e: e.matmul(psm[:, 0:NE], lhsT=xTt[:, kc, :], rhs=rw_bf[:, kc, :], start=(kc == 0), stop=(kc == KC - 1)),
                          reads=[r_xT, r_const], writes=[r_psm_r])
                sc = rt[:, 0]; sel = rt[:, 1]; eq1 = rt[:, 2]; sel2 = rt[:, 3]; ge2 = rt[:, 4]; M = rt[:, 5]; wv = rt[:, 6]
                pos1 = rt[:, 7]; vv = rt[:, 8]; sv = rt[:, 9]; tmp = rt[:, 10]; sv2 = rt[:, 11]
                m1 = m4[:, 0]; m2 = m4[:, 1]; gs = m4[:, 2]; gm = m4[:, 3]
                gmax = m4[:, 4, 0:1]; wsum = m4[:, 4, 1:2]; ihi = m4[:, 5, 0:1]; ilo = m4[:, 5, 1:2]; t1 = m4[:, 5, 2:3]
                R = [r_rt]
                kb.op("act", lambda e: e.activation(out=sc, in_=psm[:, 0:NE], func=AF.Sigmoid), reads=[r_psm_r], writes=R)
                kb.op("dve", lambda e: e.tensor_tensor(out=sel, in0=sc, in1=rbias[:], op=ALU.add), reads=R + [c2], writes=R)
                v3 = lambda a: a.rearrange("p (g j) -> p g j", g=4)
                b3 = lambda a: a.unsqueeze(2).to_broadcast([128, 4, 4])
                kb.op("dve", lambda e: e.tensor_reduce(out=m1, in_=v3(sel), axis=AX.X, op=ALU.max), reads=R, writes=R)
                kb.op("dve", lambda e: e.tensor_tensor(out=v3(eq1), in0=v3(sel), in1=b3(m1), op=ALU.is_equal), reads=R, writes=R)
                kb.op("dve", lambda e: e.scalar_tensor_tensor(out=sel2, in0=eq1, scalar=-1e9, in1=sel, op0=ALU.mult, op1=ALU.add), reads=R, writes=R)
                kb.op("dve", lambda e: e.tensor_reduce(out=m2, in_=v3(sel2), axis=AX.X, op=ALU.max), reads=R, writes=R)
                kb.op("dve", lambda e: e.tensor_tensor(out=gs, in0=m1, in1=m2, op=ALU.add), reads=R, writes=R)
                kb.op("dve", lambda e: e.tensor_reduce(out=gmax, in_=gs, axis=AX.X, op=ALU.max), reads=R, writes=R)
                kb.op("dve", lambda e: e.tensor_scalar(out=gm, in0=gs, scalar1=gmax, scalar2=None, op0=ALU.is_equal), reads=R, writes=R)
                kb.op("dve", lambda e: e.tensor_tensor(out=v3(ge2), in0=v3(sel), in1=b3(m2), op=ALU.is_ge), reads=R, writes=R)
                kb.op("dve", lambda e: e.tensor_tensor(out=v3(M), in0=v3(ge2), in1=b3(gm), op=ALU.mult), reads=R, writes=R)
                kb.op("dve", lambda e: e.tensor_tensor(out=wv, in0=sc, in1=M, op=ALU.mult), reads=R, writes=R)
                kb.op("dve", lambda e: e.tensor_reduce(out=wsum, in_=wv, axis=AX.X, op=ALU.add), reads=R, writes=R)
                kb.op("dve", lambda e: e.reciprocal(out=wsum, in_=wsum), reads=R, writes=R)
                kb.op("dve", lambda e: e.tensor_scalar(out=gate_all[:, i, :], in0=wv, scalar1=wsum, scalar2=None, op0=ALU.mult), reads=R, writes=[r_gate])
                mb, r_mb = m_ring.next()
                kb.op("dve", lambda e: e.tensor_copy(out=mb, in_=M), reads=R, writes=[r_mb])
                kb.op("pe", lambda e: e.matmul(psm[:, 32:32 + NE], lhsT=lstrict[:], rhs=mb, start=True, stop=False),
                      reads=[r_mb, c6], writes=[r_psm_p])
                kb.op("pe", lambda e: e.matmul(psm[:, 32:32 + NE], lhsT=ones_bf[:], rhs=mcum[:], start=False, stop=True),
                      reads=[r_mcum, c6], writes=[r_psm_p])
                kb.op("dve", lambda e: e.tensor_scalar(out=vv, in0=psm[:, 32:32 + NE], scalar1=float(CAP), scalar2=None, op0=ALU.is_lt), reads=[r_psm_p] + R, writes=R)
                kb.op("dve", lambda e: e.scalar_tensor_tensor(out=pos1, in0=psm[:, 32:32 + NE], scalar=1.0, in1=M, op0=ALU.add, op1=ALU.mult), reads=[r_psm_p] + R, writes=R)
                kb.op("dve", lambda e: e.tensor_tensor(out=pos1, in0=pos1, in1=vv, op=ALU.mult), reads=R, writes=R)
                kb.op("dve", lambda e: e.tensor_scalar_add(out=posm_all[:, i, :], in0=pos1, scalar1=-1.0), reads=R, writes=[r_posm])
                kb.op("dve", lambda e: e.tensor_tensor(out=mcum[:], in0=mcum[:], in1=mb, op=ALU.add), reads=[r_mb, r_mcum], writes=[r_mcum])
                kb.op("dve", lambda e: e.tensor_scalar(out=vv, in0=pos1, scalar1=0.0, scalar2=None, op0=ALU.is_gt), reads=R, writes=R)
                kb.op("dve", lambda e: e.tensor_tensor(out=sv, in0=pos1, in1=ecap[:], op=ALU.add), reads=R + [c7], writes=R)
                kb.op("dve", lambda e: e.tensor_tensor(out=sv, in0=sv, in1=vv, op=ALU.mult), reads=R, writes=R)
                kb.op("dve", lambda e: e.tensor_reduce(out=ihi, in_=sv, axis=AX.X, op=ALU.max), reads=R, writes=R)
                kb.op("dve", lambda e: e.tensor_scalar(out=tmp, in0=sv, scalar1=ihi, scalar2=None, op0=ALU.not_equal), reads=R, writes=R)
                kb.op("dve", lambda e: e.tensor_tensor(out=sv2, in0=sv, in1=tmp, op=ALU.mult), reads=R, writes=R)
                kb.op("dve", lambda e: e.tensor_reduce(out=ilo, in_=sv2, axis=AX.X, op=ALU.max), reads=R, writes=R)
                kb.op("dve", lambda e: e.scalar_tensor_tensor(out=tmp, in0=sv, scalar=ihi, in1=gate_all[:, i, :], op0=ALU.is_equal, op1=ALU.mult,
                                                              accum_out=gsel[:, i, 0:1]), reads=R + [r_gate], writes=R + [r_ridx])
                kb.op("dve", lambda e: e.scalar_tensor_tensor(out=tmp, in0=sv, scalar=ilo, in1=gate_all[:, i, :], op0=ALU.is_equal, op1=ALU.mult,
                                                              accum_out=gsel[:, i, 1:2]), reads=R + [r_gate], writes=R + [r_ridx])
                for k, src in ((0, ihi), (1, ilo)):
                    kb.op("dve", lambda e: e.tensor_scalar(out=t1, in0=src, scalar1=0.0, scalar2=float(YZ + 1), op0=ALU.is_equal, op1=ALU.mult), reads=R, writes=R)
                    kb.op("dve", lambda e: e.scalar_tensor_tensor(out=t1, in0=src, scalar=-1.0, in1=t1, op0=ALU.add, op1=ALU.add), reads=R, writes=R)
                    kb.op("dve", lambda e: e.tensor_copy(out=ridx[:, i, k:k + 1], in_=t1), reads=R, writes=[r_ridx])
            pacs = sb2("pacs", [128, NE * NJ, 4], F32)
            r_tf = Res("tf")
            ptab = ps2("ptab", [128, 2, NJ, NT, 4], F32)
            ptab_ring = Ring([ptab[:, i] for i in range(2)], "ptab")
            for ex in range(NE):
                pt, r_pt = ptab_ring.next()
                for i in range(NT):
                    o, r_o = oh_ring.next()
                    kb.op("dve", lambda e: e.tensor_scalar(out=o, in0=iota_s[:], scalar1=posm_all[:, i, ex:ex + 1], scalar2=None, op0=ALU.is_equal),
                          reads=[r_posm, c4], writes=[r_o])
                    for j in range(NJ):
                        kb.op("pe", lambda e: e.matmul(pt[:, j, i, 0:4], lhsT=o[:, j * 128:(j + 1) * 128], rhs=tokinfo_s[:, i, :],
                                                       start=True, stop=True), reads=[r_o, c5], writes=[r_pt])
                kb.op("dve", lambda e: e.tensor_reduce(out=pacs[:, ex * NJ:(ex + 1) * NJ, :], in_=pt.rearrange("p j i c -> p j c i"), axis=AX.X, op=ALU.add),
                      reads=[r_pt], writes=[r_tf])
            tf = sb2("tf", [128, NE * NJ, 2], F32)
            kb.op("dve", lambda e: e.scalar_tensor_tensor(out=tf[:, :, 0], in0=pacs[:, :, 1], scalar=128.0, in1=pacs[:, :, 0], op0=ALU.mult, op1=ALU.add),
                  reads=[r_tf], writes=[r_tf])
            kb.op("dve", lambda e: e.tensor_scalar(out=tf[:, :, 1], in0=pacs[:, :, 2], scalar1=-float(ZROW), scalar2=float(ZROW), op0=ALU.mult, op1=ALU.add),
                  reads=[r_tf], writes=[r_tf])
            kb.op("dve", lambda e: e.tensor_tensor(out=tf[:, :, 0], in0=tf[:, :, 0], in1=tf[:, :, 1], op=ALU.add), reads=[r_tf], writes=[r_tf])
            kb.op("dve", lambda e: e.tensor_copy(out=tokidx[:, :], in_=tf[:, :, 0]), reads=[r_tf], writes=[r_tok])
        kb.barrier()

        with ExitStack() as es2:
            def sb2(name, shape, dt):
                return es2.enter_context(nc.sbuf_tensor(uq(name), shape, dt))

            def ps2(name, shape, dt):
                return es2.enter_context(nc.psum_tensor(uq(name), shape, dt))

            NGU = 6
            NWD = 4
            wgu = sb2("wgu", [128, NGU, 2, KC, 128], BF16)
            gu_ring = Ring([wgu[:, i] for i in range(NGU)], "wgu")
            wd = sb2("wd", [128, NWD, NF, 512], BF16)
            wd_ring = Ring([wd[:, i] for i in range(NWD)], "wd")
            xg = sb2("xg", [128, 4, D], BF16)
            xg_ring = Ring([xg[:, i] for i in range(4)], "xg")
            xgT = sb2("xgT", [128, 2, KC, CAP], BF16)
            xgT_res = [[Res("xgT%d_%d" % (b, j)) for j in range(NJ)] for b in range(2)]
            hT = sb2("hT", [128, 2, NF, CAP], BF16)
            hT_res = [[Res("hT%d_%d" % (b, f)) for f in range(NF)] for b in range(2)]
            sg = sb2("sg", [128, 2, CAP], F32)
            sg_ring = Ring([sg[:, i] for i in range(2)], "sg")
            yst = sb2("yst", [128, 4, 512], BF16)
            yst_ring = Ring([yst[:, i] for i in range(4)], "yst")
            pT = ps2("pTe", [128, 2, 1024], BF16)
            pT_ring = Ring([pT[:, i] for i in range(2)], "pTe")
            pg = ps2("pg", [128, 2, 512], F32)
            pg_ring = Ring([pg[:, i] for i in range(2)], "pg")
            pu = ps2("pu", [128, 2, 512], F32)
            pu_ring = Ring([pu[:, i] for i in range(2)], "pu")
            py = ps2("py", [128, 2, 512], F32)
            py_ring = Ring([py[:, i] for i in range(2)], "py")
            r_ys = Res("ys_dram")
            nev_box = [0]

            def prep(ex):
                b = ex % 2
                groups = []
                for j in range(NJ):
                    g, r_g = xg_ring.next()
                    col = ex * NJ + j
                    kb.dma("pool", lambda e: e.indirect_dma_start(out=g, out_offset=None, in_=xab[:, :],
                                                                   in_offset=bass.IndirectOffsetOnAxis(ap=tokidx[:, col:col + 1], axis=0)),
                           reads=[r_tok], writes=[r_g])
                    for g4 in range(4):
                        def grp(g=g, r_g=r_g, j=j, g4=g4, b=b):
                            p, r_p = pT_ring.next()
                            for q in range(4):
                                kc = g4 * 4 + q
                                kb.op("pe", lambda e: e.transpose(out=p[:, q * 128:(q + 1) * 128], in_=g[:, kc * 128:(kc + 1) * 128], identity=ident_s[:]),
                                      reads=[r_g, c3], writes=[r_p])
                            dst = xgT[:, b, g4 * 4:(g4 + 1) * 4, j * 128:(j + 1) * 128]
                            src = p[:, 0:512].rearrange("p (a b) -> p a b", a=4)
                            if nev_box[0] % 2 == 0:
                                kb.op("act", lambda e: e.copy(out=dst, in_=src), reads=[r_p], writes=[xgT_res[b][j]])
                            else:
                                kb.op("dve", lambda e: e.tensor_copy(out=dst, in_=src), reads=[r_p], writes=[xgT_res[b][j]])
                            nev_box[0] += 1
                        groups.append(grp)
                return groups

            for g_ in prep(0):
                g_()
            for ex in range(NE):
                b = ex % 2
                for f in range(NF):
                    wt, r_w = gu_ring.next()
                    kb.dma("pool", lambda e: e.dma_start(out=wt[:, 0], in_=wg_d[L, ex].rearrange("(kc p) f -> p kc f", p=128)[:, :, f * 128:(f + 1) * 128]),
                           writes=[r_w])
                    kb.dma("pool", lambda e: e.dma_start(out=wt[:, 1], in_=wu_d[L, ex].rearrange("(kc p) f -> p kc f", p=128)[:, :, f * 128:(f + 1) * 128]),
                           writes=[r_w])
                    pgt, r_pg = pg_ring.next()
                    put, r_pu = pu_ring.next()
                    for kc in range(KC):
                        kb.op("pe", lambda e: e.matmul(pgt[:, 0:CAP], lhsT=wt[:, 0, kc, :], rhs=xgT[:, b, kc, :], start=(kc == 0), stop=(kc == KC - 1)),
                              reads=[r_w] + xgT_res[b], writes=[r_pg])
                    for kc in range(KC):
                        kb.op("pe", lambda e: e.matmul(put[:, 0:CAP], lhsT=wt[:, 1, kc, :], rhs=xgT[:, b, kc, :], start=(kc == 0), stop=(kc == KC - 1)),
                              reads=[r_w] + xgT_res[b], writes=[r_pu])
                    sgt, r_sg = sg_ring.next()
                    kb.op("act", lambda e: e.activation(out=sgt, in_=pgt[:, 0:CAP], func=AF.Silu), reads=[r_pg], writes=[r_sg])
                    kb.op("dve", lambda e: e.tensor_tensor(out=hT[:, b, f, :], in0=sgt, in1=put[:, 0:CAP], op=ALU.mult), reads=[r_sg, r_pu], writes=[hT_res[b][f]])
                nxt = prep(ex + 1) if ex + 1 < NE else []
                for dc in range(4):
                    wt, r_w = wd_ring.next()
                    kb.dma("pool", lambda e: e.dma_start(out=wt, in_=wd_d[L, ex].rearrange("(fc p) d -> p fc d", p=128)[:, :, dc * 512:(dc + 1) * 512]),
                           writes=[r_w])
                    for t in range(NJ):
                        if nxt:
                            nxt.pop(0)()
                        pyt, r_py = py_ring.next()
                        for f in range(NF):
                            kb.op("pe", lambda e: e.matmul(pyt[:, 0:512], lhsT=hT[:, b, f, t * 128:(t + 1) * 128], rhs=wt[:, f, :], start=(f == 0), stop=(f == NF - 1)),
                                  reads=[r_w] + hT_res[b], writes=[r_py])
                        y, r_y = yst_ring.next()
                        if nev_box[0] % 2 == 0:
                            kb.op("act", lambda e: e.copy(out=y, in_=pyt[:, 0:512]), reads=[r_py], writes=[r_y])
                        else:
                            kb.op("dve", lambda e: e.tensor_copy(out=y, in_=pyt[:, 0:512]), reads=[r_py], writes=[r_y])
                        nev_box[0] += 1
                        row0 = ex * CAP + t * 128
                        kb.dma("sp", lambda e: e.dma_start(out=ys[row0:row0 + 128, dc * 512:(dc + 1) * 512], in_=y), reads=[r_y], key=r_y)
                for g_ in nxt:
                    g_()
        kb.barrier()

        with ExitStack() as es2:
            def sb2(name, shape, dt):
                return es2.enter_context(nc.sbuf_tensor(uq(name), shape, dt))

            gam = sb2("gam", [128, D], F32)
            bet = sb2("bet", [128, D], F32)
            r_gb = Res("gb")
            kb.dma("sp", lambda e: e.dma_start(out=gam[:], in_=w["ln_ffn_g"][L].partition_broadcast(128)), writes=[r_gb])
            r_gb2 = Res("gb2")
            kb.dma("sp", lambda e: e.dma_start(out=bet[:], in_=w["ln_ffn_b"][L].partition_broadcast(128)), writes=[r_gb2])
            xin = sb2("xin", [128, 3, D], F32)
            xin_ring = Ring([xin[:, i] for i in range(3)], "xin")
            rr = sb2("rr", [128, 4, D], BF16)
            rr_ring = Ring([rr[:, i] for i in range(4)], "rr")
            zb = sb2("zb", [128, 2, D], BF16)
            zb_ring = Ring([zb[:, i] for i in range(2)], "zb")
            gs2 = sb2("gs2", [128, NT, 2], F32)
            r_gs2 = Res("gs2")
            kb.op("dve", lambda e: e.tensor_scalar(out=gs2[:], in0=gsel[:], scalar1=1.0 / ALPHA, scalar2=None, op0=ALU.mult), reads=[r_ridx], writes=[r_gs2])

            def emit(o, r_o, i):
                kb.dma("sp", lambda e: e.dma_start(out=xo[i * 128:(i + 1) * 128, :], in_=o), reads=[r_o], key=r_o)
                if xob is not None:
                    zbt, r_zb = zb_ring.next()
                    kb.op("act", lambda e: e.copy(out=zbt, in_=o), reads=[r_o], writes=[r_zb])
                    kb.dma("sp", lambda e: e.dma_start(out=xob[i * 128:(i + 1) * 128, :], in_=zbt), reads=[r_zb], key=r_zb)
            lnp = LNPipe(nc, kb, es2, gam, bet, [r_gb, r_gb2], emit, eps=LN_EPS / (ALPHA * ALPHA))
            for i in range(NT):
                x, r_x = xin_ring.next()
                kb.dma("sp", lambda e: e.dma_start(out=x, in_=xa[i * 128:(i + 1) * 128, :]), writes=[r_x])
                rh, r_rh = rr_ring.next()
                kb.dma("pool", lambda e: e.indirect_dma_start(out=rh, out_offset=None, in_=ys[:, :],
                                                               in_offset=bass.IndirectOffsetOnAxis(ap=ridx[:, i, 0:1], axis=0)), reads=[r_ridx], writes=[r_rh])
                rl, r_rl = rr_ring.next()
                kb.dma("pool", lambda e: e.indirect_dma_start(out=rl, out_offset=None, in_=ys[:, :],
                                                               in_offset=bass.IndirectOffsetOnAxis(ap=ridx[:, i, 1:2], axis=0)), reads=[r_ridx], writes=[r_rl])
                kb.op("dve", lambda e: e.scalar_tensor_tensor(out=x, in0=rh, scalar=gs2[:, i, 0:1], in1=x, op0=ALU.mult, op1=ALU.add),
                      reads=[r_rh, r_x, r_gs2], writes=[r_x])
                kb.op("dve", lambda e: e.scalar_tensor_tensor(out=x, in0=rl, scalar=gs2[:, i, 1:2], in1=x, op0=ALU.mult, op1=ALU.add),
                      reads=[r_rl, r_x, r_gs2], writes=[r_x])
                lnp.feed(x, r_x, i)
            lnp.flush()
        kb.barrier()


def build_xT(nc, kb, es, src, xT, r_xT, ident_s, r_id, pT_ring):
    xt_b = es.enter_context(nc.sbuf_tensor(uq("xtb"), [128, 2, D], BF16))
    ring = Ring([xt_b[:, i] for i in range(2)], "xtb")
    n = 0
    for i in range(NT):
        xt, r_xt = ring.next()
        kb.dma("pool", lambda e: e.dma_start(out=xt, in_=src[i * 128:(i + 1) * 128, :]), writes=[r_xt])
        for g4 in range(4):
            p, r_p = pT_ring.next()
            for q in range(4):
                kc = g4 * 4 + q
                kb.op("pe", lambda e: e.transpose(out=p[:, q * 128:(q + 1) * 128], in_=xt[:, kc * 128:(kc + 1) * 128], identity=ident_s[:]),
                      reads=[r_xt, r_id], writes=[r_p])
            dst = xT[:, g4 * 4:(g4 + 1) * 4, i * 128:(i + 1) * 128]
            srcp = p[:, 0:512].rearrange("p (a b) -> p a b", a=4)
            if n % 2 == 0:
                kb.op("act", lambda e: e.copy(out=dst, in_=srcp), reads=[r_p], writes=[r_xT[i]])
            else:
                kb.op("dve", lambda e: e.tensor_copy(out=dst, in_=srcp), reads=[r_p], writes=[r_xT[i]])
            n += 1


def mix_out_phase(nc, kb, es, L, mixT, r_mix, wout_d, x_src, xa, xab, w, C):
    def sb(name, shape, dt):
        return es.enter_context(nc.sbuf_tensor(uq(name), shape, dt))
    wo = sb("wo", [128, KC, D], BF16)
    r_wo = [Res("wo%d" % i) for i in range(4)]
    for q in range(4):
        kb.dma("pool", lambda e: e.dma_start(out=wo[:, q * 4:(q + 1) * 4, :], in_=wout_d.rearrange("(kc p) d -> p kc d", p=128)[:, q * 4:(q + 1) * 4, :]),
               writes=[r_wo[q]])
    gam = sb("gam", [128, D], F32)
    bet = sb("bet", [128, D], F32)
    r_g1 = Res("g1"); r_g2 = Res("g2")
    kb.dma("sp", lambda e: e.dma_start(out=gam[:], in_=w["ln_mix_g"][L].partition_broadcast(128)), writes=[r_g1])
    kb.dma("sp", lambda e: e.dma_start(out=bet[:], in_=w["ln_mix_b"][L].partition_broadcast(128)), writes=[r_g2])
    xin = sb("xin", [128, 3, D], F32)
    xin_ring = Ring([xin[:, i] for i in range(3)], "xin")
    zb = sb("zb", [128, 2, D], BF16)
    zb_ring = Ring([zb[:, i] for i in range(2)], "zb")

    def emit(o, r_o, i):
        kb.dma("sp", lambda e: e.dma_start(out=xa[i * 128:(i + 1) * 128, :], in_=o), reads=[r_o], key=r_o)
        zbt, r_zb = zb_ring.next()
        kb.op("act", lambda e: e.copy(out=zbt, in_=o), reads=[r_o], writes=[r_zb])
        kb.dma("sp", lambda e: e.dma_start(out=xab[i * 128:(i + 1) * 128, :], in_=zbt), reads=[r_zb], key=r_zb)
    lnp = LNPipe(nc, kb, es, gam, bet, [r_g1, r_g2], emit)
    pm = es.enter_context(nc.psum_tensor(uq("pm"), [128, 4, 512], F32))
    pm_ring = Ring([pm[:, i] for i in range(4)], "pm")
    for i in range(NT):
        x, r_x = xin_ring.next()
        kb.dma("sp", lambda e: e.dma_start(out=x, in_=x_src[i * 128:(i + 1) * 128, :]), writes=[r_x])
        for dc in range(4):
            p, r_p = pm_ring.next()
            for kc in range(KC):
                kb.op("pe", lambda e: e.matmul(p[:, 0:512], lhsT=mixT[kc][:, i * 128:(i + 1) * 128], rhs=wo[:, kc, dc * 512:(dc + 1) * 512],
                                               start=(kc == 0), stop=(kc == KC - 1)), reads=r_mix + r_wo, writes=[r_p])
            kb.op("dve", lambda e: e.scalar_tensor_tensor(out=x[:, dc * 512:(dc + 1) * 512], in0=x[:, dc * 512:(dc + 1) * 512], scalar=ALPHA, in1=p[:, 0:512],
                                                          op0=ALU.mult, op1=ALU.add), reads=[r_x, r_p], writes=[r_x])
        lnp.feed(x, r_x, i)
    lnp.flush()


FOXH = 8
CQ, CK, CV, CF, CA, CG = 0, 1024, 2048, 3072, 3080, 4104


def even_phase(nc, kb, L, C, x_src, uT_d, vt_d, xa, xab, w, dbg=None):
    from contextlib import ExitStack
    win = w["even_w_in"][0]
    winv = win.rearrange("(kc p) n -> p kc n", p=128)
    with ExitStack() as es0:
        def sb0(name, shape, dt):
            return es0.enter_context(nc.sbuf_tensor(uq(name), shape, dt))
        ident_s = sb0("ident", [128, 128], BF16)
        r_id = Res("ident")
        kb.dma("sp", lambda e: e.dma_start(out=ident_s[:], in_=C["ident"]), writes=[r_id])
        attT = sb0("attT", [128, FOXH, S], BF16)
        r_att = [Res("att%d" % h) for h in range(FOXH)]
        with ExitStack() as es2:
            def sb2(name, shape, dt):
                return es2.enter_context(nc.sbuf_tensor(uq(name), shape, dt))

            def ps2(name, shape, dt):
                return es2.enter_context(nc.psum_tensor(uq(name), shape, dt))
            xT = sb2("xT", [128, KC, S], BF16)
            r_xT = [Res("xT%d" % i) for i in range(NT)]
            pT = ps2("pT", [128, 2, 1024], BF16)
            pT_ring = Ring([pT[:, i] for i in range(2)], "pT")
            build_xT(nc, kb, es2, x_src, xT, r_xT, ident_s, r_id, pT_ring)
            wch = sb2("wch", [128, 3, KC, 128], BF16)
            wch_ring = Ring([wch[:, i] for i in range(3)], "wch")
            pp = ps2("pp", [128, 4, 512], F32)
            pp_ring = Ring([pp[:, i] for i in range(4)], "pp")

            def proj_fm(col0, tg, wt, r_w):
                p, r_p = pp_ring.next()
                for kc in range(KC):
                    kb.op("pe", lambda e: e.matmul(p[:, 0:512], lhsT=wt[:, kc, :], rhs=xT[:, kc, tg * 512:(tg + 1) * 512],
                                                   start=(kc == 0), stop=(kc == KC - 1)), reads=[r_w] + r_xT[tg * 4:(tg + 1) * 4], writes=[r_p])
                return p, r_p

            def load_w(col0):
                wt, r_w = wch_ring.next()
                kb.dma("pool", lambda e: e.dma_start(out=wt, in_=winv[:, :, col0:col0 + 128]), writes=[r_w])
                return wt, r_w
            with ExitStack() as es3:
                def sb3(name, shape, dt):
                    return es3.enter_context(nc.sbuf_tensor(uq(name), shape, dt))
                identf = sb3("identf", [128, 128], F32)
                onesm = sb3("onesm", [128, 128], F32)
                r_c = Res("cc")
                kb.dma("sp", lambda e: e.dma_start(out=identf[:], in_=C["identf"]), writes=[r_c])
                kb.op("dve", lambda e: e.memset(onesm[:], 1.0 / 128.0), writes=[r_c])
                cw31 = sb3("cw31", [31, 1024], F32)
                r_cw = Res("cw31")
                kb.dma("sp", lambda e: e.dma_start(out=cw31[:], in_=w["even_conv_w"][0].rearrange("j o c -> j (o c)")), writes=[r_cw])
                cwT = sb3("cwT", [128, 8, 32], F32)
                r_cwT = Res("cwT")
                prm = sb3("prm", [128, 3, 8], F32)
                r_prm = Res("prm")
                with nc.allow_non_contiguous_dma(reason="tiny per-channel params"):
                    for k, nm in enumerate(("even_conv_b", "even_conv_norm_g", "even_conv_norm_b")):
                        kb.dma("sp", lambda e: e.dma_start(out=prm[:, k, :], in_=w[nm][0].rearrange("(c p) -> p c", p=128)), writes=[r_prm])
                for c in range(8):
                    p, r_p = pp_ring.next()
                    kb.op("pe", lambda e: e.transpose(out=p[:, 0:31], in_=cw31[0:31, c * 128:(c + 1) * 128], identity=identf[0:31, 0:31]),
                          reads=[r_cw, r_c], writes=[r_p])
                    kb.op("dve", lambda e: e.tensor_copy(out=cwT[:, c, 0:31], in_=p[:, 0:31]), reads=[r_p], writes=[r_cwT])
                cin = sb3("cin", [128, 2, 32 + S], BF16)
                cin_ring = Ring([cin[:, i] for i in range(2)], "cin")
                kb.op("dve", lambda e: e.memset(cin[:, :, 0:32], 0.0), writes=cin_ring.r)
                dg = sb3("dg", [128, 2, 31, 128], BF16)
                dg_ring = Ring([dg[:, i] for i in range(2)], "dg")
                sgs = sb3("sgs", [128, 2, 512], F32)
                sg_ring = Ring([sgs[:, i] for i in range(2)], "sgs")
                tb = sb3("tb", [128, 2, 4, 512], F32)
                tb_ring = Ring([tb[:, i] for i in range(2)], "tb")
                uo = sb3("uo", [128, 2, 512], BF16)
                uo_ring = Ring([uo[:, i] for i in range(2)], "uo")
                for c in range(8):
                    wa, r_wa = load_w(CA + c * 128)
                    wg, r_wg = load_w(CG + c * 128)
                    ci, r_ci = cin_ring.next()
                    for tg in range(4):
                        pa, r_pa = proj_fm(CA, tg, wa, r_wa)
                        pg, r_pg = proj_fm(CG, tg, wg, r_wg)
                        sg, r_sg = sg_ring.next()
                        kb.op("act", lambda e: e.activation(out=sg, in_=pg[:, 0:512], func=AF.Sigmoid), reads=[r_pg], writes=[r_sg])
                        kb.op("dve", lambda e: e.tensor_tensor(out=ci[:, 32 + tg * 512:32 + (tg + 1) * 512], in0=sg, in1=pa[:, 0:512], op=ALU.mult),
                              reads=[r_sg, r_pa], writes=[r_ci])
                    dgt, r_dg = dg_ring.next()
                    for j in range(31):
                        kb.op("dve", lambda e: e.tensor_scalar(out=dgt[:, j, :], in0=identf[:], scalar1=cwT[:, c, j:j + 1], scalar2=None, op0=ALU.mult),
                              reads=[r_c, r_cwT], writes=[r_dg])
                    for tg in range(4):
                        pc, r_pc = pp_ring.next()
                        for j in range(31):
                            o0 = 2 + j + tg * 512
                            kb.op("pe", lambda e: e.matmul(pc[:, 0:512], lhsT=dgt[:, j, :], rhs=ci[:, o0:o0 + 512], start=(j == 0), stop=(j == 30)),
                                  reads=[r_dg, r_ci], writes=[r_pc])
                        t4, r_t = tb_ring.next()
                        ut = t4[:, 0]; dd = t4[:, 1]; sq = t4[:, 2]; rs = t4[:, 3]
                        kb.op("act", lambda e: e.activation(out=ut, in_=pc[:, 0:512], func=AF.Identity, bias=prm[:, 0, c:c + 1]), reads=[r_pc, r_prm], writes=[r_t])
                        pmn, r_pmn = pp_ring.next()
                        kb.op("pe", lambda e: e.matmul(pmn[:, 0:512], lhsT=onesm[:], rhs=ut, start=True, stop=True), reads=[r_t, r_c], writes=[r_pmn])
                        kb.op("dve", lambda e: e.tensor_tensor(out=dd, in0=ut, in1=pmn[:, 0:512], op=ALU.subtract), reads=[r_t, r_pmn], writes=[r_t])
                        kb.op("act", lambda e: e.activation(out=sq, in_=dd, func=AF.Square), reads=[r_t], writes=[r_t])
                        pvr, r_pvr = pp_ring.next()
                        kb.op("pe", lambda e: e.matmul(pvr[:, 0:512], lhsT=onesm[:], rhs=sq, start=True, stop=True), reads=[r_t, r_c], writes=[r_pvr])
                        kb.op("dve", lambda e: e.tensor_scalar_add(out=rs, in0=pvr[:, 0:512], scalar1=LN_EPS), reads=[r_pvr], writes=[r_t])
                        kb.op("act", lambda e: e.sqrt(out=rs, in_=rs), reads=[r_t], writes=[r_t])
                        kb.op("dve", lambda e: e.reciprocal(out=rs, in_=rs), reads=[r_t], writes=[r_t])
                        kb.op("dve", lambda e: e.tensor_tensor(out=dd, in0=dd, in1=rs, op=ALU.mult), reads=[r_t], writes=[r_t])
                        kb.op("dve", lambda e: e.tensor_scalar(out=dd, in0=dd, scalar1=prm[:, 1, c:c + 1], scalar2=prm[:, 2, c:c + 1], op0=ALU.mult, op1=ALU.add),
                              reads=[r_t, r_prm], writes=[r_t])
                        uot, r_uo = uo_ring.next()
                        kb.op("act", lambda e: e.activation(out=uot, in_=dd, func=AF.Silu), reads=[r_t], writes=[r_uo])
                        kb.dma("sp", lambda e: e.dma_start(out=uT_d[c * 128:(c + 1) * 128, tg * 512:(tg + 1) * 512], in_=uot), reads=[r_uo], key=r_uo)
        kb.barrier()
        with ExitStack() as es1:
            def sb1(name, shape, dt):
                return es1.enter_context(nc.sbuf_tensor(uq(name), shape, dt))
            qT = sb1("qT", [128, FOXH, S], BF16)
            kT = sb1("kT", [128, FOXH, S], BF16)
            fl = sb1("fl", [128, NT, FOXH], F32)
            r_q = [Res("q%d" % h) for h in range(FOXH)]
            r_k = [Res("k%d" % h) for h in range(FOXH)]
            r_fl = Res("fl")
            with ExitStack() as es2:
                def sb2(name, shape, dt):
                    return es2.enter_context(nc.sbuf_tensor(uq(name), shape, dt))

                def ps2(name, shape, dt):
                    return es2.enter_context(nc.psum_tensor(uq(name), shape, dt))
                xT = sb2("xT", [128, KC, S], BF16)
                r_xT = [Res("xT%d" % i) for i in range(NT)]
                pT = ps2("pT", [128, 2, 1024], BF16)
                pT_ring = Ring([pT[:, i] for i in range(2)], "pT")
                build_xT(nc, kb, es2, x_src, xT, r_xT, ident_s, r_id, pT_ring)
                wch = sb2("wch", [128, 3, KC, 128], BF16)
                wch_ring = Ring([wch[:, i] for i in range(3)], "wch")
                pp = ps2("pp", [128, 4, 512], F32)
                pp_ring = Ring([pp[:, i] for i in range(4)], "pp")
                nev = 0

                def proj_fm(col0, tg, wt, r_w):
                    p, r_p = pp_ring.next()
                    for kc in range(KC):
                        kb.op("pe", lambda e: e.matmul(p[:, 0:512], lhsT=wt[:, kc, :], rhs=xT[:, kc, tg * 512:(tg + 1) * 512],
                                                       start=(kc == 0), stop=(kc == KC - 1)), reads=[r_w] + r_xT[tg * 4:(tg + 1) * 4], writes=[r_p])
                    return p, r_p

                def load_w(col0):
                    wt, r_w = wch_ring.next()
                    kb.dma("pool", lambda e: e.dma_start(out=wt, in_=winv[:, :, col0:col0 + 128]), writes=[r_w])
                    return wt, r_w

                for h in range(FOXH):
                    for (c0, dst, rr, scl) in ((CQ, qT, r_q, 128.0 ** -0.5), (CK, kT, r_k, 1.0)):
                        wt, r_w = load_w(c0 + h * 128)
                        for tg in range(4):
                            p, r_p = proj_fm(c0, tg, wt, r_w)
                            if nev % 2 == 0:
                                kb.op("act", lambda e: e.mul(out=dst[:, h, tg * 512:(tg + 1) * 512], in_=p[:, 0:512], mul=scl), reads=[r_p], writes=[rr[h]])
                            else:
                                kb.op("dve", lambda e: e.tensor_scalar(out=dst[:, h, tg * 512:(tg + 1) * 512], in0=p[:, 0:512], scalar1=scl, scalar2=None, op0=ALU.mult),
                                      reads=[r_p], writes=[rr[h]])
                            nev += 1
                with ExitStack() as es3:
                    wv = es3.enter_context(nc.sbuf_tensor(uq("wv"), [128, KC, 520], BF16))
                    r_wv = Res("wv")
                    wf32 = es3.enter_context(nc.sbuf_tensor(uq("wf32"), [128, KC, 8], F32))
                    wfb = es3.enter_context(nc.sbuf_tensor(uq("wfb"), [128, KC, 128], BF16))
                    r_wf = Res("wf")
                    vst = es3.enter_context(nc.sbuf_tensor(uq("vst"), [128, 4, 512], BF16))
                    vst_ring = Ring([vst[:, i] for i in range(4)], "vst")
                    for half in range(2):
                        ncol = 520 if half == 0 else 512
                        if half == 0:
                            kb.dma("pool", lambda e: e.dma_start(out=wv[:, :, 0:512], in_=winv[:, :, CV:CV + 512]), writes=[r_wv])
                            kb.dma("sp", lambda e: e.dma_start(out=wf32[:], in_=winv[:, :, CF:CF + 8]), writes=[r_wf])
                            if dbg is not None:
                                kb.dma("sp", lambda e: e.dma_start(out=dbg["wf"], in_=wf32[:].rearrange("p a b -> p (a b)")), reads=[r_wf], key=Res("dbgwf"))
                            kb.op("dve", lambda e: e.memset(wfb[:], 0.0), writes=[r_wf])
                            kb.op("dve", lambda e: e.tensor_copy(out=wfb[:, :, 0:8], in_=wf32[:]), reads=[r_wf], writes=[r_wf])
                        else:
                            kb.dma("pool", lambda e: e.dma_start(out=wv[:, :, 0:512], in_=winv[:, :, CV + 512:CV + 1024]), writes=[r_wv])
                        for i in range(NT):
                            p, r_p = pp_ring.next()
                            for kc in range(KC):
                                kb.op("pe", lambda e: e.matmul(p[:, 0:512], lhsT=xT[:, kc, i * 128:(i + 1) * 128], rhs=wv[:, kc, 0:512],
                                                               start=(kc == 0), stop=(kc == KC - 1)), reads=[r_wv, r_xT[i]], writes=[r_p])
                            vs, r_vs = vst_ring.next()
                            if nev % 2 == 0:
                                kb.op("act", lambda e: e.copy(out=vs, in_=p[:, 0:512]), reads=[r_p], writes=[r_vs])
                            else:
                                kb.op("dve", lambda e: e.tensor_copy(out=vs, in_=p[:, 0:512]), reads=[r_p], writes=[r_vs])
                            nev += 1
                            kb.dma("sp", lambda e: e.dma_start(out=vt_d[i * 128:(i + 1) * 128, half * 512:(half + 1) * 512], in_=vs), reads=[r_vs], key=r_vs)
                            if half == 0:
                                p, r_p = pp_ring.next()
                                for kc in range(KC):
                                    kb.op("pe", lambda e: e.matmul(p[:, 0:128], lhsT=xT[:, kc, i * 128:(i + 1) * 128], rhs=wfb[:, kc, :],
                                                                   start=(kc == 0), stop=(kc == KC - 1)), reads=[r_wf, r_xT[i]], writes=[r_p])
                                kb.op("act", lambda e: e.copy(out=fl[:, i, :], in_=p[:, 0:8]), reads=[r_p], writes=[r_fl])
                if dbg is not None:
                    kb.dma("sp", lambda e: e.dma_start(out=dbg["fl2"], in_=fl[:].rearrange("p a b -> p (a b)")), reads=[r_fl], key=Res("dbgfl2"))
                kb.barrier()
            kb.barrier()
            with ExitStack() as es2:
                def sb2(name, shape, dt):
                    return es2.enter_context(nc.sbuf_tensor(uq(name), shape, dt))

                def ps2(name, shape, dt):
                    return es2.enter_context(nc.psum_tensor(uq(name), shape, dt))
                vt = sb2("vt", [128, NT, 1024], BF16)
                r_v = [Res("v%d" % i) for i in range(NT)]
                for i in range(NT):
                    kb.dma("sp", lambda e: e.dma_start(out=vt[:, i, :], in_=vt_d[i * 128:(i + 1) * 128, :]), writes=[r_v[i]])
                uinc = sb2("uinc", [128, 128], F32)
                m64 = sb2("m64", [128, 128], F32)
                onesf = sb2("onesf", [128, 128], F32)
                ones_b = sb2("ones_b", [128, 128], BF16)
                cmask = sb2("cmask", [128, 128], BF16)
                bfb = sb2("bfb", [128, FOXH], F32)
                r_c = Res("attc")
                kb.dma("sp", lambda e: e.dma_start(out=uinc[:], in_=C["uinc"]), writes=[r_c])
                r_c2 = Res("attc2")
                kb.dma("sp", lambda e: e.dma_start(out=m64[:], in_=C["m64"]), writes=[r_c2])
                r_c3 = Res("attc3")
                kb.dma("sp", lambda e: e.dma_start(out=cmask[:], in_=C["cmask"]), writes=[r_c3])
                r_c4 = Res("attc4")
                kb.dma("sp", lambda e: e.dma_start(out=bfb[:], in_=w["even_b_f"][0].partition_broadcast(128)), writes=[r_c4])
                kb.op("dve", lambda e: e.memset(onesf[:], 1.0), writes=[r_c])
                kb.op("dve", lambda e: e.memset(ones_b[:], 1.0), writes=[r_c])
                lf = sb2("lf", [128, NT, FOXH], F32)
                r_lf = Res("lf")
                kb.op("dve", lambda e: e.tensor_tensor(out=lf[:], in0=fl[:], in1=bfb[:].unsqueeze(1).to_broadcast([128, NT, FOXH]), op=ALU.add),
                      reads=[r_fl, r_c4], writes=[r_lf])
                kb.op("act", lambda e: e.activation(out=lf[:], in_=lf[:], func=AF.Exp, scale=-1.0), reads=[r_lf], writes=[r_lf])
                kb.op("act", lambda e: e.activation(out=lf[:], in_=lf[:], func=AF.Ln, bias=1.0), reads=[r_lf], writes=[r_lf])
                kb.op("dve", lambda e: e.tensor_scalar(out=lf[:], in0=lf[:], scalar1=-1.0, scalar2=None, op0=ALU.mult), reads=[r_lf], writes=[r_lf])
                c_all = sb2("c_all", [128, NT, FOXH], F32)
                cref = sb2("cref", [128, NT, FOXH], F32)
                lfcum = sb2("lfcum", [128, FOXH], F32)
                r_call = Res("c_all"); r_cref = Res("cref"); r_lfc = Res("lfcum")
                kb.op("dve", lambda e: e.memset(lfcum[:], 0.0), writes=[r_lfc])
                pcs = ps2("pcs", [128, 2, 512], F32)
                pcs_ring = Ring([pcs[:, i] for i in range(2)], "pcs")
                for i in range(NT):
                    p, r_p = pcs_ring.next()
                    kb.op("pe", lambda e: e.matmul(p[:, 0:FOXH], lhsT=uinc[:], rhs=lf[:, i, :], start=True, stop=False), reads=[r_lf, r_c], writes=[r_p])
                    kb.op("pe", lambda e: e.matmul(p[:, 0:FOXH], lhsT=onesf[:], rhs=lfcum[:], start=False, stop=True), reads=[r_lfc, r_c], writes=[r_p])
                    kb.op("dve", lambda e: e.tensor_copy(out=c_all[:, i, :], in_=p[:, 0:FOXH]), reads=[r_p], writes=[r_call])
                    p2, r_p2 = pcs_ring.next()
                    kb.op("pe", lambda e: e.matmul(p2[:, 0:FOXH], lhsT=m64[:], rhs=lf[:, i, :], start=True, stop=False), reads=[r_lf, r_c2], writes=[r_p2])
                    kb.op("pe", lambda e: e.matmul(p2[:, 0:FOXH], lhsT=onesf[:], rhs=lfcum[:], start=False, stop=True), reads=[r_lfc, r_c], writes=[r_p2])
                    kb.op("dve", lambda e: e.tensor_copy(out=cref[:, i, :], in_=p2[:, 0:FOXH]), reads=[r_p2], writes=[r_cref])
                    kb.op("dve", lambda e: e.tensor_tensor(out=lfcum[:], in0=lfcum[:], in1=lf[:, i, :], op=ALU.add), reads=[r_lf, r_lfc], writes=[r_lfc])
                if dbg is not None:
                    rd2 = Res("dbg2")
                    kb.dma("sp", lambda e: e.dma_start(out=dbg["c_all"], in_=c_all[:].rearrange("p a b -> p (a b)")), reads=[r_call], key=rd2)
                    kb.dma("sp", lambda e: e.dma_start(out=dbg["cref"], in_=cref[:].rearrange("p a b -> p (a b)")), reads=[r_cref], key=rd2)
                    kb.dma("sp", lambda e: e.dma_start(out=dbg["lf"], in_=lf[:].rearrange("p a b -> p (a b)")), reads=[r_lf], key=rd2)
                    kb.dma("sp", lambda e: e.dma_start(out=dbg["fl"], in_=fl[:].rearrange("p a b -> p (a b)")), reads=[r_fl], key=rd2)
                    kb.dma("sp", lambda e: e.dma_start(out=dbg["bfb"], in_=bfb[:]), reads=[r_c4], key=rd2)
                bias_all = sb2("bias_all", [128, FOXH, NT, NT], F32)
                r_bias = Res("bias")
                for h in range(FOXH):
                    for qb in range(NT):
                        kb.op("dve", lambda e: e.tensor_scalar(out=bias_all[:, h, qb, :], in0=c_all[:, :, h], scalar1=-1.0, scalar2=cref[:, qb, h:h + 1],
                                                               op0=ALU.mult, op1=ALU.add), reads=[r_call, r_cref], writes=[r_bias])
                pst = ps2("pst", [128, 2, 512], F32)
                pst_ring = Ring([pst[:, i] for i in range(2)], "pst")
                po = ps2("po", [128, 2, 512], F32)
                po_ring = Ring([po[:, i] for i in range(2)], "po")
                pr = ps2("pr", [128, 2, 512], F32)
                pr_ring = Ring([pr[:, i] for i in range(2)], "pr")
                PT = sb2("PT", [128, 8, 128], BF16)
                PT_ring = Ring([PT[:, i] for i in range(8)], "PT")
                rcp = sb2("rcp", [128, 2, 128], F32)
                rcp_ring = Ring([rcp[:, i] for i in range(2)], "rcp")
                for h in range(FOXH):
                    for qb in range(NT):
                        pot, r_po = po_ring.next()
                        prt, r_pr = pr_ring.next()
                        nkb = qb + 1
                        for k0 in range(0, nkb, 4):
                            st, r_st = pst_ring.next()
                            kbs = list(range(k0, min(k0 + 4, nkb)))
                            for n, kbk in enumerate(kbs):
                                kb.op("pe", lambda e: e.matmul(st[:, n * 128:(n + 1) * 128], lhsT=kT[:, h, kbk * 128:(kbk + 1) * 128], rhs=qT[:, h, qb * 128:(qb + 1) * 128],
                                                               start=True, stop=True), reads=[r_k[h], r_q[h]], writes=[r_st])
                            pts = []
                            for n, kbk in enumerate(kbs):
                                pt, r_pt = PT_ring.next()
                                kb.op("act", lambda e: e.activation(out=pt, in_=st[:, n * 128:(n + 1) * 128], func=AF.Exp, bias=bias_all[:, h, qb, kbk:kbk + 1]),
                                      reads=[r_st, r_bias], writes=[r_pt])
                                if kbk == qb:
                                    kb.op("dve", lambda e: e.tensor_tensor(out=pt, in0=pt, in1=cmask[:], op=ALU.mult), reads=[r_pt, r_c3], writes=[r_pt])
                                pts.append((kbk, pt, r_pt))
                            for kbk, pt, r_pt in pts:
                                kb.op("pe", lambda e: e.matmul(pot[:, 0:128], lhsT=vt[:, kbk, h * 128:(h + 1) * 128], rhs=pt, start=(kbk == 0), stop=(kbk == qb)),
                                      reads=[r_v[kbk], r_pt], writes=[r_po])
                                kb.op("pe", lambda e: e.matmul(prt[:, 0:128], lhsT=ones_b[:], rhs=pt, start=(kbk == 0), stop=(kbk == qb)),
                                      reads=[r_c, r_pt], writes=[r_pr])
                        rc, r_rc = rcp_ring.next()
                        kb.op("dve", lambda e: e.reciprocal(out=rc, in_=prt[:, 0:128]), reads=[r_pr], writes=[r_rc])
                        kb.op("dve", lambda e: e.tensor_tensor(out=attT[:, h, qb * 128:(qb + 1) * 128], in0=rc, in1=pot[:, 0:128], op=ALU.mult),
                              reads=[r_rc, r_po], writes=[r_att[h]])
        kb.barrier()
        if dbg is not None:
            rd = Res("dbg")
            for h in range(FOXH):
                kb.dma("sp", lambda e: e.dma_start(out=dbg["att"][h * 128:(h + 1) * 128, :], in_=attT[:, h, :]), reads=[r_att[h]], key=rd)
        with ExitStack() as es2:
            uTs = es2.enter_context(nc.sbuf_tensor(uq("uTs"), [128, 8, S], BF16))
            r_u = [Res("uT%d" % c) for c in range(8)]
            for c in range(8):
                kb.dma("sp", lambda e: e.dma_start(out=uTs[:, c, :], in_=uT_d[c * 128:(c + 1) * 128, :]), writes=[r_u[c]])
            chunks = [attT[:, h, :] for h in range(FOXH)] + [uTs[:, c, :] for c in range(8)]
            mix_out_phase(nc, kb, es2, L, chunks, r_att + r_u, w["even_w_out"][0], x_src, xa, xab, w, C)
    kb.barrier()


def make_consts():
    bf = ml_dtypes.bfloat16
    c = {}
    c["ident"] = np.eye(128, dtype=np.float32).astype(bf)
    c["iota"] = np.tile(np.arange(512, dtype=np.float32)[None, :], (128, 1))
    ti = np.zeros((128, NT, 4), np.float32)
    ti[:, :, 0] = np.arange(128)[:, None]
    ti[:, :, 1] = np.arange(NT)[None, :]
    ti[:, :, 2] = 1.0
    c["tokinfo"] = ti.astype(bf)
    c["lstrict"] = np.triu(np.ones((128, 128), np.float32), 1).astype(bf)
    c["ecap"] = np.tile((np.arange(NE, dtype=np.float32) * CAP)[None, :], (128, 1))
    c["identf"] = np.eye(128, dtype=np.float32)
    c["uinc"] = np.triu(np.ones((128, 128), np.float32), 0)
    m64 = np.zeros((128, 128), np.float32); m64[:65, :] = 1.0
    c["m64"] = m64
    c["cmask"] = np.triu(np.ones((128, 128), np.float32), 0).astype(bf)
    c["lgt16"] = np.tril(np.ones((128, 128), np.float32), -1) * (-1.0 / 16.0)
    c["uinc16"] = np.triu(np.ones((128, 128), np.float32), 0) * (-1.0 / 16.0)
    return c


CONST_DT = {"ident": BF16, "iota": F32, "tokinfo": BF16, "lstrict": BF16, "ecap": F32, "identf": F32, "uinc": F32, "m64": F32, "cmask": BF16, "lgt16": F32, "uinc16": F32}


GH = 4
OQ, OK_, OV, OG, OA = 0, 1024, 2048, 4096, 6144


def odd_phase(nc, kb, L, C, x_src, kt_d, vt_d, gt_d, o_d, xa, xab, w):
    from contextlib import ExitStack
    win = w["odd_w_in"][0]
    winv = win.rearrange("(kc p) n -> p kc n", p=128)
    with ExitStack() as es0:
        def sb0(name, shape, dt):
            return es0.enter_context(nc.sbuf_tensor(uq(name), shape, dt))
        ident_s = sb0("ident", [128, 128], BF16)
        r_id = Res("ident")
        kb.dma("sp", lambda e: e.dma_start(out=ident_s[:], in_=C["ident"]), writes=[r_id])
        with ExitStack() as es1:
            def sb1(name, shape, dt):
                return es1.enter_context(nc.sbuf_tensor(uq(name), shape, dt))
            qT = sb1("qT", [128, 8, S], BF16)
            kT = sb1("kT", [128, 8, S], BF16)
            alT = sb1("alT", [16, S], BF16)
            r_q = [Res("q%d" % c) for c in range(8)]
            r_k = [Res("k%d" % c) for c in range(8)]
            r_al = Res("alT")
            with ExitStack() as es2:
                def sb2(name, shape, dt):
                    return es2.enter_context(nc.sbuf_tensor(uq(name), shape, dt))

                def ps2(name, shape, dt):
                    return es2.enter_context(nc.psum_tensor(uq(name), shape, dt))
                xT = sb2("xT", [128, KC, S], BF16)
                r_xT = [Res("xT%d" % i) for i in range(NT)]
                pT = ps2("pT", [128, 2, 1024], BF16)
                pT_ring = Ring([pT[:, i] for i in range(2)], "pT")
                build_xT(nc, kb, es2, x_src, xT, r_xT, ident_s, r_id, pT_ring)
                wch = sb2("wch", [128, 3, KC, 128], BF16)
                wch_ring = Ring([wch[:, i] for i in range(3)], "wch")
                pp = ps2("pp", [128, 4, 512], F32)
                pp_ring = Ring([pp[:, i] for i in range(4)], "pp")
                nev = 0
                for (c0, dst, rr) in ((OQ, qT, r_q), (OK_, kT, r_k)):
                    for c in range(8):
                        wt, r_w = wch_ring.next()
                        kb.dma("pool", lambda e: e.dma_start(out=wt, in_=winv[:, :, c0 + c * 128:c0 + (c + 1) * 128]), writes=[r_w])
                        for tg in range(4):
                            p, r_p = pp_ring.next()
                            for kc in range(KC):
                                kb.op("pe", lambda e: e.matmul(p[:, 0:512], lhsT=wt[:, kc, :], rhs=xT[:, kc, tg * 512:(tg + 1) * 512],
                                                               start=(kc == 0), stop=(kc == KC - 1)), reads=[r_w] + r_xT[tg * 4:(tg + 1) * 4], writes=[r_p])
                            if nev % 2 == 0:
                                kb.op("act", lambda e: e.copy(out=dst[:, c, tg * 512:(tg + 1) * 512], in_=p[:, 0:512]), reads=[r_p], writes=[rr[c]])
                            else:
                                kb.op("dve", lambda e: e.tensor_copy(out=dst[:, c, tg * 512:(tg + 1) * 512], in_=p[:, 0:512]), reads=[r_p], writes=[rr[c]])
                            nev += 1
                wa32 = sb2("wa32", [128, KC, 16], F32)
                wab = sb2("wab", [128, KC, 16], BF16)
                r_wa = Res("wa")
                kb.dma("sp", lambda e: e.dma_start(out=wa32[:], in_=winv[:, :, OA:OA + 16]), writes=[r_wa])
                kb.op("dve", lambda e: e.tensor_copy(out=wab[:], in_=wa32[:]), reads=[r_wa], writes=[r_wa])
                for tg in range(4):
                    p, r_p = pp_ring.next()
                    for kc in range(KC):
                        kb.op("pe", lambda e: e.matmul(p[0:16, 0:512], lhsT=wab[:, kc, :], rhs=xT[:, kc, tg * 512:(tg + 1) * 512],
                                                       start=(kc == 0), stop=(kc == KC - 1)), reads=[r_wa] + r_xT[tg * 4:(tg + 1) * 4], writes=[r_p])
                    kb.op("dve", lambda e: e.tensor_copy(out=alT[0:16, tg * 512:(tg + 1) * 512], in_=p[0:16, 0:512]), reads=[r_p], writes=[r_al])
                wtm = sb2("wtm", [128, 2, KC, 512], BF16)
                wtm_ring = Ring([wtm[:, i] for i in range(2)], "wtm")
                stg = sb2("stg", [128, 4, 512], BF16)
                stg_ring = Ring([stg[:, i] for i in range(4)], "stg")
                for cg in range(10):
                    col0 = OK_ + cg * 512
                    if cg < 2:
                        dd, dcol = kt_d, cg * 512
                    elif cg < 6:
                        dd, dcol = vt_d, (cg - 2) * 512
                    else:
                        dd, dcol = gt_d, (cg - 6) * 512
                    wt, r_w = wtm_ring.next()
                    kb.dma("pool", lambda e: e.dma_start(out=wt, in_=winv[:, :, col0:col0 + 512]), writes=[r_w])
                    for i in range(NT):
                        p, r_p = pp_ring.next()
                        for kc in range(KC):
                            kb.op("pe", lambda e: e.matmul(p[:, 0:512], lhsT=xT[:, kc, i * 128:(i + 1) * 128], rhs=wt[:, kc, :],
                                                           start=(kc == 0), stop=(kc == KC - 1)), reads=[r_w, r_xT[i]], writes=[r_p])
                        st, r_st = stg_ring.next()
                        if nev % 2 == 0:
                            kb.op("act", lambda e: e.copy(out=st, in_=p[:, 0:512]), reads=[r_p], writes=[r_st])
                        else:
                            kb.op("dve", lambda e: e.tensor_copy(out=st, in_=p[:, 0:512]), reads=[r_p], writes=[r_st])
                        nev += 1
                        kb.dma("sp", lambda e: e.dma_start(out=dd[i * 128:(i + 1) * 128, dcol:dcol + 512], in_=st), reads=[r_st], key=r_st)
            kb.barrier()
            with ExitStack() as es2:
                def sb2(name, shape, dt):
                    return es2.enter_context(nc.sbuf_tensor(uq(name), shape, dt))

                def ps2(name, shape, dt):
                    return es2.enter_context(nc.psum_tensor(uq(name), shape, dt))
                lgt = sb2("lgt", [128, 128], F32)
                uin = sb2("uin", [128, 128], F32)
                cmask = sb2("cmask", [128, 128], F32)
                wa2 = sb2("wa2", [16, 1024], F32)
                wa2b = sb2("wa2b", [16, 1024], BF16)
                ba = sb2("ba", [128, 1024], F32)
                ng = sb2("ng", [128, 2048], F32)
                rc = [Res("oc%d" % i) for i in range(6)]
                kb.dma("sp", lambda e: e.dma_start(out=lgt[:], in_=C["lgt16"]), writes=[rc[0]])
                kb.dma("sp", lambda e: e.dma_start(out=uin[:], in_=C["uinc16"]), writes=[rc[1]])
                kb.dma("sp", lambda e: e.dma_start(out=cmask[:], in_=C["uinc"]), writes=[rc[2]])
                kb.dma("sp", lambda e: e.dma_start(out=wa2[:], in_=w["odd_w_a2"][0]), writes=[rc[3]])
                kb.op("dve", lambda e: e.tensor_copy(out=wa2b[:], in_=wa2[:]), reads=[rc[3]], writes=[rc[3]])
                kb.dma("sp", lambda e: e.dma_start(out=ba[:], in_=w["odd_b_a"][0].partition_broadcast(128)), writes=[rc[4]])
                kb.dma("sp", lambda e: e.dma_start(out=ng[:], in_=w["odd_norm_g"][0].partition_broadcast(128)), writes=[rc[5]])
                state = sb2("state", [128, 8, 512], F32)
                stateb = sb2("stateb", [128, 8, 512], BF16)
                r_state = [Res("st%d" % c) for c in range(8)]
                r_stateb = [Res("stb%d" % c) for c in range(8)]
                kb.op("dve", lambda e: e.memset(state[:], 0.0), writes=r_state)
                kb.op("dve", lambda e: e.memset(stateb[:], 0.0), writes=r_stateb)
                lnv = sb2("lnv", [128, 2, 1024], F32)
                lnv_ring = Ring([lnv[:, i] for i in range(2)], "lnv")
                ktk = sb2("ktk", [128, 2, 1024], BF16)
                ktk_ring = Ring([ktk[:, i] for i in range(2)], "ktk")
                vtk = sb2("vtk", [128, 2, 2048], BF16)
                vtk_ring = Ring([vtk[:, i] for i in range(2)], "vtk")
                gtk = sb2("gtk", [128, 2, 2048], BF16)
                gtk_ring = Ring([gtk[:, i] for i in range(2)], "gtk")
                gg = sb2("gg", [128, 2, 2048], BF16)
                gg_ring = Ring([gg[:, i] for i in range(2)], "gg")
                ebm = sb2("ebm", [128, 1, 1024], F32)
                ebm_ring = Ring([ebm[:, i] for i in range(1)], "ebm")
                kend = sb2("kend", [128, 2, 1024], BF16)
                kend_ring = Ring([kend[:, i] for i in range(2)], "kend")
                eb = sb2("eb", [128, 2, 8, 128], F32)
                eb_ring = Ring([eb[:, i] for i in range(2)], "eb")
                enb = sb2("enb", [128, 2, 8, 128], F32)
                enb_ring = Ring([enb[:, i] for i in range(2)], "enb")
                qt = sb2("qt", [128, 2, 8, 128], BF16)
                qt_ring = Ring([qt[:, i] for i in range(2)], "qt")
                ktt = sb2("ktt", [128, 2, 8, 128], BF16)
                ktt_ring = Ring([ktt[:, i] for i in range(2)], "ktt")
                attn = sb2("attn", [128, 2, 128], BF16)
                attn_ring = Ring([attn[:, i] for i in range(2)], "attn")
                osb = sb2("osb", [128, 2, 2048], BF16)
                osb_ring = Ring([osb[:, i] for i in range(2)], "osb")
                sm = sb2("sm", [128, 8], F32)
                r_sm = Res("sm")
                junk = sb2("junk", [128, 512], F32)
                r_junk = Res("junk")
                pA = ps2("pA", [128, 2, 512], F32)
                pA_ring = Ring([pA[:, i] for i in range(2)], "pA")
                pB = ps2("pB", [128, 2, 512], F32)
                pB_ring = Ring([pB[:, i] for i in range(2)], "pB")
                pS = ps2("pS", [128, 1, 512], F32)
                pS_ring = Ring([pS[:, i] for i in range(1)], "pS")
                pO = ps2("pO", [128, 1, 512], F32)
                pO_ring = Ring([pO[:, i] for i in range(1)], "pO")
                pU = ps2("pU", [128, 2, 512], F32)
                pU_ring = Ring([pU[:, i] for i in range(2)], "pU")
                QS = 256.0 ** -0.5
                def make_pre(i):
                    ts = slice(i * 128, (i + 1) * 128)
                    P = {}

                    def q0():
                        P["kt"], P["r_kt"] = ktk_ring.next()
                        kb.dma("sp", lambda e: e.dma_start(out=P["kt"], in_=kt_d[ts, :]), writes=[P["r_kt"]])
                        P["vt"], P["r_vt"] = vtk_ring.next()
                        kb.dma("sp", lambda e: e.dma_start(out=P["vt"], in_=vt_d[ts, :]), writes=[P["r_vt"]])
                        gt_, r_gt = gtk_ring.next()
                        kb.dma("sp", lambda e: e.dma_start(out=gt_, in_=gt_d[ts, :]), writes=[r_gt])
                        lv, r_lv = lnv_ring.next()
                        P["lv"], P["r_lv"] = lv, r_lv
                        for hf in range(2):
                            p, r_p = pA_ring.next()
                            kb.op("pe", lambda e: e.matmul(p[:, 0:512], lhsT=alT[0:16, ts], rhs=wa2b[0:16, hf * 512:(hf + 1) * 512], start=True, stop=True),
                                  reads=[r_al, rc[3]], writes=[r_p])
                            kb.op("dve", lambda e: e.tensor_tensor(out=lv[:, hf * 512:(hf + 1) * 512], in0=p[:, 0:512], in1=ba[:, hf * 512:(hf + 1) * 512], op=ALU.add),
                                  reads=[r_p, rc[4]], writes=[r_lv])
                        kb.op("act", lambda e: e.activation(out=lv, in_=lv, func=AF.Exp, scale=-1.0), reads=[r_lv], writes=[r_lv])
                        kb.op("act", lambda e: e.activation(out=lv, in_=lv, func=AF.Ln, bias=1.0), reads=[r_lv], writes=[r_lv])
                        ggt, r_gg = gg_ring.next()
                        P["gg"], P["r_gg"] = ggt, r_gg
                        kb.op("act", lambda e: e.activation(out=ggt, in_=gt_, func=AF.Silu), reads=[r_gt], writes=[r_gg])
                        kb.op("dve", lambda e: e.tensor_tensor(out=ggt, in0=ggt, in1=ng[:], op=ALU.mult), reads=[r_gg, rc[5]], writes=[r_gg])

                    def q1():
                        lv, r_lv = P["lv"], P["r_lv"]
                        em, r_em = ebm_ring.next()
                        ke, r_ke = kend_ring.next()
                        P["ke"], P["r_ke"] = ke, r_ke
                        for hf in range(2):
                            p, r_p = pA_ring.next()
                            kb.op("pe", lambda e: e.matmul(p[:, 0:512], lhsT=lgt[:], rhs=lv[:, hf * 512:(hf + 1) * 512], start=True, stop=True),
                                  reads=[r_lv, rc[0]], writes=[r_p])
                            kb.op("act", lambda e: e.activation(out=em[:, hf * 512:(hf + 1) * 512], in_=p[:, 0:512], func=AF.Exp), reads=[r_p], writes=[r_em])
                        kb.op("dve", lambda e: e.tensor_tensor(out=ke, in0=em, in1=P["kt"], op=ALU.mult), reads=[r_em, P["r_kt"]], writes=[r_ke])

                    def q2():
                        lv, r_lv = P["lv"], P["r_lv"]
                        P["eb"], P["r_eb"] = eb_ring.next()
                        P["en"], P["r_en"] = enb_ring.next()
                        for hf in range(2):
                            p, r_p = pB_ring.next()
                            for q in range(4):
                                c = hf * 4 + q
                                kb.op("pe", lambda e: e.matmul(p[:, q * 128:(q + 1) * 128], lhsT=lv[:, c * 128:(c + 1) * 128], rhs=uin[:], start=True, stop=True),
                                      reads=[r_lv, rc[1]], writes=[r_p])
                            pv = p[:, 0:512].rearrange("p (a b) -> p a b", a=4)
                            kb.op("act", lambda e: e.activation(out=P["eb"][:, hf * 4:(hf + 1) * 4, :], in_=pv, func=AF.Exp), reads=[r_p], writes=[P["r_eb"]])
                            kb.op("act", lambda e: e.activation(out=P["en"][:, hf * 4:(hf + 1) * 4, :], in_=pv, func=AF.Exp, scale=-1.0), reads=[r_p], writes=[P["r_en"]])

                    def q3():
                        P["qt"], P["r_qt"] = qt_ring.next()
                        P["kt2"], P["r_kt2"] = ktt_ring.next()
                        kb.op("dve", lambda e: e.scalar_tensor_tensor(out=P["qt"], in0=qT[:, :, ts], scalar=QS, in1=P["eb"], op0=ALU.mult, op1=ALU.mult),
                              reads=r_q + [P["r_eb"]], writes=[P["r_qt"]])
                        kb.op("dve", lambda e: e.tensor_tensor(out=P["kt2"], in0=kT[:, :, ts], in1=P["en"], op=ALU.mult), reads=r_k + [P["r_en"]], writes=[P["r_kt2"]])
                    return P, [q0, q1, q2, q3]

                def head(i, h, P, ot, r_ot):
                    qtt, r_qt, kt2, r_kt2 = P["qt"], P["r_qt"], P["kt2"], P["r_kt2"]
                    ke, r_ke, vt_, r_vt = P["ke"], P["r_ke"], P["vt"], P["r_vt"]
                    ebt, r_eb, ggt, r_gg = P["eb"], P["r_eb"], P["gg"], P["r_gg"]
                    pSt, r_pS = pS_ring.next()
                    for cc in range(2):
                        c = 2 * h + cc
                        kb.op("pe", lambda e: e.matmul(pSt[:, 0:128], lhsT=kt2[:, c, :], rhs=qtt[:, c, :], start=(cc == 0), stop=(cc == 1)),
                              reads=[r_kt2, r_qt], writes=[r_pS])
                    at, r_at = attn_ring.next()
                    kb.op("dve", lambda e: e.tensor_tensor(out=at, in0=pSt[:, 0:128], in1=cmask[:], op=ALU.mult), reads=[r_pS, rc[2]], writes=[r_at])
                    vh = vt_[:, h * 512:(h + 1) * 512]
                    pus = []
                    for cc in range(2):
                        c = 2 * h + cc
                        pu, r_pu = pU_ring.next()
                        kb.op("pe", lambda e: e.matmul(pu[:, 0:512], lhsT=ke[:, c * 128:(c + 1) * 128], rhs=vh, start=True, stop=True),
                              reads=[r_ke, r_vt], writes=[r_pu])
                        pus.append((c, pu, r_pu))
                    pOt, r_pO = pO_ring.next()
                    kb.op("pe", lambda e: e.matmul(pOt[:, 0:512], lhsT=at, rhs=vh, start=True, stop=False), reads=[r_at, r_vt], writes=[r_pO])
                    for cc in range(2):
                        c = 2 * h + cc
                        kb.op("pe", lambda e: e.matmul(pOt[:, 0:512], lhsT=qtt[:, c, :], rhs=stateb[:, c, :], start=False, stop=(cc == 1)),
                              reads=[r_qt, r_stateb[c]], writes=[r_pO])
                    for c, pu, r_pu in pus:
                        kb.op("dve", lambda e: e.scalar_tensor_tensor(out=state[:, c, :], in0=state[:, c, :], scalar=ebt[:, c, 127:128], in1=pu[:, 0:512],
                                                                      op0=ALU.mult, op1=ALU.add), reads=[r_state[c], r_eb, r_pu], writes=[r_state[c]])
                        kb.op("act", lambda e: e.copy(out=stateb[:, c, :], in_=state[:, c, :]), reads=[r_state[c]], writes=[r_stateb[c]])
                    kb.op("act", lambda e: e.activation(out=junk[:], in_=pOt[:, 0:512], func=AF.Square, accum_out=sm[:, h:h + 1]), reads=[r_pO], writes=[r_junk, r_sm])
                    kb.op("dve", lambda e: e.tensor_scalar(out=sm[:, 4 + h:5 + h], in0=sm[:, h:h + 1], scalar1=1.0 / 512.0, scalar2=LN_EPS, op0=ALU.mult, op1=ALU.add),
                          reads=[r_sm], writes=[r_sm])
                    kb.op("act", lambda e: e.sqrt(out=sm[:, 4 + h:5 + h], in_=sm[:, 4 + h:5 + h]), reads=[r_sm], writes=[r_sm])
                    kb.op("dve", lambda e: e.reciprocal(out=sm[:, 4 + h:5 + h], in_=sm[:, 4 + h:5 + h]), reads=[r_sm], writes=[r_sm])
                    kb.op("dve", lambda e: e.scalar_tensor_tensor(out=ot[:, h * 512:(h + 1) * 512], in0=pOt[:, 0:512], scalar=sm[:, 4 + h:5 + h],
                                                                  in1=ggt[:, h * 512:(h + 1) * 512], op0=ALU.mult, op1=ALU.mult),
                          reads=[r_pO, r_sm, r_gg], writes=[r_ot])

                Pcur, qs = make_pre(0)
                for q in qs:
                    q()
                for i in range(NT):
                    ts = slice(i * 128, (i + 1) * 128)
                    if i + 1 < NT:
                        Pn, qn = make_pre(i + 1)
                    else:
                        Pn, qn = None, []
                    ot, r_ot = osb_ring.next()
                    for h in range(GH):
                        head(i, h, Pcur, ot, r_ot)
                        if qn:
                            qn.pop(0)()
                    kb.dma("sp", lambda e: e.dma_start(out=o_d[ts, :], in_=ot), reads=[r_ot], key=r_ot)
                    Pcur = Pn
        kb.barrier()
        with ExitStack() as es2:
            mixT = es2.enter_context(nc.sbuf_tensor(uq("mixT"), [128, KC, S], BF16))
            r_mT = [Res("mT%d" % i) for i in range(NT)]
            pT = es2.enter_context(nc.psum_tensor(uq("pT"), [128, 2, 1024], BF16))
            pT_ring = Ring([pT[:, i] for i in range(2)], "pT")
            with ExitStack() as es3:
                build_xT(nc, kb, es3, o_d, mixT, r_mT, ident_s, r_id, pT_ring)
            kb.barrier()
            mix_out_phase(nc, kb, es2, L, [mixT[:, kc, :] for kc in range(KC)], r_mT, w["odd_w_out"][0], x_src, xa, xab, w, C)
    kb.barrier()


W_NAMES = ["even_w_in", "even_b_f", "even_conv_w", "even_conv_b", "even_conv_norm_g", "even_conv_norm_b", "even_w_out",
           "odd_w_in", "odd_w_a2", "odd_b_a", "odd_norm_g", "odd_w_out", "ln_mix_g", "ln_mix_b", "ln_ffn_g", "ln_ffn_b",
           "router_w", "router_bias", "expert_w_gate", "expert_w_up", "expert_w_down"]
W_SHAPES = {"even_w_in": (1, 2048, 5128), "even_b_f": (1, 8), "even_conv_w": (1, 31, 1, 1024), "even_conv_b": (1, 1024),
            "even_conv_norm_g": (1, 1024), "even_conv_norm_b": (1, 1024), "even_w_out": (1, 2048, 2048),
            "odd_w_in": (1, 2048, 6160), "odd_w_a2": (1, 16, 1024), "odd_b_a": (1, 1024), "odd_norm_g": (1, 2048),
            "odd_w_out": (1, 2048, 2048), "ln_mix_g": (2, 2048), "ln_mix_b": (2, 2048), "ln_ffn_g": (2, 2048),
            "ln_ffn_b": (2, 2048), "router_w": (2048, 16), "router_bias": (16,),
            "expert_w_gate": (2, 16, 2048, 1408), "expert_w_up": (2, 16, 2048, 1408), "expert_w_down": (2, 16, 1408, 2048)}


def build_program():
    nc = bass.Bass("TRN2", target_bir_lowering=False)
    kb = KB(nc)

    def din(name, shape, dt):
        return nc.dram_tensor(name, list(shape), dt, kind="ExternalInput").ap()

    def dsc(name, shape, dt):
        return nc.dram_tensor(name, list(shape), dt, kind="Internal").ap()
    consts = make_consts()
    C = {k: din("c_" + k, v.shape, CONST_DT[k]) for k, v in consts.items()}
    w = {k: din(k, W_SHAPES[k], F32) for k in W_NAMES}
    x = din("x", (S, D), F32)
    out = nc.dram_tensor("out", [S, D], F32, kind="ExternalOutput").ap()
    xa = dsc("xa", (S, D), F32)
    xab = dsc("xab", (S + 128, D), BF16)
    x2 = dsc("x2", (S, D), F32)
    ys = dsc("ys", (NE * CAP + 128, D), BF16)
    uT_d = dsc("uT_d", (1024, S), BF16)
    vte_d = dsc("vte_d", (S, 1024), BF16)
    kt_d = dsc("kt_d", (S, 1024), BF16)
    vto_d = dsc("vto_d", (S, 2048), BF16)
    gt_d = dsc("gt_d", (S, 2048), BF16)
    o_d = dsc("o_d", (S, 2048), BF16)
    with nc.sbuf_tensor(uq("zt"), [128, D], BF16) as zt:
        rz = Res("zt")
        kb.op("dve", lambda e: e.memset(zt[:], 0.0), writes=[rz])
        kb.dma("sp", lambda e: e.dma_start(out=ys[YZ:YZ + 128, :], in_=zt[:]), reads=[rz], key=rz)
        kb.dma("sp", lambda e: e.dma_start(out=xab[S:S + 128, :], in_=zt[:]), reads=[rz], key=rz)
        kb.barrier()
    even_phase(nc, kb, 0, C, x, uT_d, vte_d, xa, xab, w)
    moe_phase(nc, kb, 0, C, xa, xab, ys, x2, None, w)
    odd_phase(nc, kb, 1, C, x2, kt_d, vto_d, gt_d, o_d, xa, xab, w)
    moe_phase(nc, kb, 1, C, xa, xab, ys, out, None, w)
    return nc, consts


def kernel(**inputs):
    n = 8
    nc, consts = build_program()
    x = np.ascontiguousarray(np.asarray(inputs["x"], dtype=np.float32))
    shared = {("c_" + k): v for k, v in consts.items()}
    for k in W_NAMES:
        shared[k] = np.ascontiguousarray(np.asarray(inputs[k], dtype=np.float32))
    in_maps = []
    for b in range(n):
        m = dict(shared)
        m["x"] = x[b]
        in_maps.append(m)
    res = run_bass_kernel_spmd(nc, in_maps, core_ids=list(range(n)))
    return np.stack([np.asarray(r["out"], dtype=np.float32) for r in res.results], axis=0)
```

```python
import numpy as np
import ml_dtypes
import concourse.bass as bass
import concourse.mybir as mybir
from concourse.bass_utils import run_bass_kernel_spmd

F32 = mybir.dt.float32
BF16 = mybir.dt.bfloat16
I32 = mybir.dt.int32
AF = mybir.ActivationFunctionType
ALU = mybir.AluOpType
AX = mybir.AxisListType

S = 2048
D = 2048
NT = S // 128
KC = D // 128
DEPTH = 2
ALPHA = (2 * DEPTH) ** 0.25
LN_EPS = 1e-5
NE = 16
FE = 1408
NF = FE // 128
CAP = 512
NJ = CAP // 128
ZROW = S
YZ = NE * CAP


class Res:
    __slots__ = ("name", "w", "rs", "dsem")

    def __init__(self, name):
        self.name = name
        self.w = None
        self.rs = []
        self.dsem = None


class KB:
    ENGS = ("pe", "act", "dve", "pool", "sp")

    def __init__(self, nc, n_dma_sems=48, same_engine_sync=True):
        self.nc = nc
        self.eng = {"pe": nc.tensor, "act": nc.scalar, "dve": nc.vector,
                    "pool": nc.gpsimd, "sp": nc.sync}
        self.sem = {}
        self.cnt = {}
        self.waited = {}
        self.same_engine_sync = same_engine_sync
        for e in self.ENGS:
            self._mksem("p_" + e)
        self._mksem("bar")
        self.dma_pool = []
        for i in range(n_dma_sems):
            self._mksem("d%d" % i)
            self.dma_pool.append("d%d" % i)
        self.dma_next = 0
        self.dma_res = []

    def _mksem(self, name):
        self.sem[name] = self.nc.alloc_semaphore(name)
        self.cnt[name] = 0

    def _wait(self, e, dep):
        if dep is None:
            return
        s, c = dep
        if s == "p_" + e and (e in ("pe", "sp") or not self.same_engine_sync):
            return
        if self.waited.get((e, s), 0) >= c:
            return
        self.eng[e].wait_ge(self.sem[s], c)
        self.waited[(e, s)] = c

    def _pre(self, e, reads, writes):
        for r in reads:
            self._wait(e, r.w)
        for w in writes:
            self._wait(e, w.w)
            for d in w.rs:
                self._wait(e, d)

    def _post(self, dep, reads, writes):
        for r in reads:
            r.rs.append(dep)
            if len(r.rs) > 8:
                m = {}
                for s, c in r.rs:
                    m[s] = max(m.get(s, 0), c)
                r.rs = list(m.items())
        for w in writes:
            w.w = dep
            w.rs = []

    def op(self, e, fn, reads=(), writes=()):
        self._pre(e, reads, writes)
        ins = fn(self.eng[e])
        s = "p_" + e
        self.cnt[s] += 1
        ins.then_inc(self.sem[s], 1)
        self._post((s, self.cnt[s]), reads, writes)
        return ins

    def dma(self, q, fn, reads=(), writes=(), key=None):
        self._pre(q, reads, writes)
        key = key or (writes[0] if writes else reads[0])
        if key.dsem is None:
            assert self.dma_next < len(self.dma_pool), "out of dma sems"
            key.dsem = self.dma_pool[self.dma_next]
            self.dma_next += 1
            self.dma_res.append(key)
        ins = fn(self.eng[q])
        s = key.dsem
        self.cnt[s] += 16
        ins.then_inc(self.sem[s], 16)
        self._post((s, self.cnt[s]), reads, writes)
        return ins

    def barrier(self):
        sp = self.eng["sp"]
        for s, c in self.cnt.items():
            if s in ("bar", "p_sp") or c == 0:
                continue
            if self.waited.get(("sp", s), 0) >= c:
                continue
            sp.wait_ge(self.sem[s], c)
            self.waited[("sp", s)] = c
        self.cnt["bar"] += 1
        sp.nop().then_inc(self.sem["bar"], 1)
        for e in self.ENGS:
            if e != "sp":
                self.eng[e].wait_ge(self.sem["bar"], self.cnt["bar"])
            for s, c in self.cnt.items():
                self.waited[(e, s)] = c
        for r in self.dma_res:
            r.dsem = None
        self.dma_res = []
        self.dma_next = 0


class Ring:
    def __init__(self, views, name):
        self.v = views
        self.r = [Res("%s%d" % (name, i)) for i in range(len(views))]
        self.i = -1

    def next(self):
        self.i = (self.i + 1) % len(self.v)
        return self.v[self.i], self.r[self.i]


_UNIQ = [0]


def uq(name):
    _UNIQ[0] += 1
    return "%s_u%d" % (name, _UNIQ[0])


def ln_tile(kb, z, zr, gam, bet, rg, st, rst, out, rout, eng2="dve"):
    stats, mv, rstd = st
    for c in range(4):
        kb.op("dve", lambda e: e.bn_stats(out=stats[:, c * 6:(c + 1) * 6], in_=z[:, c * 512:(c + 1) * 512]),
              reads=[zr], writes=[rst])
    kb.op("dve", lambda e: e.bn_aggr(out=mv[:, 0:2], in_=stats[:, 0:24]), reads=[rst], writes=[rst])
    kb.op("dve", lambda e: e.tensor_scalar_add(out=rstd[:, 0:1], in0=mv[:, 1:2], scalar1=LN_EPS), reads=[rst], writes=[rst])
    kb.op("act", lambda e: e.sqrt(out=rstd[:, 0:1], in_=rstd[:, 0:1]), reads=[rst], writes=[rst])
    kb.op("dve", lambda e: e.reciprocal(out=rstd[:, 0:1], in_=rstd[:, 0:1]), reads=[rst], writes=[rst])
    kb.op("dve", lambda e: e.tensor_scalar(out=z[:, :], in0=z[:, :], scalar1=mv[:, 0:1], scalar2=rstd[:, 0:1],
                                           op0=ALU.subtract, op1=ALU.mult), reads=[zr, rst], writes=[zr])
    kb.op(eng2, lambda e: e.tensor_tensor(out=z[:, :], in0=z[:, :], in1=gam[:, :], op=ALU.mult), reads=[zr] + rg, writes=[zr])
    kb.op(eng2, lambda e: e.tensor_tensor(out=out[:, :], in0=z[:, :], in1=bet[:, :], op=ALU.add), reads=[zr] + rg, writes=[rout])


class LNPipe:
    def __init__(self, nc, kb, es, gam, bet, rgs, emit, eps=LN_EPS):
        self.kb = kb
        self.gam, self.bet, self.rgs, self.emit, self.eps = gam, bet, rgs, emit, eps
        st = es.enter_context(nc.sbuf_tensor(uq("lnst"), [128, 2, 32], F32))
        self.st_ring = Ring([st[:, i] for i in range(2)], "lnst")
        zo = es.enter_context(nc.sbuf_tensor(uq("lnzo"), [128, 2, D], F32))
        self.zo_ring = Ring([zo[:, i] for i in range(2)], "lnzo")
        self.pending = None

    def _apply(self):
        kb = self.kb
        z, r_z, tag = self.pending
        o, r_o = self.zo_ring.next()
        kb.op("dve", lambda e: e.tensor_tensor(out=z, in0=z, in1=self.gam[:, :], op=ALU.mult), reads=[r_z] + self.rgs, writes=[r_z])
        kb.op("dve", lambda e: e.tensor_tensor(out=o, in0=z, in1=self.bet[:, :], op=ALU.add), reads=[r_z] + self.rgs, writes=[r_o])
        self.pending = None
        self.emit(o, r_o, tag)

    def feed(self, z, r_z, tag):
        kb = self.kb
        st, r_st = self.st_ring.next()
        for c in range(4):
            kb.op("dve", lambda e: e.bn_stats(out=st[:, c * 6:(c + 1) * 6], in_=z[:, c * 512:(c + 1) * 512]), reads=[r_z], writes=[r_st])
        kb.op("dve", lambda e: e.bn_aggr(out=st[:, 24:26], in_=st[:, 0:24]), reads=[r_st], writes=[r_st])
        kb.op("dve", lambda e: e.tensor_scalar_add(out=st[:, 26:27], in0=st[:, 25:26], scalar1=self.eps), reads=[r_st], writes=[r_st])
        kb.op("act", lambda e: e.sqrt(out=st[:, 26:27], in_=st[:, 26:27]), reads=[r_st], writes=[r_st])
        if self.pending is not None:
            self._apply()
        kb.op("dve", lambda e: e.reciprocal(out=st[:, 27:28], in_=st[:, 26:27]), reads=[r_st], writes=[r_st])
        kb.op("dve", lambda e: e.scalar_tensor_tensor(out=st[:, 28:29], in0=st[:, 24:25], scalar=-1.0, in1=st[:, 27:28], op0=ALU.mult, op1=ALU.mult),
              reads=[r_st], writes=[r_st])
        kb.op("act", lambda e: e.activation(out=z, in_=z, func=AF.Identity, bias=st[:, 28:29], scale=st[:, 27:28]), reads=[r_z, r_st], writes=[r_z])
        self.pending = (z, r_z, tag)

    def flush(self):
        if self.pending is not None:
            self._apply()


def bcast_rows(ap1d, n):
    return ap1d.partition_broadcast(128)


def moe_phase(nc, kb, L, C, xa, xab, ys, xo, xob, w):
    from contextlib import ExitStack
    ident, iota, tokinfo = C["ident"], C["iota"], C["tokinfo"]
    wg_d = w["expert_w_gate"]
    wu_d = w["expert_w_up"]
    wd_d = w["expert_w_down"]

    with ExitStack() as es:
        def sb(name, shape, dt):
            return es.enter_context(nc.sbuf_tensor(uq(name), shape, dt))

        def ps(name, shape, dt):
            return es.enter_context(nc.psum_tensor(uq(name), shape, dt))

        gate_all = sb("gate_all", [128, NT, NE], F32)
        posm_all = sb("posm_all", [128, NT, NE], F32)
        ridx = sb("ridx", [128, NT, 2], I32)
        gsel = sb("gsel", [128, NT, 2], F32)
        tokidx = sb("tokidx", [128, NE * NJ], I32)
        rw_bf = sb("rw_bf", [128, KC, NE], BF16)
        rbias = sb("rbias", [128, NE], F32)
        ecap = sb("ecap", [128, NE], F32)
        ident_s = sb("ident_s", [128, 128], BF16)
        iota_s = sb("iota_s", [128, CAP], F32)
        tokinfo_s = sb("tokinfo_s", [128, NT, 4], BF16)
        lstrict = sb("lstrict", [128, 128], BF16)
        ones_bf = sb("ones_bf", [128, 128], BF16)
        r_const = Res("const")
        r_gate = Res("gate_all")
        r_posm = Res("posm_all")
        r_ridx = Res("ridx")
        r_tok = Res("tokidx")

        kb.dma("pool", lambda e: e.dma_start(out=rw_bf[:], in_=w["router_w"].rearrange("(kc p) e -> p kc e", p=128)), writes=[r_const])
        c2 = Res("c2"); c3 = Res("c3"); c4 = Res("c4"); c5 = Res("c5"); c6 = Res("c6"); c7 = Res("c7")
        kb.dma("sp", lambda e: e.dma_start(out=rbias[:], in_=w["router_bias"].partition_broadcast(128)), writes=[c2])
        kb.dma("sp", lambda e: e.dma_start(out=ident_s[:], in_=ident), writes=[c3])
        kb.dma("sp", lambda e: e.dma_start(out=iota_s[:], in_=iota[:, 0:CAP]), writes=[c4])
        kb.dma("sp", lambda e: e.dma_start(out=tokinfo_s[:], in_=tokinfo), writes=[c5])
        kb.dma("sp", lambda e: e.dma_start(out=lstrict[:], in_=C["lstrict"]), writes=[c6])
        kb.dma("sp", lambda e: e.dma_start(out=ecap[:], in_=C["ecap"]), writes=[c7])
        kb.op("dve", lambda e: e.memset(ones_bf[:], 1.0), writes=[c6])
        consts = [r_const, c2, c3, c4, c5, c6, c7]

        with ExitStack() as es2:
            def sb2(name, shape, dt):
                return es2.enter_context(nc.sbuf_tensor(uq(name), shape, dt))

            def ps2(name, shape, dt):
                return es2.enter_context(nc.psum_tensor(uq(name), shape, dt))

            xt_b = sb2("xt_b", [128, 2, D], BF16)
            xt_ring = Ring([xt_b[:, i] for i in range(2)], "xt_b")
            xT = sb2("xT", [128, 2, KC, 128], BF16)
            xT_ring = Ring([xT[:, i] for i in range(2)], "xT")
            pT = ps2("pT", [128, 2, 1024], BF16)
            pT_ring = Ring([pT[:, i] for i in range(2)], "pT")
            psm = ps2("psm", [128, 512], F32)
            r_psm_r = Res("psm_r"); r_psm_p = Res("psm_p")
            rt = sb2("rt", [128, 16, NE], F32)
            r_rt = Res("rt")
            m4 = sb2("m4", [128, 8, 4], F32)
            mcum = sb2("mcum", [128, NE], BF16)
            m_bf = sb2("m_bf", [128, 2, NE], BF16)
            m_ring = Ring([m_bf[:, i] for i in range(2)], "m_bf")
            r_mcum = Res("mcum")
            oh = sb2("oh", [128, 4, CAP], BF16)
            oh_ring = Ring([oh[:, i] for i in range(4)], "oh")
            kb.op("dve", lambda e: e.memset(mcum[:], 0.0), writes=[r_mcum])

            for i in range(NT):
                xt, r_xt = xt_ring.next()
                kb.dma("sp", lambda e: e.dma_start(out=xt, in_=xab[i * 128:(i + 1) * 128, :]), writes=[r_xt])
                xTt, r_xT = xT_ring.next()
                for g4 in range(4):
                    p, r_p = pT_ring.next()
                    for q in range(4):
                        kc = g4 * 4 + q
                        kb.op("pe", lambda e: e.transpose(out=p[:, q * 128:(q + 1) * 128], in_=xt[:, kc * 128:(kc + 1) * 128], identity=ident_s[:]),
                              reads=[r_xt, c3], writes=[r_p])
                    en = "act" if g4 % 2 == 0 else "dve"
                    if en == "act":
                        kb.op("act", lambda e: e.copy(out=xTt[:, g4 * 4:(g4 + 1) * 4, :], in_=p[:, 0:512].rearrange("p (a b) -> p a b", a=4)),
                              reads=[r_p], writes=[r_xT])
                    else:
                        kb.op("dve", lambda e: e.tensor_copy(out=xTt[:, g4 * 4:(g4 + 1) * 4, :], in_=p[:, 0:512].rearrange("p (a b) -> p a b", a=4)),
                              reads=[r_p], writes=[r_xT])
                for kc in range(KC):
                    kb.op("pe", lambda e: e.matmul(psm[:, 0:NE], lhsT=xTt[:, kc, :], rhs=rw_bf[:, kc, :], start=(kc == 0), stop=(kc == KC - 1)),
                          reads=[r_xT, r_const], writes=[r_psm_r])
                sc = rt[:, 0]; sel = rt[:, 1]; eq1 = rt[:, 2]; sel2 = rt[:, 3]; ge2 = rt[:, 4]; M = rt[:, 5]; wv = rt[:, 6]
                pos1 = rt[:, 7]; vv = rt[:, 8]; sv = rt[:, 9]; tmp = rt[:, 10]; sv2 = rt[:, 11]
                m1 = m4[:, 0]; m2 = m4[:, 1]; gs = m4[:, 2]; gm = m4[:, 3]
                gmax = m4[:, 4, 0:1]; wsum = m4[:, 4, 1:2]; ihi = m4[:, 5, 0:1]; ilo = m4[:, 5, 1:2]; t1 = m4[:, 5, 2:3]
                R = [r_rt]
                kb.op("act", lambda e: e.activation(out=sc, in_=psm[:, 0:NE], func=AF.Sigmoid), reads=[r_psm_r], writes=R)
                kb.op("dve", lambda e: e.tensor_tensor(out=sel, in0=sc, in1=rbias[:], op=ALU.add), reads=R + [c2], writes=R)
                v3 = lambda a: a.rearrange("p (g j) -> p g j", g=4)
                b3 = lambda a: a.unsqueeze(2).to_broadcast([128, 4, 4])
                kb.op("dve", lambda e: e.tensor_reduce(out=m1, in_=v3(sel), axis=AX.X, op=ALU.max), reads=R, writes=R)
                kb.op("dve", lambda e: e.tensor_tensor(out=v3(eq1), in0=v3(sel), in1=b3(m1), op=ALU.is_equal), reads=R, writes=R)
                kb.op("dve", lambda e: e.scalar_tensor_tensor(out=sel2, in0=eq1, scalar=-1e9, in1=sel, op0=ALU.mult, op1=ALU.add), reads=R, writes=R)
                kb.op("dve", lambda e: e.tensor_reduce(out=m2, in_=v3(sel2), axis=AX.X, op=ALU.max), reads=R, writes=R)
                kb.op("dve", lambda e: e.tensor_tensor(out=gs, in0=m1, in1=m2, op=ALU.add), reads=R, writes=R)
                kb.op("dve", lambda e: e.tensor_reduce(out=gmax, in_=gs, axis=AX.X, op=ALU.max), reads=R, writes=R)
                kb.op("dve", lambda e: e.tensor_scalar(out=gm, in0=gs, scalar1=gmax, scalar2=None, op0=ALU.is_equal), reads=R, writes=R)
                kb.op("dve", lambda e: e.tensor_tensor(out=v3(ge2), in0=v3(sel), in1=b3(m2), op=ALU.is_ge), reads=R, writes=R)
                kb.op("dve", lambda e: e.tensor_tensor(out=v3(M), in0=v3(ge2), in1=b3(gm), op=ALU.mult), reads=R, writes=R)
                kb.op("dve", lambda e: e.tensor_tensor(out=wv, in0=sc, in1=M, op=ALU.mult), reads=R, writes=R)
                kb.op("dve", lambda e: e.tensor_reduce(out=wsum, in_=wv, axis=AX.X, op=ALU.add), reads=R, writes=R)
                kb.op("dve", lambda e: e.reciprocal(out=wsum, in_=wsum), reads=R, writes=R)
                kb.op("dve", lambda e: e.tensor_scalar(out=gate_all[:, i, :], in0=wv, scalar1=wsum, scalar2=None, op0=ALU.mult), reads=R, writes=[r_gate])
                mb, r_mb = m_ring.next()
                kb.op("dve", lambda e: e.tensor_copy(out=mb, in_=M), reads=R, writes=[r_mb])
                kb.op("pe", lambda e: e.matmul(psm[:, 32:32 + NE], lhsT=lstrict[:], rhs=mb, start=True, stop=False),
                      reads=[r_mb, c6], writes=[r_psm_p])
                kb.op("pe", lambda e: e.matmul(psm[:, 32:32 + NE], lhsT=ones_bf[:], rhs=mcum[:], start=False, stop=True),
                      reads=[r_mcum, c6], writes=[r_psm_p])
                kb.op("dve", lambda e: e.tensor_scalar(out=vv, in0=psm[:, 32:32 + NE], scalar1=float(CAP), scalar2=None, op0=ALU.is_lt), reads=[r_psm_p] + R, writes=R)
                kb.op("dve", lambda e: e.scalar_tensor_tensor(out=pos1, in0=psm[:, 32:32 + NE], scalar=1.0, in1=M, op0=ALU.add, op1=ALU.mult), reads=[r_psm_p] + R, writes=R)
                kb.op("dve", lambda e: e.tensor_tensor(out=pos1, in0=pos1, in1=vv, op=ALU.mult), reads=R, writes=R)
                kb.op("dve", lambda e: e.tensor_scalar_add(out=posm_all[:, i, :], in0=pos1, scalar1=-1.0), reads=R, writes=[r_posm])
                kb.op("dve", lambda e: e.tensor_tensor(out=mcum[:], in0=mcum[:], in1=mb, op=ALU.add), reads=[r_mb, r_mcum], writes=[r_mcum])
                kb.op("dve", lambda e: e.tensor_scalar(out=vv, in0=pos1, scalar1=0.0, scalar2=None, op0=ALU.is_gt), reads=R, writes=R)
                kb.op("dve", lambda e: e.tensor_tensor(out=sv, in0=pos1, in1=ecap[:], op=ALU.add), reads=R + [c7], writes=R)
                kb.op("dve", lambda e: e.tensor_tensor(out=sv, in0=sv, in1=vv, op=ALU.mult), reads=R, writes=R)
                kb.op("dve", lambda e: e.tensor_reduce(out=ihi, in_=sv, axis=AX.X, op=ALU.max), reads=R, writes=R)
                kb.op("dve", lambda e: e.tensor_scalar(out=tmp, in0=sv, scalar1=ihi, scalar2=None, op0=ALU.not_equal), reads=R, writes=R)
                kb.op("dve", lambda e: e.tensor_tensor(out=sv2, in0=sv, in1=tmp, op=ALU.mult), reads=R, writes=R)
                kb.op("dve", lambda e: e.tensor_reduce(out=ilo, in_=sv2, axis=AX.X, op=ALU.max), reads=R, writes=R)
                kb.op("dve", lambda e: e.scalar_tensor_tensor(out=tmp, in0=sv, scalar=ihi, in1=gate_all[:, i, :], op0=ALU.is_equal, op1=ALU.mult,
                                                              accum_out=gsel[:, i, 0:1]), reads=R + [r_gate], writes=R + [r_ridx])
                kb.op("dve", lambda e: e.scalar_tensor_tensor(out=tmp, in0=sv, scalar=ilo, in1=gate_all[:, i, :], op0=ALU.is_equal, op1=ALU.mult,
                                                              accum_out=gsel[:, i, 1:2]), reads=R + [r_gate], writes=R + [r_ridx])
                for k, src in ((0, ihi), (1, ilo)):
                    kb.op("dve", lambda e: e.tensor_scalar(out=t1, in0=src, scalar1=0.0, scalar2=float(YZ + 1), op0=ALU.is_equal, op1=ALU.mult), reads=R, writes=R)
                    kb.op("dve", lambda e: e.scalar_tensor_tensor(out=t1, in0=src, scalar=-1.0, in1=t1, op0=ALU.add, op1=ALU.add), reads=R, writes=R)
                    kb.op("dve", lambda e: e.tensor_copy(out=ridx[:, i, k:k + 1], in_=t1), reads=R, writes=[r_ridx])
            pacs = sb2("pacs", [128, NE * NJ, 4], F32)
            r_tf = Res("tf")
            ptab = ps2("ptab", [128, 2, NJ, NT, 4], F32)
            ptab_ring = Ring([ptab[:, i] for i in range(2)], "ptab")
            for ex in range(NE):
                pt, r_pt = ptab_ring.next()
                for i in range(NT):
                    o, r_o = oh_ring.next()
                    kb.op("dve", lambda e: e.tensor_scalar(out=o, in0=iota_s[:], scalar1=posm_all[:, i, ex:ex + 1], scalar2=None, op0=ALU.is_equal),
                          reads=[r_posm, c4], writes=[r_o])
                    for j in range(NJ):
                        kb.op("pe", lambda e: e.matmul(pt[:, j, i, 0:4], lhsT=o[:, j * 128:(j + 1) * 128], rhs=tokinfo_s[:, i, :],
                                                       start=True, stop=True), reads=[r_o, c5], writes=[r_pt])
                kb.op("dve", lambda e: e.tensor_reduce(out=pacs[:, ex * NJ:(ex + 1) * NJ, :], in_=pt.rearrange("p j i c -> p j c i"), axis=AX.X, op=ALU.add),
                      reads=[r_pt], writes=[r_tf])
            tf = sb2("tf", [128, NE * NJ, 2], F32)
            kb.op("dve", lambda e: e.scalar_tensor_tensor(out=tf[:, :, 0], in0=pacs[:, :, 1], scalar=128.0, in1=pacs[:, :, 0], op0=ALU.mult, op1=ALU.add),
                  reads=[r_tf], writes=[r_tf])
            kb.op("dve", lambda e: e.tensor_scalar(out=tf[:, :, 1], in0=pacs[:, :, 2], scalar1=-float(ZROW), scalar2=float(ZROW), op0=ALU.mult, op1=ALU.add),
                  reads=[r_tf], writes=[r_tf])
            kb.op("dve", lambda e: e.tensor_tensor(out=tf[:, :, 0], in0=tf[:, :, 0], in1=tf[:, :, 1], op=ALU.add), reads=[r_tf], writes=[r_tf])
            kb.op("dve", lambda e: e.tensor_copy(out=tokidx[:, :], in_=tf[:, :, 0]), reads=[r_tf], writes=[r_tok])
        kb.barrier()

        with ExitStack() as es2:
            def sb2(name, shape, dt):
                return es2.enter_context(nc.sbuf_tensor(uq(name), shape, dt))

            def ps2(name, shape, dt):
                return es2.enter_context(nc.psum_tensor(uq(name), shape, dt))

            NGU = 8
            NWD = 4
            wgu = sb2("wgu", [128, NGU, 2, KC, 128], BF16)
            gu_ring = Ring([wgu[:, i] for i in range(NGU)], "wgu")
            wd = sb2("wd", [128, NWD, NF, 512], BF16)
            wd_ring = Ring([wd[:, i] for i in range(NWD)], "wd")
            xg = sb2("xg", [128, 4, D], BF16)
            xg_ring = Ring([xg[:, i] for i in range(4)], "xg")
            xgT = sb2("xgT", [128, 2, KC, CAP], BF16)
            xgT_res = [[Res("xgT%d_%d" % (b, j)) for j in range(NJ)] for b in range(2)]
            hT = sb2("hT", [128, 2, NF, CAP], BF16)
            hT_res = [[Res("hT%d_%d" % (b, f)) for f in range(NF)] for b in range(2)]
            sg = sb2("sg", [128, 2, CAP], F32)
            sg_ring = Ring([sg[:, i] for i in range(2)], "sg")
            yst = sb2("yst", [128, 4, 512], BF16)
            yst_ring = Ring([yst[:, i] for i in range(4)], "yst")
            pT = ps2("pTe", [128, 2, 1024], BF16)
            pT_ring = Ring([pT[:, i] for i in range(2)], "pTe")
            pg = ps2("pg", [128, 2, 512], F32)
            pg_ring = Ring([pg[:, i] for i in range(2)], "pg")
            pu = ps2("pu", [128, 2, 512], F32)
            pu_ring = Ring([pu[:, i] for i in range(2)], "pu")
            py = ps2("py", [128, 2, 512], F32)
            py_ring = Ring([py[:, i] for i in range(2)], "py")
            r_ys = Res("ys_dram")
            nev_box = [0]

            def prep(ex):
                b = ex % 2
                groups = []
                for j in range(NJ):
                    g, r_g = xg_ring.next()
                    col = ex * NJ + j
                    kb.dma("pool", lambda e: e.indirect_dma_start(out=g, out_offset=None, in_=xab[:, :],
                                                                   in_offset=bass.IndirectOffsetOnAxis(ap=tokidx[:, col:col + 1], axis=0)),
                           reads=[r_tok], writes=[r_g])
                    for g4 in range(4):
                        def grp(g=g, r_g=r_g, j=j, g4=g4, b=b):
                            p, r_p = pT_ring.next()
                            for q in range(4):
                                kc = g4 * 4 + q
                                kb.op("pe", lambda e: e.transpose(out=p[:, q * 128:(q + 1) * 128], in_=g[:, kc * 128:(kc + 1) * 128], identity=ident_s[:]),
                                      reads=[r_g, c3], writes=[r_p])
                            dst = xgT[:, b, g4 * 4:(g4 + 1) * 4, j * 128:(j + 1) * 128]
                            src = p[:, 0:512].rearrange("p (a b) -> p a b", a=4)
                            if nev_box[0] % 2 == 0:
                                kb.op("act", lambda e: e.copy(out=dst, in_=src), reads=[r_p], writes=[xgT_res[b][j]])
                            else:
                                kb.op("dve", lambda e: e.tensor_copy(out=dst, in_=src), reads=[r_p], writes=[xgT_res[b][j]])
                            nev_box[0] += 1
                        groups.append(grp)
                return groups

            for g_ in prep(0):
                g_()
            for ex in range(NE):
                b = ex % 2
                wd_slots = []
                for f in range(NF):
                    wt, r_w = gu_ring.next()
                    kb.dma("pool", lambda e: e.dma_start(out=wt[:, 0], in_=wg_d[L, ex].rearrange("(kc p) f -> p kc f", p=128)[:, :, f * 128:(f + 1) * 128]),
                           writes=[r_w])
                    kb.dma("pool", lambda e: e.dma_start(out=wt[:, 1], in_=wu_d[L, ex].rearrange("(kc p) f -> p kc f", p=128)[:, :, f * 128:(f + 1) * 128]),
                           writes=[r_w])
                    pgt, r_pg = pg_ring.next()
                    put, r_pu = pu_ring.next()
                    for kc in range(KC):
                        kb.op("pe", lambda e: e.matmul(pgt[:, 0:CAP], lhsT=wt[:, 0, kc, :], rhs=xgT[:, b, kc, :], start=(kc == 0), stop=(kc == KC - 1)),
                              reads=[r_w] + xgT_res[b], writes=[r_pg])
                    for kc in range(KC):
                        kb.op("pe", lambda e: e.matmul(put[:, 0:CAP], lhsT=wt[:, 1, kc, :], rhs=xgT[:, b, kc, :], start=(kc == 0), stop=(kc == KC - 1)),
                              reads=[r_w] + xgT_res[b], writes=[r_pu])
                    sgt, r_sg = sg_ring.next()
                    kb.op("act", lambda e: e.activation(out=sgt, in_=pgt[:, 0:CAP], func=AF.Silu), reads=[r_pg], writes=[r_sg])
                    kb.op("dve", lambda e: e.tensor_tensor(out=hT[:, b, f, :], in0=sgt, in1=put[:, 0:CAP], op=ALU.mult), reads=[r_sg, r_pu], writes=[hT_res[b][f]])
                for dcl in range(4):
                    wdt, r_wd = wd_ring.next()
                    kb.dma("pool", lambda e: e.dma_start(out=wdt, in_=wd_d[L, ex].rearrange("(fc p) d -> p fc d", p=128)[:, :, dcl * 512:(dcl + 1) * 512]),
                           writes=[r_wd])
                    wd_slots.append((wdt, r_wd))
                nxt = prep(ex + 1) if ex + 1 < NE else []
                for dc in range(4):
                    wt, r_w = wd_slots[dc]
                    for t in range(NJ):
                        if nxt:
                            nxt.pop(0)()
                        pyt, r_py = py_ring.next()
                        for f in range(NF):
                            kb.op("pe", lambda e: e.matmul(pyt[:, 0:512], lhsT=hT[:, b, f, t * 128:(t + 1) * 128], rhs=wt[:, f, :], start=(f == 0), stop=(f == NF - 1)),
                                  reads=[r_w] + hT_res[b], writes=[r_py])
                        y, r_y = yst_ring.next()
                        if nev_box[0] % 2 == 0:
                            kb.op("act", lambda e: e.copy(out=y, in_=pyt[:, 0:512]), reads=[r_py], writes=[r_y])
                        else:
                            kb.op("dve", lambda e: e.tensor_copy(out=y, in_=pyt[:, 0:512]), reads=[r_py], writes=[r_y])
                        nev_box[0] += 1
                        row0 = ex * CAP + t * 128
                        kb.dma("sp", lambda e: e.dma_start(out=ys[row0:row0 + 128, dc * 512:(dc + 1) * 512], in_=y), reads=[r_y], key=r_y)
                for g_ in nxt:
                    g_()
        kb.barrier()

        with ExitStack() as es2:
            def sb2(name, shape, dt):
                return es2.enter_context(nc.sbuf_tensor(uq(name), shape, dt))

            gam = sb2("gam", [128, D], F32)
            bet = sb2("bet", [128, D], F32)
            r_gb = Res("gb")
            kb.dma("sp", lambda e: e.dma_start(out=gam[:], in_=w["ln_ffn_g"][L].partition_broadcast(128)), writes=[r_gb])
            r_gb2 = Res("gb2")
            kb.dma("sp", lambda e: e.dma_start(out=bet[:], in_=w["ln_ffn_b"][L].partition_broadcast(128)), writes=[r_gb2])
            xin = sb2("xin", [128, 3, D], F32)
            xin_ring = Ring([xin[:, i] for i in range(3)], "xin")
            rr = sb2("rr", [128, 4, D], BF16)
            rr_ring = Ring([rr[:, i] for i in range(4)], "rr")
            zb = sb2("zb", [128, 2, D], BF16)
            zb_ring = Ring([zb[:, i] for i in range(2)], "zb")
            gs2 = sb2("gs2", [128, NT, 2], F32)
            r_gs2 = Res("gs2")
            kb.op("dve", lambda e: e.tensor_scalar(out=gs2[:], in0=gsel[:], scalar1=1.0 / ALPHA, scalar2=None, op0=ALU.mult), reads=[r_ridx], writes=[r_gs2])

            def emit(o, r_o, i):
                kb.dma("sp", lambda e: e.dma_start(out=xo[i * 128:(i + 1) * 128, :], in_=o), reads=[r_o], key=r_o)
                if xob is not None:
                    zbt, r_zb = zb_ring.next()
                    kb.op("act", lambda e: e.copy(out=zbt, in_=o), reads=[r_o], writes=[r_zb])
                    kb.dma("sp", lambda e: e.dma_start(out=xob[i * 128:(i + 1) * 128, :], in_=zbt), reads=[r_zb], key=r_zb)
            lnp = LNPipe(nc, kb, es2, gam, bet, [r_gb, r_gb2], emit, eps=LN_EPS / (ALPHA * ALPHA))
            for i in range(NT):
                x, r_x = xin_ring.next()
                kb.dma("sp", lambda e: e.dma_start(out=x, in_=xa[i * 128:(i + 1) * 128, :]), writes=[r_x])
                rh, r_rh = rr_ring.next()
                kb.dma("pool", lambda e: e.indirect_dma_start(out=rh, out_offset=None, in_=ys[:, :],
                                                               in_offset=bass.IndirectOffsetOnAxis(ap=ridx[:, i, 0:1], axis=0)), reads=[r_ridx], writes=[r_rh])
                rl, r_rl = rr_ring.next()
                kb.dma("pool", lambda e: e.indirect_dma_start(out=rl, out_offset=None, in_=ys[:, :],
                                                               in_offset=bass.IndirectOffsetOnAxis(ap=ridx[:, i, 1:2], axis=0)), reads=[r_ridx], writes=[r_rl])
                kb.op("dve", lambda e: e.scalar_tensor_tensor(out=x, in0=rh, scalar=gs2[:, i, 0:1], in1=x, op0=ALU.mult, op1=ALU.add),
                      reads=[r_rh, r_x, r_gs2], writes=[r_x])
                kb.op("dve", lambda e: e.scalar_tensor_tensor(out=x, in0=rl, scalar=gs2[:, i, 1:2], in1=x, op0=ALU.mult, op1=ALU.add),
                      reads=[r_rl, r_x, r_gs2], writes=[r_x])
                lnp.feed(x, r_x, i)
            lnp.flush()
        kb.barrier()


def build_xT(nc, kb, es, src, xT, r_xT, ident_s, r_id, pT_ring):
    xt_b = es.enter_context(nc.sbuf_tensor(uq("xtb"), [128, 2, D], BF16))
    ring = Ring([xt_b[:, i] for i in range(2)], "xtb")
    n = 0
    for i in range(NT):
        xt, r_xt = ring.next()
        kb.dma("pool", lambda e: e.dma_start(out=xt, in_=src[i * 128:(i + 1) * 128, :]), writes=[r_xt])
        for g4 in range(4):
            p, r_p = pT_ring.next()
            for q in range(4):
                kc = g4 * 4 + q
                kb.op("pe", lambda e: e.transpose(out=p[:, q * 128:(q + 1) * 128], in_=xt[:, kc * 128:(kc + 1) * 128], identity=ident_s[:]),
                      reads=[r_xt, r_id], writes=[r_p])
            dst = xT[:, g4 * 4:(g4 + 1) * 4, i * 128:(i + 1) * 128]
            srcp = p[:, 0:512].rearrange("p (a b) -> p a b", a=4)
            if n % 2 == 0:
                kb.op("act", lambda e: e.copy(out=dst, in_=srcp), reads=[r_p], writes=[r_xT[i]])
            else:
                kb.op("dve", lambda e: e.tensor_copy(out=dst, in_=srcp), reads=[r_p], writes=[r_xT[i]])
            n += 1


def mix_out_phase(nc, kb, es, L, mixT, r_mix, wout_d, x_src, xa, xab, w, C):
    def sb(name, shape, dt):
        return es.enter_context(nc.sbuf_tensor(uq(name), shape, dt))
    wo = sb("wo", [128, KC, D], BF16)
    r_wo = [Res("wo%d" % i) for i in range(4)]
    for q in range(4):
        kb.dma("pool", lambda e: e.dma_start(out=wo[:, q * 4:(q + 1) * 4, :], in_=wout_d.rearrange("(kc p) d -> p kc d", p=128)[:, q * 4:(q + 1) * 4, :]),
               writes=[r_wo[q]])
    gam = sb("gam", [128, D], F32)
    bet = sb("bet", [128, D], F32)
    r_g1 = Res("g1"); r_g2 = Res("g2")
    kb.dma("sp", lambda e: e.dma_start(out=gam[:], in_=w["ln_mix_g"][L].partition_broadcast(128)), writes=[r_g1])
    kb.dma("sp", lambda e: e.dma_start(out=bet[:], in_=w["ln_mix_b"][L].partition_broadcast(128)), writes=[r_g2])
    xin = sb("xin", [128, 3, D], F32)
    xin_ring = Ring([xin[:, i] for i in range(3)], "xin")
    zb = sb("zb", [128, 2, D], BF16)
    zb_ring = Ring([zb[:, i] for i in range(2)], "zb")

    def emit(o, r_o, i):
        kb.dma("sp", lambda e: e.dma_start(out=xa[i * 128:(i + 1) * 128, :], in_=o), reads=[r_o], key=r_o)
        zbt, r_zb = zb_ring.next()
        kb.op("act", lambda e: e.copy(out=zbt, in_=o), reads=[r_o], writes=[r_zb])
        kb.dma("sp", lambda e: e.dma_start(out=xab[i * 128:(i + 1) * 128, :], in_=zbt), reads=[r_zb], key=r_zb)
    lnp = LNPipe(nc, kb, es, gam, bet, [r_g1, r_g2], emit)
    pm = es.enter_context(nc.psum_tensor(uq("pm"), [128, 4, 512], F32))
    pm_ring = Ring([pm[:, i] for i in range(4)], "pm")
    for i in range(NT):
        x, r_x = xin_ring.next()
        kb.dma("sp", lambda e: e.dma_start(out=x, in_=x_src[i * 128:(i + 1) * 128, :]), writes=[r_x])
        for dc in range(4):
            p, r_p = pm_ring.next()
            for kc in range(KC):
                kb.op("pe", lambda e: e.matmul(p[:, 0:512], lhsT=mixT[kc][:, i * 128:(i + 1) * 128], rhs=wo[:, kc, dc * 512:(dc + 1) * 512],
                                               start=(kc == 0), stop=(kc == KC - 1)), reads=r_mix + r_wo, writes=[r_p])
            kb.op("dve", lambda e: e.scalar_tensor_tensor(out=x[:, dc * 512:(dc + 1) * 512], in0=x[:, dc * 512:(dc + 1) * 512], scalar=ALPHA, in1=p[:, 0:512],
                                                          op0=ALU.mult, op1=ALU.add), reads=[r_x, r_p], writes=[r_x])
        lnp.feed(x, r_x, i)
    lnp.flush()


FOXH = 8
CQ, CK, CV, CF, CA, CG = 0, 1024, 2048, 3072, 3080, 4104


def even_phase(nc, kb, L, C, x_src, uT_d, vt_d, xa, xab, w, dbg=None):
    from contextlib import ExitStack
    win = w["even_w_in"][0]
    winv = win.rearrange("(kc p) n -> p kc n", p=128)
    with ExitStack() as es0:
        def sb0(name, shape, dt):
            return es0.enter_context(nc.sbuf_tensor(uq(name), shape, dt))
        ident_s = sb0("ident", [128, 128], BF16)
        r_id = Res("ident")
        kb.dma("sp", lambda e: e.dma_start(out=ident_s[:], in_=C["ident"]), writes=[r_id])
        attT = sb0("attT", [128, FOXH, S], BF16)
        r_att = [Res("att%d" % h) for h in range(FOXH)]
        with ExitStack() as es2:
            def sb2(name, shape, dt):
                return es2.enter_context(nc.sbuf_tensor(uq(name), shape, dt))

            def ps2(name, shape, dt):
                return es2.enter_context(nc.psum_tensor(uq(name), shape, dt))
            xT = sb2("xT", [128, KC, S], BF16)
            r_xT = [Res("xT%d" % i) for i in range(NT)]
            pT = ps2("pT", [128, 2, 1024], BF16)
            pT_ring = Ring([pT[:, i] for i in range(2)], "pT")
            build_xT(nc, kb, es2, x_src, xT, r_xT, ident_s, r_id, pT_ring)
            wch = sb2("wch", [128, 3, KC, 128], BF16)
            wch_ring = Ring([wch[:, i] for i in range(3)], "wch")
            pp = ps2("pp", [128, 6, 512], F32)
            pp_ring = Ring([pp[:, i] for i in range(6)], "pp")

            def proj_fm(col0, tg, wt, r_w):
                p, r_p = pp_ring.next()
                for kc in range(KC):
                    kb.op("pe", lambda e: e.matmul(p[:, 0:512], lhsT=wt[:, kc, :], rhs=xT[:, kc, tg * 512:(tg + 1) * 512],
                                                   start=(kc == 0), stop=(kc == KC - 1)), reads=[r_w] + r_xT[tg * 4:(tg + 1) * 4], writes=[r_p])
                return p, r_p

            def load_w(col0):
                wt, r_w = wch_ring.next()
                kb.dma("pool", lambda e: e.dma_start(out=wt, in_=winv[:, :, col0:col0 + 128]), writes=[r_w])
                return wt, r_w
            with ExitStack() as es3:
                def sb3(name, shape, dt):
                    return es3.enter_context(nc.sbuf_tensor(uq(name), shape, dt))
                identf = sb3("identf", [128, 128], F32)
                onesm = sb3("onesm", [128, 128], F32)
                r_c = Res("cc")
                kb.dma("sp", lambda e: e.dma_start(out=identf[:], in_=C["identf"]), writes=[r_c])
                kb.op("dve", lambda e: e.memset(onesm[:], 1.0 / 128.0), writes=[r_c])
                cw31 = sb3("cw31", [31, 1024], F32)
                r_cw = Res("cw31")
                kb.dma("sp", lambda e: e.dma_start(out=cw31[:], in_=w["even_conv_w"][0].rearrange("j o c -> j (o c)")), writes=[r_cw])
                cwT = sb3("cwT", [128, 8, 32], F32)
                r_cwT = Res("cwT")
                prm = sb3("prm", [128, 3, 8], F32)
                r_prm = Res("prm")
                with nc.allow_non_contiguous_dma(reason="tiny per-channel params"):
                    for k, nm in enumerate(("even_conv_b", "even_conv_norm_g", "even_conv_norm_b")):
                        kb.dma("sp", lambda e: e.dma_start(out=prm[:, k, :], in_=w[nm][0].rearrange("(c p) -> p c", p=128)), writes=[r_prm])
                for c in range(8):
                    p, r_p = pp_ring.next()
                    kb.op("pe", lambda e: e.transpose(out=p[:, 0:31], in_=cw31[0:31, c * 128:(c + 1) * 128], identity=identf[0:31, 0:31]),
                          reads=[r_cw, r_c], writes=[r_p])
                    kb.op("dve", lambda e: e.tensor_copy(out=cwT[:, c, 0:31], in_=p[:, 0:31]), reads=[r_p], writes=[r_cwT])
                cin = sb3("cin", [128, 2, 32 + S], BF16)
                cin_ring = Ring([cin[:, i] for i in range(2)], "cin")
                kb.op("dve", lambda e: e.memset(cin[:, :, 0:32], 0.0), writes=cin_ring.r)
                dg = sb3("dg", [128, 2, 31, 128], BF16)
                dg_ring = Ring([dg[:, i] for i in range(2)], "dg")
                sgs = sb3("sgs", [128, 2, 512], F32)
                sg_ring = Ring([sgs[:, i] for i in range(2)], "sgs")
                tb = sb3("tb", [128, 2, 4, 512], F32)
                tb_res = [[Res("tb%d_%d" % (a, b)) for b in range(4)] for a in range(2)]
                sq = sb3("sq", [128, 4, 512], F32)
                sq_res = [Res("sq%d" % b) for b in range(4)]
                uo = sb3("uo", [128, 2, 512], BF16)
                uo_ring = Ring([uo[:, i] for i in range(2)], "uo")

                def proj_stage(c):
                    wa, r_wa = load_w(CA + c * 128)
                    wg, r_wg = load_w(CG + c * 128)
                    ci, r_ci = cin_ring.next()
                    for tg in range(4):
                        pa, r_pa = proj_fm(CA, tg, wa, r_wa)
                        pg, r_pg = proj_fm(CG, tg, wg, r_wg)
                        sg, r_sg = sg_ring.next()
                        kb.op("act", lambda e: e.activation(out=sg, in_=pg[:, 0:512], func=AF.Sigmoid), reads=[r_pg], writes=[r_sg])
                        kb.op("dve", lambda e: e.tensor_tensor(out=ci[:, 32 + tg * 512:32 + (tg + 1) * 512], in0=sg, in1=pa[:, 0:512], op=ALU.mult),
                              reads=[r_sg, r_pa], writes=[r_ci])
                    dgt, r_dg = dg_ring.next()
                    for j in range(31):
                        kb.op("dve", lambda e: e.tensor_scalar(out=dgt[:, j, :], in0=identf[:], scalar1=cwT[:, c, j:j + 1], scalar2=None, op0=ALU.mult),
                              reads=[r_c, r_cwT], writes=[r_dg])
                    return ci, r_ci, dgt, r_dg

                def conv_stage(c, ci, r_ci, dgt, r_dg):
                    for tg in range(4):
                        pc, r_pc = pp_ring.next()
                        for j in range(31):
                            o0 = 2 + j + tg * 512
                            kb.op("pe", lambda e: e.matmul(pc[:, 0:512], lhsT=dgt[:, j, :], rhs=ci[:, o0:o0 + 512], start=(j == 0), stop=(j == 30)),
                                  reads=[r_dg, r_ci], writes=[r_pc])
                        kb.op("act", lambda e: e.activation(out=tb[:, c % 2, tg], in_=pc[:, 0:512], func=AF.Identity, bias=prm[:, 0, c:c + 1]),
                              reads=[r_pc, r_prm], writes=[tb_res[c % 2][tg]])

                def mean_stage(c):
                    for tg in range(4):
                        ut = tb[:, c % 2, tg]; r_t = tb_res[c % 2][tg]
                        pmn, r_pmn = pp_ring.next()
                        kb.op("pe", lambda e: e.matmul(pmn[:, 0:512], lhsT=onesm[:], rhs=ut, start=True, stop=True), reads=[r_t, r_c], writes=[r_pmn])
                        kb.op("dve", lambda e: e.tensor_tensor(out=ut, in0=ut, in1=pmn[:, 0:512], op=ALU.subtract), reads=[r_t, r_pmn], writes=[r_t])
                        kb.op("act", lambda e: e.activation(out=sq[:, tg], in_=ut, func=AF.Square), reads=[r_t], writes=[sq_res[tg]])

                def var_stage(c):
                    for tg in range(4):
                        dd = tb[:, c % 2, tg]; r_t = tb_res[c % 2][tg]
                        rs = sq[:, tg]; r_s = sq_res[tg]
                        pvr, r_pvr = pp_ring.next()
                        kb.op("pe", lambda e: e.matmul(pvr[:, 0:512], lhsT=onesm[:], rhs=rs, start=True, stop=True), reads=[r_s, r_c], writes=[r_pvr])
                        kb.op("dve", lambda e: e.tensor_scalar_add(out=rs, in0=pvr[:, 0:512], scalar1=LN_EPS), reads=[r_pvr], writes=[r_s])
                        kb.op("act", lambda e: e.sqrt(out=rs, in_=rs), reads=[r_s], writes=[r_s])
                        kb.op("dve", lambda e: e.reciprocal(out=rs, in_=rs), reads=[r_s], writes=[r_s])
                        kb.op("dve", lambda e: e.tensor_tensor(out=dd, in0=dd, in1=rs, op=ALU.mult), reads=[r_t, r_s], writes=[r_t])
                        kb.op("dve", lambda e: e.tensor_scalar(out=dd, in0=dd, scalar1=prm[:, 1, c:c + 1], scalar2=prm[:, 2, c:c + 1], op0=ALU.mult, op1=ALU.add),
                              reads=[r_t, r_prm], writes=[r_t])
                        uot, r_uo = uo_ring.next()
                        kb.op("act", lambda e: e.activation(out=uot, in_=dd, func=AF.Silu), reads=[r_t], writes=[r_uo])
                        kb.dma("sp", lambda e: e.dma_start(out=uT_d[c * 128:(c + 1) * 128, tg * 512:(tg + 1) * 512], in_=uot), reads=[r_uo], key=r_uo)

                for c in range(9):
                    if c < 8:
                        st = proj_stage(c)
                    if c >= 1:
                        mean_stage(c - 1)
                    if c < 8:
                        conv_stage(c, *st)
                    if c >= 1:
                        var_stage(c - 1)
        kb.barrier()
        with ExitStack() as es1:
            def sb1(name, shape, dt):
                return es1.enter_context(nc.sbuf_tensor(uq(name), shape, dt))
            qT = sb1("qT", [128, FOXH, S], BF16)
            kT = sb1("kT", [128, FOXH, S], BF16)
            fl = sb1("fl", [128, NT, FOXH], F32)
            r_q = [Res("q%d" % h) for h in range(FOXH)]
            r_k = [Res("k%d" % h) for h in range(FOXH)]
            r_fl = Res("fl")
            with ExitStack() as es2:
                def sb2(name, shape, dt):
                    return es2.enter_context(nc.sbuf_tensor(uq(name), shape, dt))

                def ps2(name, shape, dt):
                    return es2.enter_context(nc.psum_tensor(uq(name), shape, dt))
                xT = sb2("xT", [128, KC, S], BF16)
                r_xT = [Res("xT%d" % i) for i in range(NT)]
                pT = ps2("pT", [128, 2, 1024], BF16)
                pT_ring = Ring([pT[:, i] for i in range(2)], "pT")
                build_xT(nc, kb, es2, x_src, xT, r_xT, ident_s, r_id, pT_ring)
                wch = sb2("wch", [128, 3, KC, 128], BF16)
                wch_ring = Ring([wch[:, i] for i in range(3)], "wch")
                pp = ps2("pp", [128, 4, 512], F32)
                pp_ring = Ring([pp[:, i] for i in range(4)], "pp")
                nev = 0

                def proj_fm(col0, tg, wt, r_w):
                    p, r_p = pp_ring.next()
                    for kc in range(KC):
                        kb.op("pe", lambda e: e.matmul(p[:, 0:512], lhsT=wt[:, kc, :], rhs=xT[:, kc, tg * 512:(tg + 1) * 512],
                                                       start=(kc == 0), stop=(kc == KC - 1)), reads=[r_w] + r_xT[tg * 4:(tg + 1) * 4], writes=[r_p])
                    return p, r_p

                def load_w(col0):
                    wt, r_w = wch_ring.next()
                    kb.dma("pool", lambda e: e.dma_start(out=wt, in_=winv[:, :, col0:col0 + 128]), writes=[r_w])
                    return wt, r_w

                for h in range(FOXH):
                    for (c0, dst, rr, scl) in ((CQ, qT, r_q, 128.0 ** -0.5), (CK, kT, r_k, 1.0)):
                        wt, r_w = load_w(c0 + h * 128)
                        for tg in range(4):
                            p, r_p = proj_fm(c0, tg, wt, r_w)
                            if nev % 2 == 0:
                                kb.op("act", lambda e: e.mul(out=dst[:, h, tg * 512:(tg + 1) * 512], in_=p[:, 0:512], mul=scl), reads=[r_p], writes=[rr[h]])
                            else:
                                kb.op("dve", lambda e: e.tensor_scalar(out=dst[:, h, tg * 512:(tg + 1) * 512], in0=p[:, 0:512], scalar1=scl, scalar2=None, op0=ALU.mult),
                                      reads=[r_p], writes=[rr[h]])
                            nev += 1
                with ExitStack() as es3:
                    wv = es3.enter_context(nc.sbuf_tensor(uq("wv"), [128, KC, 520], BF16))
                    r_wv = Res("wv")
                    wf32 = es3.enter_context(nc.sbuf_tensor(uq("wf32"), [128, KC, 8], F32))
                    wfb = es3.enter_context(nc.sbuf_tensor(uq("wfb"), [128, KC, 128], BF16))
                    r_wf = Res("wf")
                    vst = es3.enter_context(nc.sbuf_tensor(uq("vst"), [128, 4, 512], BF16))
                    vst_ring = Ring([vst[:, i] for i in range(4)], "vst")
                    for half in range(2):
                        ncol = 520 if half == 0 else 512
                        if half == 0:
                            kb.dma("pool", lambda e: e.dma_start(out=wv[:, :, 0:512], in_=winv[:, :, CV:CV + 512]), writes=[r_wv])
                            kb.dma("sp", lambda e: e.dma_start(out=wf32[:], in_=winv[:, :, CF:CF + 8]), writes=[r_wf])
                            if dbg is not None:
                                kb.dma("sp", lambda e: e.dma_start(out=dbg["wf"], in_=wf32[:].rearrange("p a b -> p (a b)")), reads=[r_wf], key=Res("dbgwf"))
                            kb.op("dve", lambda e: e.memset(wfb[:], 0.0), writes=[r_wf])
                            kb.op("dve", lambda e: e.tensor_copy(out=wfb[:, :, 0:8], in_=wf32[:]), reads=[r_wf], writes=[r_wf])
                        else:
                            kb.dma("pool", lambda e: e.dma_start(out=wv[:, :, 0:512], in_=winv[:, :, CV + 512:CV + 1024]), writes=[r_wv])
                        for i in range(NT):
                            p, r_p = pp_ring.next()
                            for kc in range(KC):
                                kb.op("pe", lambda e: e.matmul(p[:, 0:512], lhsT=xT[:, kc, i * 128:(i + 1) * 128], rhs=wv[:, kc, 0:512],
                                                               start=(kc == 0), stop=(kc == KC - 1)), reads=[r_wv, r_xT[i]], writes=[r_p])
                            vs, r_vs = vst_ring.next()
                            if nev % 2 == 0:
                                kb.op("act", lambda e: e.copy(out=vs, in_=p[:, 0:512]), reads=[r_p], writes=[r_vs])
                            else:
                                kb.op("dve", lambda e: e.tensor_copy(out=vs, in_=p[:, 0:512]), reads=[r_p], writes=[r_vs])
                            nev += 1
                            kb.dma("sp", lambda e: e.dma_start(out=vt_d[i * 128:(i + 1) * 128, half * 512:(half + 1) * 512], in_=vs), reads=[r_vs], key=r_vs)
                            if half == 0:
                                p, r_p = pp_ring.next()
                                for kc in range(KC):
                                    kb.op("pe", lambda e: e.matmul(p[:, 0:128], lhsT=xT[:, kc, i * 128:(i + 1) * 128], rhs=wfb[:, kc, :],
                                                                   start=(kc == 0), stop=(kc == KC - 1)), reads=[r_wf, r_xT[i]], writes=[r_p])
                                kb.op("act", lambda e: e.copy(out=fl[:, i, :], in_=p[:, 0:8]), reads=[r_p], writes=[r_fl])
                if dbg is not None:
                    kb.dma("sp", lambda e: e.dma_start(out=dbg["fl2"], in_=fl[:].rearrange("p a b -> p (a b)")), reads=[r_fl], key=Res("dbgfl2"))
                kb.barrier()
            kb.barrier()
            with ExitStack() as es2:
                def sb2(name, shape, dt):
                    return es2.enter_context(nc.sbuf_tensor(uq(name), shape, dt))

                def ps2(name, shape, dt):
                    return es2.enter_context(nc.psum_tensor(uq(name), shape, dt))
                vt = sb2("vt", [128, NT, 1024], BF16)
                r_v = [Res("v%d" % i) for i in range(NT)]
                for i in range(NT):
                    kb.dma("sp", lambda e: e.dma_start(out=vt[:, i, :], in_=vt_d[i * 128:(i + 1) * 128, :]), writes=[r_v[i]])
                uinc = sb2("uinc", [128, 128], F32)
                m64 = sb2("m64", [128, 128], F32)
                onesf = sb2("onesf", [128, 128], F32)
                ones_b = sb2("ones_b", [128, 128], BF16)
                cmask = sb2("cmask", [128, 128], BF16)
                bfb = sb2("bfb", [128, FOXH], F32)
                r_c = Res("attc")
                kb.dma("sp", lambda e: e.dma_start(out=uinc[:], in_=C["uinc"]), writes=[r_c])
                r_c2 = Res("attc2")
                kb.dma("sp", lambda e: e.dma_start(out=m64[:], in_=C["m64"]), writes=[r_c2])
                r_c3 = Res("attc3")
                kb.dma("sp", lambda e: e.dma_start(out=cmask[:], in_=C["cmask"]), writes=[r_c3])
                r_c4 = Res("attc4")
                kb.dma("sp", lambda e: e.dma_start(out=bfb[:], in_=w["even_b_f"][0].partition_broadcast(128)), writes=[r_c4])
                kb.op("dve", lambda e: e.memset(onesf[:], 1.0), writes=[r_c])
                kb.op("dve", lambda e: e.memset(ones_b[:], 1.0), writes=[r_c])
                lf = sb2("lf", [128, NT, FOXH], F32)
                r_lf = Res("lf")
                kb.op("dve", lambda e: e.tensor_tensor(out=lf[:], in0=fl[:], in1=bfb[:].unsqueeze(1).to_broadcast([128, NT, FOXH]), op=ALU.add),
                      reads=[r_fl, r_c4], writes=[r_lf])
                kb.op("act", lambda e: e.activation(out=lf[:], in_=lf[:], func=AF.Exp, scale=-1.0), reads=[r_lf], writes=[r_lf])
                kb.op("act", lambda e: e.activation(out=lf[:], in_=lf[:], func=AF.Ln, bias=1.0), reads=[r_lf], writes=[r_lf])
                kb.op("dve", lambda e: e.tensor_scalar(out=lf[:], in0=lf[:], scalar1=-1.0, scalar2=None, op0=ALU.mult), reads=[r_lf], writes=[r_lf])
                c_all = sb2("c_all", [128, NT, FOXH], F32)
                cref = sb2("cref", [128, NT, FOXH], F32)
                lfcum = sb2("lfcum", [128, FOXH], F32)
                r_call = Res("c_all"); r_cref = Res("cref"); r_lfc = Res("lfcum")
                kb.op("dve", lambda e: e.memset(lfcum[:], 0.0), writes=[r_lfc])
                pcs = ps2("pcs", [128, 2, 512], F32)
                pcs_ring = Ring([pcs[:, i] for i in range(2)], "pcs")
                for i in range(NT):
                    p, r_p = pcs_ring.next()
                    kb.op("pe", lambda e: e.matmul(p[:, 0:FOXH], lhsT=uinc[:], rhs=lf[:, i, :], start=True, stop=False), reads=[r_lf, r_c], writes=[r_p])
                    kb.op("pe", lambda e: e.matmul(p[:, 0:FOXH], lhsT=onesf[:], rhs=lfcum[:], start=False, stop=True), reads=[r_lfc, r_c], writes=[r_p])
                    kb.op("dve", lambda e: e.tensor_copy(out=c_all[:, i, :], in_=p[:, 0:FOXH]), reads=[r_p], writes=[r_call])
                    p2, r_p2 = pcs_ring.next()
                    kb.op("pe", lambda e: e.matmul(p2[:, 0:FOXH], lhsT=m64[:], rhs=lf[:, i, :], start=True, stop=False), reads=[r_lf, r_c2], writes=[r_p2])
                    kb.op("pe", lambda e: e.matmul(p2[:, 0:FOXH], lhsT=onesf[:], rhs=lfcum[:], start=False, stop=True), reads=[r_lfc, r_c], writes=[r_p2])
                    kb.op("dve", lambda e: e.tensor_copy(out=cref[:, i, :], in_=p2[:, 0:FOXH]), reads=[r_p2], writes=[r_cref])
                    kb.op("dve", lambda e: e.tensor_tensor(out=lfcum[:], in0=lfcum[:], in1=lf[:, i, :], op=ALU.add), reads=[r_lf, r_lfc], writes=[r_lfc])
                if dbg is not None:
                    rd2 = Res("dbg2")
                    kb.dma("sp", lambda e: e.dma_start(out=dbg["c_all"], in_=c_all[:].rearrange("p a b -> p (a b)")), reads=[r_call], key=rd2)
                    kb.dma("sp", lambda e: e.dma_start(out=dbg["cref"], in_=cref[:].rearrange("p a b -> p (a b)")), reads=[r_cref], key=rd2)
                    kb.dma("sp", lambda e: e.dma_start(out=dbg["lf"], in_=lf[:].rearrange("p a b -> p (a b)")), reads=[r_lf], key=rd2)
                    kb.dma("sp", lambda e: e.dma_start(out=dbg["fl"], in_=fl[:].rearrange("p a b -> p (a b)")), reads=[r_fl], key=rd2)
                    kb.dma("sp", lambda e: e.dma_start(out=dbg["bfb"], in_=bfb[:]), reads=[r_c4], key=rd2)
                bias_all = sb2("bias_all", [128, FOXH, NT, NT], F32)
                r_bias = Res("bias")
                for h in range(FOXH):
                    for qb in range(NT):
                        kb.op("dve", lambda e: e.tensor_scalar(out=bias_all[:, h, qb, :], in0=c_all[:, :, h], scalar1=-1.0, scalar2=cref[:, qb, h:h + 1],
                                                               op0=ALU.mult, op1=ALU.add), reads=[r_call, r_cref], writes=[r_bias])
                pst = ps2("pst", [128, 2, 512], F32)
                pst_ring = Ring([pst[:, i] for i in range(2)], "pst")
                po = ps2("po", [128, 2, 512], F32)
                po_ring = Ring([po[:, i] for i in range(2)], "po")
                pr = ps2("pr", [128, 2, 512], F32)
                pr_ring = Ring([pr[:, i] for i in range(2)], "pr")
                PT = sb2("PT", [128, 8, 128], BF16)
                PT_ring = Ring([PT[:, i] for i in range(8)], "PT")
                rcp = sb2("rcp", [128, 2, 128], F32)
                rcp_ring = Ring([rcp[:, i] for i in range(2)], "rcp")
                for h in range(FOXH):
                    for qb in range(NT):
                        pot, r_po = po_ring.next()
                        prt, r_pr = pr_ring.next()
                        nkb = qb + 1
                        for k0 in range(0, nkb, 4):
                            st, r_st = pst_ring.next()
                            kbs = list(range(k0, min(k0 + 4, nkb)))
                            for n, kbk in enumerate(kbs):
                                kb.op("pe", lambda e: e.matmul(st[:, n * 128:(n + 1) * 128], lhsT=kT[:, h, kbk * 128:(kbk + 1) * 128], rhs=qT[:, h, qb * 128:(qb + 1) * 128],
                                                               start=True, stop=True), reads=[r_k[h], r_q[h]], writes=[r_st])
                            pts = []
                            for n, kbk in enumerate(kbs):
                                pt, r_pt = PT_ring.next()
                                kb.op("act", lambda e: e.activation(out=pt, in_=st[:, n * 128:(n + 1) * 128], func=AF.Exp, bias=bias_all[:, h, qb, kbk:kbk + 1]),
                                      reads=[r_st, r_bias], writes=[r_pt])
                                if kbk == qb:
                                    kb.op("dve", lambda e: e.tensor_tensor(out=pt, in0=pt, in1=cmask[:], op=ALU.mult), reads=[r_pt, r_c3], writes=[r_pt])
                                pts.append((kbk, pt, r_pt))
                            for kbk, pt, r_pt in pts:
                                kb.op("pe", lambda e: e.matmul(pot[:, 0:128], lhsT=vt[:, kbk, h * 128:(h + 1) * 128], rhs=pt, start=(kbk == 0), stop=(kbk == qb)),
                                      reads=[r_v[kbk], r_pt], writes=[r_po])
                                kb.op("pe", lambda e: e.matmul(prt[:, 0:128], lhsT=ones_b[:], rhs=pt, start=(kbk == 0), stop=(kbk == qb)),
                                      reads=[r_c, r_pt], writes=[r_pr])
                        rc, r_rc = rcp_ring.next()
                        kb.op("dve", lambda e: e.reciprocal(out=rc, in_=prt[:, 0:128]), reads=[r_pr], writes=[r_rc])
                        kb.op("dve", lambda e: e.tensor_tensor(out=attT[:, h, qb * 128:(qb + 1) * 128], in0=rc, in1=pot[:, 0:128], op=ALU.mult),
                              reads=[r_rc, r_po], writes=[r_att[h]])
        kb.barrier()
        if dbg is not None:
            rd = Res("dbg")
            for h in range(FOXH):
                kb.dma("sp", lambda e: e.dma_start(out=dbg["att"][h * 128:(h + 1) * 128, :], in_=attT[:, h, :]), reads=[r_att[h]], key=rd)
        with ExitStack() as es2:
            uTs = es2.enter_context(nc.sbuf_tensor(uq("uTs"), [128, 8, S], BF16))
            r_u = [Res("uT%d" % c) for c in range(8)]
            for c in range(8):
                kb.dma("sp", lambda e: e.dma_start(out=uTs[:, c, :], in_=uT_d[c * 128:(c + 1) * 128, :]), writes=[r_u[c]])
            chunks = [attT[:, h, :] for h in range(FOXH)] + [uTs[:, c, :] for c in range(8)]
            mix_out_phase(nc, kb, es2, L, chunks, r_att + r_u, w["even_w_out"][0], x_src, xa, xab, w, C)
    kb.barrier()


def make_consts():
    bf = ml_dtypes.bfloat16
    c = {}
    c["ident"] = np.eye(128, dtype=np.float32).astype(bf)
    c["iota"] = np.tile(np.arange(512, dtype=np.float32)[None, :], (128, 1))
    ti = np.zeros((128, NT, 4), np.float32)
    ti[:, :, 0] = np.arange(128)[:, None]
    ti[:, :, 1] = np.arange(NT)[None, :]
    ti[:, :, 2] = 1.0
    c["tokinfo"] = ti.astype(bf)
    c["lstrict"] = np.triu(np.ones((128, 128), np.float32), 1).astype(bf)
    c["ecap"] = np.tile((np.arange(NE, dtype=np.float32) * CAP)[None, :], (128, 1))
    c["identf"] = np.eye(128, dtype=np.float32)
    c["uinc"] = np.triu(np.ones((128, 128), np.float32), 0)
    m64 = np.zeros((128, 128), np.float32); m64[:65, :] = 1.0
    c["m64"] = m64
    c["cmask"] = np.triu(np.ones((128, 128), np.float32), 0).astype(bf)
    c["lgt16"] = np.tril(np.ones((128, 128), np.float32), -1) * (-1.0 / 16.0)
    c["uinc16"] = np.triu(np.ones((128, 128), np.float32), 0) * (-1.0 / 16.0)
    return c


CONST_DT = {"ident": BF16, "iota": F32, "tokinfo": BF16, "lstrict": BF16, "ecap": F32, "identf": F32, "uinc": F32, "m64": F32, "cmask": BF16, "lgt16": F32, "uinc16": F32}


GH = 4
OQ, OK_, OV, OG, OA = 0, 1024, 2048, 4096, 6144


def odd_phase(nc, kb, L, C, x_src, kt_d, vt_d, gt_d, o_d, xa, xab, w):
    from contextlib import ExitStack
    win = w["odd_w_in"][0]
    winv = win.rearrange("(kc p) n -> p kc n", p=128)
    with ExitStack() as es0:
        def sb0(name, shape, dt):
            return es0.enter_context(nc.sbuf_tensor(uq(name), shape, dt))
        ident_s = sb0("ident", [128, 128], BF16)
        r_id = Res("ident")
        kb.dma("sp", lambda e: e.dma_start(out=ident_s[:], in_=C["ident"]), writes=[r_id])
        with ExitStack() as es1:
            def sb1(name, shape, dt):
                return es1.enter_context(nc.sbuf_tensor(uq(name), shape, dt))
            qT = sb1("qT", [128, 8, S], BF16)
            kT = sb1("kT", [128, 8, S], BF16)
            alT = sb1("alT", [16, S], BF16)
            r_q = [Res("q%d" % c) for c in range(8)]
            r_k = [Res("k%d" % c) for c in range(8)]
            r_al = Res("alT")
            with ExitStack() as es2:
                def sb2(name, shape, dt):
                    return es2.enter_context(nc.sbuf_tensor(uq(name), shape, dt))

                def ps2(name, shape, dt):
                    return es2.enter_context(nc.psum_tensor(uq(name), shape, dt))
                xT = sb2("xT", [128, KC, S], BF16)
                r_xT = [Res("xT%d" % i) for i in range(NT)]
                pT = ps2("pT", [128, 2, 1024], BF16)
                pT_ring = Ring([pT[:, i] for i in range(2)], "pT")
                build_xT(nc, kb, es2, x_src, xT, r_xT, ident_s, r_id, pT_ring)
                wch = sb2("wch", [128, 3, KC, 128], BF16)
                wch_ring = Ring([wch[:, i] for i in range(3)], "wch")
                pp = ps2("pp", [128, 4, 512], F32)
                pp_ring = Ring([pp[:, i] for i in range(4)], "pp")
                nev = 0
                for (c0, dst, rr) in ((OQ, qT, r_q), (OK_, kT, r_k)):
                    for c in range(8):
                        wt, r_w = wch_ring.next()
                        kb.dma("pool", lambda e: e.dma_start(out=wt, in_=winv[:, :, c0 + c * 128:c0 + (c + 1) * 128]), writes=[r_w])
                        for tg in range(4):
                            p, r_p = pp_ring.next()
                            for kc in range(KC):
                                kb.op("pe", lambda e: e.matmul(p[:, 0:512], lhsT=wt[:, kc, :], rhs=xT[:, kc, tg * 512:(tg + 1) * 512],
                                                               start=(kc == 0), stop=(kc == KC - 1)), reads=[r_w] + r_xT[tg * 4:(tg + 1) * 4], writes=[r_p])
                            if nev % 2 == 0:
                                kb.op("act", lambda e: e.copy(out=dst[:, c, tg * 512:(tg + 1) * 512], in_=p[:, 0:512]), reads=[r_p], writes=[rr[c]])
                            else:
                                kb.op("dve", lambda e: e.tensor_copy(out=dst[:, c, tg * 512:(tg + 1) * 512], in_=p[:, 0:512]), reads=[r_p], writes=[rr[c]])
                            nev += 1
                wa32 = sb2("wa32", [128, KC, 16], F32)
                wab = sb2("wab", [128, KC, 16], BF16)
                r_wa = Res("wa")
                kb.dma("sp", lambda e: e.dma_start(out=wa32[:], in_=winv[:, :, OA:OA + 16]), writes=[r_wa])
                kb.op("dve", lambda e: e.tensor_copy(out=wab[:], in_=wa32[:]), reads=[r_wa], writes=[r_wa])
                for tg in range(4):
                    p, r_p = pp_ring.next()
                    for kc in range(KC):
                        kb.op("pe", lambda e: e.matmul(p[0:16, 0:512], lhsT=wab[:, kc, :], rhs=xT[:, kc, tg * 512:(tg + 1) * 512],
                                                       start=(kc == 0), stop=(kc == KC - 1)), reads=[r_wa] + r_xT[tg * 4:(tg + 1) * 4], writes=[r_p])
                    kb.op("dve", lambda e: e.tensor_copy(out=alT[0:16, tg * 512:(tg + 1) * 512], in_=p[0:16, 0:512]), reads=[r_p], writes=[r_al])
                wtm = sb2("wtm", [128, 2, KC, 512], BF16)
                wtm_ring = Ring([wtm[:, i] for i in range(2)], "wtm")
                stg = sb2("stg", [128, 4, 512], BF16)
                stg_ring = Ring([stg[:, i] for i in range(4)], "stg")
                for cg in range(10):
                    col0 = OK_ + cg * 512
                    if cg < 2:
                        dd, dcol = kt_d, cg * 512
                    elif cg < 6:
                        dd, dcol = vt_d, (cg - 2) * 512
                    else:
                        dd, dcol = gt_d, (cg - 6) * 512
                    wt, r_w = wtm_ring.next()
                    kb.dma("pool", lambda e: e.dma_start(out=wt, in_=winv[:, :, col0:col0 + 512]), writes=[r_w])
                    for i in range(NT):
                        p, r_p = pp_ring.next()
                        for kc in range(KC):
                            kb.op("pe", lambda e: e.matmul(p[:, 0:512], lhsT=xT[:, kc, i * 128:(i + 1) * 128], rhs=wt[:, kc, :],
                                                           start=(kc == 0), stop=(kc == KC - 1)), reads=[r_w, r_xT[i]], writes=[r_p])
                        st, r_st = stg_ring.next()
                        if nev % 2 == 0:
                            kb.op("act", lambda e: e.copy(out=st, in_=p[:, 0:512]), reads=[r_p], writes=[r_st])
                        else:
                            kb.op("dve", lambda e: e.tensor_copy(out=st, in_=p[:, 0:512]), reads=[r_p], writes=[r_st])
                        nev += 1
                        kb.dma("sp", lambda e: e.dma_start(out=dd[i * 128:(i + 1) * 128, dcol:dcol + 512], in_=st), reads=[r_st], key=r_st)
            kb.barrier()
            with ExitStack() as es2:
                def sb2(name, shape, dt):
                    return es2.enter_context(nc.sbuf_tensor(uq(name), shape, dt))

                def ps2(name, shape, dt):
                    return es2.enter_context(nc.psum_tensor(uq(name), shape, dt))
                lgt = sb2("lgt", [128, 128], F32)
                uin = sb2("uin", [128, 128], F32)
                cmask = sb2("cmask", [128, 128], F32)
                wa2 = sb2("wa2", [16, 1024], F32)
                wa2b = sb2("wa2b", [16, 1024], BF16)
                ba = sb2("ba", [128, 1024], F32)
                ng = sb2("ng", [128, 2048], F32)
                rc = [Res("oc%d" % i) for i in range(6)]
                kb.dma("sp", lambda e: e.dma_start(out=lgt[:], in_=C["lgt16"]), writes=[rc[0]])
                kb.dma("sp", lambda e: e.dma_start(out=uin[:], in_=C["uinc16"]), writes=[rc[1]])
                kb.dma("sp", lambda e: e.dma_start(out=cmask[:], in_=C["uinc"]), writes=[rc[2]])
                kb.dma("sp", lambda e: e.dma_start(out=wa2[:], in_=w["odd_w_a2"][0]), writes=[rc[3]])
                kb.op("dve", lambda e: e.tensor_copy(out=wa2b[:], in_=wa2[:]), reads=[rc[3]], writes=[rc[3]])
                kb.dma("sp", lambda e: e.dma_start(out=ba[:], in_=w["odd_b_a"][0].partition_broadcast(128)), writes=[rc[4]])
                kb.dma("sp", lambda e: e.dma_start(out=ng[:], in_=w["odd_norm_g"][0].partition_broadcast(128)), writes=[rc[5]])
                state = sb2("state", [128, 8, 512], F32)
                stateb = sb2("stateb", [128, 8, 512], BF16)
                r_state = [Res("st%d" % c) for c in range(8)]
                r_stateb = [Res("stb%d" % c) for c in range(8)]
                kb.op("dve", lambda e: e.memset(state[:], 0.0), writes=r_state)
                kb.op("dve", lambda e: e.memset(stateb[:], 0.0), writes=r_stateb)
                lnv = sb2("lnv", [128, 2, 1024], F32)
                lnv_ring = Ring([lnv[:, i] for i in range(2)], "lnv")
                ktk = sb2("ktk", [128, 2, 1024], BF16)
                ktk_ring = Ring([ktk[:, i] for i in range(2)], "ktk")
                vtk = sb2("vtk", [128, 2, 2048], BF16)
                vtk_ring = Ring([vtk[:, i] for i in range(2)], "vtk")
                gtk = sb2("gtk", [128, 2, 2048], BF16)
                gtk_ring = Ring([gtk[:, i] for i in range(2)], "gtk")
                gg = sb2("gg", [128, 2, 2048], BF16)
                gg_ring = Ring([gg[:, i] for i in range(2)], "gg")
                ebm = sb2("ebm", [128, 1, 1024], F32)
                ebm_ring = Ring([ebm[:, i] for i in range(1)], "ebm")
                kend = sb2("kend", [128, 2, 1024], BF16)
                kend_ring = Ring([kend[:, i] for i in range(2)], "kend")
                eb = sb2("eb", [128, 2, 8, 128], F32)
                eb_ring = Ring([eb[:, i] for i in range(2)], "eb")
                enb = sb2("enb", [128, 2, 8, 128], F32)
                enb_ring = Ring([enb[:, i] for i in range(2)], "enb")
                qt = sb2("qt", [128, 2, 8, 128], BF16)
                qt_ring = Ring([qt[:, i] for i in range(2)], "qt")
                ktt = sb2("ktt", [128, 2, 8, 128], BF16)
                ktt_ring = Ring([ktt[:, i] for i in range(2)], "ktt")
                attn = sb2("attn", [128, 2, 128], BF16)
                attn_ring = Ring([attn[:, i] for i in range(2)], "attn")
                osb = sb2("osb", [128, 2, 2048], BF16)
                osb_ring = Ring([osb[:, i] for i in range(2)], "osb")
                sm = sb2("sm", [128, 8], F32)
                r_sm = Res("sm")
                junk = sb2("junk", [128, 512], F32)
                r_junk = Res("junk")
                pA = ps2("pA", [128, 2, 512], F32)
                pA_ring = Ring([pA[:, i] for i in range(2)], "pA")
                pB = ps2("pB", [128, 2, 512], F32)
                pB_ring = Ring([pB[:, i] for i in range(2)], "pB")
                pS = ps2("pS", [128, 1, 512], F32)
                pS_ring = Ring([pS[:, i] for i in range(1)], "pS")
                pO = ps2("pO", [128, 1, 512], F32)
                pO_ring = Ring([pO[:, i] for i in range(1)], "pO")
                pU = ps2("pU", [128, 2, 512], F32)
                pU_ring = Ring([pU[:, i] for i in range(2)], "pU")
                QS = 256.0 ** -0.5
                def make_pre(i):
                    ts = slice(i * 128, (i + 1) * 128)
                    P = {}

                    def q0():
                        P["kt"], P["r_kt"] = ktk_ring.next()
                        kb.dma("sp", lambda e: e.dma_start(out=P["kt"], in_=kt_d[ts, :]), writes=[P["r_kt"]])
                        P["vt"], P["r_vt"] = vtk_ring.next()
                        kb.dma("sp", lambda e: e.dma_start(out=P["vt"], in_=vt_d[ts, :]), writes=[P["r_vt"]])
                        gt_, r_gt = gtk_ring.next()
                        kb.dma("sp", lambda e: e.dma_start(out=gt_, in_=gt_d[ts, :]), writes=[r_gt])
                        lv, r_lv = lnv_ring.next()
                        P["lv"], P["r_lv"] = lv, r_lv
                        for hf in range(2):
                            p, r_p = pA_ring.next()
                            kb.op("pe", lambda e: e.matmul(p[:, 0:512], lhsT=alT[0:16, ts], rhs=wa2b[0:16, hf * 512:(hf + 1) * 512], start=True, stop=True),
                                  reads=[r_al, rc[3]], writes=[r_p])
                            kb.op("dve", lambda e: e.tensor_tensor(out=lv[:, hf * 512:(hf + 1) * 512], in0=p[:, 0:512], in1=ba[:, hf * 512:(hf + 1) * 512], op=ALU.add),
                                  reads=[r_p, rc[4]], writes=[r_lv])
                        kb.op("act", lambda e: e.activation(out=lv, in_=lv, func=AF.Exp, scale=-1.0), reads=[r_lv], writes=[r_lv])
                        kb.op("act", lambda e: e.activation(out=lv, in_=lv, func=AF.Ln, bias=1.0), reads=[r_lv], writes=[r_lv])
                        ggt, r_gg = gg_ring.next()
                        P["gg"], P["r_gg"] = ggt, r_gg
                        kb.op("act", lambda e: e.activation(out=ggt, in_=gt_, func=AF.Silu), reads=[r_gt], writes=[r_gg])
                        kb.op("dve", lambda e: e.tensor_tensor(out=ggt, in0=ggt, in1=ng[:], op=ALU.mult), reads=[r_gg, rc[5]], writes=[r_gg])

                    def q1():
                        lv, r_lv = P["lv"], P["r_lv"]
                        em, r_em = ebm_ring.next()
                        ke, r_ke = kend_ring.next()
                        P["ke"], P["r_ke"] = ke, r_ke
                        for hf in range(2):
                            p, r_p = pA_ring.next()
                            kb.op("pe", lambda e: e.matmul(p[:, 0:512], lhsT=lgt[:], rhs=lv[:, hf * 512:(hf + 1) * 512], start=True, stop=True),
                                  reads=[r_lv, rc[0]], writes=[r_p])
                            kb.op("act", lambda e: e.activation(out=em[:, hf * 512:(hf + 1) * 512], in_=p[:, 0:512], func=AF.Exp), reads=[r_p], writes=[r_em])
                        kb.op("dve", lambda e: e.tensor_tensor(out=ke, in0=em, in1=P["kt"], op=ALU.mult), reads=[r_em, P["r_kt"]], writes=[r_ke])

                    def q2():
                        lv, r_lv = P["lv"], P["r_lv"]
                        P["eb"], P["r_eb"] = eb_ring.next()
                        P["en"], P["r_en"] = enb_ring.next()
                        for hf in range(2):
                            p, r_p = pB_ring.next()
                            for q in range(4):
                                c = hf * 4 + q
                                kb.op("pe", lambda e: e.matmul(p[:, q * 128:(q + 1) * 128], lhsT=lv[:, c * 128:(c + 1) * 128], rhs=uin[:], start=True, stop=True),
                                      reads=[r_lv, rc[1]], writes=[r_p])
                            pv = p[:, 0:512].rearrange("p (a b) -> p a b", a=4)
                            kb.op("act", lambda e: e.activation(out=P["eb"][:, hf * 4:(hf + 1) * 4, :], in_=pv, func=AF.Exp), reads=[r_p], writes=[P["r_eb"]])
                            kb.op("act", lambda e: e.activation(out=P["en"][:, hf * 4:(hf + 1) * 4, :], in_=pv, func=AF.Exp, scale=-1.0), reads=[r_p], writes=[P["r_en"]])

                    def q3():
                        P["qt"], P["r_qt"] = qt_ring.next()
                        P["kt2"], P["r_kt2"] = ktt_ring.next()
                        kb.op("dve", lambda e: e.scalar_tensor_tensor(out=P["qt"], in0=qT[:, :, ts], scalar=QS, in1=P["eb"], op0=ALU.mult, op1=ALU.mult),
                              reads=r_q + [P["r_eb"]], writes=[P["r_qt"]])
                        kb.op("dve", lambda e: e.tensor_tensor(out=P["kt2"], in0=kT[:, :, ts], in1=P["en"], op=ALU.mult), reads=r_k + [P["r_en"]], writes=[P["r_kt2"]])
                    return P, [q0, q1, q2, q3]

                def head(i, h, P, ot, r_ot):
                    qtt, r_qt, kt2, r_kt2 = P["qt"], P["r_qt"], P["kt2"], P["r_kt2"]
                    ke, r_ke, vt_, r_vt = P["ke"], P["r_ke"], P["vt"], P["r_vt"]
                    ebt, r_eb, ggt, r_gg = P["eb"], P["r_eb"], P["gg"], P["r_gg"]
                    pSt, r_pS = pS_ring.next()
                    for cc in range(2):
                        c = 2 * h + cc
                        kb.op("pe", lambda e: e.matmul(pSt[:, 0:128], lhsT=kt2[:, c, :], rhs=qtt[:, c, :], start=(cc == 0), stop=(cc == 1)),
                              reads=[r_kt2, r_qt], writes=[r_pS])
                    at, r_at = attn_ring.next()
                    kb.op("dve", lambda e: e.tensor_tensor(out=at, in0=pSt[:, 0:128], in1=cmask[:], op=ALU.mult), reads=[r_pS, rc[2]], writes=[r_at])
                    vh = vt_[:, h * 512:(h + 1) * 512]
                    pus = []
                    for cc in range(2):
                        c = 2 * h + cc
                        pu, r_pu = pU_ring.next()
                        kb.op("pe", lambda e: e.matmul(pu[:, 0:512], lhsT=ke[:, c * 128:(c + 1) * 128], rhs=vh, start=True, stop=True),
                              reads=[r_ke, r_vt], writes=[r_pu])
                        pus.append((c, pu, r_pu))
                    pOt, r_pO = pO_ring.next()
                    kb.op("pe", lambda e: e.matmul(pOt[:, 0:512], lhsT=at, rhs=vh, start=True, stop=False), reads=[r_at, r_vt], writes=[r_pO])
                    for cc in range(2):
                        c = 2 * h + cc
                        kb.op("pe", lambda e: e.matmul(pOt[:, 0:512], lhsT=qtt[:, c, :], rhs=stateb[:, c, :], start=False, stop=(cc == 1)),
                              reads=[r_qt, r_stateb[c]], writes=[r_pO])
                    for c, pu, r_pu in pus:
                        kb.op("dve", lambda e: e.scalar_tensor_tensor(out=state[:, c, :], in0=state[:, c, :], scalar=ebt[:, c, 127:128], in1=pu[:, 0:512],
                                                                      op0=ALU.mult, op1=ALU.add), reads=[r_state[c], r_eb, r_pu], writes=[r_state[c]])
                        kb.op("act", lambda e: e.copy(out=stateb[:, c, :], in_=state[:, c, :]), reads=[r_state[c]], writes=[r_stateb[c]])
                    kb.op("act", lambda e: e.activation(out=junk[:], in_=pOt[:, 0:512], func=AF.Square, accum_out=sm[:, h:h + 1]), reads=[r_pO], writes=[r_junk, r_sm])
                    kb.op("dve", lambda e: e.tensor_scalar(out=sm[:, 4 + h:5 + h], in0=sm[:, h:h + 1], scalar1=1.0 / 512.0, scalar2=LN_EPS, op0=ALU.mult, op1=ALU.add),
                          reads=[r_sm], writes=[r_sm])
                    kb.op("act", lambda e: e.sqrt(out=sm[:, 4 + h:5 + h], in_=sm[:, 4 + h:5 + h]), reads=[r_sm], writes=[r_sm])
                    kb.op("dve", lambda e: e.reciprocal(out=sm[:, 4 + h:5 + h], in_=sm[:, 4 + h:5 + h]), reads=[r_sm], writes=[r_sm])
                    kb.op("dve", lambda e: e.scalar_tensor_tensor(out=ot[:, h * 512:(h + 1) * 512], in0=pOt[:, 0:512], scalar=sm[:, 4 + h:5 + h],
                                                                  in1=ggt[:, h * 512:(h + 1) * 512], op0=ALU.mult, op1=ALU.mult),
                          reads=[r_pO, r_sm, r_gg], writes=[r_ot])

                Pcur, qs = make_pre(0)
                for q in qs:
                    q()
                for i in range(NT):
                    ts = slice(i * 128, (i + 1) * 128)
                    if i + 1 < NT:
                        Pn, qn = make_pre(i + 1)
                    else:
                        Pn, qn = None, []
                    ot, r_ot = osb_ring.next()
                    for h in range(GH):
                        head(i, h, Pcur, ot, r_ot)
                        if qn:
                            qn.pop(0)()
                    kb.dma("sp", lambda e: e.dma_start(out=o_d[ts, :], in_=ot), reads=[r_ot], key=r_ot)
                    Pcur = Pn
        kb.barrier()
        with ExitStack() as es2:
            mixT = es2.enter_context(nc.sbuf_tensor(uq("mixT"), [128, KC, S], BF16))
            r_mT = [Res("mT%d" % i) for i in range(NT)]
            pT = es2.enter_context(nc.psum_tensor(uq("pT"), [128, 2, 1024], BF16))
            pT_ring = Ring([pT[:, i] for i in range(2)], "pT")
            with ExitStack() as es3:
                build_xT(nc, kb, es3, o_d, mixT, r_mT, ident_s, r_id, pT_ring)
            kb.barrier()
            mix_out_phase(nc, kb, es2, L, [mixT[:, kc, :] for kc in range(KC)], r_mT, w["odd_w_out"][0], x_src, xa, xab, w, C)
    kb.barrier()


W_NAMES = ["even_w_in", "even_b_f", "even_conv_w", "even_conv_b", "even_conv_norm_g", "even_conv_norm_b", "even_w_out",
           "odd_w_in", "odd_w_a2", "odd_b_a", "odd_norm_g", "odd_w_out", "ln_mix_g", "ln_mix_b", "ln_ffn_g", "ln_ffn_b",
           "router_w", "router_bias", "expert_w_gate", "expert_w_up", "expert_w_down"]
W_SHAPES = {"even_w_in": (1, 2048, 5128), "even_b_f": (1, 8), "even_conv_w": (1, 31, 1, 1024), "even_conv_b": (1, 1024),
            "even_conv_norm_g": (1, 1024), "even_conv_norm_b": (1, 1024), "even_w_out": (1, 2048, 2048),
            "odd_w_in": (1, 2048, 6160), "odd_w_a2": (1, 16, 1024), "odd_b_a": (1, 1024), "odd_norm_g": (1, 2048),
            "odd_w_out": (1, 2048, 2048), "ln_mix_g": (2, 2048), "ln_mix_b": (2, 2048), "ln_ffn_g": (2, 2048),
            "ln_ffn_b": (2, 2048), "router_w": (2048, 16), "router_bias": (16,),
            "expert_w_gate": (2, 16, 2048, 1408), "expert_w_up": (2, 16, 2048, 1408), "expert_w_down": (2, 16, 1408, 2048)}


def build_program():
    nc = bass.Bass("TRN2", target_bir_lowering=False)
    kb = KB(nc)

    def din(name, shape, dt):
        return nc.dram_tensor(name, list(shape), dt, kind="ExternalInput").ap()

    def dsc(name, shape, dt):
        return nc.dram_tensor(name, list(shape), dt, kind="Internal").ap()
    consts = make_consts()
    C = {k: din("c_" + k, v.shape, CONST_DT[k]) for k, v in consts.items()}
    w = {k: din(k, W_SHAPES[k], F32) for k in W_NAMES}
    x = din("x", (S, D), F32)
    out = nc.dram_tensor("out", [S, D], F32, kind="ExternalOutput").ap()
    xa = dsc("xa", (S, D), F32)
    xab = dsc("xab", (S + 128, D), BF16)
    x2 = dsc("x2", (S, D), F32)
    ys = dsc("ys", (NE * CAP + 128, D), BF16)
    uT_d = dsc("uT_d", (1024, S), BF16)
    vte_d = dsc("vte_d", (S, 1024), BF16)
    kt_d = dsc("kt_d", (S, 1024), BF16)
    vto_d = dsc("vto_d", (S, 2048), BF16)
    gt_d = dsc("gt_d", (S, 2048), BF16)
    o_d = dsc("o_d", (S, 2048), BF16)
    with nc.sbuf_tensor(uq("zt"), [128, D], BF16) as zt:
        rz = Res("zt")
        kb.op("dve", lambda e: e.memset(zt[:], 0.0), writes=[rz])
        kb.dma("sp", lambda e: e.dma_start(out=ys[YZ:YZ + 128, :], in_=zt[:]), reads=[rz], key=rz)
        kb.dma("sp", lambda e: e.dma_start(out=xab[S:S + 128, :], in_=zt[:]), reads=[rz], key=rz)
        kb.barrier()
    even_phase(nc, kb, 0, C, x, uT_d, vte_d, xa, xab, w)
    moe_phase(nc, kb, 0, C, xa, xab, ys, x2, None, w)
    odd_phase(nc, kb, 1, C, x2, kt_d, vto_d, gt_d, o_d, xa, xab, w)
    moe_phase(nc, kb, 1, C, xa, xab, ys, out, None, w)
    return nc, consts


def kernel(**inputs):
    n = 8
    nc, consts = build_program()
    x = np.ascontiguousarray(np.asarray(inputs["x"], dtype=np.float32))
    shared = {("c_" + k): v for k, v in consts.items()}
    for k in W_NAMES:
        shared[k] = np.ascontiguousarray(np.asarray(inputs[k], dtype=np.float32))
    in_maps = []
    for b in range(n):
        m = dict(shared)
        m["x"] = x[b]
        in_maps.append(m)
    res = run_bass_kernel_spmd(nc, in_maps, core_ids=list(range(n)))
    return np.stack([np.asarray(r["out"], dtype=np.float32) for r in res.results], axis=0)
```

```python
import numpy as np
import ml_dtypes
import concourse.bass as bass
import concourse.mybir as mybir
from concourse.bass_utils import run_bass_kernel_spmd

F32 = mybir.dt.float32
BF16 = mybir.dt.bfloat16
I32 = mybir.dt.int32
AF = mybir.ActivationFunctionType
ALU = mybir.AluOpType
AX = mybir.AxisListType

S = 2048
D = 2048
NT = S // 128
KC = D // 128
DEPTH = 2
ALPHA = (2 * DEPTH) ** 0.25
LN_EPS = 1e-5
NE = 16
FE = 1408
NF = FE // 128
CAP = 512
NJ = CAP // 128
ZROW = S
YZ = NE * CAP


class Res:
    __slots__ = ("name", "w", "rs", "dsem")

    def __init__(self, name):
        self.name = name
        self.w = None
        self.rs = []
        self.dsem = None


class KB:
    ENGS = ("pe", "act", "dve", "pool", "sp")

    def __init__(self, nc, n_dma_sems=48, same_engine_sync=True):
        self.nc = nc
        self.eng = {"pe": nc.tensor, "act": nc.scalar, "dve": nc.vector,
                    "pool": nc.gpsimd, "sp": nc.sync}
        self.sem = {}
        self.cnt = {}
        self.waited = {}
        self.same_engine_sync = same_engine_sync
        for e in self.ENGS:
            self._mksem("p_" + e)
        self._mksem("bar")
        self.dma_pool = []
        for i in range(n_dma_sems):
            self._mksem("d%d" % i)
            self.dma_pool.append("d%d" % i)
        self.dma_next = 0
        self.dma_res = []

    def _mksem(self, name):
        self.sem[name] = self.nc.alloc_semaphore(name)
        self.cnt[name] = 0

    def _wait(self, e, dep):
        if dep is None:
            return
        s, c = dep
        if s == "p_" + e and (e in ("pe", "sp") or not self.same_engine_sync):
            return
        if self.waited.get((e, s), 0) >= c:
            return
        self.eng[e].wait_ge(self.sem[s], c)
        self.waited[(e, s)] = c

    def _pre(self, e, reads, writes):
        for r in reads:
            self._wait(e, r.w)
        for w in writes:
            self._wait(e, w.w)
            for d in w.rs:
                self._wait(e, d)

    def _post(self, dep, reads, writes):
        for r in reads:
            r.rs.append(dep)
            if len(r.rs) > 8:
                m = {}
                for s, c in r.rs:
                    m[s] = max(m.get(s, 0), c)
                r.rs = list(m.items())
        for w in writes:
            w.w = dep
            w.rs = []

    def op(self, e, fn, reads=(), writes=()):
        self._pre(e, reads, writes)
        ins = fn(self.eng[e])
        s = "p_" + e
        self.cnt[s] += 1
        ins.then_inc(self.sem[s], 1)
        self._post((s, self.cnt[s]), reads, writes)
        return ins

    def dma(self, q, fn, reads=(), writes=(), key=None):
        self._pre(q, reads, writes)
        key = key or (writes[0] if writes else reads[0])
        if key.dsem is None:
            assert self.dma_next < len(self.dma_pool), "out of dma sems"
            key.dsem = self.dma_pool[self.dma_next]
            self.dma_next += 1
            self.dma_res.append(key)
        ins = fn(self.eng[q])
        s = key.dsem
        self.cnt[s] += 16
        ins.then_inc(self.sem[s], 16)
        self._post((s, self.cnt[s]), reads, writes)
        return ins

    def barrier(self):
        sp = self.eng["sp"]
        for s, c in self.cnt.items():
            if s in ("bar", "p_sp") or c == 0:
                continue
            if self.waited.get(("sp", s), 0) >= c:
                continue
            sp.wait_ge(self.sem[s], c)
            self.waited[("sp", s)] = c
        self.cnt["bar"] += 1
        sp.nop().then_inc(self.sem["bar"], 1)
        for e in self.ENGS:
            if e != "sp":
                self.eng[e].wait_ge(self.sem["bar"], self.cnt["bar"])
            for s, c in self.cnt.items():
                self.waited[(e, s)] = c
        for r in self.dma_res:
            r.dsem = None
        self.dma_res = []
        self.dma_next = 0


class Ring:
    def __init__(self, views, name):
        self.v = views
        self.r = [Res("%s%d" % (name, i)) for i in range(len(views))]
        self.i = -1

    def next(self):
        self.i = (self.i + 1) % len(self.v)
        return self.v[self.i], self.r[self.i]


_UNIQ = [0]


def uq(name):
    _UNIQ[0] += 1
    return "%s_u%d" % (name, _UNIQ[0])


def ln_tile(kb, z, zr, gam, bet, rg, st, rst, out, rout, eng2="dve"):
    stats, mv, rstd = st
    for c in range(4):
        kb.op("dve", lambda e: e.bn_stats(out=stats[:, c * 6:(c + 1) * 6], in_=z[:, c * 512:(c + 1) * 512]),
              reads=[zr], writes=[rst])
    kb.op("dve", lambda e: e.bn_aggr(out=mv[:, 0:2], in_=stats[:, 0:24]), reads=[rst], writes=[rst])
    kb.op("dve", lambda e: e.tensor_scalar_add(out=rstd[:, 0:1], in0=mv[:, 1:2], scalar1=LN_EPS), reads=[rst], writes=[rst])
    kb.op("act", lambda e: e.sqrt(out=rstd[:, 0:1], in_=rstd[:, 0:1]), reads=[rst], writes=[rst])
    kb.op("dve", lambda e: e.reciprocal(out=rstd[:, 0:1], in_=rstd[:, 0:1]), reads=[rst], writes=[rst])
    kb.op("dve", lambda e: e.tensor_scalar(out=z[:, :], in0=z[:, :], scalar1=mv[:, 0:1], scalar2=rstd[:, 0:1],
                                           op0=ALU.subtract, op1=ALU.mult), reads=[zr, rst], writes=[zr])
    kb.op(eng2, lambda e: e.tensor_tensor(out=z[:, :], in0=z[:, :], in1=gam[:, :], op=ALU.mult), reads=[zr] + rg, writes=[zr])
    kb.op(eng2, lambda e: e.tensor_tensor(out=out[:, :], in0=z[:, :], in1=bet[:, :], op=ALU.add), reads=[zr] + rg, writes=[rout])


class LNPipe:
    def __init__(self, nc, kb, es, gam, bet, rgs, emit, eps=LN_EPS):
        self.kb = kb
        self.gam, self.bet, self.rgs, self.emit, self.eps = gam, bet, rgs, emit, eps
        st = es.enter_context(nc.sbuf_tensor(uq("lnst"), [128, 2, 32], F32))
        self.st_ring = Ring([st[:, i] for i in range(2)], "lnst")
        zo = es.enter_context(nc.sbuf_tensor(uq("lnzo"), [128, 2, D], F32))
        self.zo_ring = Ring([zo[:, i] for i in range(2)], "lnzo")
        self.pending = None

    def _apply(self):
        kb = self.kb
        z, r_z, tag = self.pending
        o, r_o = self.zo_ring.next()
        kb.op("dve", lambda e: e.tensor_tensor(out=z, in0=z, in1=self.gam[:, :], op=ALU.mult), reads=[r_z] + self.rgs, writes=[r_z])
        kb.op("dve", lambda e: e.tensor_tensor(out=o, in0=z, in1=self.bet[:, :], op=ALU.add), reads=[r_z] + self.rgs, writes=[r_o])
        self.pending = None
        self.emit(o, r_o, tag)

    def feed(self, z, r_z, tag):
        kb = self.kb
        st, r_st = self.st_ring.next()
        for c in range(4):
            kb.op("dve", lambda e: e.bn_stats(out=st[:, c * 6:(c + 1) * 6], in_=z[:, c * 512:(c + 1) * 512]), reads=[r_z], writes=[r_st])
        kb.op("dve", lambda e: e.bn_aggr(out=st[:, 24:26], in_=st[:, 0:24]), reads=[r_st], writes=[r_st])
        kb.op("dve", lambda e: e.tensor_scalar_add(out=st[:, 26:27], in0=st[:, 25:26], scalar1=self.eps), reads=[r_st], writes=[r_st])
        kb.op("act", lambda e: e.sqrt(out=st[:, 26:27], in_=st[:, 26:27]), reads=[r_st], writes=[r_st])
        if self.pending is not None:
            self._apply()
        kb.op("dve", lambda e: e.reciprocal(out=st[:, 27:28], in_=st[:, 26:27]), reads=[r_st], writes=[r_st])
        kb.op("dve", lambda e: e.scalar_tensor_tensor(out=st[:, 28:29], in0=st[:, 24:25], scalar=-1.0, in1=st[:, 27:28], op0=ALU.mult, op1=ALU.mult),
              reads=[r_st], writes=[r_st])
        kb.op("act", lambda e: e.activation(out=z, in_=z, func=AF.Identity, bias=st[:, 28:29], scale=st[:, 27:28]), reads=[r_z, r_st], writes=[r_z])
        self.pending = (z, r_z, tag)

    def flush(self):
        if self.pending is not None:
            self._apply()


def bcast_rows(ap1d, n):
    return ap1d.partition_broadcast(128)


def moe_phase(nc, kb, L, C, xa, xab, ys, xo, xob, w):
    from contextlib import ExitStack
    ident, iota, tokinfo = C["ident"], C["iota"], C["tokinfo"]
    wg_d = w["expert_w_gate"]
    wu_d = w["expert_w_up"]
    wd_d = w["expert_w_down"]

    with ExitStack() as es:
        def sb(name, shape, dt):
            return es.enter_context(nc.sbuf_tensor(uq(name), shape, dt))

        def ps(name, shape, dt):
            return es.enter_context(nc.psum_tensor(uq(name), shape, dt))

        gate_all = sb("gate_all", [128, NT, NE], F32)
        posm_all = sb("posm_all", [128, NT, NE], F32)
        ridx = sb("ridx", [128, NT, 2], I32)
        gsel = sb("gsel", [128, NT, 2], F32)
        tokidx = sb("tokidx", [128, NE * NJ], I32)
        rw_bf = sb("rw_bf", [128, KC, NE], BF16)
        rbias = sb("rbias", [128, NE], F32)
        ecap = sb("ecap", [128, NE], F32)
        ident_s = sb("ident_s", [128, 128], BF16)
        iota_s = sb("iota_s", [128, CAP], F32)
        tokinfo_s = sb("tokinfo_s", [128, NT, 4], BF16)
        lstrict = sb("lstrict", [128, 128], BF16)
        ones_bf = sb("ones_bf", [128, 128], BF16)
        r_const = Res("const")
        r_gate = Res("gate_all")
        r_posm = Res("posm_all")
        r_ridx = Res("ridx")
        r_tok = Res("tokidx")

        kb.dma("pool", lambda e: e.dma_start(out=rw_bf[:], in_=w["router_w"].rearrange("(kc p) e -> p kc e", p=128)), writes=[r_const])
        c2 = Res("c2"); c3 = Res("c3"); c4 = Res("c4"); c5 = Res("c5"); c6 = Res("c6"); c7 = Res("c7")
        kb.dma("sp", lambda e: e.dma_start(out=rbias[:], in_=w["router_bias"].partition_broadcast(128)), writes=[c2])
        kb.dma("sp", lambda e: e.dma_start(out=ident_s[:], in_=ident), writes=[c3])
        kb.dma("sp", lambda e: e.dma_start(out=iota_s[:], in_=iota[:, 0:CAP]), writes=[c4])
        kb.dma("sp", lambda e: e.dma_start(out=tokinfo_s[:], in_=tokinfo), writes=[c5])
        kb.dma("sp", lambda e: e.dma_start(out=lstrict[:], in_=C["lstrict"]), writes=[c6])
        kb.dma("sp", lambda e: e.dma_start(out=ecap[:], in_=C["ecap"]), writes=[c7])
        kb.op("dve", lambda e: e.memset(ones_bf[:], 1.0), writes=[c6])
        consts = [r_const, c2, c3, c4, c5, c6, c7]

        with ExitStack() as es2:
            def sb2(name, shape, dt):
                return es2.enter_context(nc.sbuf_tensor(uq(name), shape, dt))

            def ps2(name, shape, dt):
                return es2.enter_context(nc.psum_tensor(uq(name), shape, dt))

            xt_b = sb2("xt_b", [128, 2, D], BF16)
            xt_ring = Ring([xt_b[:, i] for i in range(2)], "xt_b")
            xT = sb2("xT", [128, 2, KC, 128], BF16)
            xT_ring = Ring([xT[:, i] for i in range(2)], "xT")
            pT = ps2("pT", [128, 2, 1024], BF16)
            pT_ring = Ring([pT[:, i] for i in range(2)], "pT")
            psm = ps2("psm", [128, 2, 2, 512], F32)
            rt = sb2("rt", [128, 16, NE], F32)
            r_rt = Res("rt")
            m4 = sb2("m4", [128, 8, 4], F32)
            mcum = sb2("mcum", [128, NE], BF16)
            m_bf = sb2("m_bf", [128, 2, NE], BF16)
            m_ring = Ring([m_bf[:, i] for i in range(2)], "m_bf")
            r_mcum = Res("mcum")
            oh = sb2("oh", [128, 4, CAP], BF16)
            oh_ring = Ring([oh[:, i] for i in range(4)], "oh")
            kb.op("dve", lambda e: e.memset(mcum[:], 0.0), writes=[r_mcum])

            class Rec:
                def __init__(self):
                    self.ops = []

                def op(self, e, fn, reads=(), writes=()):
                    self.ops.append((e, fn, list(reads), list(writes)))

            def xpose_tile(i):
                xt, r_xt = xt_ring.next()
                kb.dma("sp", lambda e: e.dma_start(out=xt, in_=xab[i * 128:(i + 1) * 128, :]), writes=[r_xt])
                xTt, r_xT = xT_ring.next()
                for g4 in range(4):
                    p, r_p = pT_ring.next()
                    for q in range(4):
                        kc = g4 * 4 + q
                        kb.op("pe", lambda e: e.transpose(out=p[:, q * 128:(q + 1) * 128], in_=xt[:, kc * 128:(kc + 1) * 128], identity=ident_s[:]),
                              reads=[r_xt, c3], writes=[r_p])
                    if g4 % 2 == 0:
                        kb.op("act", lambda e: e.copy(out=xTt[:, g4 * 4:(g4 + 1) * 4, :], in_=p[:, 0:512].rearrange("p (a b) -> p a b", a=4)),
                              reads=[r_p], writes=[r_xT])
                    else:
                        kb.op("dve", lambda e: e.tensor_copy(out=xTt[:, g4 * 4:(g4 + 1) * 4, :], in_=p[:, 0:512].rearrange("p (a b) -> p a b", a=4)),
                              reads=[r_p], writes=[r_xT])
                return xTt, r_xT

            rt2 = sb2("rt2", [128, 2, 16, NE], F32)
            m42 = sb2("m42", [128, 2, 8, 4], F32)
            r_rt2 = [Res("rt_a"), Res("rt_b")]
            r_psr = [Res("psr_a"), Res("psr_b")]
            r_psp = [Res("psp_a"), Res("psp_b")]

            def route_tile(i, par, xTt, r_xT, rk):
                rt_ = rt2[:, par]
                m4_ = m42[:, par]
                pr_ = psm[:, par, 0, 0:NE]
                pp_ = psm[:, par, 1, 0:NE]
                r_psm_r, r_psm_p = r_psr[par], r_psp[par]
                for kc in range(KC):
                    rk.op("pe", lambda e, kc=kc: e.matmul(pr_, lhsT=xTt[:, kc, :], rhs=rw_bf[:, kc, :], start=(kc == 0), stop=(kc == KC - 1)),
                          reads=[r_xT, r_const], writes=[r_psm_r])
                sc = rt_[:, 0]; sel = rt_[:, 1]; eq1 = rt_[:, 2]; sel2 = rt_[:, 3]; ge2 = rt_[:, 4]; M = rt_[:, 5]; wv = rt_[:, 6]
                pos1 = rt_[:, 7]; vv = rt_[:, 8]; sv = rt_[:, 9]; tmp = rt_[:, 10]; sv2 = rt_[:, 11]
                m1 = m4_[:, 0]; m2 = m4_[:, 1]; gs = m4_[:, 2]; gm = m4_[:, 3]
                gmax = m4_[:, 4, 0:1]; wsum = m4_[:, 4, 1:2]; ihi = m4_[:, 5, 0:1]; ilo = m4_[:, 5, 1:2]; t1 = m4_[:, 5, 2:3]
                R = [r_rt2[par]]
                v3 = lambda a: a.rearrange("p (g j) -> p g j", g=4)
                b3 = lambda a: a.unsqueeze(2).to_broadcast([128, 4, 4])
                rk.op("act", lambda e: e.activation(out=sc, in_=pr_, func=AF.Sigmoid), reads=[r_psm_r], writes=R)
                rk.op("dve", lambda e: e.tensor_tensor(out=sel, in0=sc, in1=rbias[:], op=ALU.add), reads=R + [c2], writes=R)
                rk.op("dve", lambda e: e.tensor_reduce(out=m1, in_=v3(sel), axis=AX.X, op=ALU.max), reads=R, writes=R)
                rk.op("dve", lambda e: e.tensor_tensor(out=v3(eq1), in0=v3(sel), in1=b3(m1), op=ALU.is_equal), reads=R, writes=R)
                rk.op("dve", lambda e: e.scalar_tensor_tensor(out=sel2, in0=eq1, scalar=-1e9, in1=sel, op0=ALU.mult, op1=ALU.add), reads=R, writes=R)
                rk.op("dve", lambda e: e.tensor_reduce(out=m2, in_=v3(sel2), axis=AX.X, op=ALU.max), reads=R, writes=R)
                rk.op("dve", lambda e: e.tensor_tensor(out=gs, in0=m1, in1=m2, op=ALU.add), reads=R, writes=R)
                rk.op("dve", lambda e: e.tensor_reduce(out=gmax, in_=gs, axis=AX.X, op=ALU.max), reads=R, writes=R)
                rk.op("dve", lambda e: e.tensor_scalar(out=gm, in0=gs, scalar1=gmax, scalar2=None, op0=ALU.is_equal), reads=R, writes=R)
                rk.op("dve", lambda e: e.tensor_tensor(out=v3(ge2), in0=v3(sel), in1=b3(m2), op=ALU.is_ge), reads=R, writes=R)
                rk.op("dve", lambda e: e.tensor_tensor(out=v3(M), in0=v3(ge2), in1=b3(gm), op=ALU.mult), reads=R, writes=R)
                rk.op("dve", lambda e: e.tensor_tensor(out=wv, in0=sc, in1=M, op=ALU.mult), reads=R, writes=R)
                rk.op("dve", lambda e: e.tensor_reduce(out=wsum, in_=wv, axis=AX.X, op=ALU.add), reads=R, writes=R)
                rk.op("dve", lambda e: e.reciprocal(out=wsum, in_=wsum), reads=R, writes=R)
                rk.op("dve", lambda e: e.tensor_scalar(out=gate_all[:, i, :], in0=wv, scalar1=wsum, scalar2=None, op0=ALU.mult), reads=R, writes=[r_gate])
                mb, r_mb = m_ring.next()
                rk.op("dve", lambda e: e.tensor_copy(out=mb, in_=M), reads=R, writes=[r_mb])
                rk.op("pe", lambda e: e.matmul(pp_, lhsT=lstrict[:], rhs=mb, start=True, stop=False), reads=[r_mb, c6], writes=[r_psm_p])
                rk.op("pe", lambda e: e.matmul(pp_, lhsT=ones_bf[:], rhs=mcum[:], start=False, stop=True), reads=[r_mcum, c6], writes=[r_psm_p])
                rk.op("dve", lambda e: e.tensor_scalar(out=vv, in0=pp_, scalar1=float(CAP), scalar2=None, op0=ALU.is_lt), reads=[r_psm_p] + R, writes=R)
                rk.op("dve", lambda e: e.scalar_tensor_tensor(out=pos1, in0=pp_, scalar=1.0, in1=M, op0=ALU.add, op1=ALU.mult), reads=[r_psm_p] + R, writes=R)
                rk.op("dve", lambda e: e.tensor_tensor(out=pos1, in0=pos1, in1=vv, op=ALU.mult), reads=R, writes=R)
                rk.op("dve", lambda e: e.tensor_scalar_add(out=posm_all[:, i, :], in0=pos1, scalar1=-1.0), reads=R, writes=[r_posm])
                rk.op("dve", lambda e: e.tensor_tensor(out=mcum[:], in0=mcum[:], in1=mb, op=ALU.add), reads=[r_mb, r_mcum], writes=[r_mcum])
                rk.op("dve", lambda e: e.tensor_scalar(out=vv, in0=pos1, scalar1=0.0, scalar2=None, op0=ALU.is_gt), reads=R, writes=R)
                rk.op("dve", lambda e: e.tensor_tensor(out=sv, in0=pos1, in1=ecap[:], op=ALU.add), reads=R + [c7], writes=R)
                rk.op("dve", lambda e: e.tensor_tensor(out=sv, in0=sv, in1=vv, op=ALU.mult), reads=R, writes=R)
                rk.op("dve", lambda e: e.tensor_reduce(out=ihi, in_=sv, axis=AX.X, op=ALU.max), reads=R, writes=R)
                rk.op("dve", lambda e: e.tensor_scalar(out=tmp, in0=sv, scalar1=ihi, scalar2=None, op0=ALU.not_equal), reads=R, writes=R)
                rk.op("dve", lambda e: e.tensor_tensor(out=sv2, in0=sv, in1=tmp, op=ALU.mult), reads=R, writes=R)
                rk.op("dve", lambda e: e.tensor_reduce(out=ilo, in_=sv2, axis=AX.X, op=ALU.max), reads=R, writes=R)
                rk.op("dve", lambda e: e.scalar_tensor_tensor(out=tmp, in0=sv, scalar=ihi, in1=gate_all[:, i, :], op0=ALU.is_equal, op1=ALU.mult,
                                                              accum_out=gsel[:, i, 0:1]), reads=R + [r_gate], writes=R + [r_ridx])
                rk.op("dve", lambda e: e.scalar_tensor_tensor(out=tmp, in0=sv, scalar=ilo, in1=gate_all[:, i, :], op0=ALU.is_equal, op1=ALU.mult,
                                                              accum_out=gsel[:, i, 1:2]), reads=R + [r_gate], writes=R + [r_ridx])
                for k, src in ((0, ihi), (1, ilo)):
                    rk.op("dve", lambda e, src=src: e.tensor_scalar(out=t1, in0=src, scalar1=0.0, scalar2=float(YZ + 1), op0=ALU.is_equal, op1=ALU.mult), reads=R, writes=R)
                    rk.op("dve", lambda e, src=src: e.scalar_tensor_tensor(out=t1, in0=src, scalar=-1.0, in1=t1, op0=ALU.add, op1=ALU.add), reads=R, writes=R)
                    rk.op("dve", lambda e, k=k: e.tensor_copy(out=ridx[:, i, k:k + 1], in_=t1), reads=R, writes=[r_ridx])

            for i0_ in range(0, NT, 2):
                recs = []
                for par in range(2):
                    xTt, r_xT = xpose_tile(i0_ + par)
                    rk = Rec()
                    route_tile(i0_ + par, par, xTt, r_xT, rk)
                    recs.append(rk.ops)
                LAG = 8
                order = []
                na, nb = len(recs[0]), len(recs[1])
                for n in range(max(na, nb + LAG)):
                    if n < na:
                        order.append(recs[0][n])
                    if 0 <= n - LAG < nb:
                        order.append(recs[1][n - LAG])
                for e_, fn_, rd_, wr_ in order:
                    kb.op(e_, fn_, reads=rd_, writes=wr_)
            pacs = sb2("pacs", [128, NE * NJ, 4], F32)
            r_tf = Res("tf")
            ptab = ps2("ptab", [128, 2, NJ, NT, 4], F32)
            ptab_ring = Ring([ptab[:, i] for i in range(2)], "ptab")
            for ex in range(NE):
                pt, r_pt = ptab_ring.next()
                for i in range(NT):
                    o, r_o = oh_ring.next()
                    kb.op("dve", lambda e: e.tensor_scalar(out=o, in0=iota_s[:], scalar1=posm_all[:, i, ex:ex + 1], scalar2=None, op0=ALU.is_equal),
                          reads=[r_posm, c4], writes=[r_o])
                    for j in range(NJ):
                        kb.op("pe", lambda e: e.matmul(pt[:, j, i, 0:4], lhsT=o[:, j * 128:(j + 1) * 128], rhs=tokinfo_s[:, i, :],
                                                       start=True, stop=True), reads=[r_o, c5], writes=[r_pt])
                kb.op("dve", lambda e: e.tensor_reduce(out=pacs[:, ex * NJ:(ex + 1) * NJ, :], in_=pt.rearrange("p j i c -> p j c i"), axis=AX.X, op=ALU.add),
                      reads=[r_pt], writes=[r_tf])
            tf = sb2("tf", [128, NE * NJ, 2], F32)
            kb.op("dve", lambda e: e.scalar_tensor_tensor(out=tf[:, :, 0], in0=pacs[:, :, 1], scalar=128.0, in1=pacs[:, :, 0], op0=ALU.mult, op1=ALU.add),
                  reads=[r_tf], writes=[r_tf])
            kb.op("dve", lambda e: e.tensor_scalar(out=tf[:, :, 1], in0=pacs[:, :, 2], scalar1=-float(ZROW), scalar2=float(ZROW), op0=ALU.mult, op1=ALU.add),
                  reads=[r_tf], writes=[r_tf])
            kb.op("dve", lambda e: e.tensor_tensor(out=tf[:, :, 0], in0=tf[:, :, 0], in1=tf[:, :, 1], op=ALU.add), reads=[r_tf], writes=[r_tf])
            kb.op("dve", lambda e: e.tensor_copy(out=tokidx[:, :], in_=tf[:, :, 0]), reads=[r_tf], writes=[r_tok])
        kb.barrier()

        with ExitStack() as es2:
            def sb2(name, shape, dt):
                return es2.enter_context(nc.sbuf_tensor(uq(name), shape, dt))

            def ps2(name, shape, dt):
                return es2.enter_context(nc.psum_tensor(uq(name), shape, dt))

            NGU = 4
            NWD = 4
            wgu = sb2("wgu", [128, NGU, 2, KC, 256], BF16)
            gu_ring = Ring([wgu[:, i] for i in range(NGU)], "wgu")
            wd = sb2("wd", [128, NWD, NF, 512], BF16)
            wd_ring = Ring([wd[:, i] for i in range(NWD)], "wd")
            xg = sb2("xg", [128, 4, D], BF16)
            xg_ring = Ring([xg[:, i] for i in range(4)], "xg")
            xgT = sb2("xgT", [128, 2, KC, CAP], BF16)
            xgT_res = [[Res("xgT%d_%d" % (b, j)) for j in range(NJ)] for b in range(2)]
            hT = sb2("hT", [128, 2, NF, CAP], BF16)
            hT_res = [[Res("hT%d_%d" % (b, f)) for f in range(NF)] for b in range(2)]
            sg = sb2("sg", [128, 2, CAP], F32)
            sg_ring = Ring([sg[:, i] for i in range(2)], "sg")
            yst = sb2("yst", [128, 4, 512], BF16)
            yst_ring = Ring([yst[:, i] for i in range(4)], "yst")
            pT = ps2("pTe", [128, 2, 1024], BF16)
            pT_ring = Ring([pT[:, i] for i in range(2)], "pTe")
            pg = ps2("pg", [128, 2, 512], F32)
            pg_ring = Ring([pg[:, i] for i in range(2)], "pg")
            pu = ps2("pu", [128, 2, 512], F32)
            pu_ring = Ring([pu[:, i] for i in range(2)], "pu")
            py = ps2("py", [128, 2, 512], F32)
            py_ring = Ring([py[:, i] for i in range(2)], "py")
            r_ys = Res("ys_dram")
            nev_box = [0]

            def prep(ex):
                b = ex % 2
                groups = []
                for j in range(NJ):
                    g, r_g = xg_ring.next()
                    col = ex * NJ + j
                    kb.dma("pool", lambda e: e.indirect_dma_start(out=g, out_offset=None, in_=xab[:, :],
                                                                   in_offset=bass.IndirectOffsetOnAxis(ap=tokidx[:, col:col + 1], axis=0)),
                           reads=[r_tok], writes=[r_g])
                    for g4 in range(4):
                        def grp(g=g, r_g=r_g, j=j, g4=g4, b=b):
                            p, r_p = pT_ring.next()
                            for q in range(4):
                                kc = g4 * 4 + q
                                kb.op("pe", lambda e: e.transpose(out=p[:, q * 128:(q + 1) * 128], in_=g[:, kc * 128:(kc + 1) * 128], identity=ident_s[:]),
                                      reads=[r_g, c3], writes=[r_p])
                            dst = xgT[:, b, g4 * 4:(g4 + 1) * 4, j * 128:(j + 1) * 128]
                            src = p[:, 0:512].rearrange("p (a b) -> p a b", a=4)
                            if nev_box[0] % 2 == 0:
                                kb.op("act", lambda e: e.copy(out=dst, in_=src), reads=[r_p], writes=[xgT_res[b][j]])
                            else:
                                kb.op("dve", lambda e: e.tensor_copy(out=dst, in_=src), reads=[r_p], writes=[xgT_res[b][j]])
                            nev_box[0] += 1
                        groups.append(grp)
                return groups

            for g_ in prep(0):
                g_()
            chunks = [(ex, f) for ex in range(NE) for f in range(NF)]
            loaded = {}
            LOOK = 4

            def emit_load(g):
                ex, f = chunks[g]
                if f % 2 == 1:
                    return
                nf = 2 if f + 1 < NF else 1
                wt, r_w = gu_ring.next()
                kb.dma("pool", lambda e: e.dma_start(out=wt[:, 0, :, 0:nf * 128], in_=wg_d[L, ex].rearrange("(kc p) f -> p kc f", p=128)[:, :, f * 128:(f + nf) * 128]),
                       writes=[r_w])
                kb.dma("pool", lambda e: e.dma_start(out=wt[:, 1, :, 0:nf * 128], in_=wu_d[L, ex].rearrange("(kc p) f -> p kc f", p=128)[:, :, f * 128:(f + nf) * 128]),
                       writes=[r_w])
                for k in range(nf):
                    loaded[g + k] = (wt[:, :, :, k * 128:(k + 1) * 128], r_w)

            def load_wd(ex, dc):
                wdt, r_wd = wd_ring.next()
                kb.dma("pool", lambda e: e.dma_start(out=wdt, in_=wd_d[L, ex].rearrange("(fc p) d -> p fc d", p=128)[:, :, dc * 512:(dc + 1) * 512]),
                       writes=[r_wd])
                return (wdt, r_wd)

            def a_step(ex, f, wt, r_w):
                b = ex % 2
                pgt, r_pg = pg_ring.next()
                put, r_pu = pu_ring.next()
                for kc in range(KC):
                    kb.op("pe", lambda e: e.matmul(pgt[:, 0:CAP], lhsT=wt[:, 0, kc, :], rhs=xgT[:, b, kc, :], start=(kc == 0), stop=(kc == KC - 1)),
                          reads=[r_w] + xgT_res[b], writes=[r_pg])
                for kc in range(KC):
                    kb.op("pe", lambda e: e.matmul(put[:, 0:CAP], lhsT=wt[:, 1, kc, :], rhs=xgT[:, b, kc, :], start=(kc == 0), stop=(kc == KC - 1)),
                          reads=[r_w] + xgT_res[b], writes=[r_pu])
                sgt, r_sg = sg_ring.next()
                kb.op("act", lambda e: e.activation(out=sgt, in_=pgt[:, 0:CAP], func=AF.Silu), reads=[r_pg], writes=[r_sg])
                kb.op("dve", lambda e: e.tensor_tensor(out=hT[:, b, f, :], in0=sgt, in1=put[:, 0:CAP], op=ALU.mult), reads=[r_sg, r_pu], writes=[hT_res[b][f]])

            def b_group(ex, dc, t, wt, r_w):
                b = ex % 2
                pyt, r_py = py_ring.next()
                for f in range(NF):
                    kb.op("pe", lambda e: e.matmul(pyt[:, 0:512], lhsT=hT[:, b, f, t * 128:(t + 1) * 128], rhs=wt[:, f, :], start=(f == 0), stop=(f == NF - 1)),
                          reads=[r_w] + hT_res[b], writes=[r_py])
                y, r_y = yst_ring.next()
                if nev_box[0] % 2 == 0:
                    kb.op("act", lambda e: e.copy(out=y, in_=pyt[:, 0:512]), reads=[r_py], writes=[r_y])
                else:
                    kb.op("dve", lambda e: e.tensor_copy(out=y, in_=pyt[:, 0:512]), reads=[r_py], writes=[r_y])
                nev_box[0] += 1
                row0 = ex * CAP + t * 128
                kb.dma("sp", lambda e: e.dma_start(out=ys[row0:row0 + 128, dc * 512:(dc + 1) * 512], in_=y), reads=[r_y], key=r_y)

            for g in range(LOOK):
                emit_load(g)
            next_load = LOOK
            wd_cur = [load_wd(0, dc) for dc in range(4)]
            sched = [2, 1, 2, 1, 2, 1, 2, 1, 2, 1, 1]
            schedp = [0, 0, 0, 0, 2, 2, 2, 2, 2, 3, 3]
            for it in range(NE + 1):
                bgroups = [(dc, t) for dc in range(4) for t in range(NJ)] if it >= 1 else []
                pgroups = prep(it + 1) if it + 1 < NE else []
                wd_next = [None] * 4

                def side_work(n, npre):
                    for _ in range(npre):
                        if pgroups:
                            pgroups.pop(0)()
                    for _ in range(n):
                        if bgroups:
                            dc, t = bgroups.pop(0)
                            b_group(it - 1, dc, t, *wd_cur[dc])
                            if t == NJ - 1 and it < NE:
                                wd_next[dc] = load_wd(it, dc)
                if it < NE:
                    for f in range(NF):
                        if next_load < len(chunks):
                            emit_load(next_load)
                            next_load += 1
                        a_step(it, f, *loaded.pop(it * NF + f))
                        side_work(sched[f], schedp[f])
                side_work(16, 16)
                if it >= 1:
                    wd_cur = wd_next
        kb.barrier()

        with ExitStack() as es2:
            def sb2(name, shape, dt):
                return es2.enter_context(nc.sbuf_tensor(uq(name), shape, dt))

            gam = sb2("gam", [128, D], F32)
            bet = sb2("bet", [128, D], F32)
            r_gb = Res("gb")
            kb.dma("sp", lambda e: e.dma_start(out=gam[:], in_=w["ln_ffn_g"][L].partition_broadcast(128)), writes=[r_gb])
            r_gb2 = Res("gb2")
            kb.dma("sp", lambda e: e.dma_start(out=bet[:], in_=w["ln_ffn_b"][L].partition_broadcast(128)), writes=[r_gb2])
            xin = sb2("xin", [128, 3, D], F32)
            xin_ring = Ring([xin[:, i] for i in range(3)], "xin")
            rr = sb2("rr", [128, 4, D], BF16)
            rr_ring = Ring([rr[:, i] for i in range(4)], "rr")
            zb = sb2("zb", [128, 2, D], BF16)
            zb_ring = Ring([zb[:, i] for i in range(2)], "zb")
            gs2 = sb2("gs2", [128, NT, 2], F32)
            r_gs2 = Res("gs2")
            kb.op("dve", lambda e: e.tensor_scalar(out=gs2[:], in0=gsel[:], scalar1=1.0 / ALPHA, scalar2=None, op0=ALU.mult), reads=[r_ridx], writes=[r_gs2])

            def emit(o, r_o, i):
                kb.dma("sp", lambda e: e.dma_start(out=xo[i * 128:(i + 1) * 128, :], in_=o), reads=[r_o], key=r_o)
                if xob is not None:
                    zbt, r_zb = zb_ring.next()
                    kb.op("act", lambda e: e.copy(out=zbt, in_=o), reads=[r_o], writes=[r_zb])
                    kb.dma("sp", lambda e: e.dma_start(out=xob[i * 128:(i + 1) * 128, :], in_=zbt), reads=[r_zb], key=r_zb)
            lnp = LNPipe(nc, kb, es2, gam, bet, [r_gb, r_gb2], emit, eps=LN_EPS / (ALPHA * ALPHA))
            for i in range(NT):
                x, r_x = xin_ring.next()
                kb.dma("sp", lambda e: e.dma_start(out=x, in_=xa[i * 128:(i + 1) * 128, :]), writes=[r_x])
                rh, r_rh = rr_ring.next()
                kb.dma("pool", lambda e: e.indirect_dma_start(out=rh, out_offset=None, in_=ys[:, :],
                                                               in_offset=bass.IndirectOffsetOnAxis(ap=ridx[:, i, 0:1], axis=0)), reads=[r_ridx], writes=[r_rh])
                rl, r_rl = rr_ring.next()
                kb.dma("pool", lambda e: e.indirect_dma_start(out=rl, out_offset=None, in_=ys[:, :],
                                                               in_offset=bass.IndirectOffsetOnAxis(ap=ridx[:, i, 1:2], axis=0)), reads=[r_ridx], writes=[r_rl])
                kb.op("dve", lambda e: e.scalar_tensor_tensor(out=x, in0=rh, scalar=gs2[:, i, 0:1], in1=x, op0=ALU.mult, op1=ALU.add),
                      reads=[r_rh, r_x, r_gs2], writes=[r_x])
                kb.op("dve", lambda e: e.scalar_tensor_tensor(out=x, in0=rl, scalar=gs2[:, i, 1:2], in1=x, op0=ALU.mult, op1=ALU.add),
                      reads=[r_rl, r_x, r_gs2], writes=[r_x])
                lnp.feed(x, r_x, i)
            lnp.flush()
        kb.barrier()


def build_xT(nc, kb, es, src, xT, r_xT, ident_s, r_id, pT_ring):
    xt_b = es.enter_context(nc.sbuf_tensor(uq("xtb"), [128, 2, D], BF16))
    ring = Ring([xt_b[:, i] for i in range(2)], "xtb")
    n = 0
    for i in range(NT):
        xt, r_xt = ring.next()
        kb.dma("pool", lambda e: e.dma_start(out=xt, in_=src[i * 128:(i + 1) * 128, :]), writes=[r_xt])
        for g4 in range(4):
            p, r_p = pT_ring.next()
            for q in range(4):
                kc = g4 * 4 + q
                kb.op("pe", lambda e: e.transpose(out=p[:, q * 128:(q + 1) * 128], in_=xt[:, kc * 128:(kc + 1) * 128], identity=ident_s[:]),
                      reads=[r_xt, r_id], writes=[r_p])
            dst = xT[:, g4 * 4:(g4 + 1) * 4, i * 128:(i + 1) * 128]
            srcp = p[:, 0:512].rearrange("p (a b) -> p a b", a=4)
            if n % 2 == 0:
                kb.op("act", lambda e: e.copy(out=dst, in_=srcp), reads=[r_p], writes=[r_xT[i]])
            else:
                kb.op("dve", lambda e: e.tensor_copy(out=dst, in_=srcp), reads=[r_p], writes=[r_xT[i]])
            n += 1


def mix_out_phase(nc, kb, es, L, mixT, r_mix, wout_d, x_src, xa, xab, w, C):
    def sb(name, shape, dt):
        return es.enter_context(nc.sbuf_tensor(uq(name), shape, dt))
    wo = sb("wo", [128, KC, D], BF16)
    r_wo = [Res("wo%d" % i) for i in range(4)]
    for q in range(4):
        kb.dma("pool", lambda e: e.dma_start(out=wo[:, q * 4:(q + 1) * 4, :], in_=wout_d.rearrange("(kc p) d -> p kc d", p=128)[:, q * 4:(q + 1) * 4, :]),
               writes=[r_wo[q]])
    gam = sb("gam", [128, D], F32)
    bet = sb("bet", [128, D], F32)
    r_g1 = Res("g1"); r_g2 = Res("g2")
    kb.dma("sp", lambda e: e.dma_start(out=gam[:], in_=w["ln_mix_g"][L].partition_broadcast(128)), writes=[r_g1])
    kb.dma("sp", lambda e: e.dma_start(out=bet[:], in_=w["ln_mix_b"][L].partition_broadcast(128)), writes=[r_g2])
    xin = sb("xin", [128, 3, D], F32)
    xin_ring = Ring([xin[:, i] for i in range(3)], "xin")
    zb = sb("zb", [128, 2, D], BF16)
    zb_ring = Ring([zb[:, i] for i in range(2)], "zb")

    def emit(o, r_o, i):
        kb.dma("sp", lambda e: e.dma_start(out=xa[i * 128:(i + 1) * 128, :], in_=o), reads=[r_o], key=r_o)
        zbt, r_zb = zb_ring.next()
        kb.op("act", lambda e: e.copy(out=zbt, in_=o), reads=[r_o], writes=[r_zb])
        kb.dma("sp", lambda e: e.dma_start(out=xab[i * 128:(i + 1) * 128, :], in_=zbt), reads=[r_zb], key=r_zb)
    lnp = LNPipe(nc, kb, es, gam, bet, [r_g1, r_g2], emit)
    pm = es.enter_context(nc.psum_tensor(uq("pm"), [128, 6, 512], F32))
    pm_ring = Ring([pm[:, i] for i in range(6)], "pm")
    for i in range(NT):
        x, r_x = xin_ring.next()
        kb.dma("sp", lambda e: e.dma_start(out=x, in_=x_src[i * 128:(i + 1) * 128, :]), writes=[r_x])
        for dc in range(4):
            p, r_p = pm_ring.next()
            for kc in range(KC):
                kb.op("pe", lambda e: e.matmul(p[:, 0:512], lhsT=mixT[kc][:, i * 128:(i + 1) * 128], rhs=wo[:, kc, dc * 512:(dc + 1) * 512],
                                               start=(kc == 0), stop=(kc == KC - 1)), reads=[r_mix[kc] if len(r_mix) == KC else r_mix[i], r_wo[kc // 4]], writes=[r_p])
            kb.op("dve", lambda e: e.scalar_tensor_tensor(out=x[:, dc * 512:(dc + 1) * 512], in0=x[:, dc * 512:(dc + 1) * 512], scalar=ALPHA, in1=p[:, 0:512],
                                                          op0=ALU.mult, op1=ALU.add), reads=[r_x, r_p], writes=[r_x])
        lnp.feed(x, r_x, i)
    lnp.flush()


FOXH = 8
CQ, CK, CV, CF, CA, CG = 0, 1024, 2048, 3072, 3080, 4104


def even_phase(nc, kb, L, C, x_src, uT_d, vt_d, xa, xab, w, dbg=None):
    from contextlib import ExitStack
    win = w["even_w_in"][0]
    winv = win.rearrange("(kc p) n -> p kc n", p=128)
    with ExitStack() as es0:
        def sb0(name, shape, dt):
            return es0.enter_context(nc.sbuf_tensor(uq(name), shape, dt))
        ident_s = sb0("ident", [128, 128], BF16)
        r_id = Res("ident")
        kb.dma("sp", lambda e: e.dma_start(out=ident_s[:], in_=C["ident"]), writes=[r_id])
        attT = sb0("attT", [128, FOXH, S], BF16)
        r_att = [Res("att%d" % h) for h in range(FOXH)]
        with ExitStack() as es2:
            def sb2(name, shape, dt):
                return es2.enter_context(nc.sbuf_tensor(uq(name), shape, dt))

            def ps2(name, shape, dt):
                return es2.enter_context(nc.psum_tensor(uq(name), shape, dt))
            xT = sb2("xT", [128, KC, S], BF16)
            r_xT = [Res("xT%d" % i) for i in range(NT)]
            pT = ps2("pT", [128, 2, 1024], BF16)
            pT_ring = Ring([pT[:, i] for i in range(2)], "pT")
            build_xT(nc, kb, es2, x_src, xT, r_xT, ident_s, r_id, pT_ring)
            wch = sb2("wch", [128, 3, KC, 128], BF16)
            wch_ring = Ring([wch[:, i] for i in range(3)], "wch")
            pp = ps2("pp", [128, 6, 512], F32)
            pp_ring = Ring([pp[:, i] for i in range(6)], "pp")

            def proj_fm(col0, tg, wt, r_w):
                p, r_p = pp_ring.next()
                for kc in range(KC):
                    kb.op("pe", lambda e: e.matmul(p[:, 0:512], lhsT=wt[:, kc, :], rhs=xT[:, kc, tg * 512:(tg + 1) * 512],
                                                   start=(kc == 0), stop=(kc == KC - 1)), reads=[r_w] + r_xT[tg * 4:(tg + 1) * 4], writes=[r_p])
                return p, r_p

            def load_w(col0):
                wt, r_w = wch_ring.next()
                kb.dma("pool", lambda e: e.dma_start(out=wt, in_=winv[:, :, col0:col0 + 128]), writes=[r_w])
                return wt, r_w
            with ExitStack() as es3:
                def sb3(name, shape, dt):
                    return es3.enter_context(nc.sbuf_tensor(uq(name), shape, dt))
                identf = sb3("identf", [128, 128], F32)
                onesm = sb3("onesm", [128, 128], F32)
                r_c = Res("cc")
                kb.dma("sp", lambda e: e.dma_start(out=identf[:], in_=C["identf"]), writes=[r_c])
                kb.op("dve", lambda e: e.memset(onesm[:], 1.0 / 128.0), writes=[r_c])
                cw31 = sb3("cw31", [31, 1024], F32)
                r_cw = Res("cw31")
                kb.dma("sp", lambda e: e.dma_start(out=cw31[:], in_=w["even_conv_w"][0].rearrange("j o c -> j (o c)")), writes=[r_cw])
                cwT = sb3("cwT", [128, 8, 32], F32)
                r_cwT = Res("cwT")
                prm = sb3("prm", [128, 3, 8], F32)
                r_prm = Res("prm")
                with nc.allow_non_contiguous_dma(reason="tiny per-channel params"):
                    for k, nm in enumerate(("even_conv_b", "even_conv_norm_g", "even_conv_norm_b")):
                        kb.dma("sp", lambda e: e.dma_start(out=prm[:, k, :], in_=w[nm][0].rearrange("(c p) -> p c", p=128)), writes=[r_prm])
                for c in range(8):
                    p, r_p = pp_ring.next()
                    kb.op("pe", lambda e: e.transpose(out=p[:, 0:31], in_=cw31[0:31, c * 128:(c + 1) * 128], identity=identf[0:31, 0:31]),
                          reads=[r_cw, r_c], writes=[r_p])
                    kb.op("dve", lambda e: e.tensor_copy(out=cwT[:, c, 0:31], in_=p[:, 0:31]), reads=[r_p], writes=[r_cwT])
                cin = sb3("cin", [128, 2, 32 + S], BF16)
                cin_ring = Ring([cin[:, i] for i in range(2)], "cin")
                kb.op("dve", lambda e: e.memset(cin[:, :, 0:32], 0.0), writes=cin_ring.r)
                dg = sb3("dg", [128, 2, 31, 128], BF16)
                dg_ring = Ring([dg[:, i] for i in range(2)], "dg")
                sgs = sb3("sgs", [128, 2, 512], F32)
                sg_ring = Ring([sgs[:, i] for i in range(2)], "sgs")
                tb = sb3("tb", [128, 2, 4, 512], F32)
                tb_res = [[Res("tb%d_%d" % (a, b)) for b in range(4)] for a in range(2)]
                sq = sb3("sq", [128, 4, 512], F32)
                sq_res = [Res("sq%d" % b) for b in range(4)]
                uo = sb3("uo", [128, 2, 512], BF16)
                uo_ring = Ring([uo[:, i] for i in range(2)], "uo")

                def proj_stage(c):
                    wa, r_wa = load_w(CA + c * 128)
                    wg, r_wg = load_w(CG + c * 128)
                    ci, r_ci = cin_ring.next()
                    for tg in range(4):
                        pa, r_pa = proj_fm(CA, tg, wa, r_wa)
                        pg, r_pg = proj_fm(CG, tg, wg, r_wg)
                        sg, r_sg = sg_ring.next()
                        kb.op("act", lambda e: e.activation(out=sg, in_=pg[:, 0:512], func=AF.Sigmoid), reads=[r_pg], writes=[r_sg])
                        kb.op("dve", lambda e: e.tensor_tensor(out=ci[:, 32 + tg * 512:32 + (tg + 1) * 512], in0=sg, in1=pa[:, 0:512], op=ALU.mult),
                              reads=[r_sg, r_pa], writes=[r_ci])
                    dgt, r_dg = dg_ring.next()
                    for j in range(31):
                        kb.op("dve", lambda e: e.tensor_scalar(out=dgt[:, j, :], in0=identf[:], scalar1=cwT[:, c, j:j + 1], scalar2=None, op0=ALU.mult),
                              reads=[r_c, r_cwT], writes=[r_dg])
                    return ci, r_ci, dgt, r_dg

                def conv_stage(c, ci, r_ci, dgt, r_dg):
                    for tg in range(4):
                        pc, r_pc = pp_ring.next()
                        for j in range(31):
                            o0 = 2 + j + tg * 512
                            kb.op("pe", lambda e: e.matmul(pc[:, 0:512], lhsT=dgt[:, j, :], rhs=ci[:, o0:o0 + 512], start=(j == 0), stop=(j == 30)),
                                  reads=[r_dg, r_ci], writes=[r_pc])
                        kb.op("act", lambda e: e.activation(out=tb[:, c % 2, tg], in_=pc[:, 0:512], func=AF.Identity, bias=prm[:, 0, c:c + 1]),
                              reads=[r_pc, r_prm], writes=[tb_res[c % 2][tg]])

                def mean_stage(c):
                    for tg in range(4):
                        ut = tb[:, c % 2, tg]; r_t = tb_res[c % 2][tg]
                        pmn, r_pmn = pp_ring.next()
                        kb.op("pe", lambda e: e.matmul(pmn[:, 0:512], lhsT=onesm[:], rhs=ut, start=True, stop=True), reads=[r_t, r_c], writes=[r_pmn])
                        kb.op("dve", lambda e: e.tensor_tensor(out=ut, in0=ut, in1=pmn[:, 0:512], op=ALU.subtract), reads=[r_t, r_pmn], writes=[r_t])
                        kb.op("act", lambda e: e.activation(out=sq[:, tg], in_=ut, func=AF.Square), reads=[r_t], writes=[sq_res[tg]])

                def var_stage(c):
                    for tg in range(4):
                        dd = tb[:, c % 2, tg]; r_t = tb_res[c % 2][tg]
                        rs = sq[:, tg]; r_s = sq_res[tg]
                        pvr, r_pvr = pp_ring.next()
                        kb.op("pe", lambda e: e.matmul(pvr[:, 0:512], lhsT=onesm[:], rhs=rs, start=True, stop=True), reads=[r_s, r_c], writes=[r_pvr])
                        kb.op("dve", lambda e: e.tensor_scalar_add(out=rs, in0=pvr[:, 0:512], scalar1=LN_EPS), reads=[r_pvr], writes=[r_s])
                        kb.op("act", lambda e: e.sqrt(out=rs, in_=rs), reads=[r_s], writes=[r_s])
                        kb.op("dve", lambda e: e.reciprocal(out=rs, in_=rs), reads=[r_s], writes=[r_s])
                        kb.op("dve", lambda e: e.tensor_tensor(out=dd, in0=dd, in1=rs, op=ALU.mult), reads=[r_t, r_s], writes=[r_t])
                        kb.op("dve", lambda e: e.tensor_scalar(out=dd, in0=dd, scalar1=prm[:, 1, c:c + 1], scalar2=prm[:, 2, c:c + 1], op0=ALU.mult, op1=ALU.add),
                              reads=[r_t, r_prm], writes=[r_t])
                        uot, r_uo = uo_ring.next()
                        kb.op("act", lambda e: e.activation(out=uot, in_=dd, func=AF.Silu), reads=[r_t], writes=[r_uo])
                        kb.dma("sp", lambda e: e.dma_start(out=uT_d[c * 128:(c + 1) * 128, tg * 512:(tg + 1) * 512], in_=uot), reads=[r_uo], key=r_uo)

                for c in range(9):
                    if c < 8:
                        st = proj_stage(c)
                    if c >= 1:
                        mean_stage(c - 1)
                    if c < 8:
                        conv_stage(c, *st)
                    if c >= 1:
                        var_stage(c - 1)
        kb.barrier()
        with ExitStack() as es1:
            def sb1(name, shape, dt):
                return es1.enter_context(nc.sbuf_tensor(uq(name), shape, dt))
            qT = sb1("qT", [128, FOXH, S], BF16)
            kT = sb1("kT", [128, FOXH, S], BF16)
            fl = sb1("fl", [128, NT, FOXH], F32)
            r_q = [Res("q%d" % h) for h in range(FOXH)]
            r_k = [Res("k%d" % h) for h in range(FOXH)]
            r_fl = Res("fl")
            with ExitStack() as es2:
                def sb2(name, shape, dt):
                    return es2.enter_context(nc.sbuf_tensor(uq(name), shape, dt))

                def ps2(name, shape, dt):
                    return es2.enter_context(nc.psum_tensor(uq(name), shape, dt))
                xT = sb2("xT", [128, KC, S], BF16)
                r_xT = [Res("xT%d" % i) for i in range(NT)]
                pT = ps2("pT", [128, 2, 1024], BF16)
                pT_ring = Ring([pT[:, i] for i in range(2)], "pT")
                build_xT(nc, kb, es2, x_src, xT, r_xT, ident_s, r_id, pT_ring)
                wch = sb2("wch", [128, 3, KC, 128], BF16)
                wch_ring = Ring([wch[:, i] for i in range(3)], "wch")
                pp = ps2("pp", [128, 4, 512], F32)
                pp_ring = Ring([pp[:, i] for i in range(4)], "pp")
                nev = 0

                def proj_fm(col0, tg, wt, r_w):
                    p, r_p = pp_ring.next()
                    for kc in range(KC):
                        kb.op("pe", lambda e: e.matmul(p[:, 0:512], lhsT=wt[:, kc, :], rhs=xT[:, kc, tg * 512:(tg + 1) * 512],
                                                       start=(kc == 0), stop=(kc == KC - 1)), reads=[r_w] + r_xT[tg * 4:(tg + 1) * 4], writes=[r_p])
                    return p, r_p

                def load_w(col0):
                    wt, r_w = wch_ring.next()
                    kb.dma("pool", lambda e: e.dma_start(out=wt, in_=winv[:, :, col0:col0 + 128]), writes=[r_w])
                    return wt, r_w

                for h in range(FOXH):
                    for (c0, dst, rr, scl) in ((CQ, qT, r_q, 128.0 ** -0.5), (CK, kT, r_k, 1.0)):
                        wt, r_w = load_w(c0 + h * 128)
                        for tg in range(4):
                            p, r_p = proj_fm(c0, tg, wt, r_w)
                            if nev % 2 == 0:
                                kb.op("act", lambda e: e.mul(out=dst[:, h, tg * 512:(tg + 1) * 512], in_=p[:, 0:512], mul=scl), reads=[r_p], writes=[rr[h]])
                            else:
                                kb.op("dve", lambda e: e.tensor_scalar(out=dst[:, h, tg * 512:(tg + 1) * 512], in0=p[:, 0:512], scalar1=scl, scalar2=None, op0=ALU.mult),
                                      reads=[r_p], writes=[rr[h]])
                            nev += 1
                with ExitStack() as es3:
                    wv = es3.enter_context(nc.sbuf_tensor(uq("wv"), [128, KC, 520], BF16))
                    r_wv = Res("wv")
                    wf32 = es3.enter_context(nc.sbuf_tensor(uq("wf32"), [128, KC, 8], F32))
                    wfb = es3.enter_context(nc.sbuf_tensor(uq("wfb"), [128, KC, 128], BF16))
                    r_wf = Res("wf")
                    vst = es3.enter_context(nc.sbuf_tensor(uq("vst"), [128, 4, 512], BF16))
                    vst_ring = Ring([vst[:, i] for i in range(4)], "vst")
                    for half in range(2):
                        ncol = 520 if half == 0 else 512
                        if half == 0:
                            kb.dma("pool", lambda e: e.dma_start(out=wv[:, :, 0:512], in_=winv[:, :, CV:CV + 512]), writes=[r_wv])
                            kb.dma("sp", lambda e: e.dma_start(out=wf32[:], in_=winv[:, :, CF:CF + 8]), writes=[r_wf])
                            if dbg is not None:
                                kb.dma("sp", lambda e: e.dma_start(out=dbg["wf"], in_=wf32[:].rearrange("p a b -> p (a b)")), reads=[r_wf], key=Res("dbgwf"))
                            kb.op("dve", lambda e: e.memset(wfb[:], 0.0), writes=[r_wf])
                            kb.op("dve", lambda e: e.tensor_copy(out=wfb[:, :, 0:8], in_=wf32[:]), reads=[r_wf], writes=[r_wf])
                        else:
                            kb.dma("pool", lambda e: e.dma_start(out=wv[:, :, 0:512], in_=winv[:, :, CV + 512:CV + 1024]), writes=[r_wv])
                        for i in range(NT):
                            p, r_p = pp_ring.next()
                            for kc in range(KC):
                                kb.op("pe", lambda e: e.matmul(p[:, 0:512], lhsT=xT[:, kc, i * 128:(i + 1) * 128], rhs=wv[:, kc, 0:512],
                                                               start=(kc == 0), stop=(kc == KC - 1)), reads=[r_wv, r_xT[i]], writes=[r_p])
                            vs, r_vs = vst_ring.next()
                            if nev % 2 == 0:
                                kb.op("act", lambda e: e.copy(out=vs, in_=p[:, 0:512]), reads=[r_p], writes=[r_vs])
                            else:
                                kb.op("dve", lambda e: e.tensor_copy(out=vs, in_=p[:, 0:512]), reads=[r_p], writes=[r_vs])
                            nev += 1
                            kb.dma("sp", lambda e: e.dma_start(out=vt_d[i * 128:(i + 1) * 128, half * 512:(half + 1) * 512], in_=vs), reads=[r_vs], key=r_vs)
                            if half == 0:
                                p, r_p = pp_ring.next()
                                for kc in range(KC):
                                    kb.op("pe", lambda e: e.matmul(p[:, 0:128], lhsT=xT[:, kc, i * 128:(i + 1) * 128], rhs=wfb[:, kc, :],
                                                                   start=(kc == 0), stop=(kc == KC - 1)), reads=[r_wf, r_xT[i]], writes=[r_p])
                                kb.op("act", lambda e: e.copy(out=fl[:, i, :], in_=p[:, 0:8]), reads=[r_p], writes=[r_fl])
                if dbg is not None:
                    kb.dma("sp", lambda e: e.dma_start(out=dbg["fl2"], in_=fl[:].rearrange("p a b -> p (a b)")), reads=[r_fl], key=Res("dbgfl2"))
                kb.barrier()
            kb.barrier()
            with ExitStack() as es2:
                def sb2(name, shape, dt):
                    return es2.enter_context(nc.sbuf_tensor(uq(name), shape, dt))

                def ps2(name, shape, dt):
                    return es2.enter_context(nc.psum_tensor(uq(name), shape, dt))
                vt = sb2("vt", [128, NT, 1024], BF16)
                r_v = [Res("v%d" % i) for i in range(NT)]
                for i in range(NT):
                    kb.dma("sp", lambda e: e.dma_start(out=vt[:, i, :], in_=vt_d[i * 128:(i + 1) * 128, :]), writes=[r_v[i]])
                uinc = sb2("uinc", [128, 128], F32)
                m64 = sb2("m64", [128, 128], F32)
                onesf = sb2("onesf", [128, 128], F32)
                ones_b = sb2("ones_b", [128, 128], BF16)
                cmask = sb2("cmask", [128, 128], BF16)
                bfb = sb2("bfb", [128, FOXH], F32)
                r_c = Res("attc")
                kb.dma("sp", lambda e: e.dma_start(out=uinc[:], in_=C["uinc"]), writes=[r_c])
                r_c2 = Res("attc2")
                kb.dma("sp", lambda e: e.dma_start(out=m64[:], in_=C["m64"]), writes=[r_c2])
                r_c3 = Res("attc3")
                kb.dma("sp", lambda e: e.dma_start(out=cmask[:], in_=C["cmask"]), writes=[r_c3])
                r_c4 = Res("attc4")
                kb.dma("sp", lambda e: e.dma_start(out=bfb[:], in_=w["even_b_f"][0].partition_broadcast(128)), writes=[r_c4])
                kb.op("dve", lambda e: e.memset(onesf[:], 1.0), writes=[r_c])
                kb.op("dve", lambda e: e.memset(ones_b[:], 1.0), writes=[r_c])
                lf = sb2("lf", [128, NT, FOXH], F32)
                r_lf = Res("lf")
                kb.op("dve", lambda e: e.tensor_tensor(out=lf[:], in0=fl[:], in1=bfb[:].unsqueeze(1).to_broadcast([128, NT, FOXH]), op=ALU.add),
                      reads=[r_fl, r_c4], writes=[r_lf])
                kb.op("act", lambda e: e.activation(out=lf[:], in_=lf[:], func=AF.Exp, scale=-1.0), reads=[r_lf], writes=[r_lf])
                kb.op("act", lambda e: e.activation(out=lf[:], in_=lf[:], func=AF.Ln, bias=1.0), reads=[r_lf], writes=[r_lf])
                kb.op("dve", lambda e: e.tensor_scalar(out=lf[:], in0=lf[:], scalar1=-1.0, scalar2=None, op0=ALU.mult), reads=[r_lf], writes=[r_lf])
                c_all = sb2("c_all", [128, NT, FOXH], F32)
                cref = sb2("cref", [128, NT, FOXH], F32)
                lfcum = sb2("lfcum", [128, FOXH], F32)
                r_call = Res("c_all"); r_cref = Res("cref"); r_lfc = Res("lfcum")
                kb.op("dve", lambda e: e.memset(lfcum[:], 0.0), writes=[r_lfc])
                pcs = ps2("pcs", [128, 2, 512], F32)
                pcs_ring = Ring([pcs[:, i] for i in range(2)], "pcs")
                for i in range(NT):
                    p, r_p = pcs_ring.next()
                    kb.op("pe", lambda e: e.matmul(p[:, 0:FOXH], lhsT=uinc[:], rhs=lf[:, i, :], start=True, stop=False), reads=[r_lf, r_c], writes=[r_p])
                    kb.op("pe", lambda e: e.matmul(p[:, 0:FOXH], lhsT=onesf[:], rhs=lfcum[:], start=False, stop=True), reads=[r_lfc, r_c], writes=[r_p])
                    kb.op("dve", lambda e: e.tensor_copy(out=c_all[:, i, :], in_=p[:, 0:FOXH]), reads=[r_p], writes=[r_call])
                    p2, r_p2 = pcs_ring.next()
                    kb.op("pe", lambda e: e.matmul(p2[:, 0:FOXH], lhsT=m64[:], rhs=lf[:, i, :], start=True, stop=False), reads=[r_lf, r_c2], writes=[r_p2])
                    kb.op("pe", lambda e: e.matmul(p2[:, 0:FOXH], lhsT=onesf[:], rhs=lfcum[:], start=False, stop=True), reads=[r_lfc, r_c], writes=[r_p2])
                    kb.op("dve", lambda e: e.tensor_copy(out=cref[:, i, :], in_=p2[:, 0:FOXH]), reads=[r_p2], writes=[r_cref])
                    kb.op("dve", lambda e: e.tensor_tensor(out=lfcum[:], in0=lfcum[:], in1=lf[:, i, :], op=ALU.add), reads=[r_lf, r_lfc], writes=[r_lfc])
                if dbg is not None:
                    rd2 = Res("dbg2")
                    kb.dma("sp", lambda e: e.dma_start(out=dbg["c_all"], in_=c_all[:].rearrange("p a b -> p (a b)")), reads=[r_call], key=rd2)
                    kb.dma("sp", lambda e: e.dma_start(out=dbg["cref"], in_=cref[:].rearrange("p a b -> p (a b)")), reads=[r_cref], key=rd2)
                    kb.dma("sp", lambda e: e.dma_start(out=dbg["lf"], in_=lf[:].rearrange("p a b -> p (a b)")), reads=[r_lf], key=rd2)
                    kb.dma("sp", lambda e: e.dma_start(out=dbg["fl"], in_=fl[:].rearrange("p a b -> p (a b)")), reads=[r_fl], key=rd2)
                    kb.dma("sp", lambda e: e.dma_start(out=dbg["bfb"], in_=bfb[:]), reads=[r_c4], key=rd2)
                bias_all = sb2("bias_all", [128, FOXH, NT, NT], F32)
                r_bias = Res("bias")
                for h in range(FOXH):
                    for qb in range(NT):
                        kb.op("dve", lambda e: e.tensor_scalar(out=bias_all[:, h, qb, :], in0=c_all[:, :, h], scalar1=-1.0, scalar2=cref[:, qb, h:h + 1],
                                                               op0=ALU.mult, op1=ALU.add), reads=[r_call, r_cref], writes=[r_bias])
                pst = ps2("pst", [128, 2, 512], F32)
                pst_ring = Ring([pst[:, i] for i in range(2)], "pst")
                po = ps2("po", [128, 2, 512], F32)
                po_ring = Ring([po[:, i] for i in range(2)], "po")
                pr = ps2("pr", [128, 2, 512], F32)
                pr_ring = Ring([pr[:, i] for i in range(2)], "pr")
                PT = sb2("PT", [128, 8, 128], BF16)
                PT_ring = Ring([PT[:, i] for i in range(8)], "PT")
                rcp = sb2("rcp", [128, 2, 128], F32)
                rcp_ring = Ring([rcp[:, i] for i in range(2)], "rcp")
                groups = []
                for h in range(FOXH):
                    for qb in range(NT):
                        nkb = qb + 1
                        for k0 in range(0, nkb, 4):
                            groups.append((h, qb, list(range(k0, min(k0 + 4, nkb)))))
                acc = {}

                def do_scores(gi):
                    h, qb, kbs = groups[gi]
                    st, r_st = pst_ring.next()
                    for n, kbk in enumerate(kbs):
                        kb.op("pe", lambda e: e.matmul(st[:, n * 128:(n + 1) * 128], lhsT=kT[:, h, kbk * 128:(kbk + 1) * 128], rhs=qT[:, h, qb * 128:(qb + 1) * 128],
                                                       start=True, stop=True), reads=[r_k[h], r_q[h]], writes=[r_st])
                    pts = []
                    for n, kbk in enumerate(kbs):
                        pt, r_pt = PT_ring.next()
                        kb.op("act", lambda e: e.activation(out=pt, in_=st[:, n * 128:(n + 1) * 128], func=AF.Exp, bias=bias_all[:, h, qb, kbk:kbk + 1]),
                              reads=[r_st, r_bias], writes=[r_pt])
                        if kbk == qb:
                            kb.op("dve", lambda e: e.tensor_tensor(out=pt, in0=pt, in1=cmask[:], op=ALU.mult), reads=[r_pt, r_c3], writes=[r_pt])
                        pts.append((kbk, pt, r_pt))
                    return pts

                def do_pv(gi, pts):
                    h, qb, kbs = groups[gi]
                    if kbs[0] == 0:
                        acc[(h, qb)] = (po_ring.next(), pr_ring.next())
                    (pot, r_po), (prt, r_pr) = acc[(h, qb)]
                    for kbk, pt, r_pt in pts:
                        kb.op("pe", lambda e: e.matmul(pot[:, 0:128], lhsT=vt[:, kbk, h * 128:(h + 1) * 128], rhs=pt, start=(kbk == 0), stop=(kbk == qb)),
                              reads=[r_v[kbk], r_pt], writes=[r_po])
                        kb.op("pe", lambda e: e.matmul(prt[:, 0:128], lhsT=ones_b[:], rhs=pt, start=(kbk == 0), stop=(kbk == qb)),
                              reads=[r_c, r_pt], writes=[r_pr])
                    if kbs[-1] == qb:
                        rc, r_rc = rcp_ring.next()
                        kb.op("dve", lambda e: e.reciprocal(out=rc, in_=prt[:, 0:128]), reads=[r_pr], writes=[r_rc])
                        kb.op("dve", lambda e: e.tensor_tensor(out=attT[:, h, qb * 128:(qb + 1) * 128], in0=rc, in1=pot[:, 0:128], op=ALU.mult),
                              reads=[r_rc, r_po], writes=[r_att[h]])
                        del acc[(h, qb)]

                prev = do_scores(0)
                for gi in range(len(groups)):
                    nxt_pts = do_scores(gi + 1) if gi + 1 < len(groups) else None
                    do_pv(gi, prev)
                    prev = nxt_pts
        kb.barrier()
        if dbg is not None:
            rd = Res("dbg")
            for h in range(FOXH):
                kb.dma("sp", lambda e: e.dma_start(out=dbg["att"][h * 128:(h + 1) * 128, :], in_=attT[:, h, :]), reads=[r_att[h]], key=rd)
        with ExitStack() as es2:
            uTs = es2.enter_context(nc.sbuf_tensor(uq("uTs"), [128, 8, S], BF16))
            r_u = [Res("uT%d" % c) for c in range(8)]
            for c in range(8):
                kb.dma("sp", lambda e: e.dma_start(out=uTs[:, c, :], in_=uT_d[c * 128:(c + 1) * 128, :]), writes=[r_u[c]])
            chunks = [attT[:, h, :] for h in range(FOXH)] + [uTs[:, c, :] for c in range(8)]
            mix_out_phase(nc, kb, es2, L, chunks, r_att + r_u, w["even_w_out"][0], x_src, xa, xab, w, C)
    kb.barrier()


def make_consts():
    bf = ml_dtypes.bfloat16
    c = {}
    c["ident"] = np.eye(128, dtype=np.float32).astype(bf)
    c["iota"] = np.tile(np.arange(512, dtype=np.float32)[None, :], (128, 1))
    ti = np.zeros((128, NT, 4), np.float32)
    ti[:, :, 0] = np.arange(128)[:, None]
    ti[:, :, 1] = np.arange(NT)[None, :]
    ti[:, :, 2] = 1.0
    c["tokinfo"] = ti.astype(bf)
    c["lstrict"] = np.triu(np.ones((128, 128), np.float32), 1).astype(bf)
    c["ecap"] = np.tile((np.arange(NE, dtype=np.float32) * CAP)[None, :], (128, 1))
    c["identf"] = np.eye(128, dtype=np.float32)
    c["uinc"] = np.triu(np.ones((128, 128), np.float32), 0)
    m64 = np.zeros((128, 128), np.float32); m64[:65, :] = 1.0
    c["m64"] = m64
    c["cmask"] = np.triu(np.ones((128, 128), np.float32), 0).astype(bf)
    c["lgt16"] = np.tril(np.ones((128, 128), np.float32), -1) * (-1.0 / 16.0)
    c["uinc16"] = np.triu(np.ones((128, 128), np.float32), 0) * (-1.0 / 16.0)
    return c


CONST_DT = {"ident": BF16, "iota": F32, "tokinfo": BF16, "lstrict": BF16, "ecap": F32, "identf": F32, "uinc": F32, "m64": F32, "cmask": BF16, "lgt16": F32, "uinc16": F32}


GH = 4
OQ, OK_, OV, OG, OA = 0, 1024, 2048, 4096, 6144


def odd_phase(nc, kb, L, C, x_src, kt_d, vt_d, gt_d, o_d, xa, xab, w):
    from contextlib import ExitStack
    win = w["odd_w_in"][0]
    winv = win.rearrange("(kc p) n -> p kc n", p=128)
    with ExitStack() as es0:
        def sb0(name, shape, dt):
            return es0.enter_context(nc.sbuf_tensor(uq(name), shape, dt))
        ident_s = sb0("ident", [128, 128], BF16)
        r_id = Res("ident")
        kb.dma("sp", lambda e: e.dma_start(out=ident_s[:], in_=C["ident"]), writes=[r_id])
        with ExitStack() as es1:
            def sb1(name, shape, dt):
                return es1.enter_context(nc.sbuf_tensor(uq(name), shape, dt))
            qT = sb1("qT", [128, 8, S], BF16)
            kT = sb1("kT", [128, 8, S], BF16)
            alT = sb1("alT", [16, S], BF16)
            r_q = [Res("q%d" % c) for c in range(8)]
            r_k = [Res("k%d" % c) for c in range(8)]
            r_al = Res("alT")
            with ExitStack() as es2:
                def sb2(name, shape, dt):
                    return es2.enter_context(nc.sbuf_tensor(uq(name), shape, dt))

                def ps2(name, shape, dt):
                    return es2.enter_context(nc.psum_tensor(uq(name), shape, dt))
                xT = sb2("xT", [128, KC, S], BF16)
                r_xT = [Res("xT%d" % i) for i in range(NT)]
                pT = ps2("pT", [128, 2, 1024], BF16)
                pT_ring = Ring([pT[:, i] for i in range(2)], "pT")
                build_xT(nc, kb, es2, x_src, xT, r_xT, ident_s, r_id, pT_ring)
                wch = sb2("wch", [128, 3, KC, 128], BF16)
                wch_ring = Ring([wch[:, i] for i in range(3)], "wch")
                pp = ps2("pp", [128, 4, 512], F32)
                pp_ring = Ring([pp[:, i] for i in range(4)], "pp")
                nev = 0
                for (c0, dst, rr) in ((OQ, qT, r_q), (OK_, kT, r_k)):
                    for c in range(8):
                        wt, r_w = wch_ring.next()
                        kb.dma("pool", lambda e: e.dma_start(out=wt, in_=winv[:, :, c0 + c * 128:c0 + (c + 1) * 128]), writes=[r_w])
                        for tg in range(4):
                            p, r_p = pp_ring.next()
                            for kc in range(KC):
                                kb.op("pe", lambda e: e.matmul(p[:, 0:512], lhsT=wt[:, kc, :], rhs=xT[:, kc, tg * 512:(tg + 1) * 512],
                                                               start=(kc == 0), stop=(kc == KC - 1)), reads=[r_w] + r_xT[tg * 4:(tg + 1) * 4], writes=[r_p])
                            if nev % 2 == 0:
                                kb.op("act", lambda e: e.copy(out=dst[:, c, tg * 512:(tg + 1) * 512], in_=p[:, 0:512]), reads=[r_p], writes=[rr[c]])
                            else:
                                kb.op("dve", lambda e: e.tensor_copy(out=dst[:, c, tg * 512:(tg + 1) * 512], in_=p[:, 0:512]), reads=[r_p], writes=[rr[c]])
                            nev += 1
                wa32 = sb2("wa32", [128, KC, 16], F32)
                wab = sb2("wab", [128, KC, 16], BF16)
                r_wa = Res("wa")
                kb.dma("sp", lambda e: e.dma_start(out=wa32[:], in_=winv[:, :, OA:OA + 16]), writes=[r_wa])
                kb.op("dve", lambda e: e.tensor_copy(out=wab[:], in_=wa32[:]), reads=[r_wa], writes=[r_wa])
                for tg in range(4):
                    p, r_p = pp_ring.next()
                    for kc in range(KC):
                        kb.op("pe", lambda e: e.matmul(p[0:16, 0:512], lhsT=wab[:, kc, :], rhs=xT[:, kc, tg * 512:(tg + 1) * 512],
                                                       start=(kc == 0), stop=(kc == KC - 1)), reads=[r_wa] + r_xT[tg * 4:(tg + 1) * 4], writes=[r_p])
                    kb.op("dve", lambda e: e.tensor_copy(out=alT[0:16, tg * 512:(tg + 1) * 512], in_=p[0:16, 0:512]), reads=[r_p], writes=[r_al])
                wtm = sb2("wtm", [128, 2, KC, 512], BF16)
                wtm_ring = Ring([wtm[:, i] for i in range(2)], "wtm")
                stg = sb2("stg", [128, 4, 512], BF16)
                stg_ring = Ring([stg[:, i] for i in range(4)], "stg")
                for cg in range(10):
                    col0 = OK_ + cg * 512
                    if cg < 2:
                        dd, dcol = kt_d, cg * 512
                    elif cg < 6:
                        dd, dcol = vt_d, (cg - 2) * 512
                    else:
                        dd, dcol = gt_d, (cg - 6) * 512
                    wt, r_w = wtm_ring.next()
                    kb.dma("pool", lambda e: e.dma_start(out=wt, in_=winv[:, :, col0:col0 + 512]), writes=[r_w])
                    for i in range(NT):
                        p, r_p = pp_ring.next()
                        for kc in range(KC):
                            kb.op("pe", lambda e: e.matmul(p[:, 0:512], lhsT=xT[:, kc, i * 128:(i + 1) * 128], rhs=wt[:, kc, :],
                                                           start=(kc == 0), stop=(kc == KC - 1)), reads=[r_w, r_xT[i]], writes=[r_p])
                        st, r_st = stg_ring.next()
                        if nev % 2 == 0:
                            kb.op("act", lambda e: e.copy(out=st, in_=p[:, 0:512]), reads=[r_p], writes=[r_st])
                        else:
                            kb.op("dve", lambda e: e.tensor_copy(out=st, in_=p[:, 0:512]), reads=[r_p], writes=[r_st])
                        nev += 1
                        kb.dma("sp", lambda e: e.dma_start(out=dd[i * 128:(i + 1) * 128, dcol:dcol + 512], in_=st), reads=[r_st], key=r_st)
            kb.barrier()
            with ExitStack() as es2:
                def sb2(name, shape, dt):
                    return es2.enter_context(nc.sbuf_tensor(uq(name), shape, dt))

                def ps2(name, shape, dt):
                    return es2.enter_context(nc.psum_tensor(uq(name), shape, dt))
                lgt = sb2("lgt", [128, 128], F32)
                uin = sb2("uin", [128, 128], F32)
                cmask = sb2("cmask", [128, 128], F32)
                wa2 = sb2("wa2", [16, 1024], F32)
                wa2b = sb2("wa2b", [16, 1024], BF16)
                ba = sb2("ba", [128, 1024], F32)
                ng = sb2("ng", [128, 2048], F32)
                rc = [Res("oc%d" % i) for i in range(6)]
                kb.dma("sp", lambda e: e.dma_start(out=lgt[:], in_=C["lgt16"]), writes=[rc[0]])
                kb.dma("sp", lambda e: e.dma_start(out=uin[:], in_=C["uinc16"]), writes=[rc[1]])
                kb.dma("sp", lambda e: e.dma_start(out=cmask[:], in_=C["uinc"]), writes=[rc[2]])
                kb.dma("sp", lambda e: e.dma_start(out=wa2[:], in_=w["odd_w_a2"][0]), writes=[rc[3]])
                kb.op("dve", lambda e: e.tensor_copy(out=wa2b[:], in_=wa2[:]), reads=[rc[3]], writes=[rc[3]])
                kb.dma("sp", lambda e: e.dma_start(out=ba[:], in_=w["odd_b_a"][0].partition_broadcast(128)), writes=[rc[4]])
                kb.dma("sp", lambda e: e.dma_start(out=ng[:], in_=w["odd_norm_g"][0].partition_broadcast(128)), writes=[rc[5]])
                state = sb2("state", [128, 8, 512], F32)
                stateb = sb2("stateb", [128, 8, 512], BF16)
                r_state = [Res("st%d" % c) for c in range(8)]
                r_stateb = [Res("stb%d" % c) for c in range(8)]
                kb.op("dve", lambda e: e.memset(state[:], 0.0), writes=r_state)
                kb.op("dve", lambda e: e.memset(stateb[:], 0.0), writes=r_stateb)
                lnv = sb2("lnv", [128, 2, 1024], F32)
                lnv_ring = Ring([lnv[:, i] for i in range(2)], "lnv")
                ktk = sb2("ktk", [128, 2, 1024], BF16)
                ktk_ring = Ring([ktk[:, i] for i in range(2)], "ktk")
                vtk = sb2("vtk", [128, 2, 2048], BF16)
                vtk_ring = Ring([vtk[:, i] for i in range(2)], "vtk")
                gtk = sb2("gtk", [128, 2, 2048], BF16)
                gtk_ring = Ring([gtk[:, i] for i in range(2)], "gtk")
                gg = sb2("gg", [128, 2, 2048], BF16)
                gg_ring = Ring([gg[:, i] for i in range(2)], "gg")
                ebm = sb2("ebm", [128, 1, 1024], F32)
                ebm_ring = Ring([ebm[:, i] for i in range(1)], "ebm")
                kend = sb2("kend", [128, 2, 1024], BF16)
                kend_ring = Ring([kend[:, i] for i in range(2)], "kend")
                eb = sb2("eb", [128, 2, 8, 128], F32)
                eb_ring = Ring([eb[:, i] for i in range(2)], "eb")
                enb = sb2("enb", [128, 2, 8, 128], F32)
                enb_ring = Ring([enb[:, i] for i in range(2)], "enb")
                qt = sb2("qt", [128, 2, 8, 128], BF16)
                qt_ring = Ring([qt[:, i] for i in range(2)], "qt")
                ktt = sb2("ktt", [128, 2, 8, 128], BF16)
                ktt_ring = Ring([ktt[:, i] for i in range(2)], "ktt")
                attn = sb2("attn", [128, 2, 128], BF16)
                attn_ring = Ring([attn[:, i] for i in range(2)], "attn")
                osb = sb2("osb", [128, 2, 2048], BF16)
                osb_ring = Ring([osb[:, i] for i in range(2)], "osb")
                sm = sb2("sm", [128, 8], F32)
                r_sm = Res("sm")
                junk = sb2("junk", [128, 512], F32)
                r_junk = Res("junk")
                pA = ps2("pA", [128, 2, 512], F32)
                pA_ring = Ring([pA[:, i] for i in range(2)], "pA")
                pB = ps2("pB", [128, 2, 512], F32)
                pB_ring = Ring([pB[:, i] for i in range(2)], "pB")
                pS = ps2("pS", [128, 1, 512], F32)
                pS_ring = Ring([pS[:, i] for i in range(1)], "pS")
                pO = ps2("pO", [128, 1, 512], F32)
                pO_ring = Ring([pO[:, i] for i in range(1)], "pO")
                pU = ps2("pU", [128, 2, 512], F32)
                pU_ring = Ring([pU[:, i] for i in range(2)], "pU")
                QS = 256.0 ** -0.5
                def make_pre(i):
                    ts = slice(i * 128, (i + 1) * 128)
                    P = {}

                    def q0():
                        P["kt"], P["r_kt"] = ktk_ring.next()
                        kb.dma("sp", lambda e: e.dma_start(out=P["kt"], in_=kt_d[ts, :]), writes=[P["r_kt"]])
                        P["vt"], P["r_vt"] = vtk_ring.next()
                        kb.dma("sp", lambda e: e.dma_start(out=P["vt"], in_=vt_d[ts, :]), writes=[P["r_vt"]])
                        gt_, r_gt = gtk_ring.next()
                        kb.dma("sp", lambda e: e.dma_start(out=gt_, in_=gt_d[ts, :]), writes=[r_gt])
                        lv, r_lv = lnv_ring.next()
                        P["lv"], P["r_lv"] = lv, r_lv
                        for hf in range(2):
                            p, r_p = pA_ring.next()
                            kb.op("pe", lambda e: e.matmul(p[:, 0:512], lhsT=alT[0:16, ts], rhs=wa2b[0:16, hf * 512:(hf + 1) * 512], start=True, stop=True),
                                  reads=[r_al, rc[3]], writes=[r_p])
                            kb.op("dve", lambda e: e.tensor_tensor(out=lv[:, hf * 512:(hf + 1) * 512], in0=p[:, 0:512], in1=ba[:, hf * 512:(hf + 1) * 512], op=ALU.add),
                                  reads=[r_p, rc[4]], writes=[r_lv])
                        kb.op("act", lambda e: e.activation(out=lv, in_=lv, func=AF.Exp, scale=-1.0), reads=[r_lv], writes=[r_lv])
                        kb.op("act", lambda e: e.activation(out=lv, in_=lv, func=AF.Ln, bias=1.0), reads=[r_lv], writes=[r_lv])
                        ggt, r_gg = gg_ring.next()
                        P["gg"], P["r_gg"] = ggt, r_gg
                        kb.op("act", lambda e: e.activation(out=ggt, in_=gt_, func=AF.Silu), reads=[r_gt], writes=[r_gg])
                        kb.op("dve", lambda e: e.tensor_tensor(out=ggt, in0=ggt, in1=ng[:], op=ALU.mult), reads=[r_gg, rc[5]], writes=[r_gg])

                    def q1():
                        lv, r_lv = P["lv"], P["r_lv"]
                        em, r_em = ebm_ring.next()
                        ke, r_ke = kend_ring.next()
                        P["ke"], P["r_ke"] = ke, r_ke
                        for hf in range(2):
                            p, r_p = pA_ring.next()
                            kb.op("pe", lambda e: e.matmul(p[:, 0:512], lhsT=lgt[:], rhs=lv[:, hf * 512:(hf + 1) * 512], start=True, stop=True),
                                  reads=[r_lv, rc[0]], writes=[r_p])
                            kb.op("act", lambda e: e.activation(out=em[:, hf * 512:(hf + 1) * 512], in_=p[:, 0:512], func=AF.Exp), reads=[r_p], writes=[r_em])
                        kb.op("dve", lambda e: e.tensor_tensor(out=ke, in0=em, in1=P["kt"], op=ALU.mult), reads=[r_em, P["r_kt"]], writes=[r_ke])

                    def q2():
                        lv, r_lv = P["lv"], P["r_lv"]
                        P["eb"], P["r_eb"] = eb_ring.next()
                        P["en"], P["r_en"] = enb_ring.next()
                        for hf in range(2):
                            p, r_p = pB_ring.next()
                            for q in range(4):
                                c = hf * 4 + q
                                kb.op("pe", lambda e: e.matmul(p[:, q * 128:(q + 1) * 128], lhsT=lv[:, c * 128:(c + 1) * 128], rhs=uin[:], start=True, stop=True),
                                      reads=[r_lv, rc[1]], writes=[r_p])
                            pv = p[:, 0:512].rearrange("p (a b) -> p a b", a=4)
                            kb.op("act", lambda e: e.activation(out=P["eb"][:, hf * 4:(hf + 1) * 4, :], in_=pv, func=AF.Exp), reads=[r_p], writes=[P["r_eb"]])
                            kb.op("act", lambda e: e.activation(out=P["en"][:, hf * 4:(hf + 1) * 4, :], in_=pv, func=AF.Exp, scale=-1.0), reads=[r_p], writes=[P["r_en"]])

                    def q3():
                        P["qt"], P["r_qt"] = qt_ring.next()
                        P["kt2"], P["r_kt2"] = ktt_ring.next()
                        kb.op("dve", lambda e: e.scalar_tensor_tensor(out=P["qt"], in0=qT[:, :, ts], scalar=QS, in1=P["eb"], op0=ALU.mult, op1=ALU.mult),
                              reads=r_q + [P["r_eb"]], writes=[P["r_qt"]])
                        kb.op("dve", lambda e: e.tensor_tensor(out=P["kt2"], in0=kT[:, :, ts], in1=P["en"], op=ALU.mult), reads=r_k + [P["r_en"]], writes=[P["r_kt2"]])
                    return P, [q0, q1, q2, q3]

                def head(i, h, P, ot, r_ot):
                    qtt, r_qt, kt2, r_kt2 = P["qt"], P["r_qt"], P["kt2"], P["r_kt2"]
                    ke, r_ke, vt_, r_vt = P["ke"], P["r_ke"], P["vt"], P["r_vt"]
                    ebt, r_eb, ggt, r_gg = P["eb"], P["r_eb"], P["gg"], P["r_gg"]
                    pSt, r_pS = pS_ring.next()
                    for cc in range(2):
                        c = 2 * h + cc
                        kb.op("pe", lambda e: e.matmul(pSt[:, 0:128], lhsT=kt2[:, c, :], rhs=qtt[:, c, :], start=(cc == 0), stop=(cc == 1)),
                              reads=[r_kt2, r_qt], writes=[r_pS])
                    at, r_at = attn_ring.next()
                    kb.op("dve", lambda e: e.tensor_tensor(out=at, in0=pSt[:, 0:128], in1=cmask[:], op=ALU.mult), reads=[r_pS, rc[2]], writes=[r_at])
                    vh = vt_[:, h * 512:(h + 1) * 512]
                    pus = []
                    for cc in range(2):
                        c = 2 * h + cc
                        pu, r_pu = pU_ring.next()
                        kb.op("pe", lambda e: e.matmul(pu[:, 0:512], lhsT=ke[:, c * 128:(c + 1) * 128], rhs=vh, start=True, stop=True),
                              reads=[r_ke, r_vt], writes=[r_pu])
                        pus.append((c, pu, r_pu))
                    pOt, r_pO = pO_ring.next()
                    kb.op("pe", lambda e: e.matmul(pOt[:, 0:512], lhsT=at, rhs=vh, start=True, stop=False), reads=[r_at, r_vt], writes=[r_pO])
                    for cc in range(2):
                        c = 2 * h + cc
                        kb.op("pe", lambda e: e.matmul(pOt[:, 0:512], lhsT=qtt[:, c, :], rhs=stateb[:, c, :], start=False, stop=(cc == 1)),
                              reads=[r_qt, r_stateb[c]], writes=[r_pO])
                    for c, pu, r_pu in pus:
                        kb.op("dve", lambda e: e.scalar_tensor_tensor(out=state[:, c, :], in0=state[:, c, :], scalar=ebt[:, c, 127:128], in1=pu[:, 0:512],
                                                                      op0=ALU.mult, op1=ALU.add), reads=[r_state[c], r_eb, r_pu], writes=[r_state[c]])
                        kb.op("pool", lambda e: e.tensor_copy(out=stateb[:, c, :], in_=state[:, c, :]), reads=[r_state[c]], writes=[r_stateb[c]])
                    kb.op("act", lambda e: e.activation(out=junk[:], in_=pOt[:, 0:512], func=AF.Square, accum_out=sm[:, h:h + 1]), reads=[r_pO], writes=[r_junk, r_sm])
                    kb.op("dve", lambda e: e.tensor_scalar(out=sm[:, 4 + h:5 + h], in0=sm[:, h:h + 1], scalar1=1.0 / 512.0, scalar2=LN_EPS, op0=ALU.mult, op1=ALU.add),
                          reads=[r_sm], writes=[r_sm])
                    kb.op("act", lambda e: e.sqrt(out=sm[:, 4 + h:5 + h], in_=sm[:, 4 + h:5 + h]), reads=[r_sm], writes=[r_sm])
                    kb.op("dve", lambda e: e.reciprocal(out=sm[:, 4 + h:5 + h], in_=sm[:, 4 + h:5 + h]), reads=[r_sm], writes=[r_sm])
                    kb.op("dve", lambda e: e.scalar_tensor_tensor(out=ot[:, h * 512:(h + 1) * 512], in0=pOt[:, 0:512], scalar=sm[:, 4 + h:5 + h],
                                                                  in1=ggt[:, h * 512:(h + 1) * 512], op0=ALU.mult, op1=ALU.mult),
                          reads=[r_pO, r_sm, r_gg], writes=[r_ot])

                Pcur, qs = make_pre(0)
                for q in qs:
                    q()
                for i in range(NT):
                    ts = slice(i * 128, (i + 1) * 128)
                    if i + 1 < NT:
                        Pn, qn = make_pre(i + 1)
                    else:
                        Pn, qn = None, []
                    ot, r_ot = osb_ring.next()
                    for h in range(GH):
                        head(i, h, Pcur, ot, r_ot)
                        if qn:
                            qn.pop(0)()
                    kb.dma("sp", lambda e: e.dma_start(out=o_d[ts, :], in_=ot), reads=[r_ot], key=r_ot)
                    Pcur = Pn
        kb.barrier()
        with ExitStack() as es2:
            mixT = es2.enter_context(nc.sbuf_tensor(uq("mixT"), [128, KC, S], BF16))
            r_mT = [Res("mT%d" % i) for i in range(NT)]
            pT = es2.enter_context(nc.psum_tensor(uq("pT"), [128, 2, 1024], BF16))
            pT_ring = Ring([pT[:, i] for i in range(2)], "pT")
            with ExitStack() as es3:
                build_xT(nc, kb, es3, o_d, mixT, r_mT, ident_s, r_id, pT_ring)
            kb.barrier()
            mix_out_phase(nc, kb, es2, L, [mixT[:, kc, :] for kc in range(KC)], r_mT, w["odd_w_out"][0], x_src, xa, xab, w, C)
    kb.barrier()


W_NAMES = ["even_w_in", "even_b_f", "even_conv_w", "even_conv_b", "even_conv_norm_g", "even_conv_norm_b", "even_w_out",
           "odd_w_in", "odd_w_a2", "odd_b_a", "odd_norm_g", "odd_w_out", "ln_mix_g", "ln_mix_b", "ln_ffn_g", "ln_ffn_b",
           "router_w", "router_bias", "expert_w_gate", "expert_w_up", "expert_w_down"]
W_SHAPES = {"even_w_in": (1, 2048, 5128), "even_b_f": (1, 8), "even_conv_w": (1, 31, 1, 1024), "even_conv_b": (1, 1024),
            "even_conv_norm_g": (1, 1024), "even_conv_norm_b": (1, 1024), "even_w_out": (1, 2048, 2048),
            "odd_w_in": (1, 2048, 6160), "odd_w_a2": (1, 16, 1024), "odd_b_a": (1, 1024), "odd_norm_g": (1, 2048),
            "odd_w_out": (1, 2048, 2048), "ln_mix_g": (2, 2048), "ln_mix_b": (2, 2048), "ln_ffn_g": (2, 2048),
            "ln_ffn_b": (2, 2048), "router_w": (2048, 16), "router_bias": (16,),
            "expert_w_gate": (2, 16, 2048, 1408), "expert_w_up": (2, 16, 2048, 1408), "expert_w_down": (2, 16, 1408, 2048)}


def build_program():
    nc = bass.Bass("TRN2", target_bir_lowering=False)
    kb = KB(nc)

    def din(name, shape, dt):
        return nc.dram_tensor(name, list(shape), dt, kind="ExternalInput").ap()

    def dsc(name, shape, dt):
        return nc.dram_tensor(name, list(shape), dt, kind="Internal").ap()
    consts = make_consts()
    C = {k: din("c_" + k, v.shape, CONST_DT[k]) for k, v in consts.items()}
    w = {k: din(k, W_SHAPES[k], F32) for k in W_NAMES}
    x = din("x", (S, D), F32)
    out = nc.dram_tensor("out", [S, D], F32, kind="ExternalOutput").ap()
    xa = dsc("xa", (S, D), F32)
    xab = dsc("xab", (S + 128, D), BF16)
    x2 = dsc("x2", (S, D), F32)
    ys = dsc("ys", (NE * CAP + 128, D), BF16)
    uT_d = dsc("uT_d", (1024, S), BF16)
    vte_d = dsc("vte_d", (S, 1024), BF16)
    kt_d = dsc("kt_d", (S, 1024), BF16)
    vto_d = dsc("vto_d", (S, 2048), BF16)
    gt_d = dsc("gt_d", (S, 2048), BF16)
    o_d = dsc("o_d", (S, 2048), BF16)
    with nc.sbuf_tensor(uq("zt"), [128, D], BF16) as zt:
        rz = Res("zt")
        kb.op("dve", lambda e: e.memset(zt[:], 0.0), writes=[rz])
        kb.dma("sp", lambda e: e.dma_start(out=ys[YZ:YZ + 128, :], in_=zt[:]), reads=[rz], key=rz)
        kb.dma("sp", lambda e: e.dma_start(out=xab[S:S + 128, :], in_=zt[:]), reads=[rz], key=rz)
        kb.barrier()
    even_phase(nc, kb, 0, C, x, uT_d, vte_d, xa, xab, w)
    moe_phase(nc, kb, 0, C, xa, xab, ys, x2, None, w)
    odd_phase(nc, kb, 1, C, x2, kt_d, vto_d, gt_d, o_d, xa, xab, w)
    moe_phase(nc, kb, 1, C, xa, xab, ys, out, None, w)
    return nc, consts


def kernel(**inputs):
    n = 8
    nc, consts = build_program()
    x = np.ascontiguousarray(np.asarray(inputs["x"], dtype=np.float32))
    shared = {("c_" + k): v for k, v in consts.items()}
    for k in W_NAMES:
        shared[k] = np.ascontiguousarray(np.asarray(inputs[k], dtype=np.float32))
    in_maps = []
    for b in range(n):
        m = dict(shared)
        m["x"] = x[b]
        in_maps.append(m)
    res = run_bass_kernel_spmd(nc, in_maps, core_ids=list(range(n)))
    return np.stack([np.asarray(r["out"], dtype=np.float32) for r in res.results], axis=0)
```

```python
import numpy as np
import ml_dtypes
import concourse.bass as bass
import concourse.mybir as mybir
from concourse.bass_utils import run_bass_kernel_spmd

F32 = mybir.dt.float32
BF16 = mybir.dt.bfloat16
I32 = mybir.dt.int32
AF = mybir.ActivationFunctionType
ALU = mybir.AluOpType
AX = mybir.AxisListType

S = 2048
D = 2048
NT = S // 128
KC = D // 128
DEPTH = 2
ALPHA = (2 * DEPTH) ** 0.25
LN_EPS = 1e-5
NE = 16
FE = 1408
NF = FE // 128
CAP = 512
NJ = CAP // 128
ZROW = S
YZ = NE * CAP


class Res:
    __slots__ = ("name", "w", "rs", "dsem")

    def __init__(self, name):
        self.name = name
        self.w = None
        self.rs = []
        self.dsem = None


class KB:
    ENGS = ("pe", "act", "dve", "pool", "sp")

    def __init__(self, nc, n_dma_sems=48, same_engine_sync=True):
        self.nc = nc
        self.eng = {"pe": nc.tensor, "act": nc.scalar, "dve": nc.vector,
                    "pool": nc.gpsimd, "sp": nc.sync}
        self.sem = {}
        self.cnt = {}
        self.waited = {}
        self.same_engine_sync = same_engine_sync
        for e in self.ENGS:
            self._mksem("p_" + e)
        self._mksem("bar")
        self.dma_pool = []
        for i in range(n_dma_sems):
            self._mksem("d%d" % i)
            self.dma_pool.append("d%d" % i)
        self.dma_next = 0
        self.dma_res = []

    def _mksem(self, name):
        self.sem[name] = self.nc.alloc_semaphore(name)
        self.cnt[name] = 0

    def _wait(self, e, dep):
        if dep is None:
            return
        s, c = dep
        if s == "p_" + e and (e in ("pe", "sp") or not self.same_engine_sync):
            return
        if self.waited.get((e, s), 0) >= c:
            return
        self.eng[e].wait_ge(self.sem[s], c)
        self.waited[(e, s)] = c

    def _pre(self, e, reads, writes):
        for r in reads:
            self._wait(e, r.w)
        for w in writes:
            self._wait(e, w.w)
            for d in w.rs:
                self._wait(e, d)

    def _post(self, dep, reads, writes):
        for r in reads:
            r.rs.append(dep)
            if len(r.rs) > 8:
                m = {}
                for s, c in r.rs:
                    m[s] = max(m.get(s, 0), c)
                r.rs = list(m.items())
        for w in writes:
            w.w = dep
            w.rs = []

    def op(self, e, fn, reads=(), writes=()):
        self._pre(e, reads, writes)
        ins = fn(self.eng[e])
        s = "p_" + e
        self.cnt[s] += 1
        ins.then_inc(self.sem[s], 1)
        self._post((s, self.cnt[s]), reads, writes)
        return ins

    def dma(self, q, fn, reads=(), writes=(), key=None):
        self._pre(q, reads, writes)
        key = key or (writes[0] if writes else reads[0])
        if key.dsem is None:
            assert self.dma_next < len(self.dma_pool), "out of dma sems"
            key.dsem = self.dma_pool[self.dma_next]
            self.dma_next += 1
            self.dma_res.append(key)
        ins = fn(self.eng[q])
        s = key.dsem
        self.cnt[s] += 16
        ins.then_inc(self.sem[s], 16)
        self._post((s, self.cnt[s]), reads, writes)
        return ins

    def barrier(self):
        sp = self.eng["sp"]
        for s, c in self.cnt.items():
            if s in ("bar", "p_sp") or c == 0:
                continue
            if self.waited.get(("sp", s), 0) >= c:
                continue
            sp.wait_ge(self.sem[s], c)
            self.waited[("sp", s)] = c
        self.cnt["bar"] += 1
        sp.nop().then_inc(self.sem["bar"], 1)
        for e in self.ENGS:
            if e != "sp":
                self.eng[e].wait_ge(self.sem["bar"], self.cnt["bar"])
            for s, c in self.cnt.items():
                self.waited[(e, s)] = c
        for r in self.dma_res:
            r.dsem = None
        self.dma_res = []
        self.dma_next = 0


class Ring:
    def __init__(self, views, name):
        self.v = views
        self.r = [Res("%s%d" % (name, i)) for i in range(len(views))]
        self.i = -1

    def next(self):
        self.i = (self.i + 1) % len(self.v)
        return self.v[self.i], self.r[self.i]


_UNIQ = [0]


def uq(name):
    _UNIQ[0] += 1
    return "%s_u%d" % (name, _UNIQ[0])


def ln_tile(kb, z, zr, gam, bet, rg, st, rst, out, rout, eng2="dve"):
    stats, mv, rstd = st
    for c in range(4):
        kb.op("dve", lambda e: e.bn_stats(out=stats[:, c * 6:(c + 1) * 6], in_=z[:, c * 512:(c + 1) * 512]),
              reads=[zr], writes=[rst])
    kb.op("dve", lambda e: e.bn_aggr(out=mv[:, 0:2], in_=stats[:, 0:24]), reads=[rst], writes=[rst])
    kb.op("dve", lambda e: e.tensor_scalar_add(out=rstd[:, 0:1], in0=mv[:, 1:2], scalar1=LN_EPS), reads=[rst], writes=[rst])
    kb.op("act", lambda e: e.sqrt(out=rstd[:, 0:1], in_=rstd[:, 0:1]), reads=[rst], writes=[rst])
    kb.op("dve", lambda e: e.reciprocal(out=rstd[:, 0:1], in_=rstd[:, 0:1]), reads=[rst], writes=[rst])
    kb.op("dve", lambda e: e.tensor_scalar(out=z[:, :], in0=z[:, :], scalar1=mv[:, 0:1], scalar2=rstd[:, 0:1],
                                           op0=ALU.subtract, op1=ALU.mult), reads=[zr, rst], writes=[zr])
    kb.op(eng2, lambda e: e.tensor_tensor(out=z[:, :], in0=z[:, :], in1=gam[:, :], op=ALU.mult), reads=[zr] + rg, writes=[zr])
    kb.op(eng2, lambda e: e.tensor_tensor(out=out[:, :], in0=z[:, :], in1=bet[:, :], op=ALU.add), reads=[zr] + rg, writes=[rout])


class LNPipe:
    def __init__(self, nc, kb, es, gam, bet, rgs, emit, eps=LN_EPS):
        self.kb = kb
        self.gam, self.bet, self.rgs, self.emit, self.eps = gam, bet, rgs, emit, eps
        st = es.enter_context(nc.sbuf_tensor(uq("lnst"), [128, 2, 32], F32))
        self.st_ring = Ring([st[:, i] for i in range(2)], "lnst")
        zo = es.enter_context(nc.sbuf_tensor(uq("lnzo"), [128, 2, D], F32))
        self.zo_ring = Ring([zo[:, i] for i in range(2)], "lnzo")
        self.pending = None

    def _apply(self):
        kb = self.kb
        z, r_z, tag = self.pending
        o, r_o = self.zo_ring.next()
        kb.op("dve", lambda e: e.tensor_tensor(out=z, in0=z, in1=self.gam[:, :], op=ALU.mult), reads=[r_z] + self.rgs, writes=[r_z])
        kb.op("dve", lambda e: e.tensor_tensor(out=o, in0=z, in1=self.bet[:, :], op=ALU.add), reads=[r_z] + self.rgs, writes=[r_o])
        self.pending = None
        self.emit(o, r_o, tag)

    def feed(self, z, r_z, tag):
        kb = self.kb
        st, r_st = self.st_ring.next()
        for c in range(4):
            kb.op("dve", lambda e: e.bn_stats(out=st[:, c * 6:(c + 1) * 6], in_=z[:, c * 512:(c + 1) * 512]), reads=[r_z], writes=[r_st])
        kb.op("dve", lambda e: e.bn_aggr(out=st[:, 24:26], in_=st[:, 0:24]), reads=[r_st], writes=[r_st])
        kb.op("dve", lambda e: e.tensor_scalar_add(out=st[:, 26:27], in0=st[:, 25:26], scalar1=self.eps), reads=[r_st], writes=[r_st])
        kb.op("act", lambda e: e.sqrt(out=st[:, 26:27], in_=st[:, 26:27]), reads=[r_st], writes=[r_st])
        if self.pending is not None:
            self._apply()
        kb.op("dve", lambda e: e.reciprocal(out=st[:, 27:28], in_=st[:, 26:27]), reads=[r_st], writes=[r_st])
        kb.op("dve", lambda e: e.scalar_tensor_tensor(out=st[:, 28:29], in0=st[:, 24:25], scalar=-1.0, in1=st[:, 27:28], op0=ALU.mult, op1=ALU.mult),
              reads=[r_st], writes=[r_st])
        kb.op("act", lambda e: e.activation(out=z, in_=z, func=AF.Identity, bias=st[:, 28:29], scale=st[:, 27:28]), reads=[r_z, r_st], writes=[r_z])
        self.pending = (z, r_z, tag)

    def flush(self):
        if self.pending is not None:
            self._apply()


def bcast_rows(ap1d, n):
    return ap1d.partition_broadcast(128)


def moe_phase(nc, kb, L, C, xa, xab, ys, xo, xob, w):
    from contextlib import ExitStack
    ident, iota, tokinfo = C["ident"], C["iota"], C["tokinfo"]
    wg_d = w["expert_w_gate"]
    wu_d = w["expert_w_up"]
    wd_d = w["expert_w_down"]

    with ExitStack() as es:
        def sb(name, shape, dt):
            return es.enter_context(nc.sbuf_tensor(uq(name), shape, dt))

        def ps(name, shape, dt):
            return es.enter_context(nc.psum_tensor(uq(name), shape, dt))

        gate_all = sb("gate_all", [128, NT, NE], F32)
        posm_all = sb("posm_all", [128, NT, NE], F32)
        ridx = sb("ridx", [128, NT, 2], I32)
        gsel = sb("gsel", [128, NT, 2], F32)
        tokidx = sb("tokidx", [128, NE * NJ], I32)
        rw_bf = sb("rw_bf", [128, KC, NE], BF16)
        rbias = sb("rbias", [128, NE], F32)
        ecap = sb("ecap", [128, NE], F32)
        ident_s = sb("ident_s", [128, 128], BF16)
        iota_s = sb("iota_s", [128, CAP], F32)
        tokinfo_s = sb("tokinfo_s", [128, NT, 4], BF16)
        lstrict = sb("lstrict", [128, 128], BF16)
        ones_bf = sb("ones_bf", [128, 128], BF16)
        r_const = Res("const")
        r_gate = Res("gate_all")
        r_posm = Res("posm_all")
        r_ridx = Res("ridx")
        r_tok = Res("tokidx")

        kb.dma("pool", lambda e: e.dma_start(out=rw_bf[:], in_=w["router_w"].rearrange("(kc p) e -> p kc e", p=128)), writes=[r_const])
        c2 = Res("c2"); c3 = Res("c3"); c4 = Res("c4"); c5 = Res("c5"); c6 = Res("c6"); c7 = Res("c7")
        kb.dma("sp", lambda e: e.dma_start(out=rbias[:], in_=w["router_bias"].partition_broadcast(128)), writes=[c2])
        kb.dma("sp", lambda e: e.dma_start(out=ident_s[:], in_=ident), writes=[c3])
        kb.dma("sp", lambda e: e.dma_start(out=iota_s[:], in_=iota[:, 0:CAP]), writes=[c4])
        kb.dma("sp", lambda e: e.dma_start(out=tokinfo_s[:], in_=tokinfo), writes=[c5])
        kb.dma("sp", lambda e: e.dma_start(out=lstrict[:], in_=C["lstrict"]), writes=[c6])
        kb.dma("sp", lambda e: e.dma_start(out=ecap[:], in_=C["ecap"]), writes=[c7])
        kb.op("dve", lambda e: e.memset(ones_bf[:], 1.0), writes=[c6])
        consts = [r_const, c2, c3, c4, c5, c6, c7]

        with ExitStack() as es2:
            def sb2(name, shape, dt):
                return es2.enter_context(nc.sbuf_tensor(uq(name), shape, dt))

            def ps2(name, shape, dt):
                return es2.enter_context(nc.psum_tensor(uq(name), shape, dt))

            xt_b = sb2("xt_b", [128, 2, D], BF16)
            xt_ring = Ring([xt_b[:, i] for i in range(2)], "xt_b")
            xT = sb2("xT", [128, 2, KC, 128], BF16)
            xT_ring = Ring([xT[:, i] for i in range(2)], "xT")
            pT = ps2("pT", [128, 2, 1024], BF16)
            pT_ring = Ring([pT[:, i] for i in range(2)], "pT")
            psm = ps2("psm", [128, 2, 2, 512], F32)
            rt = sb2("rt", [128, 16, NE], F32)
            r_rt = Res("rt")
            m4 = sb2("m4", [128, 8, 4], F32)
            mcum = sb2("mcum", [128, NE], BF16)
            m_bf = sb2("m_bf", [128, 2, NE], BF16)
            m_ring = Ring([m_bf[:, i] for i in range(2)], "m_bf")
            r_mcum = Res("mcum")
            oh = sb2("oh", [128, 4, CAP], BF16)
            oh_ring = Ring([oh[:, i] for i in range(4)], "oh")
            kb.op("dve", lambda e: e.memset(mcum[:], 0.0), writes=[r_mcum])

            class Rec:
                def __init__(self):
                    self.ops = []

                def op(self, e, fn, reads=(), writes=()):
                    self.ops.append((e, fn, list(reads), list(writes)))

            def xpose_tile(i):
                xt, r_xt = xt_ring.next()
                kb.dma("sp", lambda e: e.dma_start(out=xt, in_=xab[i * 128:(i + 1) * 128, :]), writes=[r_xt])
                xTt, r_xT = xT_ring.next()
                for g4 in range(4):
                    p, r_p = pT_ring.next()
                    for q in range(4):
                        kc = g4 * 4 + q
                        kb.op("pe", lambda e: e.transpose(out=p[:, q * 128:(q + 1) * 128], in_=xt[:, kc * 128:(kc + 1) * 128], identity=ident_s[:]),
                              reads=[r_xt, c3], writes=[r_p])
                    if g4 % 2 == 0:
                        kb.op("act", lambda e: e.copy(out=xTt[:, g4 * 4:(g4 + 1) * 4, :], in_=p[:, 0:512].rearrange("p (a b) -> p a b", a=4)),
                              reads=[r_p], writes=[r_xT])
                    else:
                        kb.op("dve", lambda e: e.tensor_copy(out=xTt[:, g4 * 4:(g4 + 1) * 4, :], in_=p[:, 0:512].rearrange("p (a b) -> p a b", a=4)),
                              reads=[r_p], writes=[r_xT])
                return xTt, r_xT

            rt2 = sb2("rt2", [128, 2, 16, NE], F32)
            m42 = sb2("m42", [128, 2, 8, 4], F32)
            r_rt2 = [Res("rt_a"), Res("rt_b")]
            r_psr = [Res("psr_a"), Res("psr_b")]
            r_psp = [Res("psp_a"), Res("psp_b")]

            def route_tile(i, par, xTt, r_xT, rk):
                rt_ = rt2[:, par]
                m4_ = m42[:, par]
                pr_ = psm[:, par, 0, 0:NE]
                pp_ = psm[:, par, 1, 0:NE]
                r_psm_r, r_psm_p = r_psr[par], r_psp[par]
                for kc in range(KC):
                    rk.op("pe", lambda e, kc=kc: e.matmul(pr_, lhsT=xTt[:, kc, :], rhs=rw_bf[:, kc, :], start=(kc == 0), stop=(kc == KC - 1)),
                          reads=[r_xT, r_const], writes=[r_psm_r])
                sc = rt_[:, 0]; sel = rt_[:, 1]; eq1 = rt_[:, 2]; sel2 = rt_[:, 3]; ge2 = rt_[:, 4]; M = rt_[:, 5]; wv = rt_[:, 6]
                pos1 = rt_[:, 7]; vv = rt_[:, 8]; sv = rt_[:, 9]; tmp = rt_[:, 10]; sv2 = rt_[:, 11]
                m1 = m4_[:, 0]; m2 = m4_[:, 1]; gs = m4_[:, 2]; gm = m4_[:, 3]
                gmax = m4_[:, 4, 0:1]; wsum = m4_[:, 4, 1:2]; ihi = m4_[:, 5, 0:1]; ilo = m4_[:, 5, 1:2]; t1 = m4_[:, 5, 2:3]
                R = [r_rt2[par]]
                v3 = lambda a: a.rearrange("p (g j) -> p g j", g=4)
                b3 = lambda a: a.unsqueeze(2).to_broadcast([128, 4, 4])
                rk.op("act", lambda e: e.activation(out=sc, in_=pr_, func=AF.Sigmoid), reads=[r_psm_r], writes=R)
                rk.op("dve", lambda e: e.tensor_tensor(out=sel, in0=sc, in1=rbias[:], op=ALU.add), reads=R + [c2], writes=R)
                rk.op("dve", lambda e: e.tensor_reduce(out=m1, in_=v3(sel), axis=AX.X, op=ALU.max), reads=R, writes=R)
                rk.op("dve", lambda e: e.tensor_tensor(out=v3(eq1), in0=v3(sel), in1=b3(m1), op=ALU.is_equal), reads=R, writes=R)
                rk.op("dve", lambda e: e.scalar_tensor_tensor(out=sel2, in0=eq1, scalar=-1e9, in1=sel, op0=ALU.mult, op1=ALU.add), reads=R, writes=R)
                rk.op("dve", lambda e: e.tensor_reduce(out=m2, in_=v3(sel2), axis=AX.X, op=ALU.max), reads=R, writes=R)
                rk.op("dve", lambda e: e.tensor_tensor(out=gs, in0=m1, in1=m2, op=ALU.add), reads=R, writes=R)
                rk.op("dve", lambda e: e.tensor_reduce(out=gmax, in_=gs, axis=AX.X, op=ALU.max), reads=R, writes=R)
                rk.op("dve", lambda e: e.tensor_scalar(out=gm, in0=gs, scalar1=gmax, scalar2=None, op0=ALU.is_equal), reads=R, writes=R)
                rk.op("dve", lambda e: e.tensor_tensor(out=v3(ge2), in0=v3(sel), in1=b3(m2), op=ALU.is_ge), reads=R, writes=R)
                rk.op("dve", lambda e: e.tensor_tensor(out=v3(M), in0=v3(ge2), in1=b3(gm), op=ALU.mult), reads=R, writes=R)
                rk.op("dve", lambda e: e.tensor_tensor(out=wv, in0=sc, in1=M, op=ALU.mult), reads=R, writes=R)
                rk.op("dve", lambda e: e.tensor_reduce(out=wsum, in_=wv, axis=AX.X, op=ALU.add), reads=R, writes=R)
                rk.op("dve", lambda e: e.reciprocal(out=wsum, in_=wsum), reads=R, writes=R)
                rk.op("dve", lambda e: e.tensor_scalar(out=gate_all[:, i, :], in0=wv, scalar1=wsum, scalar2=None, op0=ALU.mult), reads=R, writes=[r_gate])
                mb, r_mb = m_ring.next()
                rk.op("dve", lambda e: e.tensor_copy(out=mb, in_=M), reads=R, writes=[r_mb])
                rk.op("pe", lambda e: e.matmul(pp_, lhsT=lstrict[:], rhs=mb, start=True, stop=False), reads=[r_mb, c6], writes=[r_psm_p])
                rk.op("pe", lambda e: e.matmul(pp_, lhsT=ones_bf[:], rhs=mcum[:], start=False, stop=True), reads=[r_mcum, c6], writes=[r_psm_p])
                rk.op("dve", lambda e: e.tensor_scalar(out=vv, in0=pp_, scalar1=float(CAP), scalar2=None, op0=ALU.is_lt), reads=[r_psm_p] + R, writes=R)
                rk.op("dve", lambda e: e.scalar_tensor_tensor(out=pos1, in0=pp_, scalar=1.0, in1=M, op0=ALU.add, op1=ALU.mult), reads=[r_psm_p] + R, writes=R)
                rk.op("dve", lambda e: e.tensor_tensor(out=pos1, in0=pos1, in1=vv, op=ALU.mult), reads=R, writes=R)
                rk.op("dve", lambda e: e.tensor_scalar_add(out=posm_all[:, i, :], in0=pos1, scalar1=-1.0), reads=R, writes=[r_posm])
                rk.op("dve", lambda e: e.tensor_tensor(out=mcum[:], in0=mcum[:], in1=mb, op=ALU.add), reads=[r_mb, r_mcum], writes=[r_mcum])
                rk.op("dve", lambda e: e.tensor_scalar(out=vv, in0=pos1, scalar1=0.0, scalar2=None, op0=ALU.is_gt), reads=R, writes=R)
                rk.op("dve", lambda e: e.tensor_tensor(out=sv, in0=pos1, in1=ecap[:], op=ALU.add), reads=R + [c7], writes=R)
                rk.op("dve", lambda e: e.tensor_tensor(out=sv, in0=sv, in1=vv, op=ALU.mult), reads=R, writes=R)
                rk.op("dve", lambda e: e.tensor_reduce(out=ihi, in_=sv, axis=AX.X, op=ALU.max), reads=R, writes=R)
                rk.op("dve", lambda e: e.tensor_scalar(out=tmp, in0=sv, scalar1=ihi, scalar2=None, op0=ALU.not_equal), reads=R, writes=R)
                rk.op("dve", lambda e: e.tensor_tensor(out=sv2, in0=sv, in1=tmp, op=ALU.mult), reads=R, writes=R)
                rk.op("dve", lambda e: e.tensor_reduce(out=ilo, in_=sv2, axis=AX.X, op=ALU.max), reads=R, writes=R)
                rk.op("dve", lambda e: e.scalar_tensor_tensor(out=tmp, in0=sv, scalar=ihi, in1=gate_all[:, i, :], op0=ALU.is_equal, op1=ALU.mult,
                                                              accum_out=gsel[:, i, 0:1]), reads=R + [r_gate], writes=R + [r_ridx])
                rk.op("dve", lambda e: e.scalar_tensor_tensor(out=tmp, in0=sv, scalar=ilo, in1=gate_all[:, i, :], op0=ALU.is_equal, op1=ALU.mult,
                                                              accum_out=gsel[:, i, 1:2]), reads=R + [r_gate], writes=R + [r_ridx])
                for k, src in ((0, ihi), (1, ilo)):
                    rk.op("dve", lambda e, src=src: e.tensor_scalar(out=t1, in0=src, scalar1=0.0, scalar2=float(YZ + 1), op0=ALU.is_equal, op1=ALU.mult), reads=R, writes=R)
                    rk.op("dve", lambda e, src=src: e.scalar_tensor_tensor(out=t1, in0=src, scalar=-1.0, in1=t1, op0=ALU.add, op1=ALU.add), reads=R, writes=R)
                    rk.op("dve", lambda e, k=k: e.tensor_copy(out=ridx[:, i, k:k + 1], in_=t1), reads=R, writes=[r_ridx])

            for i0_ in range(0, NT, 2):
                recs = []
                for par in range(2):
                    xTt, r_xT = xpose_tile(i0_ + par)
                    rk = Rec()
                    route_tile(i0_ + par, par, xTt, r_xT, rk)
                    recs.append(rk.ops)
                LAG = 8
                order = []
                na, nb = len(recs[0]), len(recs[1])
                for n in range(max(na, nb + LAG)):
                    if n < na:
                        order.append(recs[0][n])
                    if 0 <= n - LAG < nb:
                        order.append(recs[1][n - LAG])
                for e_, fn_, rd_, wr_ in order:
                    kb.op(e_, fn_, reads=rd_, writes=wr_)
            pacs = sb2("pacs", [128, NE * NJ, 4], F32)
            r_tf = Res("tf")
            ptab = ps2("ptab", [128, 2, NJ, NT, 4], F32)
            ptab_ring = Ring([ptab[:, i] for i in range(2)], "ptab")
            for ex in range(NE):
                pt, r_pt = ptab_ring.next()
                for i in range(NT):
                    o, r_o = oh_ring.next()
                    kb.op("dve", lambda e: e.tensor_scalar(out=o, in0=iota_s[:], scalar1=posm_all[:, i, ex:ex + 1], scalar2=None, op0=ALU.is_equal),
                          reads=[r_posm, c4], writes=[r_o])
                    for j in range(NJ):
                        kb.op("pe", lambda e: e.matmul(pt[:, j, i, 0:4], lhsT=o[:, j * 128:(j + 1) * 128], rhs=tokinfo_s[:, i, :],
                                                       start=True, stop=True), reads=[r_o, c5], writes=[r_pt])
                kb.op("dve", lambda e: e.tensor_reduce(out=pacs[:, ex * NJ:(ex + 1) * NJ, :], in_=pt.rearrange("p j i c -> p j c i"), axis=AX.X, op=ALU.add),
                      reads=[r_pt], writes=[r_tf])
            tf = sb2("tf", [128, NE * NJ, 2], F32)
            kb.op("dve", lambda e: e.scalar_tensor_tensor(out=tf[:, :, 0], in0=pacs[:, :, 1], scalar=128.0, in1=pacs[:, :, 0], op0=ALU.mult, op1=ALU.add),
                  reads=[r_tf], writes=[r_tf])
            kb.op("dve", lambda e: e.tensor_scalar(out=tf[:, :, 1], in0=pacs[:, :, 2], scalar1=-float(ZROW), scalar2=float(ZROW), op0=ALU.mult, op1=ALU.add),
                  reads=[r_tf], writes=[r_tf])
            kb.op("dve", lambda e: e.tensor_tensor(out=tf[:, :, 0], in0=tf[:, :, 0], in1=tf[:, :, 1], op=ALU.add), reads=[r_tf], writes=[r_tf])
            kb.op("dve", lambda e: e.tensor_copy(out=tokidx[:, :], in_=tf[:, :, 0]), reads=[r_tf], writes=[r_tok])
        kb.barrier()

        with ExitStack() as es2:
            def sb2(name, shape, dt):
                return es2.enter_context(nc.sbuf_tensor(uq(name), shape, dt))

            def ps2(name, shape, dt):
                return es2.enter_context(nc.psum_tensor(uq(name), shape, dt))

            NGU = 4
            NWD = 4
            wgu = sb2("wgu", [128, NGU, 2, 2, KC, 128], BF16)
            gu_ring = Ring([wgu[:, i] for i in range(NGU)], "wgu")
            wd = sb2("wd", [128, NWD, NF, 512], BF16)
            wd_ring = Ring([wd[:, i] for i in range(NWD)], "wd")
            xg = sb2("xg", [128, 4, D], BF16)
            xg_ring = Ring([xg[:, i] for i in range(4)], "xg")
            xgT = sb2("xgT", [128, 2, KC, CAP], BF16)
            xgT_res = [[Res("xgT%d_%d" % (b, j)) for j in range(NJ)] for b in range(2)]
            hT = sb2("hT", [128, 2, NF, CAP], BF16)
            hT_res = [[Res("hT%d_%d" % (b, f)) for f in range(NF)] for b in range(2)]
            sg = sb2("sg", [128, 2, CAP], F32)
            sg_ring = Ring([sg[:, i] for i in range(2)], "sg")
            yst = sb2("yst", [128, 4, 512], BF16)
            yst_ring = Ring([yst[:, i] for i in range(4)], "yst")
            pT = ps2("pTe", [128, 2, 1024], BF16)
            pT_ring = Ring([pT[:, i] for i in range(2)], "pTe")
            pg = ps2("pg", [128, 2, 512], F32)
            pg_ring = Ring([pg[:, i] for i in range(2)], "pg")
            pu = ps2("pu", [128, 2, 512], F32)
            pu_ring = Ring([pu[:, i] for i in range(2)], "pu")
            py = ps2("py", [128, 2, 512], F32)
            py_ring = Ring([py[:, i] for i in range(2)], "py")
            r_ys = Res("ys_dram")
            nev_box = [0]

            def prep(ex):
                b = ex % 2
                groups = []
                for j in range(NJ):
                    g, r_g = xg_ring.next()
                    col = ex * NJ + j
                    kb.dma("pool", lambda e: e.indirect_dma_start(out=g, out_offset=None, in_=xab[:, :],
                                                                   in_offset=bass.IndirectOffsetOnAxis(ap=tokidx[:, col:col + 1], axis=0)),
                           reads=[r_tok], writes=[r_g])
                    for g4 in range(4):
                        def grp(g=g, r_g=r_g, j=j, g4=g4, b=b):
                            p, r_p = pT_ring.next()
                            for q in range(4):
                                kc = g4 * 4 + q
                                kb.op("pe", lambda e: e.transpose(out=p[:, q * 128:(q + 1) * 128], in_=g[:, kc * 128:(kc + 1) * 128], identity=ident_s[:]),
                                      reads=[r_g, c3], writes=[r_p])
                            dst = xgT[:, b, g4 * 4:(g4 + 1) * 4, j * 128:(j + 1) * 128]
                            src = p[:, 0:512].rearrange("p (a b) -> p a b", a=4)
                            if nev_box[0] % 2 == 0:
                                kb.op("act", lambda e: e.copy(out=dst, in_=src), reads=[r_p], writes=[xgT_res[b][j]])
                            else:
                                kb.op("dve", lambda e: e.tensor_copy(out=dst, in_=src), reads=[r_p], writes=[xgT_res[b][j]])
                            nev_box[0] += 1
                        groups.append(grp)
                return groups

            for g_ in prep(0):
                g_()
            chunks = [(ex, f) for ex in range(NE) for f in range(NF)]
            loaded = {}
            LOOK = 4

            def emit_load(g):
                ex, f = chunks[g]
                if f % 2 == 1:
                    return
                nf = 2 if f + 1 < NF else 1
                wt, r_w = gu_ring.next()
                kb.dma("pool", lambda e: e.dma_start(out=wt[:, 0, 0:nf], in_=wg_d[L, ex][:, f:f + nf]), writes=[r_w])
                kb.dma("pool", lambda e: e.dma_start(out=wt[:, 1, 0:nf], in_=wu_d[L, ex][:, f:f + nf]), writes=[r_w])
                for k in range(nf):
                    loaded[g + k] = (wt[:, :, k], r_w)

            def load_wd(ex, dc):
                wdt, r_wd = wd_ring.next()
                kb.dma("pool", lambda e: e.dma_start(out=wdt, in_=wd_d[L, ex][:, dc]), writes=[r_wd])
                return (wdt, r_wd)

            def a_step(ex, f, wt, r_w):
                b = ex % 2
                pgt, r_pg = pg_ring.next()
                put, r_pu = pu_ring.next()
                for kc in range(KC):
                    kb.op("pe", lambda e: e.matmul(pgt[:, 0:CAP], lhsT=wt[:, 0, kc, :], rhs=xgT[:, b, kc, :], start=(kc == 0), stop=(kc == KC - 1)),
                          reads=[r_w] + xgT_res[b], writes=[r_pg])
                for kc in range(KC):
                    kb.op("pe", lambda e: e.matmul(put[:, 0:CAP], lhsT=wt[:, 1, kc, :], rhs=xgT[:, b, kc, :], start=(kc == 0), stop=(kc == KC - 1)),
                          reads=[r_w] + xgT_res[b], writes=[r_pu])
                sgt, r_sg = sg_ring.next()
                kb.op("act", lambda e: e.activation(out=sgt, in_=pgt[:, 0:CAP], func=AF.Silu), reads=[r_pg], writes=[r_sg])
                kb.op("dve", lambda e: e.tensor_tensor(out=hT[:, b, f, :], in0=sgt, in1=put[:, 0:CAP], op=ALU.mult), reads=[r_sg, r_pu], writes=[hT_res[b][f]])

            def b_group(ex, dc, t, wt, r_w):
                b = ex % 2
                pyt, r_py = py_ring.next()
                for f in range(NF):
                    kb.op("pe", lambda e: e.matmul(pyt[:, 0:512], lhsT=hT[:, b, f, t * 128:(t + 1) * 128], rhs=wt[:, f, :], start=(f == 0), stop=(f == NF - 1)),
                          reads=[r_w] + hT_res[b], writes=[r_py])
                y, r_y = yst_ring.next()
                if nev_box[0] % 2 == 0:
                    kb.op("act", lambda e: e.copy(out=y, in_=pyt[:, 0:512]), reads=[r_py], writes=[r_y])
                else:
                    kb.op("dve", lambda e: e.tensor_copy(out=y, in_=pyt[:, 0:512]), reads=[r_py], writes=[r_y])
                nev_box[0] += 1
                row0 = ex * CAP + t * 128
                kb.dma("sp", lambda e: e.dma_start(out=ys[row0:row0 + 128, dc * 512:(dc + 1) * 512], in_=y), reads=[r_y], key=r_y)

            for g in range(LOOK):
                emit_load(g)
            next_load = LOOK
            wd_cur = [load_wd(0, dc) for dc in range(4)]
            sched = [2, 1, 2, 1, 2, 1, 2, 1, 2, 1, 1]
            schedp = [0, 0, 0, 0, 2, 2, 2, 2, 2, 3, 3]
            for it in range(NE + 1):
                bgroups = [(dc, t) for dc in range(4) for t in range(NJ)] if it >= 1 else []
                pgroups = prep(it + 1) if it + 1 < NE else []
                wd_next = [None] * 4

                def side_work(n, npre):
                    for _ in range(npre):
                        if pgroups:
                            pgroups.pop(0)()
                    for _ in range(n):
                        if bgroups:
                            dc, t = bgroups.pop(0)
                            b_group(it - 1, dc, t, *wd_cur[dc])
                            if t == NJ - 1 and it < NE:
                                wd_next[dc] = load_wd(it, dc)
                if it < NE:
                    for f in range(NF):
                        if next_load < len(chunks):
                            emit_load(next_load)
                            next_load += 1
                        a_step(it, f, *loaded.pop(it * NF + f))
                        side_work(sched[f], schedp[f])
                side_work(16, 16)
                if it >= 1:
                    wd_cur = wd_next
        kb.barrier()

        with ExitStack() as es2:
            def sb2(name, shape, dt):
                return es2.enter_context(nc.sbuf_tensor(uq(name), shape, dt))

            gam = sb2("gam", [128, D], F32)
            bet = sb2("bet", [128, D], F32)
            r_gb = Res("gb")
            kb.dma("sp", lambda e: e.dma_start(out=gam[:], in_=w["ln_ffn_g"][L].partition_broadcast(128)), writes=[r_gb])
            r_gb2 = Res("gb2")
            kb.dma("sp", lambda e: e.dma_start(out=bet[:], in_=w["ln_ffn_b"][L].partition_broadcast(128)), writes=[r_gb2])
            xin = sb2("xin", [128, 3, D], F32)
            xin_ring = Ring([xin[:, i] for i in range(3)], "xin")
            rr = sb2("rr", [128, 4, D], BF16)
            rr_ring = Ring([rr[:, i] for i in range(4)], "rr")
            zb = sb2("zb", [128, 2, D], BF16)
            zb_ring = Ring([zb[:, i] for i in range(2)], "zb")
            gs2 = sb2("gs2", [128, NT, 2], F32)
            r_gs2 = Res("gs2")
            kb.op("dve", lambda e: e.tensor_scalar(out=gs2[:], in0=gsel[:], scalar1=1.0 / ALPHA, scalar2=None, op0=ALU.mult), reads=[r_ridx], writes=[r_gs2])

            def emit(o, r_o, i):
                kb.dma("sp", lambda e: e.dma_start(out=xo[i * 128:(i + 1) * 128, :], in_=o), reads=[r_o], key=r_o)
                if xob is not None:
                    zbt, r_zb = zb_ring.next()
                    kb.op("act", lambda e: e.copy(out=zbt, in_=o), reads=[r_o], writes=[r_zb])
                    kb.dma("sp", lambda e: e.dma_start(out=xob[i * 128:(i + 1) * 128, :], in_=zbt), reads=[r_zb], key=r_zb)
            lnp = LNPipe(nc, kb, es2, gam, bet, [r_gb, r_gb2], emit, eps=LN_EPS / (ALPHA * ALPHA))
            for i in range(NT):
                x, r_x = xin_ring.next()
                kb.dma("sp", lambda e: e.dma_start(out=x, in_=xa[i * 128:(i + 1) * 128, :]), writes=[r_x])
                rh, r_rh = rr_ring.next()
                kb.dma("pool", lambda e: e.indirect_dma_start(out=rh, out_offset=None, in_=ys[:, :],
                                                               in_offset=bass.IndirectOffsetOnAxis(ap=ridx[:, i, 0:1], axis=0)), reads=[r_ridx], writes=[r_rh])
                rl, r_rl = rr_ring.next()
                kb.dma("pool", lambda e: e.indirect_dma_start(out=rl, out_offset=None, in_=ys[:, :],
                                                               in_offset=bass.IndirectOffsetOnAxis(ap=ridx[:, i, 1:2], axis=0)), reads=[r_ridx], writes=[r_rl])
                kb.op("dve", lambda e: e.scalar_tensor_tensor(out=x, in0=rh, scalar=gs2[:, i, 0:1], in1=x, op0=ALU.mult, op1=ALU.add),
                      reads=[r_rh, r_x, r_gs2], writes=[r_x])
                kb.op("dve", lambda e: e.scalar_tensor_tensor(out=x, in0=rl, scalar=gs2[:, i, 1:2], in1=x, op0=ALU.mult, op1=ALU.add),
                      reads=[r_rl, r_x, r_gs2], writes=[r_x])
                lnp.feed(x, r_x, i)
            lnp.flush()
        kb.barrier()


def build_xT(nc, kb, es, src, xT, r_xT, ident_s, r_id, pT_ring):
    xt_b = es.enter_context(nc.sbuf_tensor(uq("xtb"), [128, 2, D], BF16))
    ring = Ring([xt_b[:, i] for i in range(2)], "xtb")
    n = 0
    for i in range(NT):
        xt, r_xt = ring.next()
        kb.dma("pool", lambda e: e.dma_start(out=xt, in_=src[i * 128:(i + 1) * 128, :]), writes=[r_xt])
        for g4 in range(4):
            p, r_p = pT_ring.next()
            for q in range(4):
                kc = g4 * 4 + q
                kb.op("pe", lambda e: e.transpose(out=p[:, q * 128:(q + 1) * 128], in_=xt[:, kc * 128:(kc + 1) * 128], identity=ident_s[:]),
                      reads=[r_xt, r_id], writes=[r_p])
            dst = xT[:, g4 * 4:(g4 + 1) * 4, i * 128:(i + 1) * 128]
            srcp = p[:, 0:512].rearrange("p (a b) -> p a b", a=4)
            if n % 2 == 0:
                kb.op("act", lambda e: e.copy(out=dst, in_=srcp), reads=[r_p], writes=[r_xT[i]])
            else:
                kb.op("dve", lambda e: e.tensor_copy(out=dst, in_=srcp), reads=[r_p], writes=[r_xT[i]])
            n += 1


def mix_out_phase(nc, kb, es, L, mixT, r_mix, wout_d, x_src, xa, xab, w, C):
    def sb(name, shape, dt):
        return es.enter_context(nc.sbuf_tensor(uq(name), shape, dt))
    wo = sb("wo", [128, KC, D], BF16)
    r_wo = [Res("wo%d" % i) for i in range(4)]
    for q in range(4):
        kb.dma("pool", lambda e: e.dma_start(out=wo[:, q * 4:(q + 1) * 4, :], in_=wout_d.rearrange("(kc p) d -> p kc d", p=128)[:, q * 4:(q + 1) * 4, :]),
               writes=[r_wo[q]])
    gam = sb("gam", [128, D], F32)
    bet = sb("bet", [128, D], F32)
    r_g1 = Res("g1"); r_g2 = Res("g2")
    kb.dma("sp", lambda e: e.dma_start(out=gam[:], in_=w["ln_mix_g"][L].partition_broadcast(128)), writes=[r_g1])
    kb.dma("sp", lambda e: e.dma_start(out=bet[:], in_=w["ln_mix_b"][L].partition_broadcast(128)), writes=[r_g2])
    xin = sb("xin", [128, 3, D], F32)
    xin_ring = Ring([xin[:, i] for i in range(3)], "xin")
    zb = sb("zb", [128, 2, D], BF16)
    zb_ring = Ring([zb[:, i] for i in range(2)], "zb")

    def emit(o, r_o, i):
        kb.dma("sp", lambda e: e.dma_start(out=xa[i * 128:(i + 1) * 128, :], in_=o), reads=[r_o], key=r_o)
        zbt, r_zb = zb_ring.next()
        kb.op("act", lambda e: e.copy(out=zbt, in_=o), reads=[r_o], writes=[r_zb])
        kb.dma("sp", lambda e: e.dma_start(out=xab[i * 128:(i + 1) * 128, :], in_=zbt), reads=[r_zb], key=r_zb)
    lnp = LNPipe(nc, kb, es, gam, bet, [r_g1, r_g2], emit)
    pm = es.enter_context(nc.psum_tensor(uq("pm"), [128, 6, 512], F32))
    pm_ring = Ring([pm[:, i] for i in range(6)], "pm")
    for i in range(NT):
        x, r_x = xin_ring.next()
        kb.dma("sp", lambda e: e.dma_start(out=x, in_=x_src[i * 128:(i + 1) * 128, :]), writes=[r_x])
        for dc in range(4):
            p, r_p = pm_ring.next()
            for kc in range(KC):
                kb.op("pe", lambda e: e.matmul(p[:, 0:512], lhsT=mixT[kc][:, i * 128:(i + 1) * 128], rhs=wo[:, kc, dc * 512:(dc + 1) * 512],
                                               start=(kc == 0), stop=(kc == KC - 1)), reads=[r_mix[kc] if len(r_mix) == KC else r_mix[i], r_wo[kc // 4]], writes=[r_p])
            kb.op("dve", lambda e: e.scalar_tensor_tensor(out=x[:, dc * 512:(dc + 1) * 512], in0=x[:, dc * 512:(dc + 1) * 512], scalar=ALPHA, in1=p[:, 0:512],
                                                          op0=ALU.mult, op1=ALU.add), reads=[r_x, r_p], writes=[r_x])
        lnp.feed(x, r_x, i)
    lnp.flush()


FOXH = 8
CQ, CK, CV, CF, CA, CG = 0, 1024, 2048, 3072, 3080, 4104


def even_phase(nc, kb, L, C, x_src, uT_d, vt_d, xa, xab, w, dbg=None):
    from contextlib import ExitStack
    win = w["even_w_in"][0]
    winv = win.rearrange("(kc p) n -> p kc n", p=128)
    with ExitStack() as es0:
        def sb0(name, shape, dt):
            return es0.enter_context(nc.sbuf_tensor(uq(name), shape, dt))
        ident_s = sb0("ident", [128, 128], BF16)
        r_id = Res("ident")
        kb.dma("sp", lambda e: e.dma_start(out=ident_s[:], in_=C["ident"]), writes=[r_id])
        attT = sb0("attT", [128, FOXH, S], BF16)
        r_att = [Res("att%d" % h) for h in range(FOXH)]
        with ExitStack() as es2:
            def sb2(name, shape, dt):
                return es2.enter_context(nc.sbuf_tensor(uq(name), shape, dt))

            def ps2(name, shape, dt):
                return es2.enter_context(nc.psum_tensor(uq(name), shape, dt))
            xT = sb2("xT", [128, KC, S], BF16)
            r_xT = [Res("xT%d" % i) for i in range(NT)]
            pT = ps2("pT", [128, 2, 1024], BF16)
            pT_ring = Ring([pT[:, i] for i in range(2)], "pT")
            build_xT(nc, kb, es2, x_src, xT, r_xT, ident_s, r_id, pT_ring)
            wch = sb2("wch", [128, 3, KC, 128], BF16)
            wch_ring = Ring([wch[:, i] for i in range(3)], "wch")
            pp = ps2("pp", [128, 6, 512], F32)
            pp_ring = Ring([pp[:, i] for i in range(6)], "pp")

            def proj_fm(col0, tg, wt, r_w):
                p, r_p = pp_ring.next()
                for kc in range(KC):
                    kb.op("pe", lambda e: e.matmul(p[:, 0:512], lhsT=wt[:, kc, :], rhs=xT[:, kc, tg * 512:(tg + 1) * 512],
                                                   start=(kc == 0), stop=(kc == KC - 1)), reads=[r_w] + r_xT[tg * 4:(tg + 1) * 4], writes=[r_p])
                return p, r_p

            def load_w(col0):
                wt, r_w = wch_ring.next()
                kb.dma("pool", lambda e: e.dma_start(out=wt, in_=winv[:, :, col0:col0 + 128]), writes=[r_w])
                return wt, r_w
            with ExitStack() as es3:
                def sb3(name, shape, dt):
                    return es3.enter_context(nc.sbuf_tensor(uq(name), shape, dt))
                identf = sb3("identf", [128, 128], F32)
                onesm = sb3("onesm", [128, 128], F32)
                r_c = Res("cc")
                kb.dma("sp", lambda e: e.dma_start(out=identf[:], in_=C["identf"]), writes=[r_c])
                kb.op("dve", lambda e: e.memset(onesm[:], 1.0 / 128.0), writes=[r_c])
                cw31 = sb3("cw31", [31, 1024], F32)
                r_cw = Res("cw31")
                kb.dma("sp", lambda e: e.dma_start(out=cw31[:], in_=w["even_conv_w"][0].rearrange("j o c -> j (o c)")), writes=[r_cw])
                cwT = sb3("cwT", [128, 8, 32], F32)
                r_cwT = Res("cwT")
                prm = sb3("prm", [128, 3, 8], F32)
                r_prm = Res("prm")
                with nc.allow_non_contiguous_dma(reason="tiny per-channel params"):
                    for k, nm in enumerate(("even_conv_b", "even_conv_norm_g", "even_conv_norm_b")):
                        kb.dma("sp", lambda e: e.dma_start(out=prm[:, k, :], in_=w[nm][0].rearrange("(c p) -> p c", p=128)), writes=[r_prm])
                for c in range(8):
                    p, r_p = pp_ring.next()
                    kb.op("pe", lambda e: e.transpose(out=p[:, 0:31], in_=cw31[0:31, c * 128:(c + 1) * 128], identity=identf[0:31, 0:31]),
                          reads=[r_cw, r_c], writes=[r_p])
                    kb.op("dve", lambda e: e.tensor_copy(out=cwT[:, c, 0:31], in_=p[:, 0:31]), reads=[r_p], writes=[r_cwT])
                cin = sb3("cin", [128, 2, 32 + S], BF16)
                cin_ring = Ring([cin[:, i] for i in range(2)], "cin")
                kb.op("dve", lambda e: e.memset(cin[:, :, 0:32], 0.0), writes=cin_ring.r)
                dg = sb3("dg", [128, 2, 31, 128], BF16)
                dg_ring = Ring([dg[:, i] for i in range(2)], "dg")
                sgs = sb3("sgs", [128, 2, 512], F32)
                sg_ring = Ring([sgs[:, i] for i in range(2)], "sgs")
                tb = sb3("tb", [128, 2, 4, 512], F32)
                tb_res = [[Res("tb%d_%d" % (a, b)) for b in range(4)] for a in range(2)]
                sq = sb3("sq", [128, 4, 512], F32)
                sq_res = [Res("sq%d" % b) for b in range(4)]
                uo = sb3("uo", [128, 2, 512], BF16)
                uo_ring = Ring([uo[:, i] for i in range(2)], "uo")

                def proj_stage(c):
                    wa, r_wa = load_w(CA + c * 128)
                    wg, r_wg = load_w(CG + c * 128)
                    ci, r_ci = cin_ring.next()
                    for tg in range(4):
                        pa, r_pa = proj_fm(CA, tg, wa, r_wa)
                        pg, r_pg = proj_fm(CG, tg, wg, r_wg)
                        sg, r_sg = sg_ring.next()
                        kb.op("act", lambda e: e.activation(out=sg, in_=pg[:, 0:512], func=AF.Sigmoid), reads=[r_pg], writes=[r_sg])
                        kb.op("dve", lambda e: e.tensor_tensor(out=ci[:, 32 + tg * 512:32 + (tg + 1) * 512], in0=sg, in1=pa[:, 0:512], op=ALU.mult),
                              reads=[r_sg, r_pa], writes=[r_ci])
                    dgt, r_dg = dg_ring.next()
                    for j in range(31):
                        kb.op("dve", lambda e: e.tensor_scalar(out=dgt[:, j, :], in0=identf[:], scalar1=cwT[:, c, j:j + 1], scalar2=None, op0=ALU.mult),
                              reads=[r_c, r_cwT], writes=[r_dg])
                    return ci, r_ci, dgt, r_dg

                def conv_stage(c, ci, r_ci, dgt, r_dg):
                    for tg in range(4):
                        pc, r_pc = pp_ring.next()
                        for j in range(31):
                            o0 = 2 + j + tg * 512
                            kb.op("pe", lambda e: e.matmul(pc[:, 0:512], lhsT=dgt[:, j, :], rhs=ci[:, o0:o0 + 512], start=(j == 0), stop=(j == 30)),
                                  reads=[r_dg, r_ci], writes=[r_pc])
                        kb.op("act", lambda e: e.activation(out=tb[:, c % 2, tg], in_=pc[:, 0:512], func=AF.Identity, bias=prm[:, 0, c:c + 1]),
                              reads=[r_pc, r_prm], writes=[tb_res[c % 2][tg]])

                def mean_stage(c):
                    for tg in range(4):
                        ut = tb[:, c % 2, tg]; r_t = tb_res[c % 2][tg]
                        pmn, r_pmn = pp_ring.next()
                        kb.op("pe", lambda e: e.matmul(pmn[:, 0:512], lhsT=onesm[:], rhs=ut, start=True, stop=True), reads=[r_t, r_c], writes=[r_pmn])
                        kb.op("dve", lambda e: e.tensor_tensor(out=ut, in0=ut, in1=pmn[:, 0:512], op=ALU.subtract), reads=[r_t, r_pmn], writes=[r_t])
                        kb.op("act", lambda e: e.activation(out=sq[:, tg], in_=ut, func=AF.Square), reads=[r_t], writes=[sq_res[tg]])

                def var_stage(c):
                    for tg in range(4):
                        dd = tb[:, c % 2, tg]; r_t = tb_res[c % 2][tg]
                        rs = sq[:, tg]; r_s = sq_res[tg]
                        pvr, r_pvr = pp_ring.next()
                        kb.op("pe", lambda e: e.matmul(pvr[:, 0:512], lhsT=onesm[:], rhs=rs, start=True, stop=True), reads=[r_s, r_c], writes=[r_pvr])
                        kb.op("dve", lambda e: e.tensor_scalar_add(out=rs, in0=pvr[:, 0:512], scalar1=LN_EPS), reads=[r_pvr], writes=[r_s])
                        kb.op("act", lambda e: e.sqrt(out=rs, in_=rs), reads=[r_s], writes=[r_s])
                        kb.op("dve", lambda e: e.reciprocal(out=rs, in_=rs), reads=[r_s], writes=[r_s])
                        kb.op("dve", lambda e: e.tensor_tensor(out=dd, in0=dd, in1=rs, op=ALU.mult), reads=[r_t, r_s], writes=[r_t])
                        kb.op("dve", lambda e: e.tensor_scalar(out=dd, in0=dd, scalar1=prm[:, 1, c:c + 1], scalar2=prm[:, 2, c:c + 1], op0=ALU.mult, op1=ALU.add),
                              reads=[r_t, r_prm], writes=[r_t])
                        uot, r_uo = uo_ring.next()
                        kb.op("act", lambda e: e.activation(out=uot, in_=dd, func=AF.Silu), reads=[r_t], writes=[r_uo])
                        kb.dma("sp", lambda e: e.dma_start(out=uT_d[c * 128:(c + 1) * 128, tg * 512:(tg + 1) * 512], in_=uot), reads=[r_uo], key=r_uo)

                for c in range(9):
                    if c < 8:
                        st = proj_stage(c)
                    if c >= 1:
                        mean_stage(c - 1)
                    if c < 8:
                        conv_stage(c, *st)
                    if c >= 1:
                        var_stage(c - 1)
        kb.barrier()
        with ExitStack() as es1:
            def sb1(name, shape, dt):
                return es1.enter_context(nc.sbuf_tensor(uq(name), shape, dt))
            qT = sb1("qT", [128, FOXH, S], BF16)
            kT = sb1("kT", [128, FOXH, S], BF16)
            fl = sb1("fl", [128, NT, FOXH], F32)
            r_q = [Res("q%d" % h) for h in range(FOXH)]
            r_k = [Res("k%d" % h) for h in range(FOXH)]
            r_fl = Res("fl")
            with ExitStack() as es2:
                def sb2(name, shape, dt):
                    return es2.enter_context(nc.sbuf_tensor(uq(name), shape, dt))

                def ps2(name, shape, dt):
                    return es2.enter_context(nc.psum_tensor(uq(name), shape, dt))
                xT = sb2("xT", [128, KC, S], BF16)
                r_xT = [Res("xT%d" % i) for i in range(NT)]
                pT = ps2("pT", [128, 2, 1024], BF16)
                pT_ring = Ring([pT[:, i] for i in range(2)], "pT")
                build_xT(nc, kb, es2, x_src, xT, r_xT, ident_s, r_id, pT_ring)
                wch = sb2("wch", [128, 3, KC, 128], BF16)
                wch_ring = Ring([wch[:, i] for i in range(3)], "wch")
                pp = ps2("pp", [128, 4, 512], F32)
                pp_ring = Ring([pp[:, i] for i in range(4)], "pp")
                nev = 0

                def proj_fm(col0, tg, wt, r_w):
                    p, r_p = pp_ring.next()
                    for kc in range(KC):
                        kb.op("pe", lambda e: e.matmul(p[:, 0:512], lhsT=wt[:, kc, :], rhs=xT[:, kc, tg * 512:(tg + 1) * 512],
                                                       start=(kc == 0), stop=(kc == KC - 1)), reads=[r_w] + r_xT[tg * 4:(tg + 1) * 4], writes=[r_p])
                    return p, r_p

                def load_w(col0):
                    wt, r_w = wch_ring.next()
                    kb.dma("pool", lambda e: e.dma_start(out=wt, in_=winv[:, :, col0:col0 + 128]), writes=[r_w])
                    return wt, r_w

                for h in range(FOXH):
                    for (c0, dst, rr, scl) in ((CQ, qT, r_q, 128.0 ** -0.5), (CK, kT, r_k, 1.0)):
                        wt, r_w = load_w(c0 + h * 128)
                        for tg in range(4):
                            p, r_p = proj_fm(c0, tg, wt, r_w)
                            if nev % 2 == 0:
                                kb.op("act", lambda e: e.mul(out=dst[:, h, tg * 512:(tg + 1) * 512], in_=p[:, 0:512], mul=scl), reads=[r_p], writes=[rr[h]])
                            else:
                                kb.op("dve", lambda e: e.tensor_scalar(out=dst[:, h, tg * 512:(tg + 1) * 512], in0=p[:, 0:512], scalar1=scl, scalar2=None, op0=ALU.mult),
                                      reads=[r_p], writes=[rr[h]])
                            nev += 1
                with ExitStack() as es3:
                    wv = es3.enter_context(nc.sbuf_tensor(uq("wv"), [128, KC, 520], BF16))
                    r_wv = Res("wv")
                    wf32 = es3.enter_context(nc.sbuf_tensor(uq("wf32"), [128, KC, 8], F32))
                    wfb = es3.enter_context(nc.sbuf_tensor(uq("wfb"), [128, KC, 128], BF16))
                    r_wf = Res("wf")
                    vst = es3.enter_context(nc.sbuf_tensor(uq("vst"), [128, 4, 512], BF16))
                    vst_ring = Ring([vst[:, i] for i in range(4)], "vst")
                    for half in range(2):
                        ncol = 520 if half == 0 else 512
                        if half == 0:
                            kb.dma("pool", lambda e: e.dma_start(out=wv[:, :, 0:512], in_=winv[:, :, CV:CV + 512]), writes=[r_wv])
                            kb.dma("sp", lambda e: e.dma_start(out=wf32[:], in_=winv[:, :, CF:CF + 8]), writes=[r_wf])
                            if dbg is not None:
                                kb.dma("sp", lambda e: e.dma_start(out=dbg["wf"], in_=wf32[:].rearrange("p a b -> p (a b)")), reads=[r_wf], key=Res("dbgwf"))
                            kb.op("dve", lambda e: e.memset(wfb[:], 0.0), writes=[r_wf])
                            kb.op("dve", lambda e: e.tensor_copy(out=wfb[:, :, 0:8], in_=wf32[:]), reads=[r_wf], writes=[r_wf])
                        else:
                            kb.dma("pool", lambda e: e.dma_start(out=wv[:, :, 0:512], in_=winv[:, :, CV + 512:CV + 1024]), writes=[r_wv])
                        for i in range(NT):
                            p, r_p = pp_ring.next()
                            for kc in range(KC):
                                kb.op("pe", lambda e: e.matmul(p[:, 0:512], lhsT=xT[:, kc, i * 128:(i + 1) * 128], rhs=wv[:, kc, 0:512],
                                                               start=(kc == 0), stop=(kc == KC - 1)), reads=[r_wv, r_xT[i]], writes=[r_p])
                            vs, r_vs = vst_ring.next()
                            if nev % 2 == 0:
                                kb.op("act", lambda e: e.copy(out=vs, in_=p[:, 0:512]), reads=[r_p], writes=[r_vs])
                            else:
                                kb.op("dve", lambda e: e.tensor_copy(out=vs, in_=p[:, 0:512]), reads=[r_p], writes=[r_vs])
                            nev += 1
                            kb.dma("sp", lambda e: e.dma_start(out=vt_d[i * 128:(i + 1) * 128, half * 512:(half + 1) * 512], in_=vs), reads=[r_vs], key=r_vs)
                            if half == 0:
                                p, r_p = pp_ring.next()
                                for kc in range(KC):
                                    kb.op("pe", lambda e: e.matmul(p[:, 0:128], lhsT=xT[:, kc, i * 128:(i + 1) * 128], rhs=wfb[:, kc, :],
                                                                   start=(kc == 0), stop=(kc == KC - 1)), reads=[r_wf, r_xT[i]], writes=[r_p])
                                kb.op("act", lambda e: e.copy(out=fl[:, i, :], in_=p[:, 0:8]), reads=[r_p], writes=[r_fl])
                if dbg is not None:
                    kb.dma("sp", lambda e: e.dma_start(out=dbg["fl2"], in_=fl[:].rearrange("p a b -> p (a b)")), reads=[r_fl], key=Res("dbgfl2"))
                kb.barrier()
            kb.barrier()
            with ExitStack() as es2:
                def sb2(name, shape, dt):
                    return es2.enter_context(nc.sbuf_tensor(uq(name), shape, dt))

                def ps2(name, shape, dt):
                    return es2.enter_context(nc.psum_tensor(uq(name), shape, dt))
                vt = sb2("vt", [128, NT, 1024], BF16)
                r_v = [Res("v%d" % i) for i in range(NT)]
                for i in range(NT):
                    kb.dma("sp", lambda e: e.dma_start(out=vt[:, i, :], in_=vt_d[i * 128:(i + 1) * 128, :]), writes=[r_v[i]])
                uinc = sb2("uinc", [128, 128], F32)
                m64 = sb2("m64", [128, 128], F32)
                onesf = sb2("onesf", [128, 128], F32)
                ones_b = sb2("ones_b", [128, 128], BF16)
                cmask = sb2("cmask", [128, 128], BF16)
                bfb = sb2("bfb", [128, FOXH], F32)
                r_c = Res("attc")
                kb.dma("sp", lambda e: e.dma_start(out=uinc[:], in_=C["uinc"]), writes=[r_c])
                r_c2 = Res("attc2")
                kb.dma("sp", lambda e: e.dma_start(out=m64[:], in_=C["m64"]), writes=[r_c2])
                r_c3 = Res("attc3")
                kb.dma("sp", lambda e: e.dma_start(out=cmask[:], in_=C["cmask"]), writes=[r_c3])
                r_c4 = Res("attc4")
                kb.dma("sp", lambda e: e.dma_start(out=bfb[:], in_=w["even_b_f"][0].partition_broadcast(128)), writes=[r_c4])
                kb.op("dve", lambda e: e.memset(onesf[:], 1.0), writes=[r_c])
                kb.op("dve", lambda e: e.memset(ones_b[:], 1.0), writes=[r_c])
                lf = sb2("lf", [128, NT, FOXH], F32)
                r_lf = Res("lf")
                kb.op("dve", lambda e: e.tensor_tensor(out=lf[:], in0=fl[:], in1=bfb[:].unsqueeze(1).to_broadcast([128, NT, FOXH]), op=ALU.add),
                      reads=[r_fl, r_c4], writes=[r_lf])
                kb.op("act", lambda e: e.activation(out=lf[:], in_=lf[:], func=AF.Exp, scale=-1.0), reads=[r_lf], writes=[r_lf])
                kb.op("act", lambda e: e.activation(out=lf[:], in_=lf[:], func=AF.Ln, bias=1.0), reads=[r_lf], writes=[r_lf])
                kb.op("dve", lambda e: e.tensor_scalar(out=lf[:], in0=lf[:], scalar1=-1.0, scalar2=None, op0=ALU.mult), reads=[r_lf], writes=[r_lf])
                c_all = sb2("c_all", [128, NT, FOXH], F32)
                cref = sb2("cref", [128, NT, FOXH], F32)
                lfcum = sb2("lfcum", [128, FOXH], F32)
                r_call = Res("c_all"); r_cref = Res("cref"); r_lfc = Res("lfcum")
                kb.op("dve", lambda e: e.memset(lfcum[:], 0.0), writes=[r_lfc])
                pcs = ps2("pcs", [128, 2, 512], F32)
                pcs_ring = Ring([pcs[:, i] for i in range(2)], "pcs")
                for i in range(NT):
                    p, r_p = pcs_ring.next()
                    kb.op("pe", lambda e: e.matmul(p[:, 0:FOXH], lhsT=uinc[:], rhs=lf[:, i, :], start=True, stop=False), reads=[r_lf, r_c], writes=[r_p])
                    kb.op("pe", lambda e: e.matmul(p[:, 0:FOXH], lhsT=onesf[:], rhs=lfcum[:], start=False, stop=True), reads=[r_lfc, r_c], writes=[r_p])
                    kb.op("dve", lambda e: e.tensor_copy(out=c_all[:, i, :], in_=p[:, 0:FOXH]), reads=[r_p], writes=[r_call])
                    p2, r_p2 = pcs_ring.next()
                    kb.op("pe", lambda e: e.matmul(p2[:, 0:FOXH], lhsT=m64[:], rhs=lf[:, i, :], start=True, stop=False), reads=[r_lf, r_c2], writes=[r_p2])
                    kb.op("pe", lambda e: e.matmul(p2[:, 0:FOXH], lhsT=onesf[:], rhs=lfcum[:], start=False, stop=True), reads=[r_lfc, r_c], writes=[r_p2])
                    kb.op("dve", lambda e: e.tensor_copy(out=cref[:, i, :], in_=p2[:, 0:FOXH]), reads=[r_p2], writes=[r_cref])
                    kb.op("dve", lambda e: e.tensor_tensor(out=lfcum[:], in0=lfcum[:], in1=lf[:, i, :], op=ALU.add), reads=[r_lf, r_lfc], writes=[r_lfc])
                if dbg is not None:
                    rd2 = Res("dbg2")
                    kb.dma("sp", lambda e: e.dma_start(out=dbg["c_all"], in_=c_all[:].rearrange("p a b -> p (a b)")), reads=[r_call], key=rd2)
                    kb.dma("sp", lambda e: e.dma_start(out=dbg["cref"], in_=cref[:].rearrange("p a b -> p (a b)")), reads=[r_cref], key=rd2)
                    kb.dma("sp", lambda e: e.dma_start(out=dbg["lf"], in_=lf[:].rearrange("p a b -> p (a b)")), reads=[r_lf], key=rd2)
                    kb.dma("sp", lambda e: e.dma_start(out=dbg["fl"], in_=fl[:].rearrange("p a b -> p (a b)")), reads=[r_fl], key=rd2)
                    kb.dma("sp", lambda e: e.dma_start(out=dbg["bfb"], in_=bfb[:]), reads=[r_c4], key=rd2)
                bias_all = sb2("bias_all", [128, FOXH, NT, NT], F32)
                r_bias = Res("bias")
                for h in range(FOXH):
                    for qb in range(NT):
                        kb.op("dve", lambda e: e.tensor_scalar(out=bias_all[:, h, qb, :], in0=c_all[:, :, h], scalar1=-1.0, scalar2=cref[:, qb, h:h + 1],
                                                               op0=ALU.mult, op1=ALU.add), reads=[r_call, r_cref], writes=[r_bias])
                pst = ps2("pst", [128, 2, 512], F32)
                pst_ring = Ring([pst[:, i] for i in range(2)], "pst")
                po = ps2("po", [128, 2, 512], F32)
                po_ring = Ring([po[:, i] for i in range(2)], "po")
                pr = ps2("pr", [128, 2, 512], F32)
                pr_ring = Ring([pr[:, i] for i in range(2)], "pr")
                PT = sb2("PT", [128, 8, 128], BF16)
                PT_ring = Ring([PT[:, i] for i in range(8)], "PT")
                rcp = sb2("rcp", [128, 2, 128], F32)
                rcp_ring = Ring([rcp[:, i] for i in range(2)], "rcp")
                groups = []
                for h in range(FOXH):
                    for qb in range(NT):
                        nkb = qb + 1
                        for k0 in range(0, nkb, 4):
                            groups.append((h, qb, list(range(k0, min(k0 + 4, nkb)))))
                acc = {}

                def do_scores(gi):
                    h, qb, kbs = groups[gi]
                    st, r_st = pst_ring.next()
                    for n, kbk in enumerate(kbs):
                        kb.op("pe", lambda e: e.matmul(st[:, n * 128:(n + 1) * 128], lhsT=kT[:, h, kbk * 128:(kbk + 1) * 128], rhs=qT[:, h, qb * 128:(qb + 1) * 128],
                                                       start=True, stop=True), reads=[r_k[h], r_q[h]], writes=[r_st])
                    pts = []
                    for n, kbk in enumerate(kbs):
                        pt, r_pt = PT_ring.next()
                        kb.op("act", lambda e: e.activation(out=pt, in_=st[:, n * 128:(n + 1) * 128], func=AF.Exp, bias=bias_all[:, h, qb, kbk:kbk + 1]),
                              reads=[r_st, r_bias], writes=[r_pt])
                        if kbk == qb:
                            kb.op("dve", lambda e: e.tensor_tensor(out=pt, in0=pt, in1=cmask[:], op=ALU.mult), reads=[r_pt, r_c3], writes=[r_pt])
                        pts.append((kbk, pt, r_pt))
                    return pts

                def do_pv(gi, pts):
                    h, qb, kbs = groups[gi]
                    if kbs[0] == 0:
                        acc[(h, qb)] = (po_ring.next(), pr_ring.next())
                    (pot, r_po), (prt, r_pr) = acc[(h, qb)]
                    for kbk, pt, r_pt in pts:
                        kb.op("pe", lambda e: e.matmul(pot[:, 0:128], lhsT=vt[:, kbk, h * 128:(h + 1) * 128], rhs=pt, start=(kbk == 0), stop=(kbk == qb)),
                              reads=[r_v[kbk], r_pt], writes=[r_po])
                        kb.op("pe", lambda e: e.matmul(prt[:, 0:128], lhsT=ones_b[:], rhs=pt, start=(kbk == 0), stop=(kbk == qb)),
                              reads=[r_c, r_pt], writes=[r_pr])
                    if kbs[-1] == qb:
                        rc, r_rc = rcp_ring.next()
                        kb.op("dve", lambda e: e.reciprocal(out=rc, in_=prt[:, 0:128]), reads=[r_pr], writes=[r_rc])
                        kb.op("dve", lambda e: e.tensor_tensor(out=attT[:, h, qb * 128:(qb + 1) * 128], in0=rc, in1=pot[:, 0:128], op=ALU.mult),
                              reads=[r_rc, r_po], writes=[r_att[h]])
                        del acc[(h, qb)]

                prev = do_scores(0)
                for gi in range(len(groups)):
                    nxt_pts = do_scores(gi + 1) if gi + 1 < len(groups) else None
                    do_pv(gi, prev)
                    prev = nxt_pts
        kb.barrier()
        if dbg is not None:
            rd = Res("dbg")
            for h in range(FOXH):
                kb.dma("sp", lambda e: e.dma_start(out=dbg["att"][h * 128:(h + 1) * 128, :], in_=attT[:, h, :]), reads=[r_att[h]], key=rd)
        with ExitStack() as es2:
            uTs = es2.enter_context(nc.sbuf_tensor(uq("uTs"), [128, 8, S], BF16))
            r_u = [Res("uT%d" % c) for c in range(8)]
            for c in range(8):
                kb.dma("sp", lambda e: e.dma_start(out=uTs[:, c, :], in_=uT_d[c * 128:(c + 1) * 128, :]), writes=[r_u[c]])
            chunks = [attT[:, h, :] for h in range(FOXH)] + [uTs[:, c, :] for c in range(8)]
            mix_out_phase(nc, kb, es2, L, chunks, r_att + r_u, w["even_w_out"][0], x_src, xa, xab, w, C)
    kb.barrier()


def make_consts():
    bf = ml_dtypes.bfloat16
    c = {}
    c["ident"] = np.eye(128, dtype=np.float32).astype(bf)
    c["iota"] = np.tile(np.arange(512, dtype=np.float32)[None, :], (128, 1))
    ti = np.zeros((128, NT, 4), np.float32)
    ti[:, :, 0] = np.arange(128)[:, None]
    ti[:, :, 1] = np.arange(NT)[None, :]
    ti[:, :, 2] = 1.0
    c["tokinfo"] = ti.astype(bf)
    c["lstrict"] = np.triu(np.ones((128, 128), np.float32), 1).astype(bf)
    c["ecap"] = np.tile((np.arange(NE, dtype=np.float32) * CAP)[None, :], (128, 1))
    c["identf"] = np.eye(128, dtype=np.float32)
    c["uinc"] = np.triu(np.ones((128, 128), np.float32), 0)
    m64 = np.zeros((128, 128), np.float32); m64[:65, :] = 1.0
    c["m64"] = m64
    c["cmask"] = np.triu(np.ones((128, 128), np.float32), 0).astype(bf)
    c["lgt16"] = np.tril(np.ones((128, 128), np.float32), -1) * (-1.0 / 16.0)
    c["uinc16"] = np.triu(np.ones((128, 128), np.float32), 0) * (-1.0 / 16.0)
    return c


CONST_DT = {"ident": BF16, "iota": F32, "tokinfo": BF16, "lstrict": BF16, "ecap": F32, "identf": F32, "uinc": F32, "m64": F32, "cmask": BF16, "lgt16": F32, "uinc16": F32}


GH = 4
OQ, OK_, OV, OG, OA = 0, 1024, 2048, 4096, 6144


def odd_phase(nc, kb, L, C, x_src, kt_d, vt_d, gt_d, o_d, xa, xab, w):
    from contextlib import ExitStack
    win = w["odd_w_in"][0]
    winv = win.rearrange("(kc p) n -> p kc n", p=128)
    with ExitStack() as es0:
        def sb0(name, shape, dt):
            return es0.enter_context(nc.sbuf_tensor(uq(name), shape, dt))
        ident_s = sb0("ident", [128, 128], BF16)
        r_id = Res("ident")
        kb.dma("sp", lambda e: e.dma_start(out=ident_s[:], in_=C["ident"]), writes=[r_id])
        with ExitStack() as es1:
            def sb1(name, shape, dt):
                return es1.enter_context(nc.sbuf_tensor(uq(name), shape, dt))
            qT = sb1("qT", [128, 8, S], BF16)
            kT = sb1("kT", [128, 8, S], BF16)
            alT = sb1("alT", [16, S], BF16)
            r_q = [Res("q%d" % c) for c in range(8)]
            r_k = [Res("k%d" % c) for c in range(8)]
            r_al = Res("alT")
            with ExitStack() as es2:
                def sb2(name, shape, dt):
                    return es2.enter_context(nc.sbuf_tensor(uq(name), shape, dt))

                def ps2(name, shape, dt):
                    return es2.enter_context(nc.psum_tensor(uq(name), shape, dt))
                xT = sb2("xT", [128, KC, S], BF16)
                r_xT = [Res("xT%d" % i) for i in range(NT)]
                pT = ps2("pT", [128, 2, 1024], BF16)
                pT_ring = Ring([pT[:, i] for i in range(2)], "pT")
                build_xT(nc, kb, es2, x_src, xT, r_xT, ident_s, r_id, pT_ring)
                wch = sb2("wch", [128, 3, KC, 128], BF16)
                wch_ring = Ring([wch[:, i] for i in range(3)], "wch")
                pp = ps2("pp", [128, 4, 512], F32)
                pp_ring = Ring([pp[:, i] for i in range(4)], "pp")
                nev = 0
                for (c0, dst, rr) in ((OQ, qT, r_q), (OK_, kT, r_k)):
                    for c in range(8):
                        wt, r_w = wch_ring.next()
                        kb.dma("pool", lambda e: e.dma_start(out=wt, in_=winv[:, :, c0 + c * 128:c0 + (c + 1) * 128]), writes=[r_w])
                        for tg in range(4):
                            p, r_p = pp_ring.next()
                            for kc in range(KC):
                                kb.op("pe", lambda e: e.matmul(p[:, 0:512], lhsT=wt[:, kc, :], rhs=xT[:, kc, tg * 512:(tg + 1) * 512],
                                                               start=(kc == 0), stop=(kc == KC - 1)), reads=[r_w] + r_xT[tg * 4:(tg + 1) * 4], writes=[r_p])
                            if nev % 2 == 0:
                                kb.op("act", lambda e: e.copy(out=dst[:, c, tg * 512:(tg + 1) * 512], in_=p[:, 0:512]), reads=[r_p], writes=[rr[c]])
                            else:
                                kb.op("dve", lambda e: e.tensor_copy(out=dst[:, c, tg * 512:(tg + 1) * 512], in_=p[:, 0:512]), reads=[r_p], writes=[rr[c]])
                            nev += 1
                wa32 = sb2("wa32", [128, KC, 16], F32)
                wab = sb2("wab", [128, KC, 16], BF16)
                r_wa = Res("wa")
                kb.dma("sp", lambda e: e.dma_start(out=wa32[:], in_=winv[:, :, OA:OA + 16]), writes=[r_wa])
                kb.op("dve", lambda e: e.tensor_copy(out=wab[:], in_=wa32[:]), reads=[r_wa], writes=[r_wa])
                for tg in range(4):
                    p, r_p = pp_ring.next()
                    for kc in range(KC):
                        kb.op("pe", lambda e: e.matmul(p[0:16, 0:512], lhsT=wab[:, kc, :], rhs=xT[:, kc, tg * 512:(tg + 1) * 512],
                                                       start=(kc == 0), stop=(kc == KC - 1)), reads=[r_wa] + r_xT[tg * 4:(tg + 1) * 4], writes=[r_p])
                    kb.op("dve", lambda e: e.tensor_copy(out=alT[0:16, tg * 512:(tg + 1) * 512], in_=p[0:16, 0:512]), reads=[r_p], writes=[r_al])
                wtm = sb2("wtm", [128, 2, KC, 512], BF16)
                wtm_ring = Ring([wtm[:, i] for i in range(2)], "wtm")
                stg = sb2("stg", [128, 4, 512], BF16)
                stg_ring = Ring([stg[:, i] for i in range(4)], "stg")
                for cg in range(10):
                    col0 = OK_ + cg * 512
                    if cg < 2:
                        dd, dcol = kt_d, cg * 512
                    elif cg < 6:
                        dd, dcol = vt_d, (cg - 2) * 512
                    else:
                        dd, dcol = gt_d, (cg - 6) * 512
                    wt, r_w = wtm_ring.next()
                    kb.dma("pool", lambda e: e.dma_start(out=wt, in_=winv[:, :, col0:col0 + 512]), writes=[r_w])
                    for i in range(NT):
                        p, r_p = pp_ring.next()
                        for kc in range(KC):
                            kb.op("pe", lambda e: e.matmul(p[:, 0:512], lhsT=xT[:, kc, i * 128:(i + 1) * 128], rhs=wt[:, kc, :],
                                                           start=(kc == 0), stop=(kc == KC - 1)), reads=[r_w, r_xT[i]], writes=[r_p])
                        st, r_st = stg_ring.next()
                        if nev % 2 == 0:
                            kb.op("act", lambda e: e.copy(out=st, in_=p[:, 0:512]), reads=[r_p], writes=[r_st])
                        else:
                            kb.op("dve", lambda e: e.tensor_copy(out=st, in_=p[:, 0:512]), reads=[r_p], writes=[r_st])
                        nev += 1
                        kb.dma("sp", lambda e: e.dma_start(out=dd[i * 128:(i + 1) * 128, dcol:dcol + 512], in_=st), reads=[r_st], key=r_st)
            kb.barrier()
            with ExitStack() as es2:
                def sb2(name, shape, dt):
                    return es2.enter_context(nc.sbuf_tensor(uq(name), shape, dt))

                def ps2(name, shape, dt):
                    return es2.enter_context(nc.psum_tensor(uq(name), shape, dt))
                lgt = sb2("lgt", [128, 128], F32)
                uin = sb2("uin", [128, 128], F32)
                cmask = sb2("cmask", [128, 128], F32)
                wa2 = sb2("wa2", [16, 1024], F32)
                wa2b = sb2("wa2b", [16, 1024], BF16)
                ba = sb2("ba", [128, 1024], F32)
                ng = sb2("ng", [128, 2048], F32)
                rc = [Res("oc%d" % i) for i in range(6)]
                kb.dma("sp", lambda e: e.dma_start(out=lgt[:], in_=C["lgt16"]), writes=[rc[0]])
                kb.dma("sp", lambda e: e.dma_start(out=uin[:], in_=C["uinc16"]), writes=[rc[1]])
                kb.dma("sp", lambda e: e.dma_start(out=cmask[:], in_=C["uinc"]), writes=[rc[2]])
                kb.dma("sp", lambda e: e.dma_start(out=wa2[:], in_=w["odd_w_a2"][0]), writes=[rc[3]])
                kb.op("dve", lambda e: e.tensor_copy(out=wa2b[:], in_=wa2[:]), reads=[rc[3]], writes=[rc[3]])
                kb.dma("sp", lambda e: e.dma_start(out=ba[:], in_=w["odd_b_a"][0].partition_broadcast(128)), writes=[rc[4]])
                kb.dma("sp", lambda e: e.dma_start(out=ng[:], in_=w["odd_norm_g"][0].partition_broadcast(128)), writes=[rc[5]])
                state = sb2("state", [128, 8, 512], F32)
                stateb = sb2("stateb", [128, 8, 512], BF16)
                r_state = [Res("st%d" % c) for c in range(8)]
                r_stateb = [Res("stb%d" % c) for c in range(8)]
                kb.op("dve", lambda e: e.memset(state[:], 0.0), writes=r_state)
                kb.op("dve", lambda e: e.memset(stateb[:], 0.0), writes=r_stateb)
                lnv = sb2("lnv", [128, 2, 1024], F32)
                lnv_ring = Ring([lnv[:, i] for i in range(2)], "lnv")
                ktk = sb2("ktk", [128, 2, 1024], BF16)
                ktk_ring = Ring([ktk[:, i] for i in range(2)], "ktk")
                vtk = sb2("vtk", [128, 2, 2048], BF16)
                vtk_ring = Ring([vtk[:, i] for i in range(2)], "vtk")
                gtk = sb2("gtk", [128, 2, 2048], BF16)
                gtk_ring = Ring([gtk[:, i] for i in range(2)], "gtk")
                gg = sb2("gg", [128, 2, 2048], BF16)
                gg_ring = Ring([gg[:, i] for i in range(2)], "gg")
                ebm = sb2("ebm", [128, 1, 1024], F32)
                ebm_ring = Ring([ebm[:, i] for i in range(1)], "ebm")
                kend = sb2("kend", [128, 2, 1024], BF16)
                kend_ring = Ring([kend[:, i] for i in range(2)], "kend")
                eb = sb2("eb", [128, 2, 8, 128], F32)
                eb_ring = Ring([eb[:, i] for i in range(2)], "eb")
                enb = sb2("enb", [128, 2, 8, 128], F32)
                enb_ring = Ring([enb[:, i] for i in range(2)], "enb")
                qt = sb2("qt", [128, 2, 8, 128], BF16)
                qt_ring = Ring([qt[:, i] for i in range(2)], "qt")
                ktt = sb2("ktt", [128, 2, 8, 128], BF16)
                ktt_ring = Ring([ktt[:, i] for i in range(2)], "ktt")
                attn = sb2("attn", [128, 2, 128], BF16)
                attn_ring = Ring([attn[:, i] for i in range(2)], "attn")
                osb = sb2("osb", [128, 2, 2048], BF16)
                osb_ring = Ring([osb[:, i] for i in range(2)], "osb")
                sm = sb2("sm", [128, 8], F32)
                r_sm = Res("sm")
                junk = sb2("junk", [128, 512], F32)
                r_junk = Res("junk")
                pA = ps2("pA", [128, 2, 512], F32)
                pA_ring = Ring([pA[:, i] for i in range(2)], "pA")
                pB = ps2("pB", [128, 2, 512], F32)
                pB_ring = Ring([pB[:, i] for i in range(2)], "pB")
                pS = ps2("pS", [128, 1, 512], F32)
                pS_ring = Ring([pS[:, i] for i in range(1)], "pS")
                pO = ps2("pO", [128, 1, 512], F32)
                pO_ring = Ring([pO[:, i] for i in range(1)], "pO")
                pU = ps2("pU", [128, 2, 512], F32)
                pU_ring = Ring([pU[:, i] for i in range(2)], "pU")
                QS = 256.0 ** -0.5
                def make_pre(i):
                    ts = slice(i * 128, (i + 1) * 128)
                    P = {}

                    def q0():
                        P["kt"], P["r_kt"] = ktk_ring.next()
                        kb.dma("sp", lambda e: e.dma_start(out=P["kt"], in_=kt_d[ts, :]), writes=[P["r_kt"]])
                        P["vt"], P["r_vt"] = vtk_ring.next()
                        kb.dma("sp", lambda e: e.dma_start(out=P["vt"], in_=vt_d[ts, :]), writes=[P["r_vt"]])
                        gt_, r_gt = gtk_ring.next()
                        kb.dma("sp", lambda e: e.dma_start(out=gt_, in_=gt_d[ts, :]), writes=[r_gt])
                        lv, r_lv = lnv_ring.next()
                        P["lv"], P["r_lv"] = lv, r_lv
                        for hf in range(2):
                            p, r_p = pA_ring.next()
                            kb.op("pe", lambda e: e.matmul(p[:, 0:512], lhsT=alT[0:16, ts], rhs=wa2b[0:16, hf * 512:(hf + 1) * 512], start=True, stop=True),
                                  reads=[r_al, rc[3]], writes=[r_p])
                            kb.op("dve", lambda e: e.tensor_tensor(out=lv[:, hf * 512:(hf + 1) * 512], in0=p[:, 0:512], in1=ba[:, hf * 512:(hf + 1) * 512], op=ALU.add),
                                  reads=[r_p, rc[4]], writes=[r_lv])
                        kb.op("act", lambda e: e.activation(out=lv, in_=lv, func=AF.Exp, scale=-1.0), reads=[r_lv], writes=[r_lv])
                        kb.op("act", lambda e: e.activation(out=lv, in_=lv, func=AF.Ln, bias=1.0), reads=[r_lv], writes=[r_lv])
                        ggt, r_gg = gg_ring.next()
                        P["gg"], P["r_gg"] = ggt, r_gg
                        kb.op("act", lambda e: e.activation(out=ggt, in_=gt_, func=AF.Silu), reads=[r_gt], writes=[r_gg])
                        kb.op("dve", lambda e: e.tensor_tensor(out=ggt, in0=ggt, in1=ng[:], op=ALU.mult), reads=[r_gg, rc[5]], writes=[r_gg])

                    def q1():
                        lv, r_lv = P["lv"], P["r_lv"]
                        em, r_em = ebm_ring.next()
                        ke, r_ke = kend_ring.next()
                        P["ke"], P["r_ke"] = ke, r_ke
                        for hf in range(2):
                            p, r_p = pA_ring.next()
                            kb.op("pe", lambda e: e.matmul(p[:, 0:512], lhsT=lgt[:], rhs=lv[:, hf * 512:(hf + 1) * 512], start=True, stop=True),
                                  reads=[r_lv, rc[0]], writes=[r_p])
                            kb.op("act", lambda e: e.activation(out=em[:, hf * 512:(hf + 1) * 512], in_=p[:, 0:512], func=AF.Exp), reads=[r_p], writes=[r_em])
                        kb.op("dve", lambda e: e.tensor_tensor(out=ke, in0=em, in1=P["kt"], op=ALU.mult), reads=[r_em, P["r_kt"]], writes=[r_ke])

                    def q2():
                        lv, r_lv = P["lv"], P["r_lv"]
                        P["eb"], P["r_eb"] = eb_ring.next()
                        P["en"], P["r_en"] = enb_ring.next()
                        for hf in range(2):
                            p, r_p = pB_ring.next()
                            for q in range(4):
                                c = hf * 4 + q
                                kb.op("pe", lambda e: e.matmul(p[:, q * 128:(q + 1) * 128], lhsT=lv[:, c * 128:(c + 1) * 128], rhs=uin[:], start=True, stop=True),
                                      reads=[r_lv, rc[1]], writes=[r_p])
                            pv = p[:, 0:512].rearrange("p (a b) -> p a b", a=4)
                            kb.op("act", lambda e: e.activation(out=P["eb"][:, hf * 4:(hf + 1) * 4, :], in_=pv, func=AF.Exp), reads=[r_p], writes=[P["r_eb"]])
                            kb.op("act", lambda e: e.activation(out=P["en"][:, hf * 4:(hf + 1) * 4, :], in_=pv, func=AF.Exp, scale=-1.0), reads=[r_p], writes=[P["r_en"]])

                    def q3():
                        P["qt"], P["r_qt"] = qt_ring.next()
                        P["kt2"], P["r_kt2"] = ktt_ring.next()
                        kb.op("dve", lambda e: e.scalar_tensor_tensor(out=P["qt"], in0=qT[:, :, ts], scalar=QS, in1=P["eb"], op0=ALU.mult, op1=ALU.mult),
                              reads=r_q + [P["r_eb"]], writes=[P["r_qt"]])
                        kb.op("dve", lambda e: e.tensor_tensor(out=P["kt2"], in0=kT[:, :, ts], in1=P["en"], op=ALU.mult), reads=r_k + [P["r_en"]], writes=[P["r_kt2"]])
                    return P, [q0, q1, q2, q3]

                def head(i, h, P, ot, r_ot):
                    qtt, r_qt, kt2, r_kt2 = P["qt"], P["r_qt"], P["kt2"], P["r_kt2"]
                    ke, r_ke, vt_, r_vt = P["ke"], P["r_ke"], P["vt"], P["r_vt"]
                    ebt, r_eb, ggt, r_gg = P["eb"], P["r_eb"], P["gg"], P["r_gg"]
                    pSt, r_pS = pS_ring.next()
                    for cc in range(2):
                        c = 2 * h + cc
                        kb.op("pe", lambda e: e.matmul(pSt[:, 0:128], lhsT=kt2[:, c, :], rhs=qtt[:, c, :], start=(cc == 0), stop=(cc == 1)),
                              reads=[r_kt2, r_qt], writes=[r_pS])
                    at, r_at = attn_ring.next()
                    kb.op("dve", lambda e: e.tensor_tensor(out=at, in0=pSt[:, 0:128], in1=cmask[:], op=ALU.mult), reads=[r_pS, rc[2]], writes=[r_at])
                    vh = vt_[:, h * 512:(h + 1) * 512]
                    pus = []
                    for cc in range(2):
                        c = 2 * h + cc
                        pu, r_pu = pU_ring.next()
                        kb.op("pe", lambda e: e.matmul(pu[:, 0:512], lhsT=ke[:, c * 128:(c + 1) * 128], rhs=vh, start=True, stop=True),
                              reads=[r_ke, r_vt], writes=[r_pu])
                        pus.append((c, pu, r_pu))
                    pOt, r_pO = pO_ring.next()
                    kb.op("pe", lambda e: e.matmul(pOt[:, 0:512], lhsT=at, rhs=vh, start=True, stop=False), reads=[r_at, r_vt], writes=[r_pO])
                    for cc in range(2):
                        c = 2 * h + cc
                        kb.op("pe", lambda e: e.matmul(pOt[:, 0:512], lhsT=qtt[:, c, :], rhs=stateb[:, c, :], start=False, stop=(cc == 1)),
                              reads=[r_qt, r_stateb[c]], writes=[r_pO])
                    for c, pu, r_pu in pus:
                        kb.op("dve", lambda e: e.scalar_tensor_tensor(out=state[:, c, :], in0=state[:, c, :], scalar=ebt[:, c, 127:128], in1=pu[:, 0:512],
                                                                      op0=ALU.mult, op1=ALU.add), reads=[r_state[c], r_eb, r_pu], writes=[r_state[c]])
                        kb.op("pool", lambda e: e.tensor_copy(out=stateb[:, c, :], in_=state[:, c, :]), reads=[r_state[c]], writes=[r_stateb[c]])
                    kb.op("act", lambda e: e.activation(out=junk[:], in_=pOt[:, 0:512], func=AF.Square, accum_out=sm[:, h:h + 1]), reads=[r_pO], writes=[r_junk, r_sm])
                    kb.op("dve", lambda e: e.tensor_scalar(out=sm[:, 4 + h:5 + h], in0=sm[:, h:h + 1], scalar1=1.0 / 512.0, scalar2=LN_EPS, op0=ALU.mult, op1=ALU.add),
                          reads=[r_sm], writes=[r_sm])
                    kb.op("act", lambda e: e.sqrt(out=sm[:, 4 + h:5 + h], in_=sm[:, 4 + h:5 + h]), reads=[r_sm], writes=[r_sm])
                    kb.op("dve", lambda e: e.reciprocal(out=sm[:, 4 + h:5 + h], in_=sm[:, 4 + h:5 + h]), reads=[r_sm], writes=[r_sm])
                    kb.op("dve", lambda e: e.scalar_tensor_tensor(out=ot[:, h * 512:(h + 1) * 512], in0=pOt[:, 0:512], scalar=sm[:, 4 + h:5 + h],
                                                                  in1=ggt[:, h * 512:(h + 1) * 512], op0=ALU.mult, op1=ALU.mult),
                          reads=[r_pO, r_sm, r_gg], writes=[r_ot])

                Pcur, qs = make_pre(0)
                for q in qs:
                    q()
                for i in range(NT):
                    ts = slice(i * 128, (i + 1) * 128)
                    if i + 1 < NT:
                        Pn, qn = make_pre(i + 1)
                    else:
                        Pn, qn = None, []
                    ot, r_ot = osb_ring.next()
                    for h in range(GH):
                        head(i, h, Pcur, ot, r_ot)
                        if qn:
                            qn.pop(0)()
                    kb.dma("sp", lambda e: e.dma_start(out=o_d[ts, :], in_=ot), reads=[r_ot], key=r_ot)
                    Pcur = Pn
        kb.barrier()
        with ExitStack() as es2:
            mixT = es2.enter_context(nc.sbuf_tensor(uq("mixT"), [128, KC, S], BF16))
            r_mT = [Res("mT%d" % i) for i in range(NT)]
            pT = es2.enter_context(nc.psum_tensor(uq("pT"), [128, 2, 1024], BF16))
            pT_ring = Ring([pT[:, i] for i in range(2)], "pT")
            with ExitStack() as es3:
                build_xT(nc, kb, es3, o_d, mixT, r_mT, ident_s, r_id, pT_ring)
            kb.barrier()
            mix_out_phase(nc, kb, es2, L, [mixT[:, kc, :] for kc in range(KC)], r_mT, w["odd_w_out"][0], x_src, xa, xab, w, C)
    kb.barrier()


W_NAMES = ["even_w_in", "even_b_f", "even_conv_w", "even_conv_b", "even_conv_norm_g", "even_conv_norm_b", "even_w_out",
           "odd_w_in", "odd_w_a2", "odd_b_a", "odd_norm_g", "odd_w_out", "ln_mix_g", "ln_mix_b", "ln_ffn_g", "ln_ffn_b",
           "router_w", "router_bias", "expert_w_gate", "expert_w_up", "expert_w_down"]
W_SHAPES = {"even_w_in": (1, 2048, 5128), "even_b_f": (1, 8), "even_conv_w": (1, 31, 1, 1024), "even_conv_b": (1, 1024),
            "even_conv_norm_g": (1, 1024), "even_conv_norm_b": (1, 1024), "even_w_out": (1, 2048, 2048),
            "odd_w_in": (1, 2048, 6160), "odd_w_a2": (1, 16, 1024), "odd_b_a": (1, 1024), "odd_norm_g": (1, 2048),
            "odd_w_out": (1, 2048, 2048), "ln_mix_g": (2, 2048), "ln_mix_b": (2, 2048), "ln_ffn_g": (2, 2048),
            "ln_ffn_b": (2, 2048), "router_w": (2048, 16), "router_bias": (16,),
            "expert_w_gate": (2, 16, 128, 11, 16, 128), "expert_w_up": (2, 16, 128, 11, 16, 128), "expert_w_down": (2, 16, 128, 4, 11, 512)}


def build_program():
    nc = bass.Bass("TRN2", target_bir_lowering=False)
    kb = KB(nc)

    def din(name, shape, dt):
        return nc.dram_tensor(name, list(shape), dt, kind="ExternalInput").ap()

    def dsc(name, shape, dt):
        return nc.dram_tensor(name, list(shape), dt, kind="Internal").ap()
    consts = make_consts()
    C = {k: din("c_" + k, v.shape, CONST_DT[k]) for k, v in consts.items()}
    w = {k: din(k, W_SHAPES[k], F32) for k in W_NAMES}
    x = din("x", (S, D), F32)
    out = nc.dram_tensor("out", [S, D], F32, kind="ExternalOutput").ap()
    xa = dsc("xa", (S, D), F32)
    xab = dsc("xab", (S + 128, D), BF16)
    x2 = dsc("x2", (S, D), F32)
    ys = dsc("ys", (NE * CAP + 128, D), BF16)
    uT_d = dsc("uT_d", (1024, S), BF16)
    vte_d = dsc("vte_d", (S, 1024), BF16)
    kt_d = dsc("kt_d", (S, 1024), BF16)
    vto_d = dsc("vto_d", (S, 2048), BF16)
    gt_d = dsc("gt_d", (S, 2048), BF16)
    o_d = dsc("o_d", (S, 2048), BF16)
    with nc.sbuf_tensor(uq("zt"), [128, D], BF16) as zt:
        rz = Res("zt")
        kb.op("dve", lambda e: e.memset(zt[:], 0.0), writes=[rz])
        kb.dma("sp", lambda e: e.dma_start(out=ys[YZ:YZ + 128, :], in_=zt[:]), reads=[rz], key=rz)
        kb.dma("sp", lambda e: e.dma_start(out=xab[S:S + 128, :], in_=zt[:]), reads=[rz], key=rz)
        kb.barrier()
    even_phase(nc, kb, 0, C, x, uT_d, vte_d, xa, xab, w)
    moe_phase(nc, kb, 0, C, xa, xab, ys, x2, None, w)
    odd_phase(nc, kb, 1, C, x2, kt_d, vto_d, gt_d, o_d, xa, xab, w)
    moe_phase(nc, kb, 1, C, xa, xab, ys, out, None, w)
    return nc, consts


def relayout(name, a):
    if name in ("expert_w_gate", "expert_w_up"):
        return np.ascontiguousarray(a.reshape(2, 16, 16, 128, 11, 128).transpose(0, 1, 3, 4, 2, 5))
    if name == "expert_w_down":
        return np.ascontiguousarray(a.reshape(2, 16, 11, 128, 4, 512).transpose(0, 1, 3, 4, 2, 5))
    return np.ascontiguousarray(a)


def kernel(**inputs):
    n = 8
    nc, consts = build_program()
    x = np.ascontiguousarray(np.asarray(inputs["x"], dtype=np.float32))
    shared = {("c_" + k): v for k, v in consts.items()}
    for k in W_NAMES:
        shared[k] = relayout(k, np.asarray(inputs[k], dtype=np.float32))
    in_maps = []
    for b in range(n):
        m = dict(shared)
        m["x"] = x[b]
        in_maps.append(m)
    res = run_bass_kernel_spmd(nc, in_maps, core_ids=list(range(n)))
    return np.stack([np.asarray(r["out"], dtype=np.float32) for r in res.results], axis=0)
```

```python
import numpy as np
import ml_dtypes
import concourse.bass as bass
import concourse.mybir as mybir
from concourse.bass_utils import run_bass_kernel_spmd

F32 = mybir.dt.float32
BF16 = mybir.dt.bfloat16
I32 = mybir.dt.int32
AF = mybir.ActivationFunctionType
ALU = mybir.AluOpType
AX = mybir.AxisListType

S = 2048
D = 2048
NT = S // 128
KC = D // 128
DEPTH = 2
ALPHA = (2 * DEPTH) ** 0.25
LN_EPS = 1e-5
NE = 16
FE = 1408
NF = FE // 128
CAP = 512
CAPV = 448
NJ = CAP // 128
ZROW = S
YZ = NE * CAP


class Res:
    __slots__ = ("name", "w", "rs", "dsem")

    def __init__(self, name):
        self.name = name
        self.w = None
        self.rs = []
        self.dsem = None


class KB:
    ENGS = ("pe", "act", "dve", "pool", "sp")

    def __init__(self, nc, n_dma_sems=48, same_engine_sync=True):
        self.nc = nc
        self.eng = {"pe": nc.tensor, "act": nc.scalar, "dve": nc.vector,
                    "pool": nc.gpsimd, "sp": nc.sync}
        self.sem = {}
        self.cnt = {}
        self.waited = {}
        self.same_engine_sync = same_engine_sync
        for e in self.ENGS:
            self._mksem("p_" + e)
        self._mksem("bar")
        self.dma_pool = []
        for i in range(n_dma_sems):
            self._mksem("d%d" % i)
            self.dma_pool.append("d%d" % i)
        self.dma_next = 0
        self.dma_res = []

    def _mksem(self, name):
        self.sem[name] = self.nc.alloc_semaphore(name)
        self.cnt[name] = 0

    def _wait(self, e, dep):
        if dep is None:
            return
        s, c = dep
        if s == "p_" + e and (e in ("pe", "sp") or not self.same_engine_sync):
            return
        if self.waited.get((e, s), 0) >= c:
            return
        self.eng[e].wait_ge(self.sem[s], c)
        self.waited[(e, s)] = c

    def _pre(self, e, reads, writes):
        for r in reads:
            self._wait(e, r.w)
        for w in writes:
            self._wait(e, w.w)
            for d in w.rs:
                self._wait(e, d)

    def _post(self, dep, reads, writes):
        for r in reads:
            r.rs.append(dep)
            if len(r.rs) > 8:
                m = {}
                for s, c in r.rs:
                    m[s] = max(m.get(s, 0), c)
                r.rs = list(m.items())
        for w in writes:
            w.w = dep
            w.rs = []

    def op(self, e, fn, reads=(), writes=()):
        self._pre(e, reads, writes)
        ins = fn(self.eng[e])
        s = "p_" + e
        self.cnt[s] += 1
        ins.then_inc(self.sem[s], 1)
        self._post((s, self.cnt[s]), reads, writes)
        return ins

    def dma(self, q, fn, reads=(), writes=(), key=None):
        self._pre(q, reads, writes)
        key = key or (writes[0] if writes else reads[0])
        if key.dsem is None:
            assert self.dma_next < len(self.dma_pool), "out of dma sems"
            key.dsem = self.dma_pool[self.dma_next]
            self.dma_next += 1
            self.dma_res.append(key)
        ins = fn(self.eng[q])
        s = key.dsem
        self.cnt[s] += 16
        ins.then_inc(self.sem[s], 16)
        self._post((s, self.cnt[s]), reads, writes)
        return ins

    def barrier(self):
        sp = self.eng["sp"]
        for s, c in self.cnt.items():
            if s in ("bar", "p_sp") or c == 0:
                continue
            if self.waited.get(("sp", s), 0) >= c:
                continue
            sp.wait_ge(self.sem[s], c)
            self.waited[("sp", s)] = c
        self.cnt["bar"] += 1
        sp.nop().then_inc(self.sem["bar"], 1)
        for e in self.ENGS:
            if e != "sp":
                self.eng[e].wait_ge(self.sem["bar"], self.cnt["bar"])
            for s, c in self.cnt.items():
                self.waited[(e, s)] = c
        for r in self.dma_res:
            r.dsem = None
        self.dma_res = []
        self.dma_next = 0


class Ring:
    def __init__(self, views, name):
        self.v = views
        self.r = [Res("%s%d" % (name, i)) for i in range(len(views))]
        self.i = -1

    def next(self):
        self.i = (self.i + 1) % len(self.v)
        return self.v[self.i], self.r[self.i]


_UNIQ = [0]


def uq(name):
    _UNIQ[0] += 1
    return "%s_u%d" % (name, _UNIQ[0])


def ln_tile(kb, z, zr, gam, bet, rg, st, rst, out, rout, eng2="dve"):
    stats, mv, rstd = st
    for c in range(4):
        kb.op("dve", lambda e: e.bn_stats(out=stats[:, c * 6:(c + 1) * 6], in_=z[:, c * 512:(c + 1) * 512]),
              reads=[zr], writes=[rst])
    kb.op("dve", lambda e: e.bn_aggr(out=mv[:, 0:2], in_=stats[:, 0:24]), reads=[rst], writes=[rst])
    kb.op("dve", lambda e: e.tensor_scalar_add(out=rstd[:, 0:1], in0=mv[:, 1:2], scalar1=LN_EPS), reads=[rst], writes=[rst])
    kb.op("act", lambda e: e.sqrt(out=rstd[:, 0:1], in_=rstd[:, 0:1]), reads=[rst], writes=[rst])
    kb.op("dve", lambda e: e.reciprocal(out=rstd[:, 0:1], in_=rstd[:, 0:1]), reads=[rst], writes=[rst])
    kb.op("dve", lambda e: e.tensor_scalar(out=z[:, :], in0=z[:, :], scalar1=mv[:, 0:1], scalar2=rstd[:, 0:1],
                                           op0=ALU.subtract, op1=ALU.mult), reads=[zr, rst], writes=[zr])
    kb.op(eng2, lambda e: e.tensor_tensor(out=z[:, :], in0=z[:, :], in1=gam[:, :], op=ALU.mult), reads=[zr] + rg, writes=[zr])
    kb.op(eng2, lambda e: e.tensor_tensor(out=out[:, :], in0=z[:, :], in1=bet[:, :], op=ALU.add), reads=[zr] + rg, writes=[rout])


class LNPipe:
    def __init__(self, nc, kb, es, gam, bet, rgs, emit, eps=LN_EPS):
        self.kb = kb
        self.gam, self.bet, self.rgs, self.emit, self.eps = gam, bet, rgs, emit, eps
        st = es.enter_context(nc.sbuf_tensor(uq("lnst"), [128, 2, 32], F32))
        self.st_ring = Ring([st[:, i] for i in range(2)], "lnst")
        zo = es.enter_context(nc.sbuf_tensor(uq("lnzo"), [128, 2, D], F32))
        self.zo_ring = Ring([zo[:, i] for i in range(2)], "lnzo")
        self.pending = None

    def _apply(self):
        kb = self.kb
        z, r_z, tag = self.pending
        o, r_o = self.zo_ring.next()
        kb.op("dve", lambda e: e.tensor_tensor(out=z, in0=z, in1=self.gam[:, :], op=ALU.mult), reads=[r_z] + self.rgs, writes=[r_z])
        kb.op("dve", lambda e: e.tensor_tensor(out=o, in0=z, in1=self.bet[:, :], op=ALU.add), reads=[r_z] + self.rgs, writes=[r_o])
        self.pending = None
        self.emit(o, r_o, tag)

    def feed(self, z, r_z, tag):
        kb = self.kb
        st, r_st = self.st_ring.next()
        for c in range(4):
            kb.op("dve", lambda e: e.bn_stats(out=st[:, c * 6:(c + 1) * 6], in_=z[:, c * 512:(c + 1) * 512]), reads=[r_z], writes=[r_st])
        kb.op("dve", lambda e: e.bn_aggr(out=st[:, 24:26], in_=st[:, 0:24]), reads=[r_st], writes=[r_st])
        kb.op("dve", lambda e: e.tensor_scalar_add(out=st[:, 26:27], in0=st[:, 25:26], scalar1=self.eps), reads=[r_st], writes=[r_st])
        kb.op("act", lambda e: e.sqrt(out=st[:, 26:27], in_=st[:, 26:27]), reads=[r_st], writes=[r_st])
        if self.pending is not None:
            self._apply()
        kb.op("dve", lambda e: e.reciprocal(out=st[:, 27:28], in_=st[:, 26:27]), reads=[r_st], writes=[r_st])
        kb.op("dve", lambda e: e.scalar_tensor_tensor(out=st[:, 28:29], in0=st[:, 24:25], scalar=-1.0, in1=st[:, 27:28], op0=ALU.mult, op1=ALU.mult),
              reads=[r_st], writes=[r_st])
        kb.op("act", lambda e: e.activation(out=z, in_=z, func=AF.Identity, bias=st[:, 28:29], scale=st[:, 27:28]), reads=[r_z, r_st], writes=[r_z])
        self.pending = (z, r_z, tag)

    def flush(self):
        if self.pending is not None:
            self._apply()


def bcast_rows(ap1d, n):
    return ap1d.partition_broadcast(128)


def moe_phase(nc, kb, L, C, xa, xab, ys, xo, xob, w):
    from contextlib import ExitStack
    ident, iota, tokinfo = C["ident"], C["iota"], C["tokinfo"]
    wg_d = w["expert_w_gate"]
    wu_d = w["expert_w_up"]
    wd_d = w["expert_w_down"]

    with ExitStack() as es:
        def sb(name, shape, dt):
            return es.enter_context(nc.sbuf_tensor(uq(name), shape, dt))

        def ps(name, shape, dt):
            return es.enter_context(nc.psum_tensor(uq(name), shape, dt))

        gate_all = sb("gate_all", [128, NT, NE], F32)
        posm_all = sb("posm_all", [128, NT, NE], F32)
        ridx = sb("ridx", [128, NT, 2], I32)
        gsel = sb("gsel", [128, NT, 2], F32)
        tokidx = sb("tokidx", [128, NE * NJ], I32)
        rw_bf = sb("rw_bf", [128, KC, NE], BF16)
        rbias = sb("rbias", [128, NE], F32)
        ecap = sb("ecap", [128, NE], F32)
        ident_s = sb("ident_s", [128, 128], BF16)
        iota_s = sb("iota_s", [128, CAP], F32)
        tokinfo_s = sb("tokinfo_s", [128, NT, 4], BF16)
        lstrict = sb("lstrict", [128, 128], BF16)
        ones_bf = sb("ones_bf", [128, 128], BF16)
        r_const = Res("const")
        r_gate = Res("gate_all")
        r_posm = Res("posm_all")
        r_ridx = Res("ridx")
        r_tok = Res("tokidx")

        kb.dma("pool", lambda e: e.dma_start(out=rw_bf[:], in_=w["router_w"].rearrange("(kc p) e -> p kc e", p=128)), writes=[r_const])
        c2 = Res("c2"); c3 = Res("c3"); c4 = Res("c4"); c5 = Res("c5"); c6 = Res("c6"); c7 = Res("c7")
        kb.dma("sp", lambda e: e.dma_start(out=rbias[:], in_=w["router_bias"].partition_broadcast(128)), writes=[c2])
        kb.dma("sp", lambda e: e.dma_start(out=ident_s[:], in_=ident), writes=[c3])
        kb.dma("sp", lambda e: e.dma_start(out=iota_s[:], in_=iota[:, 0:CAP]), writes=[c4])
        kb.dma("sp", lambda e: e.dma_start(out=tokinfo_s[:], in_=tokinfo), writes=[c5])
        kb.dma("sp", lambda e: e.dma_start(out=lstrict[:], in_=C["lstrict"]), writes=[c6])
        kb.dma("sp", lambda e: e.dma_start(out=ecap[:], in_=C["ecap"]), writes=[c7])
        kb.op("dve", lambda e: e.memset(ones_bf[:], 1.0), writes=[c6])
        consts = [r_const, c2, c3, c4, c5, c6, c7]

        with ExitStack() as es2:
            def sb2(name, shape, dt):
                return es2.enter_context(nc.sbuf_tensor(uq(name), shape, dt))

            def ps2(name, shape, dt):
                return es2.enter_context(nc.psum_tensor(uq(name), shape, dt))

            xt_b = sb2("xt_b", [128, 2, D], BF16)
            xt_ring = Ring([xt_b[:, i] for i in range(2)], "xt_b")
            xT = sb2("xT", [128, 2, KC, 128], BF16)
            xT_ring = Ring([xT[:, i] for i in range(2)], "xT")
            pT = ps2("pT", [128, 2, 1024], BF16)
            pT_ring = Ring([pT[:, i] for i in range(2)], "pT")
            psm = ps2("psm", [128, 2, 2, 512], F32)
            rt = sb2("rt", [128, 16, NE], F32)
            r_rt = Res("rt")
            m4 = sb2("m4", [128, 8, 4], F32)
            mcum = sb2("mcum", [128, NE], BF16)
            m_bf = sb2("m_bf", [128, 2, NE], BF16)
            m_ring = Ring([m_bf[:, i] for i in range(2)], "m_bf")
            r_mcum = Res("mcum")
            oh = sb2("oh", [128, 4, CAP], BF16)
            oh_ring = Ring([oh[:, i] for i in range(4)], "oh")
            kb.op("dve", lambda e: e.memset(mcum[:], 0.0), writes=[r_mcum])

            class Rec:
                def __init__(self):
                    self.ops = []

                def op(self, e, fn, reads=(), writes=()):
                    self.ops.append((e, fn, list(reads), list(writes)))

            def xpose_tile(i):
                xt, r_xt = xt_ring.next()
                kb.dma("sp", lambda e: e.dma_start(out=xt, in_=xab[i * 128:(i + 1) * 128, :]), writes=[r_xt])
                xTt, r_xT = xT_ring.next()
                for g4 in range(4):
                    p, r_p = pT_ring.next()
                    for q in range(4):
                        kc = g4 * 4 + q
                        kb.op("pe", lambda e: e.transpose(out=p[:, q * 128:(q + 1) * 128], in_=xt[:, kc * 128:(kc + 1) * 128], identity=ident_s[:]),
                              reads=[r_xt, c3], writes=[r_p])
                    if g4 % 2 == 0:
                        kb.op("act", lambda e: e.copy(out=xTt[:, g4 * 4:(g4 + 1) * 4, :], in_=p[:, 0:512].rearrange("p (a b) -> p a b", a=4)),
                              reads=[r_p], writes=[r_xT])
                    else:
                        kb.op("dve", lambda e: e.tensor_copy(out=xTt[:, g4 * 4:(g4 + 1) * 4, :], in_=p[:, 0:512].rearrange("p (a b) -> p a b", a=4)),
                              reads=[r_p], writes=[r_xT])
                return xTt, r_xT

            rt2 = sb2("rt2", [128, 2, 16, NE], F32)
            m42 = sb2("m42", [128, 2, 8, 4], F32)
            r_rt2 = [Res("rt_a"), Res("rt_b")]
            r_psr = [Res("psr_a"), Res("psr_b")]
            r_psp = [Res("psp_a"), Res("psp_b")]

            def route_tile(i, par, xTt, r_xT, rk):
                rt_ = rt2[:, par]
                m4_ = m42[:, par]
                pr_ = psm[:, par, 0, 0:NE]
                pp_ = psm[:, par, 1, 0:NE]
                r_psm_r, r_psm_p = r_psr[par], r_psp[par]
                for kc in range(KC):
                    rk.op("pe", lambda e, kc=kc: e.matmul(pr_, lhsT=xTt[:, kc, :], rhs=rw_bf[:, kc, :], start=(kc == 0), stop=(kc == KC - 1)),
                          reads=[r_xT, r_const], writes=[r_psm_r])
                sc = rt_[:, 0]; sel = rt_[:, 1]; eq1 = rt_[:, 2]; sel2 = rt_[:, 3]; ge2 = rt_[:, 4]; M = rt_[:, 5]; wv = rt_[:, 6]
                pos1 = rt_[:, 7]; vv = rt_[:, 8]; sv = rt_[:, 9]; tmp = rt_[:, 10]; sv2 = rt_[:, 11]
                m1 = m4_[:, 0]; m2 = m4_[:, 1]; gs = m4_[:, 2]; gm = m4_[:, 3]
                gmax = m4_[:, 4, 0:1]; wsum = m4_[:, 4, 1:2]; ihi = m4_[:, 5, 0:1]; ilo = m4_[:, 5, 1:2]; t1 = m4_[:, 5, 2:3]
                R = [r_rt2[par]]
                v3 = lambda a: a.rearrange("p (g j) -> p g j", g=4)
                b3 = lambda a: a.unsqueeze(2).to_broadcast([128, 4, 4])
                rk.op("act", lambda e: e.activation(out=sc, in_=pr_, func=AF.Sigmoid), reads=[r_psm_r], writes=R)
                rk.op("dve", lambda e: e.tensor_tensor(out=sel, in0=sc, in1=rbias[:], op=ALU.add), reads=R + [c2], writes=R)
                rk.op("dve", lambda e: e.tensor_reduce(out=m1, in_=v3(sel), axis=AX.X, op=ALU.max), reads=R, writes=R)
                rk.op("dve", lambda e: e.tensor_tensor(out=v3(eq1), in0=v3(sel), in1=b3(m1), op=ALU.is_equal), reads=R, writes=R)
                rk.op("dve", lambda e: e.scalar_tensor_tensor(out=sel2, in0=eq1, scalar=-1e9, in1=sel, op0=ALU.mult, op1=ALU.add), reads=R, writes=R)
                rk.op("dve", lambda e: e.tensor_reduce(out=m2, in_=v3(sel2), axis=AX.X, op=ALU.max), reads=R, writes=R)
                rk.op("dve", lambda e: e.tensor_tensor(out=gs, in0=m1, in1=m2, op=ALU.add), reads=R, writes=R)
                rk.op("dve", lambda e: e.tensor_reduce(out=gmax, in_=gs, axis=AX.X, op=ALU.max), reads=R, writes=R)
                rk.op("dve", lambda e: e.tensor_scalar(out=gm, in0=gs, scalar1=gmax, scalar2=None, op0=ALU.is_equal), reads=R, writes=R)
                rk.op("dve", lambda e: e.tensor_tensor(out=v3(ge2), in0=v3(sel), in1=b3(m2), op=ALU.is_ge), reads=R, writes=R)
                rk.op("dve", lambda e: e.tensor_tensor(out=v3(M), in0=v3(ge2), in1=b3(gm), op=ALU.mult), reads=R, writes=R)
                rk.op("dve", lambda e: e.tensor_tensor(out=wv, in0=sc, in1=M, op=ALU.mult), reads=R, writes=R)
                rk.op("dve", lambda e: e.tensor_reduce(out=wsum, in_=wv, axis=AX.X, op=ALU.add), reads=R, writes=R)
                rk.op("dve", lambda e: e.reciprocal(out=wsum, in_=wsum), reads=R, writes=R)
                rk.op("dve", lambda e: e.tensor_scalar(out=gate_all[:, i, :], in0=wv, scalar1=wsum, scalar2=None, op0=ALU.mult), reads=R, writes=[r_gate])
                mb, r_mb = m_ring.next()
                rk.op("dve", lambda e: e.tensor_copy(out=mb, in_=M), reads=R, writes=[r_mb])
                rk.op("pe", lambda e: e.matmul(pp_, lhsT=lstrict[:], rhs=mb, start=True, stop=False), reads=[r_mb, c6], writes=[r_psm_p])
                rk.op("pe", lambda e: e.matmul(pp_, lhsT=ones_bf[:], rhs=mcum[:], start=False, stop=True), reads=[r_mcum, c6], writes=[r_psm_p])
                rk.op("dve", lambda e: e.tensor_scalar(out=vv, in0=pp_, scalar1=float(CAPV), scalar2=None, op0=ALU.is_lt), reads=[r_psm_p] + R, writes=R)
                rk.op("dve", lambda e: e.scalar_tensor_tensor(out=pos1, in0=pp_, scalar=1.0, in1=M, op0=ALU.add, op1=ALU.mult), reads=[r_psm_p] + R, writes=R)
                rk.op("dve", lambda e: e.tensor_tensor(out=pos1, in0=pos1, in1=vv, op=ALU.mult), reads=R, writes=R)
                rk.op("dve", lambda e: e.tensor_scalar_add(out=posm_all[:, i, :], in0=pos1, scalar1=-1.0), reads=R, writes=[r_posm])
                rk.op("dve", lambda e: e.tensor_tensor(out=mcum[:], in0=mcum[:], in1=mb, op=ALU.add), reads=[r_mb, r_mcum], writes=[r_mcum])
                rk.op("dve", lambda e: e.tensor_scalar(out=vv, in0=pos1, scalar1=0.0, scalar2=None, op0=ALU.is_gt), reads=R, writes=R)
                rk.op("dve", lambda e: e.tensor_tensor(out=sv, in0=pos1, in1=ecap[:], op=ALU.add), reads=R + [c7], writes=R)
                rk.op("dve", lambda e: e.tensor_tensor(out=sv, in0=sv, in1=vv, op=ALU.mult), reads=R, writes=R)
                rk.op("dve", lambda e: e.tensor_reduce(out=ihi, in_=sv, axis=AX.X, op=ALU.max), reads=R, writes=R)
                rk.op("dve", lambda e: e.tensor_scalar(out=tmp, in0=sv, scalar1=ihi, scalar2=None, op0=ALU.not_equal), reads=R, writes=R)
                rk.op("dve", lambda e: e.tensor_tensor(out=sv2, in0=sv, in1=tmp, op=ALU.mult), reads=R, writes=R)
                rk.op("dve", lambda e: e.tensor_reduce(out=ilo, in_=sv2, axis=AX.X, op=ALU.max), reads=R, writes=R)
                rk.op("dve", lambda e: e.scalar_tensor_tensor(out=tmp, in0=sv, scalar=ihi, in1=gate_all[:, i, :], op0=ALU.is_equal, op1=ALU.mult,
                                                              accum_out=gsel[:, i, 0:1]), reads=R + [r_gate], writes=R + [r_ridx])
                rk.op("dve", lambda e: e.scalar_tensor_tensor(out=tmp, in0=sv, scalar=ilo, in1=gate_all[:, i, :], op0=ALU.is_equal, op1=ALU.mult,
                                                              accum_out=gsel[:, i, 1:2]), reads=R + [r_gate], writes=R + [r_ridx])
                for k, src in ((0, ihi), (1, ilo)):
                    rk.op("dve", lambda e, src=src: e.tensor_scalar(out=t1, in0=src, scalar1=0.0, scalar2=float(YZ + 1), op0=ALU.is_equal, op1=ALU.mult), reads=R, writes=R)
                    rk.op("dve", lambda e, src=src: e.scalar_tensor_tensor(out=t1, in0=src, scalar=-1.0, in1=t1, op0=ALU.add, op1=ALU.add), reads=R, writes=R)
                    rk.op("dve", lambda e, k=k: e.tensor_copy(out=ridx[:, i, k:k + 1], in_=t1), reads=R, writes=[r_ridx])

            for i0_ in range(0, NT, 2):
                recs = []
                for par in range(2):
                    xTt, r_xT = xpose_tile(i0_ + par)
                    rk = Rec()
                    route_tile(i0_ + par, par, xTt, r_xT, rk)
                    recs.append(rk.ops)
                LAG = 8
                order = []
                na, nb = len(recs[0]), len(recs[1])
                for n in range(max(na, nb + LAG)):
                    if n < na:
                        order.append(recs[0][n])
                    if 0 <= n - LAG < nb:
                        order.append(recs[1][n - LAG])
                for e_, fn_, rd_, wr_ in order:
                    kb.op(e_, fn_, reads=rd_, writes=wr_)
            pacs = sb2("pacs", [128, NE * NJ, 4], F32)
            r_tf = Res("tf")
            ptab = ps2("ptab", [128, 2, NJ, NT, 4], F32)
            ptab_ring = Ring([ptab[:, i] for i in range(2)], "ptab")
            for ex in range(NE):
                pt, r_pt = ptab_ring.next()
                for i in range(NT):
                    o, r_o = oh_ring.next()
                    kb.op("dve", lambda e: e.tensor_scalar(out=o, in0=iota_s[:], scalar1=posm_all[:, i, ex:ex + 1], scalar2=None, op0=ALU.is_equal),
                          reads=[r_posm, c4], writes=[r_o])
                    for j in range(NJ):
                        kb.op("pe", lambda e: e.matmul(pt[:, j, i, 0:4], lhsT=o[:, j * 128:(j + 1) * 128], rhs=tokinfo_s[:, i, :],
                                                       start=True, stop=True), reads=[r_o, c5], writes=[r_pt])
                kb.op("dve", lambda e: e.tensor_reduce(out=pacs[:, ex * NJ:(ex + 1) * NJ, :], in_=pt.rearrange("p j i c -> p j c i"), axis=AX.X, op=ALU.add),
                      reads=[r_pt], writes=[r_tf])
            tf = sb2("tf", [128, NE * NJ, 2], F32)
            kb.op("dve", lambda e: e.scalar_tensor_tensor(out=tf[:, :, 0], in0=pacs[:, :, 1], scalar=128.0, in1=pacs[:, :, 0], op0=ALU.mult, op1=ALU.add),
                  reads=[r_tf], writes=[r_tf])
            kb.op("dve", lambda e: e.tensor_scalar(out=tf[:, :, 1], in0=pacs[:, :, 2], scalar1=-float(ZROW), scalar2=float(ZROW), op0=ALU.mult, op1=ALU.add),
                  reads=[r_tf], writes=[r_tf])
            kb.op("dve", lambda e: e.tensor_tensor(out=tf[:, :, 0], in0=tf[:, :, 0], in1=tf[:, :, 1], op=ALU.add), reads=[r_tf], writes=[r_tf])
            kb.op("dve", lambda e: e.tensor_copy(out=tokidx[:, :], in_=tf[:, :, 0]), reads=[r_tf], writes=[r_tok])
        kb.barrier()

        with ExitStack() as es2:
            def sb2(name, shape, dt):
                return es2.enter_context(nc.sbuf_tensor(uq(name), shape, dt))

            def ps2(name, shape, dt):
                return es2.enter_context(nc.psum_tensor(uq(name), shape, dt))

            NGU = 4
            NWD = 4
            wgu = sb2("wgu", [128, NGU, 2, 2, KC, 128], BF16)
            gu_ring = Ring([wgu[:, i] for i in range(NGU)], "wgu")
            wd = sb2("wd", [128, NWD, NF, 512], BF16)
            wd_ring = Ring([wd[:, i] for i in range(NWD)], "wd")
            xg = sb2("xg", [128, 4, D], BF16)
            xg_ring = Ring([xg[:, i] for i in range(4)], "xg")
            xgT = sb2("xgT", [128, 2, KC, CAP], BF16)
            xgT_res = [[Res("xgT%d_%d" % (b, j)) for j in range(NJ)] for b in range(2)]
            hT = sb2("hT", [128, 2, NF, CAP], BF16)
            hT_res = [[Res("hT%d_%d" % (b, f)) for f in range(NF)] for b in range(2)]
            sg = sb2("sg", [128, 2, CAP], F32)
            sg_ring = Ring([sg[:, i] for i in range(2)], "sg")
            yst = sb2("yst", [128, 4, 512], BF16)
            yst_ring = Ring([yst[:, i] for i in range(4)], "yst")
            pT = ps2("pTe", [128, 2, 1024], BF16)
            pT_ring = Ring([pT[:, i] for i in range(2)], "pTe")
            pg = ps2("pg", [128, 2, 512], F32)
            pg_ring = Ring([pg[:, i] for i in range(2)], "pg")
            pu = ps2("pu", [128, 2, 512], F32)
            pu_ring = Ring([pu[:, i] for i in range(2)], "pu")
            py = ps2("py", [128, 2, 512], F32)
            py_ring = Ring([py[:, i] for i in range(2)], "py")
            r_ys = Res("ys_dram")
            nev_box = [0]

            def prep(ex):
                b = ex % 2
                groups = []
                for j in range(NJ):
                    g, r_g = xg_ring.next()
                    col = ex * NJ + j
                    kb.dma("pool", lambda e: e.indirect_dma_start(out=g, out_offset=None, in_=xab[:, :],
                                                                   in_offset=bass.IndirectOffsetOnAxis(ap=tokidx[:, col:col + 1], axis=0)),
                           reads=[r_tok], writes=[r_g])
                    for g4 in range(4):
                        def grp(g=g, r_g=r_g, j=j, g4=g4, b=b):
                            p, r_p = pT_ring.next()
                            for q in range(4):
                                kc = g4 * 4 + q
                                kb.op("pe", lambda e: e.transpose(out=p[:, q * 128:(q + 1) * 128], in_=g[:, kc * 128:(kc + 1) * 128], identity=ident_s[:]),
                                      reads=[r_g, c3], writes=[r_p])
                            dst = xgT[:, b, g4 * 4:(g4 + 1) * 4, j * 128:(j + 1) * 128]
                            src = p[:, 0:512].rearrange("p (a b) -> p a b", a=4)
                            if nev_box[0] % 2 == 0:
                                kb.op("act", lambda e: e.copy(out=dst, in_=src), reads=[r_p], writes=[xgT_res[b][j]])
                            else:
                                kb.op("dve", lambda e: e.tensor_copy(out=dst, in_=src), reads=[r_p], writes=[xgT_res[b][j]])
                            nev_box[0] += 1
                        groups.append(grp)
                return groups

            for g_ in prep(0):
                g_()
            chunks = [(ex, f) for ex in range(NE) for f in range(NF)]
            loaded = {}
            LOOK = 4

            def emit_load(g):
                ex, f = chunks[g]
                if f % 2 == 1:
                    return
                nf = 2 if f + 1 < NF else 1
                wt, r_w = gu_ring.next()
                kb.dma("pool", lambda e: e.dma_start(out=wt[:, 0, 0:nf], in_=wg_d[L, ex][:, f:f + nf]), writes=[r_w])
                kb.dma("pool", lambda e: e.dma_start(out=wt[:, 1, 0:nf], in_=wu_d[L, ex][:, f:f + nf]), writes=[r_w])
                for k in range(nf):
                    loaded[g + k] = (wt[:, :, k], r_w)

            def load_wd(ex, dc):
                wdt, r_wd = wd_ring.next()
                kb.dma("pool", lambda e: e.dma_start(out=wdt, in_=wd_d[L, ex][:, dc]), writes=[r_wd])
                return (wdt, r_wd)

            def a_step(ex, f, wt, r_w):
                b = ex % 2
                pgt, r_pg = pg_ring.next()
                put, r_pu = pu_ring.next()
                for kc in range(KC):
                    kb.op("pe", lambda e: e.matmul(pgt[:, 0:CAPV], lhsT=wt[:, 0, kc, :], rhs=xgT[:, b, kc, 0:CAPV], start=(kc == 0), stop=(kc == KC - 1)),
                          reads=[r_w] + xgT_res[b], writes=[r_pg])
                for kc in range(KC):
                    kb.op("pe", lambda e: e.matmul(put[:, 0:CAPV], lhsT=wt[:, 1, kc, :], rhs=xgT[:, b, kc, 0:CAPV], start=(kc == 0), stop=(kc == KC - 1)),
                          reads=[r_w] + xgT_res[b], writes=[r_pu])
                sgt, r_sg = sg_ring.next()
                kb.op("act", lambda e: e.activation(out=sgt[:, 0:CAPV], in_=pgt[:, 0:CAPV], func=AF.Silu), reads=[r_pg], writes=[r_sg])
                kb.op("dve", lambda e: e.tensor_tensor(out=hT[:, b, f, 0:CAPV], in0=sgt[:, 0:CAPV], in1=put[:, 0:CAPV], op=ALU.mult), reads=[r_sg, r_pu], writes=[hT_res[b][f]])

            def b_group(ex, dc, t, wt, r_w):
                b = ex % 2
                pyt, r_py = py_ring.next()
                nr = min(128, CAPV - t * 128)
                for f in range(NF):
                    kb.op("pe", lambda e: e.matmul(pyt[0:nr, 0:512], lhsT=hT[:, b, f, t * 128:t * 128 + nr], rhs=wt[:, f, :], start=(f == 0), stop=(f == NF - 1)),
                          reads=[r_w] + hT_res[b], writes=[r_py])
                y, r_y = yst_ring.next()
                if nev_box[0] % 2 == 0:
                    kb.op("act", lambda e: e.copy(out=y[0:nr], in_=pyt[0:nr, 0:512]), reads=[r_py], writes=[r_y])
                else:
                    kb.op("dve", lambda e: e.tensor_copy(out=y[0:nr], in_=pyt[0:nr, 0:512]), reads=[r_py], writes=[r_y])
                nev_box[0] += 1
                row0 = ex * CAP + t * 128
                kb.dma("sp", lambda e: e.dma_start(out=ys[row0:row0 + nr, dc * 512:(dc + 1) * 512], in_=y[0:nr]), reads=[r_y], key=r_y)

            for g in range(LOOK):
                emit_load(g)
            next_load = LOOK
            wd_cur = [load_wd(0, dc) for dc in range(4)]
            sched = [2, 1, 2, 1, 2, 1, 2, 1, 2, 1, 1]
            schedp = [0, 0, 0, 0, 2, 2, 2, 2, 2, 3, 3]
            for it in range(NE + 1):
                bgroups = [(dc, t) for dc in range(4) for t in range(NJ)] if it >= 1 else []
                pgroups = prep(it + 1) if it + 1 < NE else []
                wd_next = [None] * 4

                def side_work(n, npre):
                    for _ in range(npre):
                        if pgroups:
                            pgroups.pop(0)()
                    for _ in range(n):
                        if bgroups:
                            dc, t = bgroups.pop(0)
                            b_group(it - 1, dc, t, *wd_cur[dc])
                            if t == NJ - 1 and it < NE:
                                wd_next[dc] = load_wd(it, dc)
                if it < NE:
                    for f in range(NF):
                        if next_load < len(chunks):
                            emit_load(next_load)
                            next_load += 1
                        a_step(it, f, *loaded.pop(it * NF + f))
                        side_work(sched[f], schedp[f])
                side_work(16, 16)
                if it >= 1:
                    wd_cur = wd_next
        kb.barrier()

        with ExitStack() as es2:
            def sb2(name, shape, dt):
                return es2.enter_context(nc.sbuf_tensor(uq(name), shape, dt))

            gam = sb2("gam", [128, D], F32)
            bet = sb2("bet", [128, D], F32)
            r_gb = Res("gb")
            kb.dma("sp", lambda e: e.dma_start(out=gam[:], in_=w["ln_ffn_g"][L].partition_broadcast(128)), writes=[r_gb])
            r_gb2 = Res("gb2")
            kb.dma("sp", lambda e: e.dma_start(out=bet[:], in_=w["ln_ffn_b"][L].partition_broadcast(128)), writes=[r_gb2])
            xin = sb2("xin", [128, 3, D], F32)
            xin_ring = Ring([xin[:, i] for i in range(3)], "xin")
            rr = sb2("rr", [128, 4, D], BF16)
            rr_ring = Ring([rr[:, i] for i in range(4)], "rr")
            zb = sb2("zb", [128, 2, D], BF16)
            zb_ring = Ring([zb[:, i] for i in range(2)], "zb")
            gs2 = sb2("gs2", [128, NT, 2], F32)
            r_gs2 = Res("gs2")
            kb.op("dve", lambda e: e.tensor_scalar(out=gs2[:], in0=gsel[:], scalar1=1.0 / ALPHA, scalar2=None, op0=ALU.mult), reads=[r_ridx], writes=[r_gs2])

            def emit(o, r_o, i):
                kb.dma("sp", lambda e: e.dma_start(out=xo[i * 128:(i + 1) * 128, :], in_=o), reads=[r_o], key=r_o)
                if xob is not None:
                    zbt, r_zb = zb_ring.next()
                    kb.op("act", lambda e: e.copy(out=zbt, in_=o), reads=[r_o], writes=[r_zb])
                    kb.dma("sp", lambda e: e.dma_start(out=xob[i * 128:(i + 1) * 128, :], in_=zbt), reads=[r_zb], key=r_zb)
            lnp = LNPipe(nc, kb, es2, gam, bet, [r_gb, r_gb2], emit, eps=LN_EPS / (ALPHA * ALPHA))
            for i in range(NT):
                x, r_x = xin_ring.next()
                kb.dma("sp", lambda e: e.dma_start(out=x, in_=xa[i * 128:(i + 1) * 128, :]), writes=[r_x])
                rh, r_rh = rr_ring.next()
                kb.dma("pool", lambda e: e.indirect_dma_start(out=rh, out_offset=None, in_=ys[:, :],
                                                               in_offset=bass.IndirectOffsetOnAxis(ap=ridx[:, i, 0:1], axis=0)), reads=[r_ridx], writes=[r_rh])
                rl, r_rl = rr_ring.next()
                kb.dma("pool", lambda e: e.indirect_dma_start(out=rl, out_offset=None, in_=ys[:, :],
                                                               in_offset=bass.IndirectOffsetOnAxis(ap=ridx[:, i, 1:2], axis=0)), reads=[r_ridx], writes=[r_rl])
                kb.op("dve", lambda e: e.scalar_tensor_tensor(out=x, in0=rh, scalar=gs2[:, i, 0:1], in1=x, op0=ALU.mult, op1=ALU.add),
                      reads=[r_rh, r_x, r_gs2], writes=[r_x])
                kb.op("dve", lambda e: e.scalar_tensor_tensor(out=x, in0=rl, scalar=gs2[:, i, 1:2], in1=x, op0=ALU.mult, op1=ALU.add),
                      reads=[r_rl, r_x, r_gs2], writes=[r_x])
                lnp.feed(x, r_x, i)
            lnp.flush()
        kb.barrier()


def build_xT(nc, kb, es, src, xT, r_xT, ident_s, r_id, pT_ring):
    xt_b = es.enter_context(nc.sbuf_tensor(uq("xtb"), [128, 2, D], BF16))
    ring = Ring([xt_b[:, i] for i in range(2)], "xtb")
    n = 0
    for i in range(NT):
        xt, r_xt = ring.next()
        kb.dma("pool", lambda e: e.dma_start(out=xt, in_=src[i * 128:(i + 1) * 128, :]), writes=[r_xt])
        for g4 in range(4):
            p, r_p = pT_ring.next()
            for q in range(4):
                kc = g4 * 4 + q
                kb.op("pe", lambda e: e.transpose(out=p[:, q * 128:(q + 1) * 128], in_=xt[:, kc * 128:(kc + 1) * 128], identity=ident_s[:]),
                      reads=[r_xt, r_id], writes=[r_p])
            dst = xT[:, g4 * 4:(g4 + 1) * 4, i * 128:(i + 1) * 128]
            srcp = p[:, 0:512].rearrange("p (a b) -> p a b", a=4)
            if n % 2 == 0:
                kb.op("act", lambda e: e.copy(out=dst, in_=srcp), reads=[r_p], writes=[r_xT[i]])
            else:
                kb.op("dve", lambda e: e.tensor_copy(out=dst, in_=srcp), reads=[r_p], writes=[r_xT[i]])
            n += 1


def mix_out_phase(nc, kb, es, L, mixT, r_mix, wout_d, x_src, xa, xab, w, C):
    def sb(name, shape, dt):
        return es.enter_context(nc.sbuf_tensor(uq(name), shape, dt))
    wo = sb("wo", [128, KC, D], BF16)
    r_wo = [Res("wo%d" % i) for i in range(4)]
    for q in range(4):
        kb.dma("pool", lambda e: e.dma_start(out=wo[:, q * 4:(q + 1) * 4, :], in_=wout_d.rearrange("(kc p) d -> p kc d", p=128)[:, q * 4:(q + 1) * 4, :]),
               writes=[r_wo[q]])
    gam = sb("gam", [128, D], F32)
    bet = sb("bet", [128, D], F32)
    r_g1 = Res("g1"); r_g2 = Res("g2")
    kb.dma("sp", lambda e: e.dma_start(out=gam[:], in_=w["ln_mix_g"][L].partition_broadcast(128)), writes=[r_g1])
    kb.dma("sp", lambda e: e.dma_start(out=bet[:], in_=w["ln_mix_b"][L].partition_broadcast(128)), writes=[r_g2])
    xin = sb("xin", [128, 3, D], F32)
    xin_ring = Ring([xin[:, i] for i in range(3)], "xin")
    zb = sb("zb", [128, 2, D], BF16)
    zb_ring = Ring([zb[:, i] for i in range(2)], "zb")

    def emit(o, r_o, i):
        kb.dma("sp", lambda e: e.dma_start(out=xa[i * 128:(i + 1) * 128, :], in_=o), reads=[r_o], key=r_o)
        zbt, r_zb = zb_ring.next()
        kb.op("act", lambda e: e.copy(out=zbt, in_=o), reads=[r_o], writes=[r_zb])
        kb.dma("sp", lambda e: e.dma_start(out=xab[i * 128:(i + 1) * 128, :], in_=zbt), reads=[r_zb], key=r_zb)
    lnp = LNPipe(nc, kb, es, gam, bet, [r_g1, r_g2], emit)
    pm = es.enter_context(nc.psum_tensor(uq("pm"), [128, 6, 512], F32))
    pm_ring = Ring([pm[:, i] for i in range(6)], "pm")
    for i in range(NT):
        x, r_x = xin_ring.next()
        kb.dma("sp", lambda e: e.dma_start(out=x, in_=x_src[i * 128:(i + 1) * 128, :]), writes=[r_x])
        for dc in range(4):
            p, r_p = pm_ring.next()
            for kc in range(KC):
                kb.op("pe", lambda e: e.matmul(p[:, 0:512], lhsT=mixT[kc][:, i * 128:(i + 1) * 128], rhs=wo[:, kc, dc * 512:(dc + 1) * 512],
                                               start=(kc == 0), stop=(kc == KC - 1)), reads=[r_mix[kc] if len(r_mix) == KC else r_mix[i], r_wo[kc // 4]], writes=[r_p])
            kb.op("dve", lambda e: e.scalar_tensor_tensor(out=x[:, dc * 512:(dc + 1) * 512], in0=x[:, dc * 512:(dc + 1) * 512], scalar=ALPHA, in1=p[:, 0:512],
                                                          op0=ALU.mult, op1=ALU.add), reads=[r_x, r_p], writes=[r_x])
        lnp.feed(x, r_x, i)
    lnp.flush()


FOXH = 8
CQ, CK, CV, CF, CA, CG = 0, 1024, 2048, 3072, 3080, 4104


def even_phase(nc, kb, L, C, x_src, uT_d, vt_d, xa, xab, w, dbg=None):
    from contextlib import ExitStack
    win = w["even_w_in"][0]
    winv = win.rearrange("(kc p) n -> p kc n", p=128)
    with ExitStack() as es0:
        def sb0(name, shape, dt):
            return es0.enter_context(nc.sbuf_tensor(uq(name), shape, dt))
        ident_s = sb0("ident", [128, 128], BF16)
        r_id = Res("ident")
        kb.dma("sp", lambda e: e.dma_start(out=ident_s[:], in_=C["ident"]), writes=[r_id])
        attT = sb0("attT", [128, FOXH, S], BF16)
        r_att = [Res("att%d" % h) for h in range(FOXH)]
        with ExitStack() as es2:
            def sb2(name, shape, dt):
                return es2.enter_context(nc.sbuf_tensor(uq(name), shape, dt))

            def ps2(name, shape, dt):
                return es2.enter_context(nc.psum_tensor(uq(name), shape, dt))
            xT = sb2("xT", [128, KC, S], BF16)
            r_xT = [Res("xT%d" % i) for i in range(NT)]
            pT = ps2("pT", [128, 2, 1024], BF16)
            pT_ring = Ring([pT[:, i] for i in range(2)], "pT")
            build_xT(nc, kb, es2, x_src, xT, r_xT, ident_s, r_id, pT_ring)
            wch = sb2("wch", [128, 3, KC, 128], BF16)
            wch_ring = Ring([wch[:, i] for i in range(3)], "wch")
            pp = ps2("pp", [128, 6, 512], F32)
            pp_ring = Ring([pp[:, i] for i in range(6)], "pp")

            def proj_fm(col0, tg, wt, r_w):
                p, r_p = pp_ring.next()
                for kc in range(KC):
                    kb.op("pe", lambda e: e.matmul(p[:, 0:512], lhsT=wt[:, kc, :], rhs=xT[:, kc, tg * 512:(tg + 1) * 512],
                                                   start=(kc == 0), stop=(kc == KC - 1)), reads=[r_w] + r_xT[tg * 4:(tg + 1) * 4], writes=[r_p])
                return p, r_p

            def load_w(col0):
                wt, r_w = wch_ring.next()
                kb.dma("pool", lambda e: e.dma_start(out=wt, in_=winv[:, :, col0:col0 + 128]), writes=[r_w])
                return wt, r_w
            with ExitStack() as es3:
                def sb3(name, shape, dt):
                    return es3.enter_context(nc.sbuf_tensor(uq(name), shape, dt))
                identf = sb3("identf", [128, 128], F32)
                onesm = sb3("onesm", [128, 128], F32)
                r_c = Res("cc")
                kb.dma("sp", lambda e: e.dma_start(out=identf[:], in_=C["identf"]), writes=[r_c])
                kb.op("dve", lambda e: e.memset(onesm[:], 1.0 / 128.0), writes=[r_c])
                cw31 = sb3("cw31", [31, 1024], F32)
                r_cw = Res("cw31")
                kb.dma("sp", lambda e: e.dma_start(out=cw31[:], in_=w["even_conv_w"][0].rearrange("j o c -> j (o c)")), writes=[r_cw])
                cwT = sb3("cwT", [128, 8, 32], F32)
                r_cwT = Res("cwT")
                prm = sb3("prm", [128, 3, 8], F32)
                r_prm = Res("prm")
                with nc.allow_non_contiguous_dma(reason="tiny per-channel params"):
                    for k, nm in enumerate(("even_conv_b", "even_conv_norm_g", "even_conv_norm_b")):
                        kb.dma("sp", lambda e: e.dma_start(out=prm[:, k, :], in_=w[nm][0].rearrange("(c p) -> p c", p=128)), writes=[r_prm])
                for c in range(8):
                    p, r_p = pp_ring.next()
                    kb.op("pe", lambda e: e.transpose(out=p[:, 0:31], in_=cw31[0:31, c * 128:(c + 1) * 128], identity=identf[0:31, 0:31]),
                          reads=[r_cw, r_c], writes=[r_p])
                    kb.op("dve", lambda e: e.tensor_copy(out=cwT[:, c, 0:31], in_=p[:, 0:31]), reads=[r_p], writes=[r_cwT])
                cin = sb3("cin", [128, 2, 32 + S], BF16)
                cin_ring = Ring([cin[:, i] for i in range(2)], "cin")
                kb.op("dve", lambda e: e.memset(cin[:, :, 0:32], 0.0), writes=cin_ring.r)
                dg = sb3("dg", [128, 2, 31, 128], BF16)
                dg_ring = Ring([dg[:, i] for i in range(2)], "dg")
                sgs = sb3("sgs", [128, 2, 512], F32)
                sg_ring = Ring([sgs[:, i] for i in range(2)], "sgs")
                tb = sb3("tb", [128, 2, 4, 512], F32)
                tb_res = [[Res("tb%d_%d" % (a, b)) for b in range(4)] for a in range(2)]
                sq = sb3("sq", [128, 4, 512], F32)
                sq_res = [Res("sq%d" % b) for b in range(4)]
                uo = sb3("uo", [128, 2, 512], BF16)
                uo_ring = Ring([uo[:, i] for i in range(2)], "uo")

                def proj_stage(c):
                    wa, r_wa = load_w(CA + c * 128)
                    wg, r_wg = load_w(CG + c * 128)
                    ci, r_ci = cin_ring.next()
                    for tg in range(4):
                        pa, r_pa = proj_fm(CA, tg, wa, r_wa)
                        pg, r_pg = proj_fm(CG, tg, wg, r_wg)
                        sg, r_sg = sg_ring.next()
                        kb.op("act", lambda e: e.activation(out=sg, in_=pg[:, 0:512], func=AF.Sigmoid), reads=[r_pg], writes=[r_sg])
                        kb.op("dve", lambda e: e.tensor_tensor(out=ci[:, 32 + tg * 512:32 + (tg + 1) * 512], in0=sg, in1=pa[:, 0:512], op=ALU.mult),
                              reads=[r_sg, r_pa], writes=[r_ci])
                    dgt, r_dg = dg_ring.next()
                    for j in range(31):
                        kb.op("dve", lambda e: e.tensor_scalar(out=dgt[:, j, :], in0=identf[:], scalar1=cwT[:, c, j:j + 1], scalar2=None, op0=ALU.mult),
                              reads=[r_c, r_cwT], writes=[r_dg])
                    return ci, r_ci, dgt, r_dg

                def conv_stage(c, ci, r_ci, dgt, r_dg):
                    for tg in range(4):
                        pc, r_pc = pp_ring.next()
                        for j in range(31):
                            o0 = 2 + j + tg * 512
                            kb.op("pe", lambda e: e.matmul(pc[:, 0:512], lhsT=dgt[:, j, :], rhs=ci[:, o0:o0 + 512], start=(j == 0), stop=(j == 30)),
                                  reads=[r_dg, r_ci], writes=[r_pc])
                        kb.op("act", lambda e: e.activation(out=tb[:, c % 2, tg], in_=pc[:, 0:512], func=AF.Identity, bias=prm[:, 0, c:c + 1]),
                              reads=[r_pc, r_prm], writes=[tb_res[c % 2][tg]])

                def mean_stage(c):
                    for tg in range(4):
                        ut = tb[:, c % 2, tg]; r_t = tb_res[c % 2][tg]
                        pmn, r_pmn = pp_ring.next()
                        kb.op("pe", lambda e: e.matmul(pmn[:, 0:512], lhsT=onesm[:], rhs=ut, start=True, stop=True), reads=[r_t, r_c], writes=[r_pmn])
                        kb.op("dve", lambda e: e.tensor_tensor(out=ut, in0=ut, in1=pmn[:, 0:512], op=ALU.subtract), reads=[r_t, r_pmn], writes=[r_t])
                        kb.op("act", lambda e: e.activation(out=sq[:, tg], in_=ut, func=AF.Square), reads=[r_t], writes=[sq_res[tg]])

                def var_stage(c):
                    for tg in range(4):
                        dd = tb[:, c % 2, tg]; r_t = tb_res[c % 2][tg]
                        rs = sq[:, tg]; r_s = sq_res[tg]
                        pvr, r_pvr = pp_ring.next()
                        kb.op("pe", lambda e: e.matmul(pvr[:, 0:512], lhsT=onesm[:], rhs=rs, start=True, stop=True), reads=[r_s, r_c], writes=[r_pvr])
                        kb.op("dve", lambda e: e.tensor_scalar_add(out=rs, in0=pvr[:, 0:512], scalar1=LN_EPS), reads=[r_pvr], writes=[r_s])
                        kb.op("act", lambda e: e.sqrt(out=rs, in_=rs), reads=[r_s], writes=[r_s])
                        kb.op("dve", lambda e: e.reciprocal(out=rs, in_=rs), reads=[r_s], writes=[r_s])
                        kb.op("dve", lambda e: e.tensor_tensor(out=dd, in0=dd, in1=rs, op=ALU.mult), reads=[r_t, r_s], writes=[r_t])
                        kb.op("dve", lambda e: e.tensor_scalar(out=dd, in0=dd, scalar1=prm[:, 1, c:c + 1], scalar2=prm[:, 2, c:c + 1], op0=ALU.mult, op1=ALU.add),
                              reads=[r_t, r_prm], writes=[r_t])
                        uot, r_uo = uo_ring.next()
                        kb.op("act", lambda e: e.activation(out=uot, in_=dd, func=AF.Silu), reads=[r_t], writes=[r_uo])
                        kb.dma("sp", lambda e: e.dma_start(out=uT_d[c * 128:(c + 1) * 128, tg * 512:(tg + 1) * 512], in_=uot), reads=[r_uo], key=r_uo)

                for c in range(9):
                    if c < 8:
                        st = proj_stage(c)
                    if c >= 1:
                        mean_stage(c - 1)
                    if c < 8:
                        conv_stage(c, *st)
                    if c >= 1:
                        var_stage(c - 1)
        kb.barrier()
        with ExitStack() as es1:
            def sb1(name, shape, dt):
                return es1.enter_context(nc.sbuf_tensor(uq(name), shape, dt))
            qT = sb1("qT", [128, FOXH, S], BF16)
            kT = sb1("kT", [128, FOXH, S], BF16)
            fl = sb1("fl", [128, NT, FOXH], F32)
            r_q = [Res("q%d" % h) for h in range(FOXH)]
            r_k = [Res("k%d" % h) for h in range(FOXH)]
            r_fl = Res("fl")
            with ExitStack() as es2:
                def sb2(name, shape, dt):
                    return es2.enter_context(nc.sbuf_tensor(uq(name), shape, dt))

                def ps2(name, shape, dt):
                    return es2.enter_context(nc.psum_tensor(uq(name), shape, dt))
                xT = sb2("xT", [128, KC, S], BF16)
                r_xT = [Res("xT%d" % i) for i in range(NT)]
                pT = ps2("pT", [128, 2, 1024], BF16)
                pT_ring = Ring([pT[:, i] for i in range(2)], "pT")
                build_xT(nc, kb, es2, x_src, xT, r_xT, ident_s, r_id, pT_ring)
                wch = sb2("wch", [128, 3, KC, 128], BF16)
                wch_ring = Ring([wch[:, i] for i in range(3)], "wch")
                pp = ps2("pp", [128, 4, 512], F32)
                pp_ring = Ring([pp[:, i] for i in range(4)], "pp")
                nev = 0

                def proj_fm(col0, tg, wt, r_w):
                    p, r_p = pp_ring.next()
                    for kc in range(KC):
                        kb.op("pe", lambda e: e.matmul(p[:, 0:512], lhsT=wt[:, kc, :], rhs=xT[:, kc, tg * 512:(tg + 1) * 512],
                                                       start=(kc == 0), stop=(kc == KC - 1)), reads=[r_w] + r_xT[tg * 4:(tg + 1) * 4], writes=[r_p])
                    return p, r_p

                def load_w(col0):
                    wt, r_w = wch_ring.next()
                    kb.dma("pool", lambda e: e.dma_start(out=wt, in_=winv[:, :, col0:col0 + 128]), writes=[r_w])
                    return wt, r_w

                for h in range(FOXH):
                    for (c0, dst, rr, scl) in ((CQ, qT, r_q, 128.0 ** -0.5), (CK, kT, r_k, 1.0)):
                        wt, r_w = load_w(c0 + h * 128)
                        for tg in range(4):
                            p, r_p = proj_fm(c0, tg, wt, r_w)
                            if nev % 2 == 0:
                                kb.op("act", lambda e: e.mul(out=dst[:, h, tg * 512:(tg + 1) * 512], in_=p[:, 0:512], mul=scl), reads=[r_p], writes=[rr[h]])
                            else:
                                kb.op("dve", lambda e: e.tensor_scalar(out=dst[:, h, tg * 512:(tg + 1) * 512], in0=p[:, 0:512], scalar1=scl, scalar2=None, op0=ALU.mult),
                                      reads=[r_p], writes=[rr[h]])
                            nev += 1
                with ExitStack() as es3:
                    wv = es3.enter_context(nc.sbuf_tensor(uq("wv"), [128, KC, 520], BF16))
                    r_wv = Res("wv")
                    wf32 = es3.enter_context(nc.sbuf_tensor(uq("wf32"), [128, KC, 8], F32))
                    wfb = es3.enter_context(nc.sbuf_tensor(uq("wfb"), [128, KC, 128], BF16))
                    r_wf = Res("wf")
                    vst = es3.enter_context(nc.sbuf_tensor(uq("vst"), [128, 4, 512], BF16))
                    vst_ring = Ring([vst[:, i] for i in range(4)], "vst")
                    for half in range(2):
                        ncol = 520 if half == 0 else 512
                        if half == 0:
                            kb.dma("pool", lambda e: e.dma_start(out=wv[:, :, 0:512], in_=winv[:, :, CV:CV + 512]), writes=[r_wv])
                            kb.dma("sp", lambda e: e.dma_start(out=wf32[:], in_=winv[:, :, CF:CF + 8]), writes=[r_wf])
                            if dbg is not None:
                                kb.dma("sp", lambda e: e.dma_start(out=dbg["wf"], in_=wf32[:].rearrange("p a b -> p (a b)")), reads=[r_wf], key=Res("dbgwf"))
                            kb.op("dve", lambda e: e.memset(wfb[:], 0.0), writes=[r_wf])
                            kb.op("dve", lambda e: e.tensor_copy(out=wfb[:, :, 0:8], in_=wf32[:]), reads=[r_wf], writes=[r_wf])
                        else:
                            kb.dma("pool", lambda e: e.dma_start(out=wv[:, :, 0:512], in_=winv[:, :, CV + 512:CV + 1024]), writes=[r_wv])
                        for i in range(NT):
                            p, r_p = pp_ring.next()
                            for kc in range(KC):
                                kb.op("pe", lambda e: e.matmul(p[:, 0:512], lhsT=xT[:, kc, i * 128:(i + 1) * 128], rhs=wv[:, kc, 0:512],
                                                               start=(kc == 0), stop=(kc == KC - 1)), reads=[r_wv, r_xT[i]], writes=[r_p])
                            vs, r_vs = vst_ring.next()
                            if nev % 2 == 0:
                                kb.op("act", lambda e: e.copy(out=vs, in_=p[:, 0:512]), reads=[r_p], writes=[r_vs])
                            else:
                                kb.op("dve", lambda e: e.tensor_copy(out=vs, in_=p[:, 0:512]), reads=[r_p], writes=[r_vs])
                            nev += 1
                            kb.dma("sp", lambda e: e.dma_start(out=vt_d[i * 128:(i + 1) * 128, half * 512:(half + 1) * 512], in_=vs), reads=[r_vs], key=r_vs)
                            if half == 0:
                                p, r_p = pp_ring.next()
                                for kc in range(KC):
                                    kb.op("pe", lambda e: e.matmul(p[:, 0:128], lhsT=xT[:, kc, i * 128:(i + 1) * 128], rhs=wfb[:, kc, :],
                                                                   start=(kc == 0), stop=(kc == KC - 1)), reads=[r_wf, r_xT[i]], writes=[r_p])
                                kb.op("act", lambda e: e.copy(out=fl[:, i, :], in_=p[:, 0:8]), reads=[r_p], writes=[r_fl])
                if dbg is not None:
                    kb.dma("sp", lambda e: e.dma_start(out=dbg["fl2"], in_=fl[:].rearrange("p a b -> p (a b)")), reads=[r_fl], key=Res("dbgfl2"))
                kb.barrier()
            kb.barrier()
            with ExitStack() as es2:
                def sb2(name, shape, dt):
                    return es2.enter_context(nc.sbuf_tensor(uq(name), shape, dt))

                def ps2(name, shape, dt):
                    return es2.enter_context(nc.psum_tensor(uq(name), shape, dt))
                vt = sb2("vt", [128, NT, 1024], BF16)
                r_v = [Res("v%d" % i) for i in range(NT)]
                for i in range(NT):
                    kb.dma("sp", lambda e: e.dma_start(out=vt[:, i, :], in_=vt_d[i * 128:(i + 1) * 128, :]), writes=[r_v[i]])
                uinc = sb2("uinc", [128, 128], F32)
                m64 = sb2("m64", [128, 128], F32)
                onesf = sb2("onesf", [128, 128], F32)
                ones_b = sb2("ones_b", [128, 128], BF16)
                cmask = sb2("cmask", [128, 128], BF16)
                bfb = sb2("bfb", [128, FOXH], F32)
                r_c = Res("attc")
                kb.dma("sp", lambda e: e.dma_start(out=uinc[:], in_=C["uinc"]), writes=[r_c])
                r_c2 = Res("attc2")
                kb.dma("sp", lambda e: e.dma_start(out=m64[:], in_=C["m64"]), writes=[r_c2])
                r_c3 = Res("attc3")
                kb.dma("sp", lambda e: e.dma_start(out=cmask[:], in_=C["cmask"]), writes=[r_c3])
                r_c4 = Res("attc4")
                kb.dma("sp", lambda e: e.dma_start(out=bfb[:], in_=w["even_b_f"][0].partition_broadcast(128)), writes=[r_c4])
                kb.op("dve", lambda e: e.memset(onesf[:], 1.0), writes=[r_c])
                kb.op("dve", lambda e: e.memset(ones_b[:], 1.0), writes=[r_c])
                lf = sb2("lf", [128, NT, FOXH], F32)
                r_lf = Res("lf")
                kb.op("dve", lambda e: e.tensor_tensor(out=lf[:], in0=fl[:], in1=bfb[:].unsqueeze(1).to_broadcast([128, NT, FOXH]), op=ALU.add),
                      reads=[r_fl, r_c4], writes=[r_lf])
                kb.op("act", lambda e: e.activation(out=lf[:], in_=lf[:], func=AF.Exp, scale=-1.0), reads=[r_lf], writes=[r_lf])
                kb.op("act", lambda e: e.activation(out=lf[:], in_=lf[:], func=AF.Ln, bias=1.0), reads=[r_lf], writes=[r_lf])
                kb.op("dve", lambda e: e.tensor_scalar(out=lf[:], in0=lf[:], scalar1=-1.0, scalar2=None, op0=ALU.mult), reads=[r_lf], writes=[r_lf])
                c_all = sb2("c_all", [128, NT, FOXH], F32)
                cref = sb2("cref", [128, NT, FOXH], F32)
                lfcum = sb2("lfcum", [128, FOXH], F32)
                r_call = Res("c_all"); r_cref = Res("cref"); r_lfc = Res("lfcum")
                kb.op("dve", lambda e: e.memset(lfcum[:], 0.0), writes=[r_lfc])
                pcs = ps2("pcs", [128, 2, 512], F32)
                pcs_ring = Ring([pcs[:, i] for i in range(2)], "pcs")
                for i in range(NT):
                    p, r_p = pcs_ring.next()
                    kb.op("pe", lambda e: e.matmul(p[:, 0:FOXH], lhsT=uinc[:], rhs=lf[:, i, :], start=True, stop=False), reads=[r_lf, r_c], writes=[r_p])
                    kb.op("pe", lambda e: e.matmul(p[:, 0:FOXH], lhsT=onesf[:], rhs=lfcum[:], start=False, stop=True), reads=[r_lfc, r_c], writes=[r_p])
                    kb.op("dve", lambda e: e.tensor_copy(out=c_all[:, i, :], in_=p[:, 0:FOXH]), reads=[r_p], writes=[r_call])
                    p2, r_p2 = pcs_ring.next()
                    kb.op("pe", lambda e: e.matmul(p2[:, 0:FOXH], lhsT=m64[:], rhs=lf[:, i, :], start=True, stop=False), reads=[r_lf, r_c2], writes=[r_p2])
                    kb.op("pe", lambda e: e.matmul(p2[:, 0:FOXH], lhsT=onesf[:], rhs=lfcum[:], start=False, stop=True), reads=[r_lfc, r_c], writes=[r_p2])
                    kb.op("dve", lambda e: e.tensor_copy(out=cref[:, i, :], in_=p2[:, 0:FOXH]), reads=[r_p2], writes=[r_cref])
                    kb.op("dve", lambda e: e.tensor_tensor(out=lfcum[:], in0=lfcum[:], in1=lf[:, i, :], op=ALU.add), reads=[r_lf, r_lfc], writes=[r_lfc])
                if dbg is not None:
                    rd2 = Res("dbg2")
                    kb.dma("sp", lambda e: e.dma_start(out=dbg["c_all"], in_=c_all[:].rearrange("p a b -> p (a b)")), reads=[r_call], key=rd2)
                    kb.dma("sp", lambda e: e.dma_start(out=dbg["cref"], in_=cref[:].rearrange("p a b -> p (a b)")), reads=[r_cref], key=rd2)
                    kb.dma("sp", lambda e: e.dma_start(out=dbg["lf"], in_=lf[:].rearrange("p a b -> p (a b)")), reads=[r_lf], key=rd2)
                    kb.dma("sp", lambda e: e.dma_start(out=dbg["fl"], in_=fl[:].rearrange("p a b -> p (a b)")), reads=[r_fl], key=rd2)
                    kb.dma("sp", lambda e: e.dma_start(out=dbg["bfb"], in_=bfb[:]), reads=[r_c4], key=rd2)
                bias_all = sb2("bias_all", [128, FOXH, NT, NT], F32)
                r_bias = Res("bias")
                for h in range(FOXH):
                    for qb in range(NT):
                        kb.op("dve", lambda e: e.tensor_scalar(out=bias_all[:, h, qb, :], in0=c_all[:, :, h], scalar1=-1.0, scalar2=cref[:, qb, h:h + 1],
                                                               op0=ALU.mult, op1=ALU.add), reads=[r_call, r_cref], writes=[r_bias])
                pst = ps2("pst", [128, 2, 512], F32)
                pst_ring = Ring([pst[:, i] for i in range(2)], "pst")
                po = ps2("po", [128, 2, 512], F32)
                po_ring = Ring([po[:, i] for i in range(2)], "po")
                pr = ps2("pr", [128, 2, 512], F32)
                pr_ring = Ring([pr[:, i] for i in range(2)], "pr")
                PT = sb2("PT", [128, 8, 128], BF16)
                PT_ring = Ring([PT[:, i] for i in range(8)], "PT")
                rcp = sb2("rcp", [128, 2, 128], F32)
                rcp_ring = Ring([rcp[:, i] for i in range(2)], "rcp")
                groups = []
                for h in range(FOXH):
                    for qb in range(NT):
                        nkb = qb + 1
                        for k0 in range(0, nkb, 4):
                            groups.append((h, qb, list(range(k0, min(k0 + 4, nkb)))))
                acc = {}

                def do_scores(gi):
                    h, qb, kbs = groups[gi]
                    st, r_st = pst_ring.next()
                    for n, kbk in enumerate(kbs):
                        kb.op("pe", lambda e: e.matmul(st[:, n * 128:(n + 1) * 128], lhsT=kT[:, h, kbk * 128:(kbk + 1) * 128], rhs=qT[:, h, qb * 128:(qb + 1) * 128],
                                                       start=True, stop=True), reads=[r_k[h], r_q[h]], writes=[r_st])
                    pts = []
                    for n, kbk in enumerate(kbs):
                        pt, r_pt = PT_ring.next()
                        kb.op("act", lambda e: e.activation(out=pt, in_=st[:, n * 128:(n + 1) * 128], func=AF.Exp, bias=bias_all[:, h, qb, kbk:kbk + 1]),
                              reads=[r_st, r_bias], writes=[r_pt])
                        if kbk == qb:
                            kb.op("dve", lambda e: e.tensor_tensor(out=pt, in0=pt, in1=cmask[:], op=ALU.mult), reads=[r_pt, r_c3], writes=[r_pt])
                        pts.append((kbk, pt, r_pt))
                    return pts

                def do_pv(gi, pts):
                    h, qb, kbs = groups[gi]
                    if kbs[0] == 0:
                        acc[(h, qb)] = (po_ring.next(), pr_ring.next())
                    (pot, r_po), (prt, r_pr) = acc[(h, qb)]
                    for kbk, pt, r_pt in pts:
                        kb.op("pe", lambda e: e.matmul(pot[:, 0:128], lhsT=vt[:, kbk, h * 128:(h + 1) * 128], rhs=pt, start=(kbk == 0), stop=(kbk == qb)),
                              reads=[r_v[kbk], r_pt], writes=[r_po])
                        kb.op("pe", lambda e: e.matmul(prt[:, 0:128], lhsT=ones_b[:], rhs=pt, start=(kbk == 0), stop=(kbk == qb)),
                              reads=[r_c, r_pt], writes=[r_pr])
                    if kbs[-1] == qb:
                        rc, r_rc = rcp_ring.next()
                        kb.op("dve", lambda e: e.reciprocal(out=rc, in_=prt[:, 0:128]), reads=[r_pr], writes=[r_rc])
                        kb.op("dve", lambda e: e.tensor_tensor(out=attT[:, h, qb * 128:(qb + 1) * 128], in0=rc, in1=pot[:, 0:128], op=ALU.mult),
                              reads=[r_rc, r_po], writes=[r_att[h]])
                        del acc[(h, qb)]

                prev = do_scores(0)
                for gi in range(len(groups)):
                    nxt_pts = do_scores(gi + 1) if gi + 1 < len(groups) else None
                    do_pv(gi, prev)
                    prev = nxt_pts
        kb.barrier()
        if dbg is not None:
            rd = Res("dbg")
            for h in range(FOXH):
                kb.dma("sp", lambda e: e.dma_start(out=dbg["att"][h * 128:(h + 1) * 128, :], in_=attT[:, h, :]), reads=[r_att[h]], key=rd)
        with ExitStack() as es2:
            uTs = es2.enter_context(nc.sbuf_tensor(uq("uTs"), [128, 8, S], BF16))
            r_u = [Res("uT%d" % c) for c in range(8)]
            for c in range(8):
                kb.dma("sp", lambda e: e.dma_start(out=uTs[:, c, :], in_=uT_d[c * 128:(c + 1) * 128, :]), writes=[r_u[c]])
            chunks = [attT[:, h, :] for h in range(FOXH)] + [uTs[:, c, :] for c in range(8)]
            mix_out_phase(nc, kb, es2, L, chunks, r_att + r_u, w["even_w_out"][0], x_src, xa, xab, w, C)
    kb.barrier()


def make_consts():
    bf = ml_dtypes.bfloat16
    c = {}
    c["ident"] = np.eye(128, dtype=np.float32).astype(bf)
    c["iota"] = np.tile(np.arange(512, dtype=np.float32)[None, :], (128, 1))
    ti = np.zeros((128, NT, 4), np.float32)
    ti[:, :, 0] = np.arange(128)[:, None]
    ti[:, :, 1] = np.arange(NT)[None, :]
    ti[:, :, 2] = 1.0
    c["tokinfo"] = ti.astype(bf)
    c["lstrict"] = np.triu(np.ones((128, 128), np.float32), 1).astype(bf)
    c["ecap"] = np.tile((np.arange(NE, dtype=np.float32) * CAP)[None, :], (128, 1))
    c["identf"] = np.eye(128, dtype=np.float32)
    c["uinc"] = np.triu(np.ones((128, 128), np.float32), 0)
    m64 = np.zeros((128, 128), np.float32); m64[:65, :] = 1.0
    c["m64"] = m64
    c["cmask"] = np.triu(np.ones((128, 128), np.float32), 0).astype(bf)
    c["lgt16"] = np.tril(np.ones((128, 128), np.float32), -1) * (-1.0 / 16.0)
    c["uinc16"] = np.triu(np.ones((128, 128), np.float32), 0) * (-1.0 / 16.0)
    return c


CONST_DT = {"ident": BF16, "iota": F32, "tokinfo": BF16, "lstrict": BF16, "ecap": F32, "identf": F32, "uinc": F32, "m64": F32, "cmask": BF16, "lgt16": F32, "uinc16": F32}


GH = 4
OQ, OK_, OV, OG, OA = 0, 1024, 2048, 4096, 6144


def odd_phase(nc, kb, L, C, x_src, kt_d, vt_d, gt_d, o_d, xa, xab, w):
    from contextlib import ExitStack
    win = w["odd_w_in"][0]
    winv = win.rearrange("(kc p) n -> p kc n", p=128)
    with ExitStack() as es0:
        def sb0(name, shape, dt):
            return es0.enter_context(nc.sbuf_tensor(uq(name), shape, dt))
        ident_s = sb0("ident", [128, 128], BF16)
        r_id = Res("ident")
        kb.dma("sp", lambda e: e.dma_start(out=ident_s[:], in_=C["ident"]), writes=[r_id])
        with ExitStack() as es1:
            def sb1(name, shape, dt):
                return es1.enter_context(nc.sbuf_tensor(uq(name), shape, dt))
            qT = sb1("qT", [128, 8, S], BF16)
            kT = sb1("kT", [128, 8, S], BF16)
            alT = sb1("alT", [16, S], BF16)
            r_q = [Res("q%d" % c) for c in range(8)]
            r_k = [Res("k%d" % c) for c in range(8)]
            r_al = Res("alT")
            with ExitStack() as es2:
                def sb2(name, shape, dt):
                    return es2.enter_context(nc.sbuf_tensor(uq(name), shape, dt))

                def ps2(name, shape, dt):
                    return es2.enter_context(nc.psum_tensor(uq(name), shape, dt))
                xT = sb2("xT", [128, KC, S], BF16)
                r_xT = [Res("xT%d" % i) for i in range(NT)]
                pT = ps2("pT", [128, 2, 1024], BF16)
                pT_ring = Ring([pT[:, i] for i in range(2)], "pT")
                build_xT(nc, kb, es2, x_src, xT, r_xT, ident_s, r_id, pT_ring)
                wch = sb2("wch", [128, 3, KC, 128], BF16)
                wch_ring = Ring([wch[:, i] for i in range(3)], "wch")
                pp = ps2("pp", [128, 4, 512], F32)
                pp_ring = Ring([pp[:, i] for i in range(4)], "pp")
                nev = 0
                for (c0, dst, rr) in ((OQ, qT, r_q), (OK_, kT, r_k)):
                    for c in range(8):
                        wt, r_w = wch_ring.next()
                        kb.dma("pool", lambda e: e.dma_start(out=wt, in_=winv[:, :, c0 + c * 128:c0 + (c + 1) * 128]), writes=[r_w])
                        for tg in range(4):
                            p, r_p = pp_ring.next()
                            for kc in range(KC):
                                kb.op("pe", lambda e: e.matmul(p[:, 0:512], lhsT=wt[:, kc, :], rhs=xT[:, kc, tg * 512:(tg + 1) * 512],
                                                               start=(kc == 0), stop=(kc == KC - 1)), reads=[r_w] + r_xT[tg * 4:(tg + 1) * 4], writes=[r_p])
                            if nev % 2 == 0:
                                kb.op("act", lambda e: e.copy(out=dst[:, c, tg * 512:(tg + 1) * 512], in_=p[:, 0:512]), reads=[r_p], writes=[rr[c]])
                            else:
                                kb.op("dve", lambda e: e.tensor_copy(out=dst[:, c, tg * 512:(tg + 1) * 512], in_=p[:, 0:512]), reads=[r_p], writes=[rr[c]])
                            nev += 1
                wa32 = sb2("wa32", [128, KC, 16], F32)
                wab = sb2("wab", [128, KC, 16], BF16)
                r_wa = Res("wa")
                kb.dma("sp", lambda e: e.dma_start(out=wa32[:], in_=winv[:, :, OA:OA + 16]), writes=[r_wa])
                kb.op("dve", lambda e: e.tensor_copy(out=wab[:], in_=wa32[:]), reads=[r_wa], writes=[r_wa])
                for tg in range(4):
                    p, r_p = pp_ring.next()
                    for kc in range(KC):
                        kb.op("pe", lambda e: e.matmul(p[0:16, 0:512], lhsT=wab[:, kc, :], rhs=xT[:, kc, tg * 512:(tg + 1) * 512],
                                                       start=(kc == 0), stop=(kc == KC - 1)), reads=[r_wa] + r_xT[tg * 4:(tg + 1) * 4], writes=[r_p])
                    kb.op("dve", lambda e: e.tensor_copy(out=alT[0:16, tg * 512:(tg + 1) * 512], in_=p[0:16, 0:512]), reads=[r_p], writes=[r_al])
                wtm = sb2("wtm", [128, 2, KC, 512], BF16)
                wtm_ring = Ring([wtm[:, i] for i in range(2)], "wtm")
                stg = sb2("stg", [128, 4, 512], BF16)
                stg_ring = Ring([stg[:, i] for i in range(4)], "stg")
                for cg in range(10):
                    col0 = OK_ + cg * 512
                    if cg < 2:
                        dd, dcol = kt_d, cg * 512
                    elif cg < 6:
                        dd, dcol = vt_d, (cg - 2) * 512
                    else:
                        dd, dcol = gt_d, (cg - 6) * 512
                    wt, r_w = wtm_ring.next()
                    kb.dma("pool", lambda e: e.dma_start(out=wt, in_=winv[:, :, col0:col0 + 512]), writes=[r_w])
                    for i in range(NT):
                        p, r_p = pp_ring.next()
                        for kc in range(KC):
                            kb.op("pe", lambda e: e.matmul(p[:, 0:512], lhsT=xT[:, kc, i * 128:(i + 1) * 128], rhs=wt[:, kc, :],
                                                           start=(kc == 0), stop=(kc == KC - 1)), reads=[r_w, r_xT[i]], writes=[r_p])
                        st, r_st = stg_ring.next()
                        if nev % 2 == 0:
                            kb.op("act", lambda e: e.copy(out=st, in_=p[:, 0:512]), reads=[r_p], writes=[r_st])
                        else:
                            kb.op("dve", lambda e: e.tensor_copy(out=st, in_=p[:, 0:512]), reads=[r_p], writes=[r_st])
                        nev += 1
                        kb.dma("sp", lambda e: e.dma_start(out=dd[i * 128:(i + 1) * 128, dcol:dcol + 512], in_=st), reads=[r_st], key=r_st)
            kb.barrier()
            with ExitStack() as es2:
                def sb2(name, shape, dt):
                    return es2.enter_context(nc.sbuf_tensor(uq(name), shape, dt))

                def ps2(name, shape, dt):
                    return es2.enter_context(nc.psum_tensor(uq(name), shape, dt))
                lgt = sb2("lgt", [128, 128], F32)
                uin = sb2("uin", [128, 128], F32)
                cmask = sb2("cmask", [128, 128], F32)
                wa2 = sb2("wa2", [16, 1024], F32)
                wa2b = sb2("wa2b", [16, 1024], BF16)
                ba = sb2("ba", [128, 1024], F32)
                ng = sb2("ng", [128, 2048], F32)
                rc = [Res("oc%d" % i) for i in range(6)]
                kb.dma("sp", lambda e: e.dma_start(out=lgt[:], in_=C["lgt16"]), writes=[rc[0]])
                kb.dma("sp", lambda e: e.dma_start(out=uin[:], in_=C["uinc16"]), writes=[rc[1]])
                kb.dma("sp", lambda e: e.dma_start(out=cmask[:], in_=C["uinc"]), writes=[rc[2]])
                kb.dma("sp", lambda e: e.dma_start(out=wa2[:], in_=w["odd_w_a2"][0]), writes=[rc[3]])
                kb.op("dve", lambda e: e.tensor_copy(out=wa2b[:], in_=wa2[:]), reads=[rc[3]], writes=[rc[3]])
                kb.dma("sp", lambda e: e.dma_start(out=ba[:], in_=w["odd_b_a"][0].partition_broadcast(128)), writes=[rc[4]])
                kb.dma("sp", lambda e: e.dma_start(out=ng[:], in_=w["odd_norm_g"][0].partition_broadcast(128)), writes=[rc[5]])
                state = sb2("state", [128, 8, 512], F32)
                stateb = sb2("stateb", [128, 8, 512], BF16)
                r_state = [Res("st%d" % c) for c in range(8)]
                r_stateb = [Res("stb%d" % c) for c in range(8)]
                kb.op("dve", lambda e: e.memset(state[:], 0.0), writes=r_state)
                kb.op("dve", lambda e: e.memset(stateb[:], 0.0), writes=r_stateb)
                lnv = sb2("lnv", [128, 2, 1024], F32)
                lnv_ring = Ring([lnv[:, i] for i in range(2)], "lnv")
                ktk = sb2("ktk", [128, 2, 1024], BF16)
                ktk_ring = Ring([ktk[:, i] for i in range(2)], "ktk")
                vtk = sb2("vtk", [128, 2, 2048], BF16)
                vtk_ring = Ring([vtk[:, i] for i in range(2)], "vtk")
                gtk = sb2("gtk", [128, 2, 2048], BF16)
                gtk_ring = Ring([gtk[:, i] for i in range(2)], "gtk")
                gg = sb2("gg", [128, 2, 2048], BF16)
                gg_ring = Ring([gg[:, i] for i in range(2)], "gg")
                ebm = sb2("ebm", [128, 1, 1024], F32)
                ebm_ring = Ring([ebm[:, i] for i in range(1)], "ebm")
                kend = sb2("kend", [128, 2, 1024], BF16)
                kend_ring = Ring([kend[:, i] for i in range(2)], "kend")
                eb = sb2("eb", [128, 2, 8, 128], F32)
                eb_ring = Ring([eb[:, i] for i in range(2)], "eb")
                enb = sb2("enb", [128, 2, 8, 128], F32)
                enb_ring = Ring([enb[:, i] for i in range(2)], "enb")
                qt = sb2("qt", [128, 2, 8, 128], BF16)
                qt_ring = Ring([qt[:, i] for i in range(2)], "qt")
                ktt = sb2("ktt", [128, 2, 8, 128], BF16)
                ktt_ring = Ring([ktt[:, i] for i in range(2)], "ktt")
                attn = sb2("attn", [128, 2, 128], BF16)
                attn_ring = Ring([attn[:, i] for i in range(2)], "attn")
                osb = sb2("osb", [128, 2, 2048], BF16)
                osb_ring = Ring([osb[:, i] for i in range(2)], "osb")
                sm = sb2("sm", [128, 8], F32)
                r_sm = Res("sm")
                junk = sb2("junk", [128, 512], F32)
                r_junk = Res("junk")
                pA = ps2("pA", [128, 2, 512], F32)
                pA_ring = Ring([pA[:, i] for i in range(2)], "pA")
                pB = ps2("pB", [128, 2, 512], F32)
                pB_ring = Ring([pB[:, i] for i in range(2)], "pB")
                pS = ps2("pS", [128, 1, 512], F32)
                pS_ring = Ring([pS[:, i] for i in range(1)], "pS")
                pO = ps2("pO", [128, 1, 512], F32)
                pO_ring = Ring([pO[:, i] for i in range(1)], "pO")
                pU = ps2("pU", [128, 2, 512], F32)
                pU_ring = Ring([pU[:, i] for i in range(2)], "pU")
                QS = 256.0 ** -0.5
                def make_pre(i):
                    ts = slice(i * 128, (i + 1) * 128)
                    P = {}

                    def q0():
                        P["kt"], P["r_kt"] = ktk_ring.next()
                        kb.dma("sp", lambda e: e.dma_start(out=P["kt"], in_=kt_d[ts, :]), writes=[P["r_kt"]])
                        P["vt"], P["r_vt"] = vtk_ring.next()
                        kb.dma("sp", lambda e: e.dma_start(out=P["vt"], in_=vt_d[ts, :]), writes=[P["r_vt"]])
                        gt_, r_gt = gtk_ring.next()
                        kb.dma("sp", lambda e: e.dma_start(out=gt_, in_=gt_d[ts, :]), writes=[r_gt])
                        lv, r_lv = lnv_ring.next()
                        P["lv"], P["r_lv"] = lv, r_lv
                        for hf in range(2):
                            p, r_p = pA_ring.next()
                            kb.op("pe", lambda e: e.matmul(p[:, 0:512], lhsT=alT[0:16, ts], rhs=wa2b[0:16, hf * 512:(hf + 1) * 512], start=True, stop=True),
                                  reads=[r_al, rc[3]], writes=[r_p])
                            kb.op("dve", lambda e: e.tensor_tensor(out=lv[:, hf * 512:(hf + 1) * 512], in0=p[:, 0:512], in1=ba[:, hf * 512:(hf + 1) * 512], op=ALU.add),
                                  reads=[r_p, rc[4]], writes=[r_lv])
                        kb.op("act", lambda e: e.activation(out=lv, in_=lv, func=AF.Exp, scale=-1.0), reads=[r_lv], writes=[r_lv])
                        kb.op("act", lambda e: e.activation(out=lv, in_=lv, func=AF.Ln, bias=1.0), reads=[r_lv], writes=[r_lv])
                        ggt, r_gg = gg_ring.next()
                        P["gg"], P["r_gg"] = ggt, r_gg
                        kb.op("act", lambda e: e.activation(out=ggt, in_=gt_, func=AF.Silu), reads=[r_gt], writes=[r_gg])
                        kb.op("dve", lambda e: e.tensor_tensor(out=ggt, in0=ggt, in1=ng[:], op=ALU.mult), reads=[r_gg, rc[5]], writes=[r_gg])

                    def q1():
                        lv, r_lv = P["lv"], P["r_lv"]
                        em, r_em = ebm_ring.next()
                        ke, r_ke = kend_ring.next()
                        P["ke"], P["r_ke"] = ke, r_ke
                        for hf in range(2):
                            p, r_p = pA_ring.next()
                            kb.op("pe", lambda e: e.matmul(p[:, 0:512], lhsT=lgt[:], rhs=lv[:, hf * 512:(hf + 1) * 512], start=True, stop=True),
                                  reads=[r_lv, rc[0]], writes=[r_p])
                            kb.op("act", lambda e: e.activation(out=em[:, hf * 512:(hf + 1) * 512], in_=p[:, 0:512], func=AF.Exp), reads=[r_p], writes=[r_em])
                        kb.op("dve", lambda e: e.tensor_tensor(out=ke, in0=em, in1=P["kt"], op=ALU.mult), reads=[r_em, P["r_kt"]], writes=[r_ke])

                    def q2():
                        lv, r_lv = P["lv"], P["r_lv"]
                        P["eb"], P["r_eb"] = eb_ring.next()
                        P["en"], P["r_en"] = enb_ring.next()
                        for hf in range(2):
                            p, r_p = pB_ring.next()
                            for q in range(4):
                                c = hf * 4 + q
                                kb.op("pe", lambda e: e.matmul(p[:, q * 128:(q + 1) * 128], lhsT=lv[:, c * 128:(c + 1) * 128], rhs=uin[:], start=True, stop=True),
                                      reads=[r_lv, rc[1]], writes=[r_p])
                            pv = p[:, 0:512].rearrange("p (a b) -> p a b", a=4)
                            kb.op("act", lambda e: e.activation(out=P["eb"][:, hf * 4:(hf + 1) * 4, :], in_=pv, func=AF.Exp), reads=[r_p], writes=[P["r_eb"]])
                            kb.op("act", lambda e: e.activation(out=P["en"][:, hf * 4:(hf + 1) * 4, :], in_=pv, func=AF.Exp, scale=-1.0), reads=[r_p], writes=[P["r_en"]])

                    def q3():
                        P["qt"], P["r_qt"] = qt_ring.next()
                        P["kt2"], P["r_kt2"] = ktt_ring.next()
                        kb.op("dve", lambda e: e.scalar_tensor_tensor(out=P["qt"], in0=qT[:, :, ts], scalar=QS, in1=P["eb"], op0=ALU.mult, op1=ALU.mult),
                              reads=r_q + [P["r_eb"]], writes=[P["r_qt"]])
                        kb.op("dve", lambda e: e.tensor_tensor(out=P["kt2"], in0=kT[:, :, ts], in1=P["en"], op=ALU.mult), reads=r_k + [P["r_en"]], writes=[P["r_kt2"]])
                    return P, [q0, q1, q2, q3]

                def head(i, h, P, ot, r_ot):
                    qtt, r_qt, kt2, r_kt2 = P["qt"], P["r_qt"], P["kt2"], P["r_kt2"]
                    ke, r_ke, vt_, r_vt = P["ke"], P["r_ke"], P["vt"], P["r_vt"]
                    ebt, r_eb, ggt, r_gg = P["eb"], P["r_eb"], P["gg"], P["r_gg"]
                    pSt, r_pS = pS_ring.next()
                    for cc in range(2):
                        c = 2 * h + cc
                        kb.op("pe", lambda e: e.matmul(pSt[:, 0:128], lhsT=kt2[:, c, :], rhs=qtt[:, c, :], start=(cc == 0), stop=(cc == 1)),
                              reads=[r_kt2, r_qt], writes=[r_pS])
                    at, r_at = attn_ring.next()
                    kb.op("dve", lambda e: e.tensor_tensor(out=at, in0=pSt[:, 0:128], in1=cmask[:], op=ALU.mult), reads=[r_pS, rc[2]], writes=[r_at])
                    vh = vt_[:, h * 512:(h + 1) * 512]
                    pus = []
                    for cc in range(2):
                        c = 2 * h + cc
                        pu, r_pu = pU_ring.next()
                        kb.op("pe", lambda e: e.matmul(pu[:, 0:512], lhsT=ke[:, c * 128:(c + 1) * 128], rhs=vh, start=True, stop=True),
                              reads=[r_ke, r_vt], writes=[r_pu])
                        pus.append((c, pu, r_pu))
                    pOt, r_pO = pO_ring.next()
                    kb.op("pe", lambda e: e.matmul(pOt[:, 0:512], lhsT=at, rhs=vh, start=True, stop=False), reads=[r_at, r_vt], writes=[r_pO])
                    for cc in range(2):
                        c = 2 * h + cc
                        kb.op("pe", lambda e: e.matmul(pOt[:, 0:512], lhsT=qtt[:, c, :], rhs=stateb[:, c, :], start=False, stop=(cc == 1)),
                              reads=[r_qt, r_stateb[c]], writes=[r_pO])
                    for c, pu, r_pu in pus:
                        kb.op("dve", lambda e: e.scalar_tensor_tensor(out=state[:, c, :], in0=state[:, c, :], scalar=ebt[:, c, 127:128], in1=pu[:, 0:512],
                                                                      op0=ALU.mult, op1=ALU.add), reads=[r_state[c], r_eb, r_pu], writes=[r_state[c]])
                        kb.op("pool", lambda e: e.tensor_copy(out=stateb[:, c, :], in_=state[:, c, :]), reads=[r_state[c]], writes=[r_stateb[c]])
                    kb.op("act", lambda e: e.activation(out=junk[:], in_=pOt[:, 0:512], func=AF.Square, accum_out=sm[:, h:h + 1]), reads=[r_pO], writes=[r_junk, r_sm])
                    kb.op("dve", lambda e: e.tensor_scalar(out=sm[:, 4 + h:5 + h], in0=sm[:, h:h + 1], scalar1=1.0 / 512.0, scalar2=LN_EPS, op0=ALU.mult, op1=ALU.add),
                          reads=[r_sm], writes=[r_sm])
                    kb.op("act", lambda e: e.sqrt(out=sm[:, 4 + h:5 + h], in_=sm[:, 4 + h:5 + h]), reads=[r_sm], writes=[r_sm])
                    kb.op("dve", lambda e: e.reciprocal(out=sm[:, 4 + h:5 + h], in_=sm[:, 4 + h:5 + h]), reads=[r_sm], writes=[r_sm])
                    kb.op("dve", lambda e: e.scalar_tensor_tensor(out=ot[:, h * 512:(h + 1) * 512], in0=pOt[:, 0:512], scalar=sm[:, 4 + h:5 + h],
                                                                  in1=ggt[:, h * 512:(h + 1) * 512], op0=ALU.mult, op1=ALU.mult),
                          reads=[r_pO, r_sm, r_gg], writes=[r_ot])

                Pcur, qs = make_pre(0)
                for q in qs:
                    q()
                for i in range(NT):
                    ts = slice(i * 128, (i + 1) * 128)
                    if i + 1 < NT:
                        Pn, qn = make_pre(i + 1)
                    else:
                        Pn, qn = None, []
                    ot, r_ot = osb_ring.next()
                    for h in range(GH):
                        head(i, h, Pcur, ot, r_ot)
                        if qn:
                            qn.pop(0)()
                    kb.dma("sp", lambda e: e.dma_start(out=o_d[ts, :], in_=ot), reads=[r_ot], key=r_ot)
                    Pcur = Pn
        kb.barrier()
        with ExitStack() as es2:
            mixT = es2.enter_context(nc.sbuf_tensor(uq("mixT"), [128, KC, S], BF16))
            r_mT = [Res("mT%d" % i) for i in range(NT)]
            pT = es2.enter_context(nc.psum_tensor(uq("pT"), [128, 2, 1024], BF16))
            pT_ring = Ring([pT[:, i] for i in range(2)], "pT")
            with ExitStack() as es3:
                build_xT(nc, kb, es3, o_d, mixT, r_mT, ident_s, r_id, pT_ring)
            kb.barrier()
            mix_out_phase(nc, kb, es2, L, [mixT[:, kc, :] for kc in range(KC)], r_mT, w["odd_w_out"][0], x_src, xa, xab, w, C)
    kb.barrier()


W_NAMES = ["even_w_in", "even_b_f", "even_conv_w", "even_conv_b", "even_conv_norm_g", "even_conv_norm_b", "even_w_out",
           "odd_w_in", "odd_w_a2", "odd_b_a", "odd_norm_g", "odd_w_out", "ln_mix_g", "ln_mix_b", "ln_ffn_g", "ln_ffn_b",
           "router_w", "router_bias", "expert_w_gate", "expert_w_up", "expert_w_down"]
W_SHAPES = {"even_w_in": (1, 2048, 5128), "even_b_f": (1, 8), "even_conv_w": (1, 31, 1, 1024), "even_conv_b": (1, 1024),
            "even_conv_norm_g": (1, 1024), "even_conv_norm_b": (1, 1024), "even_w_out": (1, 2048, 2048),
            "odd_w_in": (1, 2048, 6160), "odd_w_a2": (1, 16, 1024), "odd_b_a": (1, 1024), "odd_norm_g": (1, 2048),
            "odd_w_out": (1, 2048, 2048), "ln_mix_g": (2, 2048), "ln_mix_b": (2, 2048), "ln_ffn_g": (2, 2048),
            "ln_ffn_b": (2, 2048), "router_w": (2048, 16), "router_bias": (16,),
            "expert_w_gate": (2, 16, 128, 11, 16, 128), "expert_w_up": (2, 16, 128, 11, 16, 128), "expert_w_down": (2, 16, 128, 4, 11, 512)}


def build_program():
    nc = bass.Bass("TRN2", target_bir_lowering=False)
    kb = KB(nc)

    def din(name, shape, dt):
        return nc.dram_tensor(name, list(shape), dt, kind="ExternalInput").ap()

    def dsc(name, shape, dt):
        return nc.dram_tensor(name, list(shape), dt, kind="Internal").ap()
    consts = make_consts()
    C = {k: din("c_" + k, v.shape, CONST_DT[k]) for k, v in consts.items()}
    w = {k: din(k, W_SHAPES[k], F32) for k in W_NAMES}
    x = din("x", (S, D), F32)
    out = nc.dram_tensor("out", [S, D], F32, kind="ExternalOutput").ap()
    xa = dsc("xa", (S, D), F32)
    xab = dsc("xab", (S + 128, D), BF16)
    x2 = dsc("x2", (S, D), F32)
    ys = dsc("ys", (NE * CAP + 128, D), BF16)
    uT_d = dsc("uT_d", (1024, S), BF16)
    vte_d = dsc("vte_d", (S, 1024), BF16)
    kt_d = dsc("kt_d", (S, 1024), BF16)
    vto_d = dsc("vto_d", (S, 2048), BF16)
    gt_d = dsc("gt_d", (S, 2048), BF16)
    o_d = dsc("o_d", (S, 2048), BF16)
    with nc.sbuf_tensor(uq("zt"), [128, D], BF16) as zt:
        rz = Res("zt")
        kb.op("dve", lambda e: e.memset(zt[:], 0.0), writes=[rz])
        kb.dma("sp", lambda e: e.dma_start(out=ys[YZ:YZ + 128, :], in_=zt[:]), reads=[rz], key=rz)
        kb.dma("sp", lambda e: e.dma_start(out=xab[S:S + 128, :], in_=zt[:]), reads=[rz], key=rz)
        kb.barrier()
    even_phase(nc, kb, 0, C, x, uT_d, vte_d, xa, xab, w)
    moe_phase(nc, kb, 0, C, xa, xab, ys, x2, None, w)
    odd_phase(nc, kb, 1, C, x2, kt_d, vto_d, gt_d, o_d, xa, xab, w)
    moe_phase(nc, kb, 1, C, xa, xab, ys, out, None, w)
    return nc, consts


def relayout(name, a):
    if name in ("expert_w_gate", "expert_w_up"):
        return np.ascontiguousarray(a.reshape(2, 16, 16, 128, 11, 128).transpose(0, 1, 3, 4, 2, 5))
    if name == "expert_w_down":
        return np.ascontiguousarray(a.reshape(2, 16, 11, 128, 4, 512).transpose(0, 1, 3, 4, 2, 5))
    return np.ascontiguousarray(a)


def kernel(**inputs):
    n = 8
    nc, consts = build_program()
    x = np.ascontiguousarray(np.asarray(inputs["x"], dtype=np.float32))
    shared = {("c_" + k): v for k, v in consts.items()}
    for k in W_NAMES:
        shared[k] = relayout(k, np.asarray(inputs[k], dtype=np.float32))
    in_maps = []
    for b in range(n):
        m = dict(shared)
        m["x"] = x[b]
        in_maps.append(m)
    res = run_bass_kernel_spmd(nc, in_maps, core_ids=list(range(n)))
    return np.stack([np.asarray(r["out"], dtype=np.float32) for r in res.results], axis=0)
```
